# Optimizing a Trainium2 kernel written in Bass

```python
import math
import jax, jax.numpy as jnp
from jax import lax
import numpy as np

D_MODEL = 1024
BATCH = 4
SEQ = 4096
DEPTH = 2

CHUNK = 64
Q_BLOCK = 128
NUM_BUCKETS = 32
MAX_DISTANCE = 128
EPS = 1e-6
NEG = -1e30

A_HEADS = 4
A_HEAD_DIM = 64
A_V_DIM = 2 * A_HEAD_DIM
B_HEADS = 8
B_HEAD_DIM = 64
IDX_HEADS = 8
IDX_DIM = 32
TOPK_MAX = 256
N_ATTN_HEADS = A_HEADS + B_HEADS

ATTN_SPLITS = (
    2 * A_HEADS * A_HEAD_DIM,
    2 * A_HEADS * A_HEAD_DIM,
    A_HEADS * A_V_DIM,
    B_HEADS * B_HEAD_DIM,
    B_HEAD_DIM,
    B_HEAD_DIM,
    IDX_HEADS * IDX_DIM,
    IDX_DIM,
    IDX_HEADS,
)
ATTN_IN = sum(ATTN_SPLITS)
ATTN_MIX = A_HEADS * A_V_DIM + B_HEADS * B_HEAD_DIM

CONV_CH = 512
CONV_WIDTH = 31
SC_CH = 512
SC_WIDTH = 3
CONV_IN = 2 * CONV_CH + 3 * SC_CH
CONV_MIX = CONV_CH + SC_CH

D_FF = 4 * D_MODEL
N_EVEN = (DEPTH + 1) // 2
N_ODD = DEPTH // 2

kernel_name = 'hybrid_chunk_causal_diff_dsa_conv_block'


def rms_norm(x, g):
    xf = x.astype(jnp.float32)
    y = xf * lax.rsqrt(jnp.mean(xf * xf, axis=-1, keepdims=True) + EPS)
    return (y * g.astype(jnp.float32)).astype(x.dtype)


def layer_norm(x, g, b):
    xf = x.astype(jnp.float32)
    mu = jnp.mean(xf, axis=-1, keepdims=True)
    var = jnp.mean(jnp.square(xf - mu), axis=-1, keepdims=True)
    y = (xf - mu) * lax.rsqrt(var + EPS)
    return (y * g.astype(jnp.float32) + b.astype(jnp.float32)).astype(x.dtype)


def t5_bucket(rel):
    nb = NUM_BUCKETS // 2
    ret = jnp.where(rel > 0, nb, 0)
    n = jnp.abs(rel)
    max_exact = nb // 2
    nf = jnp.maximum(n, 1).astype(jnp.float32)
    large = max_exact + (jnp.log(nf / max_exact) / math.log(MAX_DISTANCE / max_exact)
                         * (nb - max_exact)).astype(jnp.int32)
    large = jnp.minimum(large, nb - 1)
    return ret + jnp.where(n < max_exact, n, large)


def admissible(q_pos, k_pos):
    return (k_pos // CHUNK) <= (q_pos // CHUNK)


def to_blocks(t, nblk):
    return t.reshape(t.shape[0], nblk, Q_BLOCK, *t.shape[2:]).swapaxes(0, 1)


def from_blocks(t):
    t = t.swapaxes(0, 1)
    return t.reshape(t.shape[0], t.shape[1] * t.shape[2], *t.shape[3:])


def diff_attention(q1, q2, k1, k2, v, bias_tab, lam, subln_g, lambda_init):
    S = q1.shape[1]
    nblk = S // Q_BLOCK
    k_pos = jnp.arange(S)
    scale = A_HEAD_DIM ** -0.5

    def one_block(args):
        qb1, qb2, blk = args
        q_pos = blk * Q_BLOCK + jnp.arange(Q_BLOCK)
        rel = k_pos[None, :] - q_pos[:, None]
        bias = bias_tab[t5_bucket(rel)].astype(jnp.float32).transpose(2, 0, 1)
        mask = admissible(q_pos[:, None], k_pos[None, :])

        def probs(q, k):
            s = jnp.einsum('bqhd,bkhd->bhqk', q, k).astype(jnp.float32) * scale + bias
            return jax.nn.softmax(jnp.where(mask, s, NEG), axis=-1)

        a = probs(qb1, k1) - lam * probs(qb2, k2)
        return jnp.einsum('bhqk,bkhd->bqhd', a.astype(v.dtype), v)

    out = from_blocks(lax.map(one_block, (to_blocks(q1, nblk), to_blocks(q2, nblk), jnp.arange(nblk))))
    out = rms_norm(out, subln_g) * (1.0 - lambda_init)
    return out.reshape(out.shape[0], S, -1)


def dsa_attention(q, qi, wi, k, v, ki, bias_tab, top_k):
    S = q.shape[1]
    nblk = S // Q_BLOCK
    k_pos = jnp.arange(S)
    scale = B_HEAD_DIM ** -0.5
    idx_scale = IDX_DIM ** -0.5
    w_scale = IDX_HEADS ** -0.5

    def one_block(args):
        qb, qib, wib, blk = args
        q_pos = blk * Q_BLOCK + jnp.arange(Q_BLOCK)
        mask = admissible(q_pos[:, None], k_pos[None, :])
        idx_logits = jnp.einsum('bqhd,bkd->bhqk', qib, ki).astype(jnp.float32) * idx_scale
        score = jnp.einsum('bhqk,bqh->bqk', jax.nn.relu(idx_logits),
                           wib.astype(jnp.float32) * w_scale)
        score = jnp.where(mask[None], score, -jnp.inf)
        top_val, top_idx = lax.top_k(score, top_k)
        valid = jnp.isfinite(top_val)
        k_sel = jax.vmap(lambda kb, ib: kb[ib])(k, top_idx)
        v_sel = jax.vmap(lambda vb, ib: vb[ib])(v, top_idx)
        rel = top_idx - q_pos[None, :, None]
        bias = bias_tab[t5_bucket(rel)].astype(jnp.float32).transpose(0, 3, 1, 2)
        s = jnp.einsum('bqhd,bqkd->bhqk', qb, k_sel).astype(jnp.float32) * scale + bias
        p = jax.nn.softmax(jnp.where(valid[:, None], s, NEG), axis=-1)
        return jnp.einsum('bhqk,bqkd->bqhd', p.astype(v_sel.dtype), v_sel)

    out = from_blocks(lax.map(one_block, (to_blocks(q, nblk), to_blocks(qi, nblk),
                                          to_blocks(wi, nblk), jnp.arange(nblk))))
    return out.reshape(out.shape[0], S, -1)


def attention_mixer(h, w_in, w_out, rel_bias, lam_vecs, subln_g, layer_idx, top_k):
    Bsz, S, _ = h.shape
    proj = h @ w_in
    offs = np.cumsum(ATTN_SPLITS)[:-1].tolist()
    aq, ak, av, bq, bk, bv, iq, ik, iw = jnp.split(proj, offs, axis=-1)
    aq = aq.reshape(Bsz, S, A_HEADS, 2, A_HEAD_DIM)
    ak = ak.reshape(Bsz, S, A_HEADS, 2, A_HEAD_DIM)
    av = av.reshape(Bsz, S, A_HEADS, A_V_DIM)
    lambda_init = 0.8 - 0.6 * math.exp(-0.3 * layer_idx)
    lv = lam_vecs.astype(jnp.float32)
    lam = jnp.exp(jnp.sum(lv[0] * lv[1])) - jnp.exp(jnp.sum(lv[2] * lv[3])) + lambda_init
    ya = diff_attention(aq[..., 0, :], aq[..., 1, :], ak[..., 0, :], ak[..., 1, :], av,
                        rel_bias[:, :A_HEADS], lam, subln_g, lambda_init)
    yb = dsa_attention(bq.reshape(Bsz, S, B_HEADS, B_HEAD_DIM),
                       iq.reshape(Bsz, S, IDX_HEADS, IDX_DIM), iw, bk, bv, ik,
                       rel_bias[:, A_HEADS:], top_k)
    return jnp.concatenate([ya, yb], axis=-1) @ w_out


def depthwise_causal_conv(u, w):
    W, C = w.shape
    return lax.conv_general_dilated(u, w[:, None, :].astype(u.dtype), window_strides=(1,),
                                    padding=[(W - 1, 0)],
                                    dimension_numbers=('NWC', 'WIO', 'NWC'),
                                    feature_group_count=C)


def conv_mixer(h, w_in, w_out, dw_w, dw_b, ln_g, ln_b, sc_w):
    proj = h @ w_in
    ca, cg, db, dc, dh = jnp.split(
        proj, [CONV_CH, 2 * CONV_CH, 2 * CONV_CH + SC_CH, 2 * CONV_CH + 2 * SC_CH], axis=-1)
    u = ca * jax.nn.sigmoid(cg)
    u = depthwise_causal_conv(u, dw_w) + dw_b.astype(u.dtype)
    u = jax.nn.silu(layer_norm(u, ln_g, ln_b))
    z = db * depthwise_causal_conv(dc * dh, sc_w)
    return jnp.concatenate([u, z], axis=-1) @ w_out


def setup_inputs(seed: int = 0) -> dict:
    key = jax.random.key(seed)
    ks = jax.random.split(key, 20)
    f32 = jnp.float32
    nrm = lambda k, shape, s: jax.random.normal(k, shape, f32) * s
    return {
        'x': nrm(ks[0], (BATCH, SEQ, D_MODEL), 1.0),
        'rel_bias': nrm(ks[1], (NUM_BUCKETS, N_ATTN_HEADS), 0.5),
        'norm_g': 1.0 + nrm(ks[2], (DEPTH, 4, D_MODEL), 0.05),
        'w_mlp_up': nrm(ks[3], (DEPTH, D_MODEL, D_FF), D_MODEL ** -0.5),
        'w_mlp_down': nrm(ks[4], (DEPTH, D_FF, D_MODEL), D_FF ** -0.5),
        'attn_w_in': nrm(ks[5], (N_EVEN, D_MODEL, ATTN_IN), D_MODEL ** -0.5),
        'attn_w_out': nrm(ks[6], (N_EVEN, ATTN_MIX, D_MODEL), ATTN_MIX ** -0.5),
        'diff_lambda': nrm(ks[7], (N_EVEN, 4, A_HEAD_DIM), 0.1),
        'diff_subln_g': 1.0 + nrm(ks[8], (N_EVEN, A_V_DIM), 0.05),
        'conv_w_in': nrm(ks[9], (N_ODD, D_MODEL, CONV_IN), D_MODEL ** -0.5),
        'conv_w_out': nrm(ks[10], (N_ODD, CONV_MIX, D_MODEL), CONV_MIX ** -0.5),
        'conv_dw_w': nrm(ks[11], (N_ODD, CONV_WIDTH, CONV_CH), CONV_WIDTH ** -0.5),
        'conv_dw_b': nrm(ks[12], (N_ODD, CONV_CH), 0.02),
        'conv_ln_g': 1.0 + nrm(ks[13], (N_ODD, CONV_CH), 0.05),
        'conv_ln_b': nrm(ks[14], (N_ODD, CONV_CH), 0.02),
        'sconv_w': nrm(ks[15], (N_ODD, SC_WIDTH, SC_CH), SC_WIDTH ** -0.5),
    }


def reference(x, rel_bias, norm_g, w_mlp_up, w_mlp_down, attn_w_in, attn_w_out, diff_lambda,
              diff_subln_g, conv_w_in, conv_w_out, conv_dw_w, conv_dw_b, conv_ln_g, conv_ln_b,
              sconv_w):
    top_k = min(TOPK_MAX, x.shape[1] // 4)
    for i in range(DEPTH):
        j = i // 2
        h = rms_norm(x, norm_g[i, 0])
        if i % 2 == 0:
            m = attention_mixer(h, attn_w_in[j], attn_w_out[j], rel_bias, diff_lambda[j],
                                diff_subln_g[j], i, top_k)
        else:
            m = conv_mixer(h, conv_w_in[j], conv_w_out[j], conv_dw_w[j], conv_dw_b[j],
                           conv_ln_g[j], conv_ln_b[j], sconv_w[j])
        x = x + rms_norm(m, norm_g[i, 1])
        h = rms_norm(x, norm_g[i, 2])
        u = jax.nn.relu(h @ w_mlp_up[i])
        x = x + rms_norm((u * u) @ w_mlp_down[i], norm_g[i, 3])
    return x
```

```python
import math
import contextlib
import numpy as np
import concourse.bass as bass
import concourse.mybir as mybir
from concourse.bass_utils import run_bass_kernel_spmd

F32 = mybir.dt.float32
BF16 = mybir.dt.bfloat16
ALU = mybir.AluOpType
AF = mybir.ActivationFunctionType
AX = mybir.AxisListType

ENGS = ["pe", "act", "dve", "pool", "sp"]
NDMA_SLOTS = 8
NEG = -1.0e30
EPS = 1e-6
NITER = 16
TOPK = 256
D = 1024
QT0 = 15
NQT = 17


class Sched:
    def __init__(self):
        self.ops = {e: [] for e in ENGS}
        self.last_w = {}
        self.readers = {}
        self.dma_slot_rr = {e: 0 for e in ENGS}
        self.dma_slot_last = {}
        self.dma_slot_uses = {}
        self.pending = {e: set() for e in ENGS}
        self.last_tok = {}
        self.open_dma = set()

    def _deps_for(self, reads, writes):
        deps = set()
        for k in reads:
            deps.update(self.last_w.get(k, {}).values())
        for k in writes:
            deps.update(self.last_w.get(k, {}).values())
            deps.update(self.readers.get(k, {}).values())
        return deps

    def _commit(self, tok, track, reads, writes):
        for k in reads:
            self.readers.setdefault(k, {})[track] = tok
        for k in writes:
            self.last_w[k] = {track: tok}
            self.readers[k] = {}

    def op(self, eng, fn, reads=(), writes=()):
        idx = len(self.ops[eng])
        deps = self._deps_for(reads, writes)
        tok = ("c", eng, idx)
        raw = set()
        if eng != "pe":
            for k in reads:
                raw.update(self.last_w.get(k, {}).values())
        deps = {d for d in deps if not (d[0] == "c" and d[1] == eng) or d in raw}
        deps |= self.pending[eng]
        self.pending[eng] = set()
        self.ops[eng].append(dict(fn=fn, deps=deps, tok=tok, dma=False))
        self._commit(tok, eng, reads, writes)
        self.last_tok[eng] = tok
        return tok

    def dma(self, eng, fn, reads=(), writes=()):
        slot = self.dma_slot_rr[eng]
        self.dma_slot_rr[eng] = (slot + 1) % NDMA_SLOTS
        use = self.dma_slot_uses.get((eng, slot), 0) + 1
        self.dma_slot_uses[(eng, slot)] = use
        tok = ("d", eng, slot, use)
        deps = self._deps_for(reads, writes)
        prev = self.dma_slot_last.get((eng, slot))
        if prev is not None:
            deps.add(prev)
            self.open_dma.discard(prev)
        self.dma_slot_last[(eng, slot)] = tok
        deps |= self.pending[eng]
        self.pending[eng] = set()
        self.ops[eng].append(dict(fn=fn, deps=deps, tok=tok, dma=True))
        self._commit(tok, ("dma", eng, slot), reads, writes)
        self.open_dma.add(tok)
        return tok

    def barrier(self):
        toks = set(self.last_tok.values()) | set(self.open_dma)
        for e in ENGS:
            self.pending[e] |= {t for t in toks if not (t[0] == "c" and t[1] == e)}
        self.open_dma = set()

    def emit(self, nc, final_wait_tokens=()):
        needed = {e: set() for e in ENGS}
        for e in ENGS:
            for o in self.ops[e]:
                for d in o["deps"]:
                    if d[0] == "c":
                        needed[d[1]].add(d[2])
        semval = {e: {} for e in ENGS}
        for e in ENGS:
            c = 0
            for i in range(len(self.ops[e])):
                if i in needed[e]:
                    c += 1
                    semval[e][i] = c
        with contextlib.ExitStack() as st:
            csem = {e: st.enter_context(nc.semaphore("c_" + e)) for e in ENGS if e != "sp"}
            dsem = {}
            for e in ENGS:
                for s in range(NDMA_SLOTS):
                    if (e, s) in self.dma_slot_uses:
                        dsem[(e, s)] = st.enter_context(nc.semaphore("d_%s_%d" % (e, s)))
            block = st.enter_context(nc.Block())

            def resolve(tok):
                if tok[0] == "c":
                    return csem[tok[1]], semval[tok[1]][tok[2]], ("c", tok[1])
                return dsem[(tok[1], tok[2])], 16 * tok[3], ("d", tok[1], tok[2])

            def run(e, engine):
                waited = {}
                for i, o in enumerate(self.ops[e]):
                    ws = {}
                    for d in o["deps"]:
                        sem, val, sk = resolve(d)
                        if waited.get(sk, 0) >= val:
                            continue
                        if sk not in ws or ws[sk][1] < val:
                            ws[sk] = (sem, val)
                    for sk, (sem, val) in ws.items():
                        engine.wait_ge(sem, val)
                        waited[sk] = val
                    ins = o["fn"](engine)
                    if o["dma"]:
                        ins.then_inc(dsem[(o["tok"][1], o["tok"][2])], 16)
                    elif i in needed[e]:
                        ins.then_inc(csem[e], 1)
                if e == "sp":
                    for d in final_wait_tokens:
                        sem, val, sk = resolve(d)
                        engine.wait_ge(sem, val)

            block.tensor(lambda eng: run("pe", eng))
            block.scalar(lambda eng: run("act", eng))
            block.vector(lambda eng: run("dve", eng))
            block.gpsimd(lambda eng: run("pool", eng))
            block.sync(lambda eng: run("sp", eng))


class Arena:
    def __init__(self, ap, nwords):
        self.ap = ap
        self.n = nwords
        self.off = 0

    def alloc(self, free_shape, dt, at=None):
        nel = int(np.prod(free_shape))
        nbytes = nel * (2 if dt == BF16 else 4)
        nw = (nbytes + 3) // 4
        off = self.off if at is None else at
        assert off + nw <= self.n, ("arena overflow", off, nw, self.n)
        a = self.ap[:, off:off + nw]
        if at is None:
            self.off += nw
        if dt == BF16:
            a = a.bitcast(BF16)[:, :nel]
        if len(free_shape) == 2:
            a = a.rearrange("p (a b) -> p a b", a=free_shape[0])
        elif len(free_shape) == 3:
            a = a.rearrange("p (a b c) -> p a b c", a=free_shape[0], b=free_shape[1])
        return a


class Ctx:
    pass


def build_program(debug=False):
    nc = bass.Bass("TRN2", target_bir_lowering=False)
    S = Sched()
    C = Ctx()
    C.nc, C.S = nc, S
    din = lambda n, s: nc.dram_tensor(n, list(s), F32, kind="ExternalInput").ap()
    xloc = din("xloc", [4096, D])
    win = din("win", [D, 2472])
    wout = din("wout", [D, D])
    wup = din("wup", [2, D, 4096])
    wdn = din("wdn", [2, 4096, D])
    cwin = din("cwin", [D, 2560])
    cwout = din("cwout", [D, D])
    gb = din("gb", [8, 128, D])
    biasA_d = din("biasA", [128, 2, 8, 128])
    biasB_d = din("biasB", [128, 2, 8, 128])
    cA_d = din("cA", [128, 8])
    cB_d = din("cB", [128, 8])
    lvb_d = din("lvb", [128, 4, 64])
    sgn_d = din("sgn", [128, 1])
    flags_d = din("flags", [128, 2])
    selA_d = din("selA", [128, 2])
    selI_d = din("selI", [128, 4])
    ident_d = din("ident", [128, 128])
    dwT_d = din("dwT", [128, 4, 31])
    cvec_d = din("cvec", [128, 3, 4])
    scw_d = din("scw", [128, 4, 3])
    out_d = nc.dram_tensor("out", [2048, D], F32, kind="ExternalOutput").ap()
    kind = "ExternalOutput"
    x1d = nc.dram_tensor("x1d", [NQT * 128, D], F32, kind=kind).ap()
    x2d = nc.dram_tensor("x2d", [NQT * 128, D], F32, kind=kind).ap()
    x3d = nc.dram_tensor("x3d", [2048, D], F32, kind=kind).ap()
    ydbg = nc.dram_tensor("ydbg", [NQT, 128, 8, 128], F32, kind="ExternalOutput").ap() if debug else None

    NW = 53000
    with contextlib.ExitStack() as st:
        arena_t = st.enter_context(nc.sbuf_tensor("arena", [128, NW], F32))
        A = Arena(arena_t[:], NW)
        pb = [st.enter_context(nc.psum_tensor("pb%d" % i, [128, 512], F32)) for i in range(7)]
        pbt = st.enter_context(nc.psum_tensor("pbt", [128, 1024], BF16))
        pbk = lambda i: ("pb", i)

        identf = A.alloc((128,), F32)
        identb = A.alloc((128,), BF16)
        onesf = A.alloc((128,), F32)
        onesb = A.alloc((128,), BF16)
        smallc = A.alloc((16,), F32)
        selA = A.alloc((2,), F32)
        selI = A.alloc((4,), F32)
        stat = A.alloc((64,), F32)
        stepc = A.alloc((NITER,), F32)
        P0 = A.off
        epsc, zeroc, flagc, pnegc = smallc[:, 0:1], smallc[:, 1:2], smallc[:, 2:3], smallc[:, 3:4]
        neglam, sgc = smallc[:, 4:5], smallc[:, 5:6]
        C.statctr = 0

        def newstat():
            i = C.statctr % 64
            C.statctr += 1
            return stat[:, i:i + 1], ("stat", i)

        S.dma("sp", lambda e: e.dma_start(out=identf, in_=ident_d), writes=["identf"])
        S.dma("sp", lambda e: e.dma_start(out=smallc[:, 2:4], in_=flags_d), writes=["flags"])
        S.dma("sp", lambda e: e.dma_start(out=selA, in_=selA_d), writes=["selA"])
        S.dma("sp", lambda e: e.dma_start(out=selI, in_=selI_d), writes=["selI"])
        S.op("dve", lambda e: e.tensor_copy(out=identb, in_=identf), reads=["identf"], writes=["identb"])
        S.op("pool", lambda e: e.memset(onesf, 1.0), writes=["onesf"])
        S.op("pool", lambda e: e.memset(onesb, 1.0), writes=["onesb"])
        S.op("pool", lambda e: e.memset(smallc[:, 0:1], EPS), writes=["eps"])
        S.op("pool", lambda e: e.memset(smallc[:, 1:2], 0.0), writes=["zero"])
        for it in range(NITER):
            S.op("pool", lambda e, it=it: e.memset(stepc[:, it:it + 1], 2.0 ** -(it + 1)), writes=["stepc"])

        def rmsnorm_rows(x_ap, x_key, g_ap, g_key, hb_ap, hb_key, junkb):
            ss, ssk = newstat()
            rs, rsk = newstat()
            S.op("act", lambda e: e.activation(out=junkb, in_=x_ap, func=AF.Square, accum_out=ss),
                 reads=[x_key], writes=["junkb", ssk])
            S.op("act", lambda e: e.activation(out=rs, in_=ss, func=AF.Sqrt, scale=1.0 / D, bias=epsc),
                 reads=[ssk, "eps"], writes=[rsk])
            S.op("dve", lambda e: e.reciprocal(out=rs, in_=rs), reads=[rsk], writes=[rsk])
            S.op("dve", lambda e: e.scalar_tensor_tensor(out=hb_ap, in0=x_ap, scalar=rs, in1=g_ap,
                                                         op0=ALU.mult, op1=ALU.mult),
                 reads=[x_key, rsk, g_key], writes=[hb_key])

        def transpose8(hb_ap, hb_key, dst_ap, dst_key, eng="act"):
            pv = pbt[:, 0:1024].rearrange("p (c t) -> p c t", c=8)
            for c in range(8):
                S.op("pe", lambda e, c=c: e.transpose(out=pv[:, c, :], in_=hb_ap[:, c * 128:(c + 1) * 128],
                                                      identity=identb),
                     reads=[hb_key, "identb"], writes=["pbt"])
            if eng == "act":
                S.op("act", lambda e: e.copy(out=dst_ap, in_=pv), reads=["pbt"], writes=[dst_key])
            else:
                S.op("dve", lambda e: e.tensor_copy(out=dst_ap, in_=pv), reads=["pbt"], writes=[dst_key])

        def norm_residual(bank0, bank1, g_ap, g_key, x_ap, x_key, tmp_ap, junkf):
            s0, k0 = newstat()
            s1, k1 = newstat()
            rs, rk = newstat()
            S.op("act", lambda e: e.activation(out=junkf, in_=pb[bank0][:], func=AF.Square, accum_out=s0),
                 reads=[pbk(bank0)], writes=["junkf", k0])
            S.op("act", lambda e: e.activation(out=junkf, in_=pb[bank1][:], func=AF.Square, accum_out=s1),
                 reads=[pbk(bank1)], writes=["junkf", k1])
            S.op("dve", lambda e: e.tensor_tensor(out=rs, in0=s0, in1=s1, op=ALU.add), reads=[k0, k1], writes=[rk])
            S.op("act", lambda e: e.activation(out=rs, in_=rs, func=AF.Sqrt, scale=1.0 / D, bias=epsc),
                 reads=[rk, "eps"], writes=[rk])
            S.op("dve", lambda e: e.reciprocal(out=rs, in_=rs), reads=[rk], writes=[rk])
            for hf, bk in enumerate((bank0, bank1)):
                S.op("dve", lambda e, hf=hf, bk=bk: e.scalar_tensor_tensor(
                    out=tmp_ap[:, hf * 512:(hf + 1) * 512], in0=pb[bk][:], scalar=rs,
                    in1=g_ap[:, hf * 512:(hf + 1) * 512], op0=ALU.mult, op1=ALU.mult),
                    reads=[pbk(bk), rk, g_key], writes=[("tmpn", hf)])
            S.op("pool", lambda e: e.tensor_tensor(out=x_ap, in0=x_ap, in1=tmp_ap, op=ALU.add),
                 reads=[x_key, ("tmpn", 0), ("tmpn", 1)], writes=[x_key])

        def load_w(dst, dst_keyf, src3, ncols, piece):
            for i, c0 in enumerate(range(0, ncols, piece)):
                c1 = min(ncols, c0 + piece)
                S.dma("pool", lambda e, c0=c0, c1=c1: e.dma_start(out=dst[:, :, c0:c1], in_=src3[:, :, c0:c1]),
                      writes=[dst_keyf(i)])

        A.off = P0
        KA = A.alloc((4, 4096), BF16)
        KB2 = A.alloc((4096,), BF16)
        KI4 = A.alloc((4096,), BF16)
        VA = A.alloc((32, 512), BF16)
        VB2 = A.alloc((32, 128), BF16)
        wq = A.alloc((8, 1288), BF16)
        wo = A.alloc((8, 1024), BF16)
        g0 = A.alloc((1024,), F32)
        g1 = A.alloc((1024,), F32)
        biasA = A.alloc((2, 8, 128), BF16)
        biasB = A.alloc((2, 8, 128), BF16)
        xt = A.alloc((1024,), F32)
        hb = A.alloc((1024,), BF16)
        junkb = A.alloc((1024,), BF16)
        OV = A.off
        wk = A.alloc((8, 1408), BF16)
        hT4 = A.alloc((8, 512), BF16)
        xt2 = A.alloc((1024,), F32)
        biasf = A.alloc((2, 8, 128), F32)
        cAB = A.alloc((16,), F32)
        lvb = A.alloc((4, 64), F32)
        lvj = A.alloc((64,), F32)
        kv_end = A.off
        A.off = OV
        sc = A.alloc((4096,), F32)
        junk = A.alloc((2048,), BF16)
        maskT = A.alloc((32, 128), BF16)
        hT = A.alloc((8, 128), BF16)
        qmA = A.alloc((4, 2, 128), BF16)
        qmB = A.alloc((4, 2, 128), BF16)
        qmI = A.alloc((2, 4, 128), BF16)
        wis = A.alloc((8,), F32)
        rbuf = [A.alloc((512,), F32) for _ in range(2)]
        ptb = [A.alloc((512,), BF16) for _ in range(2)]
        pmb = [A.alloc((512,), BF16) for _ in range(2)]
        rz = A.alloc((512,), F32)
        tt = A.alloc((512,), F32)
        dd = A.alloc((256,), F32)
        sq = A.alloc((256,), F32)
        rstd = A.alloc((256,), F32)
        yT = A.alloc((8, 128), BF16)
        tmpn = sc[:, 0:1024]
        junkf = tt
        bis = A.alloc((8,), F32)
        Rs = A.alloc((NITER,), F32)
        q_end = A.off
        assert max(kv_end, q_end) <= NW

        win3 = win.rearrange("(c p) n -> p c n", p=128)
        wkparts = [(0, 512, 512), (512, 2048, 64), (576, 2048, 64), (640, 2432, 32), (672, 2432, 32),
                   (704, 2432, 32), (736, 2432, 32), (768, 1024, 512), (1280, 2112, 64), (1344, 2112, 64)]
        for i, (d0, s0, n) in enumerate(wkparts):
            S.dma("pool", lambda e, d0=d0, s0=s0, n=n: e.dma_start(out=wk[:, :, d0:d0 + n], in_=win3[:, :, s0:s0 + n]),
                  writes=[("wk", i)])
        WK = [("wk", i) for i in range(len(wkparts))]
        wqparts = [(0, 0, 512), (512, 1536, 512), (1024, 2176, 256), (1280, 2464, 8)]
        for i, (d0, s0, n) in enumerate(wqparts):
            S.dma("pool", lambda e, d0=d0, s0=s0, n=n: e.dma_start(out=wq[:, :, d0:d0 + n], in_=win3[:, :, s0:s0 + n]),
                  writes=[("wq", i)])
        WQ = [("wq", i) for i in range(len(wqparts))]
        load_w(wo, lambda i: ("wo", i), wout.rearrange("(c p) n -> p c n", p=128), 1024, 512)
        WO = [("wo", 0), ("wo", 1)]
        S.dma("sp", lambda e: e.dma_start(out=g0, in_=gb[0]), writes=["g0"])
        S.dma("sp", lambda e: e.dma_start(out=g1, in_=gb[1]), writes=["g1"])
        S.dma("sp", lambda e: e.dma_start(out=cAB[:, 0:8], in_=cA_d), writes=["cAB"])
        S.dma("sp", lambda e: e.dma_start(out=cAB[:, 8:16], in_=cB_d), writes=["cAB2"])
        for which, (src, dstb) in enumerate(((biasA_d, biasA), (biasB_d, biasB))):
            S.dma("sp", lambda e, src=src: e.dma_start(out=biasf, in_=src), writes=["biasf"])
            for m in range(8):
                S.op("dve", lambda e, m=m, dstb=dstb, which=which: e.tensor_scalar(
                    out=dstb[:, :, m, :], in0=biasf[:, :, m, :], scalar1=cAB[:, which * 8 + m:which * 8 + m + 1],
                    scalar2=None, op0=ALU.subtract),
                    reads=["biasf", "cAB", "cAB2"], writes=[("bias", which)])
        lam_init = 0.8 - 0.6 * math.exp(-0.3 * 0)
        S.dma("sp", lambda e: e.dma_start(out=lvb, in_=lvb_d), writes=["lvb"])
        S.dma("sp", lambda e: e.dma_start(out=smallc[:, 5:6], in_=sgn_d), writes=["sgraw"])
        e1, e1k = newstat()
        e2, e2k = newstat()
        S.op("dve", lambda e: e.tensor_tensor(out=lvj, in0=lvb[:, 0, :], in1=lvb[:, 1, :], op=ALU.mult),
             reads=["lvb"], writes=["lvj"])
        S.op("dve", lambda e: e.tensor_reduce(out=e1, in_=lvj, axis=AX.X, op=ALU.add), reads=["lvj"], writes=[e1k])
        S.op("dve", lambda e: e.tensor_tensor(out=lvj, in0=lvb[:, 2, :], in1=lvb[:, 3, :], op=ALU.mult),
             reads=["lvb", e1k], writes=["lvj"])
        S.op("dve", lambda e: e.tensor_reduce(out=e2, in_=lvj, axis=AX.X, op=ALU.add), reads=["lvj"], writes=[e2k])
        S.op("act", lambda e: e.activation(out=e1, in_=e1, func=AF.Exp), reads=[e1k], writes=[e1k])
        S.op("act", lambda e: e.activation(out=e2, in_=e2, func=AF.Exp), reads=[e2k], writes=[e2k])
        S.op("dve", lambda e: e.scalar_tensor_tensor(out=neglam, in0=e2, scalar=-lam_init, in1=e1,
                                                     op0=ALU.add, op1=ALU.subtract),
             reads=[e1k, e2k], writes=["neglam"])
        S.op("dve", lambda e: e.tensor_scalar(out=sgc, in0=sgc, scalar1=1.0 - lam_init, scalar2=None, op0=ALU.mult),
             reads=["sgraw"], writes=["sg"])
        S.op("pool", lambda e: e.memset(VB2[:, :, :], 0.0), writes=["VB2"])

        xts = [xt, xt2]
        for g in range(8):
            for i in range(4):
                t = 4 * g + i
                xa = xts[t % 2]
                xk = ("xt", t % 2)
                S.dma("sp", lambda e, t=t, xa=xa: e.dma_start(out=xa, in_=xloc[t * 128:(t + 1) * 128, :]), writes=[xk])
                rmsnorm_rows(xa, xk, g0, "g0", hb, "hb", junkb)
                transpose8(hb, "hb", hT4[:, :, i * 128:(i + 1) * 128], ("hT4", i), eng="act" if i % 2 == 0 else "dve")
            HT4 = [("hT4", i) for i in range(4)]
            for c in range(6):
                bk = c % 2
                for dc in range(8):
                    S.op("pe", lambda e, c=c, dc=dc, bk=bk: e.matmul(pb[bk][:], lhsT=wk[:, dc, c * 128:(c + 1) * 128],
                                                                    rhs=hT4[:, dc, :], start=(dc == 0), stop=(dc == 7)),
                         reads=HT4 + WK, writes=[pbk(bk)])
                if c < 4:
                    dst = KA[:, c, g * 512:(g + 1) * 512]
                elif c == 4:
                    dst = KB2[:, g * 512:(g + 1) * 512]
                else:
                    dst = KI4[:, g * 512:(g + 1) * 512]
                if c % 2 == 0:
                    S.op("act", lambda e, dst=dst, bk=bk: e.copy(out=dst, in_=pb[bk][:]), reads=[pbk(bk)], writes=[("K", c, g)])
                else:
                    S.op("dve", lambda e, dst=dst, bk=bk: e.tensor_copy(out=dst, in_=pb[bk][:]), reads=[pbk(bk)], writes=[("K", c, g)])
            for i in range(4):
                t = 4 * g + i
                for dc in range(8):
                    S.op("pe", lambda e, i=i, dc=dc: e.matmul(pb[2][:], lhsT=hT4[:, dc, i * 128:(i + 1) * 128],
                                                              rhs=wk[:, dc, 768:1280], start=(dc == 0), stop=(dc == 7)),
                         reads=HT4 + WK, writes=[pbk(2)])
                for dc in range(8):
                    S.op("pe", lambda e, i=i, dc=dc: e.matmul(pb[3][:, 0:128], lhsT=hT4[:, dc, i * 128:(i + 1) * 128],
                                                              rhs=wk[:, dc, 1280:1408], start=(dc == 0), stop=(dc == 7)),
                         reads=HT4 + WK, writes=[pbk(3)])
                S.op("act", lambda e, t=t: e.copy(out=VA[:, t, :], in_=pb[2][:]), reads=[pbk(2)], writes=[("VA", t)])
                S.op("dve", lambda e, t=t: e.tensor_copy(out=VB2[:, t, :], in_=pb[3][:, 0:128]),
                     reads=[pbk(3), "VB2"], writes=[("VB", t)])
        S.barrier()

        ALLK = [("K", c, g) for c in range(6) for g in range(8)]
        outs = []
        for qi in range(NQT):
            qt = QT0 + qi
            nk = qt + 1
            N = nk * 128
            xk = ("xt", 0)
            S.dma("sp", lambda e, qt=qt: e.dma_start(out=xt, in_=xloc[qt * 128:(qt + 1) * 128, :]), writes=[xk])
            rmsnorm_rows(xt, xk, g0, "g0", hb, "hb", junkb)
            transpose8(hb, "hb", hT, "hT")
            for c in range(10):
                bank, col = (4, c) if c < 4 else ((5, c - 4) if c < 8 else (6, c - 8))
                for dc in range(8):
                    S.op("pe", lambda e, c=c, dc=dc, bank=bank, col=col: e.matmul(
                        pb[bank][:, col * 128:(col + 1) * 128], lhsT=wq[:, dc, c * 128:(c + 1) * 128], rhs=hT[:, dc, :],
                        start=(dc == 0), stop=(dc == 7)), reads=["hT"] + WQ, writes=[pbk(bank)])
            for dc in range(8):
                S.op("pe", lambda e, dc=dc: e.matmul(pb[6][:, 256:264], lhsT=hT[:, dc, :], rhs=wq[:, dc, 1280:1288],
                                                     start=(dc == 0), stop=(dc == 7)), reads=["hT"] + WQ, writes=[pbk(6)])
            cnt = 0
            for (bank, dstq, nsub, sel) in ((4, qmA, 2, selA), (5, qmB, 2, selA), (6, qmI, 4, selI)):
                for c in range(dstq.shape[1]):
                    for s_ in range(nsub):
                        src = pb[bank][:, c * 128:(c + 1) * 128]
                        dst = dstq[:, c, s_, :]
                        sca = sel[:, s_:s_ + 1]
                        if cnt % 2 == 0:
                            S.op("act", lambda e, src=src, dst=dst, sca=sca: e.activation(out=dst, in_=src, func=AF.Copy, scale=sca),
                                 reads=[pbk(bank), "selA", "selI"], writes=[("qm", bank)])
                        else:
                            S.op("dve", lambda e, src=src, dst=dst, sca=sca: e.tensor_scalar(out=dst, in0=src, scalar1=sca, scalar2=None, op0=ALU.mult),
                                 reads=[pbk(bank), "selA", "selI"], writes=[("qm", bank)])
                        cnt += 1
            S.op("act", lambda e: e.mul(out=wis, in_=pb[6][:, 256:264], mul=(32.0 ** -0.5) * (8.0 ** -0.5)),
                 reads=[pbk(6)], writes=["wis"])
            nkb = (N + 511) // 512
            for kb in range(nkb):
                cols = min(512, N - kb * 512)
                for j in range(8):
                    bk = 4 + (j % 2)
                    S.op("pe", lambda e, j=j, bk=bk, kb=kb, cols=cols: e.matmul(
                        pb[bk][:, 0:cols], lhsT=qmI[:, j // 4, j % 4, :], rhs=KI4[:, kb * 512:kb * 512 + cols],
                        start=True, stop=True), reads=[("qm", 6)] + ALLK, writes=[pbk(bk)])
                    rb = rbuf[j % 2]
                    S.op("act", lambda e, bk=bk, rb=rb, cols=cols: e.activation(out=rb[:, 0:cols], in_=pb[bk][:, 0:cols], func=AF.Relu),
                         reads=[pbk(bk)], writes=[("rbuf", j % 2)])
                    scv = sc[:, kb * 512:kb * 512 + cols]
                    if j == 0:
                        S.op("dve", lambda e, rb=rb, scv=scv, cols=cols: e.tensor_scalar(
                            out=scv, in0=rb[:, 0:cols], scalar1=wis[:, 0:1], scalar2=None, op0=ALU.mult),
                            reads=[("rbuf", 0), "wis"], writes=[("sc", kb)])
                    else:
                        S.op("dve", lambda e, rb=rb, scv=scv, cols=cols, j=j: e.scalar_tensor_tensor(
                            out=scv, in0=rb[:, 0:cols], scalar=wis[:, j:j + 1], in1=scv, op0=ALU.mult, op1=ALU.add),
                            reads=[("rbuf", j % 2), "wis", ("sc", kb)], writes=[("sc", kb)])
            SCK = [("sc", kb) for kb in range(nkb)]
            mx, mn, Rr, lo, tth, cA_, cB_, gg = [bis[:, i:i + 1] for i in range(8)]
            S.op("dve", lambda e, N=N: e.tensor_reduce(out=mx, in_=sc[:, 0:N], axis=AX.X, op=ALU.max), reads=SCK, writes=["b_mx"])
            S.op("dve", lambda e, N=N: e.tensor_reduce(out=lo, in_=sc[:, 0:N], axis=AX.X, op=ALU.min), reads=SCK, writes=["b_lo"])
            S.op("dve", lambda e: e.tensor_tensor(out=Rr, in0=mx, in1=lo, op=ALU.subtract), reads=["b_mx", "b_lo"], writes=["b_R"])
            S.op("pool", lambda e, N=N: e.memset(sc[0:64, N - 64:N], NEG), reads=SCK + ["b_mx", "b_lo"], writes=SCK)
            npv = 15 * 128 if qt == 15 else 2048
            S.op("pool", lambda e, npv=npv: e.tensor_scalar(out=sc[:, 0:npv], in0=sc[:, 0:npv], scalar1=pnegc, scalar2=None, op0=ALU.add),
                 reads=SCK + ["flags"], writes=SCK)
            halves = [(0, N)] if N <= 2048 else [(0, 2048), (2048, N)]
            S.op("dve", lambda e: e.tensor_scalar(out=Rs, in0=stepc, scalar1=Rr, scalar2=None, op0=ALU.mult),
                 reads=["b_R", "stepc"], writes=["b_Rs"])
            for it in range(NITER):
                S.op("dve", lambda e, it=it: e.tensor_tensor(out=tth, in0=Rs[:, it:it + 1], in1=lo, op=ALU.add),
                     reads=["b_Rs", "b_lo"], writes=["b_t"])
                for hi_, (a0, a1) in enumerate(halves):
                    co = cA_ if hi_ == 0 else cB_
                    S.op("dve", lambda e, a0=a0, a1=a1, co=co: e.tensor_scalar(
                        out=junk[:, 0:a1 - a0], in0=sc[:, a0:a1], scalar1=tth, scalar2=None, op0=ALU.is_ge, op1=ALU.add, accum_out=co),
                        reads=SCK + ["b_t"], writes=["junk", ("b_c", hi_)])
                if len(halves) == 2:
                    S.op("dve", lambda e: e.tensor_tensor(out=cA_, in0=cA_, in1=cB_, op=ALU.add),
                         reads=[("b_c", 0), ("b_c", 1)], writes=[("b_c", 0)])
                S.op("dve", lambda e: e.tensor_scalar(out=gg, in0=cA_, scalar1=TOPK - 0.5, scalar2=None, op0=ALU.is_ge),
                     reads=[("b_c", 0)], writes=["b_g"])
                S.op("dve", lambda e, it=it: e.scalar_tensor_tensor(out=lo, in0=gg, scalar=Rs[:, it:it + 1], in1=lo, op0=ALU.mult, op1=ALU.add),
                     reads=["b_g", "b_lo", "b_Rs"], writes=["b_lo"])
            S.op("dve", lambda e, N=N: e.tensor_scalar(out=sc[:, 0:N], in0=sc[:, 0:N], scalar1=lo, scalar2=None, op0=ALU.is_ge),
                 reads=SCK + ["b_lo"], writes=SCK)
            for k4 in range((nk + 3) // 4):
                n4 = min(4, nk - 4 * k4)
                for i in range(n4):
                    kt = 4 * k4 + i
                    S.op("pe", lambda e, kt=kt, i=i: e.transpose(out=pb[6][:, i * 128:(i + 1) * 128], in_=sc[:, kt * 128:(kt + 1) * 128],
                                                                 identity=identf), reads=SCK + ["identf"], writes=[pbk(6)])
                S.op("act", lambda e, k4=k4, n4=n4: e.copy(out=maskT[:, 4 * k4:4 * k4 + n4, :],
                                                           in_=pb[6][:, 0:n4 * 128].rearrange("p (a b) -> p a b", a=n4)),
                     reads=[pbk(6)], writes=[("maskT", k4)])
            MK = [("maskT", k4) for k4 in range((nk + 3) // 4)]
            for (typ, gi) in (("A", 0), ("A", 1), ("B", 0), ("B", 1)):
                for kt in range(nk):
                    sbk = kt % 2
                    band = kt >= qt - 1
                    if band:
                        bt = 0 if kt == qt else 1
                        bsrc = (biasA if typ == "A" else biasB)[:, bt, 4 * gi:4 * gi + 4, :]
                        S.op("pe", lambda e, sbk=sbk, bsrc=bsrc: e.matmul(pb[sbk][:].rearrange("p (a b) -> p a b", a=4), lhsT=identb, rhs=bsrc,
                                                                        start=True, stop=False),
                             reads=["identb", ("bias", 0), ("bias", 1)], writes=[pbk(sbk)])
                    for mi in range(4):
                        if typ == "A":
                            h = 2 * gi + mi // 2
                            lhsT = KA[:, h, kt * 128:(kt + 1) * 128]
                            rhs = qmA[:, h, mi % 2, :]
                            qk = ("qm", 4)
                        else:
                            j = 4 * gi + mi
                            lhsT = KB2[:, kt * 128:(kt + 1) * 128]
                            rhs = qmB[:, j // 2, j % 2, :]
                            qk = ("qm", 5)
                        S.op("pe", lambda e, sbk=sbk, mi=mi, lhsT=lhsT, rhs=rhs, band=band: e.matmul(
                            pb[sbk][:, mi * 128:(mi + 1) * 128], lhsT=lhsT, rhs=rhs, start=(not band), stop=((not band) or mi == 3)),
                            reads=[qk] + ALLK, writes=[pbk(sbk)])
                    masked = (kt <= 14) or (kt == 15 and qt >= 16)
                    bias_ap = pnegc if masked else zeroc
                    pt = ptb[kt % 2]
                    S.op("act", lambda e, sbk=sbk, pt=pt, bias_ap=bias_ap: e.activation(out=pt, in_=pb[sbk][:], func=AF.Exp, bias=bias_ap, scale=1.0),
                         reads=[pbk(sbk), "flags", "zero"], writes=[("pt", kt % 2)])
                    src, srck = pt, ("pt", kt % 2)
                    if typ == "B":
                        pm = pmb[kt % 2]
                        S.op("dve", lambda e, pt=pt, pm=pm, kt=kt: e.tensor_tensor(
                            out=pm.rearrange("p (a b) -> p a b", a=4), in0=pt.rearrange("p (a b) -> p a b", a=4),
                            in1=maskT[:, kt, :].unsqueeze(1).to_broadcast([128, 4, 128]), op=ALU.mult),
                            reads=[("pt", kt % 2)] + MK, writes=[("pm", kt % 2)])
                        src, srck = pm, ("pm", kt % 2)
                    if typ == "A":
                        for hl in range(2):
                            h = 2 * gi + hl
                            ob = 2 if hl == 0 else 4
                            S.op("pe", lambda e, hl=hl, h=h, kt=kt, src=src, nk=nk, ob=ob: e.matmul(
                                pb[ob][:, 0:256], lhsT=VA[:, kt, h * 128:(h + 1) * 128], rhs=src[:, hl * 256:(hl + 1) * 256],
                                start=(kt == 0), stop=(kt == nk - 1)), reads=[srck, ("VA", kt)], writes=[pbk(ob)])
                    else:
                        S.op("pe", lambda e, kt=kt, src=src, nk=nk: e.matmul(pb[2][:], lhsT=VB2[:, kt, :], rhs=src,
                                                                           start=(kt == 0), stop=(kt == nk - 1)),
                             reads=[srck, ("VB", kt)], writes=[pbk(2)])
                    S.op("pe", lambda e, kt=kt, src=src, nk=nk: e.matmul(pb[3][:], lhsT=onesb, rhs=src, start=(kt == 0), stop=(kt == nk - 1)),
                         reads=[srck, "onesb"], writes=[pbk(3)])
                S.op("dve", lambda e: e.reciprocal(out=rz, in_=pb[3][:]), reads=[pbk(3)], writes=["rz"])
                if typ == "A":
                    S.op("dve", lambda e: e.tensor_tensor(out=tt[:, 0:256], in0=pb[2][:, 0:256], in1=rz[:, 0:256], op=ALU.mult),
                         reads=[pbk(2), "rz"], writes=["tt"])
                    S.op("dve", lambda e: e.tensor_tensor(out=tt[:, 256:512], in0=pb[4][:, 0:256], in1=rz[:, 256:512], op=ALU.mult),
                         reads=[pbk(4), "rz", "tt"], writes=["tt"])
                    t4 = tt.rearrange("p (h m q) -> p h m q", h=2, m=2)
                    S.op("dve", lambda e, t4=t4: e.scalar_tensor_tensor(out=dd.rearrange("p (h q) -> p h q", h=2), in0=t4[:, :, 1, :], scalar=neglam,
                                                                      in1=t4[:, :, 0, :], op0=ALU.mult, op1=ALU.add),
                         reads=["tt", "neglam"], writes=["dd"])
                    S.op("pool", lambda e: e.tensor_tensor(out=sq, in0=dd, in1=dd, op=ALU.mult), reads=["dd"], writes=["sq"])
                    S.op("pe", lambda e: e.matmul(pb[6][:, 0:256], lhsT=onesf, rhs=sq, start=True, stop=True),
                         reads=["sq", "onesf"], writes=[pbk(6)])
                    S.op("act", lambda e: e.activation(out=rstd, in_=pb[6][:, 0:256], func=AF.Sqrt, scale=1.0 / 128, bias=epsc),
                         reads=[pbk(6), "eps"], writes=["rstd"])
                    S.op("dve", lambda e: e.reciprocal(out=rstd, in_=rstd), reads=["rstd"], writes=["rstd"])
                    S.op("dve", lambda e, gi=gi: e.scalar_tensor_tensor(out=yT[:, 2 * gi:2 * gi + 2, :], in0=dd.rearrange("p (h q) -> p h q", h=2),
                                                                      scalar=sgc, in1=rstd.rearrange("p (h q) -> p h q", h=2),
                                                                      op0=ALU.mult, op1=ALU.mult),
                         reads=["dd", "rstd", "sg"], writes=[("yT", gi)])
                else:
                    for cl in range(2):
                        ch = 4 + 2 * gi + cl
                        for hf in range(2):
                            jl = 2 * cl + hf
                            S.op("dve", lambda e, ch=ch, hf=hf, jl=jl: e.tensor_tensor(
                                out=yT[hf * 64:(hf + 1) * 64, ch, :], in0=pb[2][hf * 64:(hf + 1) * 64, jl * 128:(jl + 1) * 128],
                                in1=rz[hf * 64:(hf + 1) * 64, jl * 128:(jl + 1) * 128], op=ALU.mult),
                                reads=[pbk(2), "rz"], writes=[("yT", 2 + gi)])
            YT = [("yT", i) for i in range(4)]
            if debug:
                S.dma("pool", lambda e, qi=qi: e.dma_start(out=ydbg[qi], in_=yT), reads=YT, writes=["ydbg"])
            for hf in range(2):
                for c in range(8):
                    S.op("pe", lambda e, hf=hf, c=c: e.matmul(pb[hf][:], lhsT=yT[:, c, :], rhs=wo[:, c, hf * 512:(hf + 1) * 512],
                                                              start=(c == 0), stop=(c == 7)), reads=YT + WO, writes=[pbk(hf)])
            norm_residual(0, 1, g1, "g1", xt, xk, tmpn, junkf)
            outs.append(S.dma("sp", lambda e, qi=qi: e.dma_start(out=x1d[qi * 128:(qi + 1) * 128, :], in_=xt), reads=[xk], writes=["x1d"]))
        S.barrier()

        def mlp_phase(layer, ntiles, src_d, dst_d, gi0):
            A.off = P0
            Wup = A.alloc((8, 4096), BF16)
            Wdn = A.alloc((32, 1024), BF16)
            ga = A.alloc((1024,), F32)
            gb_ = A.alloc((1024,), F32)
            xtm = [A.alloc((1024,), F32) for _ in range(2)]
            hbm = A.alloc((1024,), BF16)
            jb = A.alloc((1024,), BF16)
            hTm = A.alloc((8, 256), BF16)
            uT = A.alloc((32, 256), BF16)
            rb2 = [A.alloc((2, 256), F32) for _ in range(2)]
            tmpm = A.alloc((1024,), F32)
            jf = A.alloc((512,), F32)
            assert A.off <= NW
            load_w(Wup, lambda i: ("Wup", i), wup[layer].rearrange("(c p) n -> p c n", p=128), 4096, 512)
            for p4 in range(4):
                S.dma("pool", lambda e, p4=p4: e.dma_start(out=Wdn[:, 8 * p4:8 * p4 + 8, :],
                                                          in_=wdn[layer].rearrange("(f p) n -> p f n", p=128)[:, 8 * p4:8 * p4 + 8, :]),
                      writes=[("Wdn", p4)])
            S.dma("sp", lambda e: e.dma_start(out=ga, in_=gb[gi0]), writes=["ga"])
            S.dma("sp", lambda e: e.dma_start(out=gb_, in_=gb[gi0 + 1]), writes=["gb"])
            res = []
            groups = [list(range(i, min(i + 2, ntiles))) for i in range(0, ntiles, 2)]
            for grp in groups:
                T = 128 * len(grp)
                for i, t in enumerate(grp):
                    xk = ("xtm", i)
                    S.dma("sp", lambda e, t=t, i=i: e.dma_start(out=xtm[i], in_=src_d[t * 128:(t + 1) * 128, :]), reads=["srcd"], writes=[xk])
                    rmsnorm_rows(xtm[i], xk, ga, "ga", hbm, "hbm", jb)
                    transpose8(hbm, "hbm", hTm[:, :, i * 128:(i + 1) * 128], ("hTm", i), eng="act" if i == 0 else "dve")
                HK = [("hTm", i) for i in range(len(grp))]
                for f2 in range(16):
                    bk = 4 + f2 % 2
                    for fl in range(2):
                        f = 2 * f2 + fl
                        for dc in range(8):
                            S.op("pe", lambda e, f=f, fl=fl, dc=dc, bk=bk, T=T: e.matmul(
                                pb[bk][:, fl * 256:fl * 256 + T], lhsT=Wup[:, dc, f * 128:(f + 1) * 128], rhs=hTm[:, dc, 0:T],
                                start=(dc == 0), stop=(dc == 7)), reads=HK + [("Wup", f // 4)], writes=[pbk(bk)])
                    rb = rb2[f2 % 2]
                    S.op("act", lambda e, bk=bk, rb=rb, T=T: e.activation(
                        out=rb[:, :, 0:T], in_=pb[bk][:].rearrange("p (a b) -> p a b", a=2)[:, :, 0:T], func=AF.Relu),
                        reads=[pbk(bk)], writes=[("rb2", f2 % 2)])
                    S.op("pool", lambda e, rb=rb, f2=f2, T=T: e.tensor_tensor(out=uT[:, 2 * f2:2 * f2 + 2, 0:T], in0=rb[:, :, 0:T],
                                                                           in1=rb[:, :, 0:T], op=ALU.mult),
                         reads=[("rb2", f2 % 2)], writes=[("uT", f2)])
                UK = [("uT", f2) for f2 in range(16)]
                for i, t in enumerate(grp):
                    for hf in range(2):
                        for f in range(32):
                            S.op("pe", lambda e, i=i, hf=hf, f=f: e.matmul(pb[hf][:], lhsT=uT[:, f, i * 128:(i + 1) * 128],
                                                                          rhs=Wdn[:, f, hf * 512:(hf + 1) * 512], start=(f == 0), stop=(f == 31)),
                                 reads=UK + [("Wdn", f // 8)], writes=[pbk(hf)])
                    norm_residual(0, 1, gb_, "gb", xtm[i], ("xtm", i), tmpm, jf)
                    res.append(S.dma("sp", lambda e, t=t, i=i: e.dma_start(out=dst_d[t * 128:(t + 1) * 128, :], in_=xtm[i]),
                                     reads=[("xtm", i)], writes=["dstd"]))
            S.barrier()
            return res

        mlp_phase(0, NQT, x1d, x2d, 2)

        A.off = P0
        wci = A.alloc((8, 2560), BF16)
        wco = A.alloc((8, 1024), BF16)
        dg = A.alloc((31, 4, 128), BF16)
        dwT = A.alloc((4, 31), F32)
        cvec = A.alloc((3, 4), F32)
        scw = A.alloc((4, 3), F32)
        g4 = A.alloc((1024,), F32)
        g5 = A.alloc((1024,), F32)
        xtc = [A.alloc((1024,), F32) for _ in range(2)]
        hbc = A.alloc((1024,), BF16)
        jbc = A.alloc((1024,), BF16)
        hTc = A.alloc((8, 256), BF16)
        uTb = A.alloc((4, 32 + 256), BF16)
        eTb = A.alloc((4, 2 + 256), F32)
        sgb = A.alloc((256,), F32)
        utmp = A.alloc((256,), F32)
        dhs = A.alloc((256,), F32)
        z1 = A.alloc((256,), F32)
        vv = A.alloc((4, 256), F32)
        sqv = A.alloc((4, 256), F32)
        mean = A.alloc((256,), F32)
        msq = A.alloc((256,), F32)
        rsd = A.alloc((256,), F32)
        tcb = A.alloc((256,), F32)
        ymT = A.alloc((8, 256), BF16)
        tmpc = A.alloc((1024,), F32)
        jfc = A.alloc((512,), F32)
        assert A.off <= NW
        load_w(wci, lambda i: ("wci", i), cwin.rearrange("(c p) n -> p c n", p=128), 2560, 512)
        WCI = [("wci", i) for i in range(5)]
        load_w(wco, lambda i: ("wco", i), cwout.rearrange("(c p) n -> p c n", p=128), 1024, 512)
        WCO = [("wco", 0), ("wco", 1)]
        S.dma("sp", lambda e: e.dma_start(out=dwT, in_=dwT_d), writes=["dwT"])
        S.dma("sp", lambda e: e.dma_start(out=cvec, in_=cvec_d), writes=["cvec"])
        S.dma("sp", lambda e: e.dma_start(out=scw, in_=scw_d), writes=["scw"])
        S.dma("sp", lambda e: e.dma_start(out=g4, in_=gb[4]), writes=["g4"])
        S.dma("sp", lambda e: e.dma_start(out=g5, in_=gb[5]), writes=["g5"])
        cnt = 0
        for j in range(31):
            for c in range(4):
                eng = "dve" if cnt % 2 == 0 else "pool"
                S.op(eng, lambda e, j=j, c=c: e.tensor_scalar(out=dg[:, j, c, :], in0=identf, scalar1=dwT[:, c, j:j + 1], scalar2=None, op0=ALU.mult),
                     reads=["identf", "dwT"], writes=[("dg", eng)])
                cnt += 1
        DG = [("dg", "dve"), ("dg", "pool")]
        S.op("pool", lambda e: e.memset(uTb[:, :, :], 0.0), writes=["uTb"])
        S.op("pool", lambda e: e.memset(eTb[:, :, :], 0.0), writes=["eTb"])
        cgroups = [[0]] + [[1 + 2 * i, 2 + 2 * i] for i in range(8)]
        for grp in cgroups:
            T = 128 * len(grp)
            halo = (grp == [0])
            for i, li in enumerate(grp):
                xk = ("xtc", i)
                S.dma("sp", lambda e, li=li, i=i: e.dma_start(out=xtc[i], in_=x2d[li * 128:(li + 1) * 128, :]), reads=["dstd"], writes=[xk])
                rmsnorm_rows(xtc[i], xk, g4, "g4", hbc, "hbc", jbc)
                transpose8(hbc, "hbc", hTc[:, :, i * 128:(i + 1) * 128], ("hTc", i), eng="act" if i == 0 else "dve")
            HK = [("hTc", i) for i in range(len(grp))]

            def proj(bank, col0, T=T, HK=HK):
                for dc in range(8):
                    S.op("pe", lambda e, dc=dc: e.matmul(pb[bank][:, 0:T], lhsT=wci[:, dc, col0:col0 + 128], rhs=hTc[:, dc, 0:T],
                                                         start=(dc == 0), stop=(dc == 7)), reads=HK + WCI, writes=[pbk(bank)])
            for c in range(4):
                proj(0, c * 128)
                proj(1, 512 + c * 128)
                S.op("act", lambda e, T=T: e.activation(out=sgb[:, 0:T], in_=pb[1][:, 0:T], func=AF.Sigmoid), reads=[pbk(1)], writes=["sgb"])
                if halo:
                    S.op("dve", lambda e, T=T: e.tensor_tensor(out=utmp[:, 0:T], in0=pb[0][:, 0:T], in1=sgb[:, 0:T], op=ALU.mult),
                         reads=[pbk(0), "sgb"], writes=["utmp"])
                    S.op("dve", lambda e, c=c, T=T: e.tensor_scalar(out=uTb[:, c, 32:32 + T], in0=utmp[:, 0:T], scalar1=flagc, scalar2=None, op0=ALU.mult),
                         reads=["utmp", "flags", "uTb"], writes=[("uT", c)])
                else:
                    S.op("dve", lambda e, c=c, T=T: e.tensor_tensor(out=uTb[:, c, 32:32 + T], in0=pb[0][:, 0:T], in1=sgb[:, 0:T], op=ALU.mult),
                         reads=[pbk(0), "sgb", "uTb"], writes=[("uT", c)])
            for c in range(4):
                proj(2, 1536 + c * 128)
                proj(3, 2048 + c * 128)
                S.op("act", lambda e, T=T: e.copy(out=dhs[:, 0:T], in_=pb[3][:, 0:T]), reads=[pbk(3)], writes=["dhs"])
                if halo:
                    S.op("dve", lambda e, T=T: e.tensor_tensor(out=utmp[:, 0:T], in0=pb[2][:, 0:T], in1=dhs[:, 0:T], op=ALU.mult),
                         reads=[pbk(2), "dhs"], writes=["utmp"])
                    S.op("dve", lambda e, c=c, T=T: e.tensor_scalar(out=eTb[:, c, 2:2 + T], in0=utmp[:, 0:T], scalar1=flagc, scalar2=None, op0=ALU.mult),
                         reads=["utmp", "flags", "eTb"], writes=[("eT", c)])
                else:
                    S.op("dve", lambda e, c=c, T=T: e.tensor_tensor(out=eTb[:, c, 2:2 + T], in0=pb[2][:, 0:T], in1=dhs[:, 0:T], op=ALU.mult),
                         reads=[pbk(2), "dhs", "eTb"], writes=[("eT", c)])
            if not halo:
                for c in range(4):
                    proj(4, 1024 + c * 128)
                    S.op("dve", lambda e, c=c, T=T: e.tensor_scalar(out=z1[:, 0:T], in0=eTb[:, c, 0:T], scalar1=scw[:, c, 0:1], scalar2=None, op0=ALU.mult),
                         reads=[("eT", c), "scw"], writes=["z1"])
                    for jj in (1, 2):
                        S.op("dve", lambda e, c=c, T=T, jj=jj: e.scalar_tensor_tensor(out=z1[:, 0:T], in0=eTb[:, c, jj:jj + T], scalar=scw[:, c, jj:jj + 1],
                                                                                    in1=z1[:, 0:T], op0=ALU.mult, op1=ALU.add),
                             reads=[("eT", c), "scw", "z1"], writes=["z1"])
                    S.op("dve", lambda e, c=c, T=T: e.tensor_tensor(out=ymT[:, 4 + c, 0:T], in0=pb[4][:, 0:T], in1=z1[:, 0:T], op=ALU.mult),
                         reads=[pbk(4), "z1"], writes=[("ymT", 4 + c)])
                for c in range(4):
                    for j in range(31):
                        S.op("pe", lambda e, c=c, j=j, T=T: e.matmul(pb[5][:, 0:T], lhsT=dg[:, j, c, :], rhs=uTb[:, c, 2 + j:2 + j + T],
                                                                     start=(j == 0), stop=(j == 30)),
                             reads=[("uT", c)] + DG, writes=[pbk(5)])
                    S.op("act", lambda e, c=c, T=T: e.activation(out=vv[:, c, 0:T], in_=pb[5][:, 0:T], func=AF.Identity, bias=cvec[:, 0, c:c + 1], scale=1.0),
                         reads=[pbk(5), "cvec"], writes=[("vv", c)])
                    S.op("act", lambda e, c=c, T=T: e.activation(out=sqv[:, c, 0:T], in_=vv[:, c, 0:T], func=AF.Square),
                         reads=[("vv", c)], writes=[("sqv", c)])
                for c in range(4):
                    S.op("pe", lambda e, c=c, T=T: e.matmul(pb[6][:, 0:T], lhsT=onesf, rhs=vv[:, c, 0:T], start=(c == 0), stop=(c == 3)),
                         reads=[("vv", c), "onesf"], writes=[pbk(6)])
                for c in range(4):
                    S.op("pe", lambda e, c=c, T=T: e.matmul(pb[6][:, 256:256 + T], lhsT=onesf, rhs=sqv[:, c, 0:T], start=(c == 0), stop=(c == 3)),
                         reads=[("sqv", c), "onesf"], writes=[pbk(6)])
                S.op("act", lambda e, T=T: e.mul(out=mean[:, 0:T], in_=pb[6][:, 0:T], mul=1.0 / 512), reads=[pbk(6)], writes=["mean"])
                S.op("pool", lambda e, T=T: e.tensor_tensor(out=msq[:, 0:T], in0=mean[:, 0:T], in1=mean[:, 0:T], op=ALU.mult), reads=["mean"], writes=["msq"])
                S.op("dve", lambda e, T=T: e.scalar_tensor_tensor(out=rsd[:, 0:T], in0=pb[6][:, 256:256 + T], scalar=1.0 / 512, in1=msq[:, 0:T],
                                                                op0=ALU.mult, op1=ALU.subtract), reads=[pbk(6), "msq"], writes=["rsd"])
                S.op("act", lambda e, T=T: e.activation(out=rsd[:, 0:T], in_=rsd[:, 0:T], func=AF.Sqrt, scale=1.0, bias=epsc), reads=["rsd", "eps"], writes=["rsd"])
                S.op("dve", lambda e, T=T: e.reciprocal(out=rsd[:, 0:T], in_=rsd[:, 0:T]), reads=["rsd"], writes=["rsd"])
                for c in range(4):
                    S.op("pool", lambda e, c=c, T=T: e.tensor_tensor(out=tcb[:, 0:T], in0=vv[:, c, 0:T], in1=mean[:, 0:T], op=ALU.subtract),
                         reads=[("vv", c), "mean"], writes=["tcb"])
                    S.op("pool", lambda e, T=T: e.tensor_tensor(out=tcb[:, 0:T], in0=tcb[:, 0:T], in1=rsd[:, 0:T], op=ALU.mult),
                         reads=["tcb", "rsd"], writes=["tcb"])
                    S.op("act", lambda e, c=c, T=T: e.activation(out=ymT[:, c, 0:T], in_=tcb[:, 0:T], func=AF.Silu, scale=cvec[:, 1, c:c + 1],
                                                                bias=cvec[:, 2, c:c + 1]), reads=["tcb", "cvec"], writes=[("ymT", c)])
                YM = [("ymT", c) for c in range(8)]
                for i, li in enumerate(grp):
                    for hf in range(2):
                        for c in range(8):
                            S.op("pe", lambda e, i=i, hf=hf, c=c: e.matmul(pb[hf][:], lhsT=ymT[:, c, i * 128:(i + 1) * 128],
                                                                          rhs=wco[:, c, hf * 512:(hf + 1) * 512], start=(c == 0), stop=(c == 7)),
                                 reads=YM + WCO, writes=[pbk(hf)])
                    norm_residual(0, 1, g5, "g5", xtc[i], ("xtc", i), tmpc, jfc)
                    S.dma("sp", lambda e, li=li, i=i: e.dma_start(out=x3d[(li - 1) * 128:li * 128, :], in_=xtc[i]),
                          reads=[("xtc", i)], writes=["x3d"])
            UT = [("uT", c) for c in range(4)]
            ET = [("eT", c) for c in range(4)]
            S.op("pool", lambda e, T=T: e.tensor_copy(out=uTb[:, :, 0:32], in_=uTb[:, :, T:T + 32]), reads=UT, writes=UT + ["uTb"])
            S.op("pool", lambda e, T=T: e.tensor_copy(out=eTb[:, :, 0:2], in_=eTb[:, :, T:T + 2]), reads=ET, writes=ET + ["eTb"])
        S.barrier()

        A.off = P0

        def final_src_fix():
            pass
        res = mlp_phase_final = None
        res = mlp_phase(1, 16, x3d, out_d, 6)
        S.emit(nc, final_wait_tokens=res)
    return nc


def _t5_bucket_np(rel):
    import jax
    import jax.numpy as jnp
    cpu = jax.devices("cpu")[0]
    with jax.default_device(cpu):
        rel = jnp.asarray(rel, dtype=jnp.int32)
        nb = 16
        ret = jnp.where(rel > 0, nb, 0)
        n = jnp.abs(rel)
        max_exact = nb // 2
        nf = jnp.maximum(n, 1).astype(jnp.float32)
        large = max_exact + (jnp.log(nf / max_exact) / math.log(128 / max_exact) * (nb - max_exact)).astype(jnp.int32)
        large = jnp.minimum(large, nb - 1)
        out = ret + jnp.where(n < max_exact, n, large)
        return np.asarray(out)


_PROG = {}


def _get_prog(debug=False):
    if debug not in _PROG:
        _PROG[debug] = build_program(debug)
    return _PROG[debug]


def make_in_maps(inputs):
    f = lambda a: np.ascontiguousarray(np.asarray(a, dtype=np.float32))
    x = f(inputs["x"])
    rel_bias = f(inputs["rel_bias"])
    norm_g = f(inputs["norm_g"])
    kk = np.arange(128)[:, None]
    qq = np.arange(128)[None, :]
    bD = _t5_bucket_np(kk - qq)
    bP = _t5_bucket_np(kk - 128 - qq)
    admiss = (kk // 64) <= (qq // 64)
    tabA = rel_bias[:, :4]
    tabB = rel_bias[:, 4:]
    biasA = np.zeros((128, 2, 8, 128), np.float32)
    biasB = np.zeros((128, 2, 8, 128), np.float32)
    for m in range(8):
        biasA[:, 0, m, :] = np.where(admiss, tabA[bD, m // 2], np.float32(NEG))
        biasA[:, 1, m, :] = tabA[bP, m // 2]
        biasB[:, 0, m, :] = np.where(admiss, tabB[bD, m], np.float32(NEG))
        biasB[:, 1, m, :] = tabB[bP, m]
    cA = np.ascontiguousarray(np.broadcast_to(np.repeat(tabA[15], 2)[None, :], (128, 8)))
    cB = np.ascontiguousarray(np.broadcast_to(tabB[15][None, :], (128, 8)))
    gbb = np.ascontiguousarray(np.broadcast_to(norm_g.reshape(8, 1, D), (8, 128, D)))
    lvb = np.ascontiguousarray(np.broadcast_to(f(inputs["diff_lambda"])[0][None], (128, 4, 64)))
    sgn = np.ascontiguousarray(f(inputs["diff_subln_g"])[0].reshape(128, 1))
    selA = np.zeros((128, 2), np.float32)
    selA[:64, 0] = 0.125
    selA[64:, 1] = 0.125
    selI = np.zeros((128, 4), np.float32)
    for i in range(4):
        selI[32 * i:32 * i + 32, i] = 1.0
    dwT = np.ascontiguousarray(f(inputs["conv_dw_w"])[0].reshape(31, 4, 128).transpose(2, 1, 0))
    cvec = np.ascontiguousarray(np.stack([f(inputs["conv_dw_b"])[0].reshape(4, 128).T,
                                          f(inputs["conv_ln_g"])[0].reshape(4, 128).T,
                                          f(inputs["conv_ln_b"])[0].reshape(4, 128).T], axis=1))
    scw = np.ascontiguousarray(f(inputs["sconv_w"])[0].reshape(3, 4, 128).transpose(2, 1, 0))
    common = dict(win=f(inputs["attn_w_in"])[0], wout=f(inputs["attn_w_out"])[0], wup=f(inputs["w_mlp_up"]),
                  wdn=f(inputs["w_mlp_down"]), cwin=f(inputs["conv_w_in"])[0], cwout=f(inputs["conv_w_out"])[0],
                  gb=gbb, biasA=biasA, biasB=biasB, cA=cA, cB=cB, lvb=lvb, sgn=sgn, selA=selA, selI=selI,
                  ident=np.eye(128, dtype=np.float32), dwT=dwT, cvec=cvec, scw=scw)
    in_maps = []
    for c in range(8):
        b, h = c // 2, c % 2
        if h == 1:
            xl = x[b]
        else:
            xl = np.concatenate([np.zeros((2048, D), np.float32), x[b, :2048]], axis=0)
        flags = np.zeros((128, 2), np.float32)
        flags[:, 0] = float(h)
        flags[:, 1] = 0.0 if h == 1 else NEG
        m = dict(common)
        m["xloc"] = np.ascontiguousarray(xl)
        m["flags"] = flags
        in_maps.append(m)
    return in_maps


def kernel(**inputs):
    nc = _get_prog(False)
    in_maps = make_in_maps(inputs)
    res = run_bass_kernel_spmd(nc, in_maps, core_ids=list(range(8)))
    out = np.zeros((4, 4096, D), np.float32)
    for c in range(8):
        b, h = c // 2, c % 2
        out[b, h * 2048:(h + 1) * 2048] = res.results[c]["out"]
    return out
```

```python
import math
import contextlib
import numpy as np
import concourse.bass as bass
import concourse.mybir as mybir
from concourse.bass_utils import run_bass_kernel_spmd

F32 = mybir.dt.float32
BF16 = mybir.dt.bfloat16
ALU = mybir.AluOpType
AF = mybir.ActivationFunctionType
AX = mybir.AxisListType

ENGS = ["pe", "act", "dve", "pool", "sp"]
NDMA_SLOTS = 8
NEG = -1.0e30
EPS = 1e-6
NITER = 16
TOPK = 256
D = 1024
QT0 = 15
NQT = 17


class Sched:
    def __init__(self):
        self.ops = {e: [] for e in ENGS}
        self.last_w = {}
        self.readers = {}
        self.dma_slot_rr = {e: 0 for e in ENGS}
        self.dma_slot_last = {}
        self.dma_slot_uses = {}
        self.pending = {e: set() for e in ENGS}
        self.last_tok = {}
        self.open_dma = set()

    def _deps_for(self, reads, writes):
        deps = set()
        for k in reads:
            deps.update(self.last_w.get(k, {}).values())
        for k in writes:
            deps.update(self.last_w.get(k, {}).values())
            deps.update(self.readers.get(k, {}).values())
        return deps

    def _commit(self, tok, track, reads, writes):
        for k in reads:
            self.readers.setdefault(k, {})[track] = tok
        for k in writes:
            self.last_w[k] = {track: tok}
            self.readers[k] = {}

    def op(self, eng, fn, reads=(), writes=()):
        idx = len(self.ops[eng])
        deps = self._deps_for(reads, writes)
        tok = ("c", eng, idx)
        raw = set()
        if eng != "pe":
            for k in reads:
                raw.update(self.last_w.get(k, {}).values())
        deps = {d for d in deps if not (d[0] == "c" and d[1] == eng) or d in raw}
        deps |= self.pending[eng]
        self.pending[eng] = set()
        self.ops[eng].append(dict(fn=fn, deps=deps, tok=tok, dma=False))
        self._commit(tok, eng, reads, writes)
        self.last_tok[eng] = tok
        return tok

    def dma(self, eng, fn, reads=(), writes=()):
        slot = self.dma_slot_rr[eng]
        self.dma_slot_rr[eng] = (slot + 1) % NDMA_SLOTS
        use = self.dma_slot_uses.get((eng, slot), 0) + 1
        self.dma_slot_uses[(eng, slot)] = use
        tok = ("d", eng, slot, use)
        deps = self._deps_for(reads, writes)
        prev = self.dma_slot_last.get((eng, slot))
        if prev is not None:
            deps.add(prev)
            self.open_dma.discard(prev)
        self.dma_slot_last[(eng, slot)] = tok
        deps |= self.pending[eng]
        self.pending[eng] = set()
        self.ops[eng].append(dict(fn=fn, deps=deps, tok=tok, dma=True))
        self._commit(tok, ("dma", eng, slot), reads, writes)
        self.open_dma.add(tok)
        return tok

    def barrier(self):
        toks = set(self.last_tok.values()) | set(self.open_dma)
        for e in ENGS:
            self.pending[e] |= {t for t in toks if not (t[0] == "c" and t[1] == e)}
        self.open_dma = set()

    def emit(self, nc, final_wait_tokens=()):
        needed = {e: set() for e in ENGS}
        for e in ENGS:
            for o in self.ops[e]:
                for d in o["deps"]:
                    if d[0] == "c":
                        needed[d[1]].add(d[2])
        semval = {e: {} for e in ENGS}
        for e in ENGS:
            c = 0
            for i in range(len(self.ops[e])):
                if i in needed[e]:
                    c += 1
                    semval[e][i] = c
        with contextlib.ExitStack() as st:
            csem = {e: st.enter_context(nc.semaphore("c_" + e)) for e in ENGS if e != "sp"}
            dsem = {}
            for e in ENGS:
                for s in range(NDMA_SLOTS):
                    if (e, s) in self.dma_slot_uses:
                        dsem[(e, s)] = st.enter_context(nc.semaphore("d_%s_%d" % (e, s)))
            block = st.enter_context(nc.Block())

            def resolve(tok):
                if tok[0] == "c":
                    return csem[tok[1]], semval[tok[1]][tok[2]], ("c", tok[1])
                return dsem[(tok[1], tok[2])], 16 * tok[3], ("d", tok[1], tok[2])

            def run(e, engine):
                waited = {}
                for i, o in enumerate(self.ops[e]):
                    ws = {}
                    for d in o["deps"]:
                        sem, val, sk = resolve(d)
                        if waited.get(sk, 0) >= val:
                            continue
                        if sk not in ws or ws[sk][1] < val:
                            ws[sk] = (sem, val)
                    for sk, (sem, val) in ws.items():
                        engine.wait_ge(sem, val)
                        waited[sk] = val
                    ins = o["fn"](engine)
                    if o["dma"]:
                        ins.then_inc(dsem[(o["tok"][1], o["tok"][2])], 16)
                    elif i in needed[e]:
                        ins.then_inc(csem[e], 1)
                if e == "sp":
                    for d in final_wait_tokens:
                        sem, val, sk = resolve(d)
                        engine.wait_ge(sem, val)

            block.tensor(lambda eng: run("pe", eng))
            block.scalar(lambda eng: run("act", eng))
            block.vector(lambda eng: run("dve", eng))
            block.gpsimd(lambda eng: run("pool", eng))
            block.sync(lambda eng: run("sp", eng))


class Arena:
    def __init__(self, ap, nwords):
        self.ap = ap
        self.n = nwords
        self.off = 0

    def alloc(self, free_shape, dt, at=None):
        nel = int(np.prod(free_shape))
        nbytes = nel * (2 if dt == BF16 else 4)
        nw = (nbytes + 3) // 4
        off = self.off if at is None else at
        assert off + nw <= self.n, ("arena overflow", off, nw, self.n)
        a = self.ap[:, off:off + nw]
        if at is None:
            self.off += nw
        if dt == BF16:
            a = a.bitcast(BF16)[:, :nel]
        if len(free_shape) == 2:
            a = a.rearrange("p (a b) -> p a b", a=free_shape[0])
        elif len(free_shape) == 3:
            a = a.rearrange("p (a b c) -> p a b c", a=free_shape[0], b=free_shape[1])
        return a


class Ctx:
    pass


def build_program(debug=False):
    nc = bass.Bass("TRN2", target_bir_lowering=False)
    S = Sched()
    C = Ctx()
    C.nc, C.S = nc, S
    din = lambda n, s: nc.dram_tensor(n, list(s), F32, kind="ExternalInput").ap()
    xloc = din("xloc", [4096, D])
    win = din("win", [D, 2472])
    wout = din("wout", [D, D])
    wup = din("wup", [2, D, 4096])
    wdn = din("wdn", [2, 4096, D])
    cwin = din("cwin", [D, 2560])
    cwout = din("cwout", [D, D])
    gb = din("gb", [8, 128, D])
    biasA_d = din("biasA", [128, 2, 8, 128])
    biasB_d = din("biasB", [128, 2, 8, 128])
    cA_d = din("cA", [128, 8])
    cB_d = din("cB", [128, 8])
    lvb_d = din("lvb", [128, 4, 64])
    sgn_d = din("sgn", [128, 1])
    flags_d = din("flags", [128, 2])
    selA_d = din("selA", [128, 2])
    selI_d = din("selI", [128, 4])
    ident_d = din("ident", [128, 128])
    dwT_d = din("dwT", [128, 4, 31])
    cvec_d = din("cvec", [128, 3, 4])
    scw_d = din("scw", [128, 4, 3])
    out_d = nc.dram_tensor("out", [2048, D], F32, kind="ExternalOutput").ap()
    kind = "ExternalOutput"
    x1d = nc.dram_tensor("x1d", [NQT * 128, D], F32, kind=kind).ap()
    x2d = nc.dram_tensor("x2d", [NQT * 128, D], F32, kind=kind).ap()
    x3d = nc.dram_tensor("x3d", [2048, D], F32, kind=kind).ap()
    ydbg = nc.dram_tensor("ydbg", [NQT, 128, 8, 128], F32, kind="ExternalOutput").ap() if debug else None

    NW = 53000
    with contextlib.ExitStack() as st:
        arena_t = st.enter_context(nc.sbuf_tensor("arena", [128, NW], F32))
        A = Arena(arena_t[:], NW)
        pb = [st.enter_context(nc.psum_tensor("pb%d" % i, [128, 512], F32)) for i in range(7)]
        pbt = st.enter_context(nc.psum_tensor("pbt", [128, 1024], BF16))
        pbk = lambda i: ("pb", i)

        identf = A.alloc((128,), F32)
        identb = A.alloc((128,), BF16)
        onesf = A.alloc((128,), F32)
        onesb = A.alloc((128,), BF16)
        smallc = A.alloc((16,), F32)
        selA = A.alloc((2,), F32)
        selI = A.alloc((4,), F32)
        stat = A.alloc((64,), F32)
        stepc = A.alloc((NITER,), F32)
        P0 = A.off
        epsc, zeroc, flagc, pnegc = smallc[:, 0:1], smallc[:, 1:2], smallc[:, 2:3], smallc[:, 3:4]
        neglam, sgc = smallc[:, 4:5], smallc[:, 5:6]
        C.statctr = 0

        def newstat():
            i = C.statctr % 64
            C.statctr += 1
            return stat[:, i:i + 1], ("stat", i)

        S.dma("sp", lambda e: e.dma_start(out=identf, in_=ident_d), writes=["identf"])
        S.dma("sp", lambda e: e.dma_start(out=smallc[:, 2:4], in_=flags_d), writes=["flags"])
        S.dma("sp", lambda e: e.dma_start(out=selA, in_=selA_d), writes=["selA"])
        S.dma("sp", lambda e: e.dma_start(out=selI, in_=selI_d), writes=["selI"])
        S.op("dve", lambda e: e.tensor_copy(out=identb, in_=identf), reads=["identf"], writes=["identb"])
        S.op("pool", lambda e: e.memset(onesf, 1.0), writes=["onesf"])
        S.op("pool", lambda e: e.memset(onesb, 1.0), writes=["onesb"])
        S.op("pool", lambda e: e.memset(smallc[:, 0:1], EPS), writes=["eps"])
        S.op("pool", lambda e: e.memset(smallc[:, 1:2], 0.0), writes=["zero"])
        for it in range(NITER):
            S.op("pool", lambda e, it=it: e.memset(stepc[:, it:it + 1], 2.0 ** -(it + 1)), writes=["stepc"])

        def rmsnorm_rows(x_ap, x_key, g_ap, g_key, hb_ap, hb_key, junkb):
            ss, ssk = newstat()
            rs, rsk = newstat()
            S.op("act", lambda e: e.activation(out=junkb, in_=x_ap, func=AF.Square, accum_out=ss),
                 reads=[x_key], writes=["junkb", ssk])
            S.op("act", lambda e: e.activation(out=rs, in_=ss, func=AF.Sqrt, scale=1.0 / D, bias=epsc),
                 reads=[ssk, "eps"], writes=[rsk])
            S.op("dve", lambda e: e.reciprocal(out=rs, in_=rs), reads=[rsk], writes=[rsk])
            S.op("dve", lambda e: e.scalar_tensor_tensor(out=hb_ap, in0=x_ap, scalar=rs, in1=g_ap,
                                                         op0=ALU.mult, op1=ALU.mult),
                 reads=[x_key, rsk, g_key], writes=[hb_key])

        def transpose8(hb_ap, hb_key, dst_ap, dst_key, eng="act"):
            pv = pbt[:, 0:1024].rearrange("p (c t) -> p c t", c=8)
            for c in range(8):
                S.op("pe", lambda e, c=c: e.transpose(out=pv[:, c, :], in_=hb_ap[:, c * 128:(c + 1) * 128],
                                                      identity=identb),
                     reads=[hb_key, "identb"], writes=["pbt"])
            if eng == "act":
                S.op("act", lambda e: e.copy(out=dst_ap, in_=pv), reads=["pbt"], writes=[dst_key])
            else:
                S.op("dve", lambda e: e.tensor_copy(out=dst_ap, in_=pv), reads=["pbt"], writes=[dst_key])

        def norm_residual(bank0, bank1, g_ap, g_key, x_ap, x_key, tmp_ap, junkf):
            s0, k0 = newstat()
            s1, k1 = newstat()
            rs, rk = newstat()
            S.op("act", lambda e: e.activation(out=junkf, in_=pb[bank0][:], func=AF.Square, accum_out=s0),
                 reads=[pbk(bank0)], writes=["junkf", k0])
            S.op("act", lambda e: e.activation(out=junkf, in_=pb[bank1][:], func=AF.Square, accum_out=s1),
                 reads=[pbk(bank1)], writes=["junkf", k1])
            S.op("dve", lambda e: e.tensor_tensor(out=rs, in0=s0, in1=s1, op=ALU.add), reads=[k0, k1], writes=[rk])
            S.op("act", lambda e: e.activation(out=rs, in_=rs, func=AF.Sqrt, scale=1.0 / D, bias=epsc),
                 reads=[rk, "eps"], writes=[rk])
            S.op("dve", lambda e: e.reciprocal(out=rs, in_=rs), reads=[rk], writes=[rk])
            for hf, bk in enumerate((bank0, bank1)):
                S.op("dve", lambda e, hf=hf, bk=bk: e.scalar_tensor_tensor(
                    out=tmp_ap[:, hf * 512:(hf + 1) * 512], in0=pb[bk][:], scalar=rs,
                    in1=g_ap[:, hf * 512:(hf + 1) * 512], op0=ALU.mult, op1=ALU.mult),
                    reads=[pbk(bk), rk, g_key], writes=[("tmpn", hf)])
            S.op("pool", lambda e: e.tensor_tensor(out=x_ap, in0=x_ap, in1=tmp_ap, op=ALU.add),
                 reads=[x_key, ("tmpn", 0), ("tmpn", 1)], writes=[x_key])

        def load_w(dst, dst_keyf, src3, ncols, piece):
            for i, c0 in enumerate(range(0, ncols, piece)):
                c1 = min(ncols, c0 + piece)
                S.dma("pool", lambda e, c0=c0, c1=c1: e.dma_start(out=dst[:, :, c0:c1], in_=src3[:, :, c0:c1]),
                      writes=[dst_keyf(i)])

        A.off = P0
        KA = A.alloc((4, 4096), BF16)
        KB2 = A.alloc((4096,), BF16)
        KI4 = A.alloc((4096,), BF16)
        VA = A.alloc((32, 512), BF16)
        VB2 = A.alloc((32, 128), BF16)
        wq = A.alloc((8, 1288), BF16)
        wo = A.alloc((8, 1024), BF16)
        g0 = A.alloc((1024,), F32)
        g1 = A.alloc((1024,), F32)
        biasA = A.alloc((2, 8, 128), BF16)
        biasB = A.alloc((2, 8, 128), BF16)
        xt = A.alloc((1024,), F32)
        hb = A.alloc((1024,), BF16)
        junkb = A.alloc((1024,), BF16)
        OV = A.off
        wk = A.alloc((8, 1408), BF16)
        hT4 = A.alloc((8, 512), BF16)
        xt2 = A.alloc((1024,), F32)
        biasf = A.alloc((2, 8, 128), F32)
        cAB = A.alloc((16,), F32)
        lvb = A.alloc((4, 64), F32)
        lvj = A.alloc((64,), F32)
        kv_end = A.off
        A.off = OV
        sc = A.alloc((4096,), F32)
        junk = A.alloc((2048,), BF16)
        maskT = A.alloc((32, 128), BF16)
        hT = A.alloc((8, 128), BF16)
        qmA = A.alloc((4, 2, 128), BF16)
        qmB = A.alloc((4, 2, 128), BF16)
        qmI = A.alloc((2, 4, 128), BF16)
        wis = A.alloc((8,), F32)
        rbuf = [A.alloc((512,), F32) for _ in range(2)]
        ptb = [A.alloc((512,), BF16) for _ in range(2)]
        pmb = [A.alloc((512,), BF16) for _ in range(2)]
        rz = A.alloc((512,), F32)
        tt = A.alloc((512,), F32)
        dd = A.alloc((256,), F32)
        sq = A.alloc((256,), F32)
        rstd = A.alloc((256,), F32)
        yT = A.alloc((8, 128), BF16)
        tmpn = sc[:, 0:1024]
        junkf = tt
        bis = A.alloc((8,), F32)
        Rs = A.alloc((NITER,), F32)
        q_end = A.off
        assert max(kv_end, q_end) <= NW

        win3 = win.rearrange("(c p) n -> p c n", p=128)
        wkparts = [(0, 512, 512), (512, 2048, 64), (576, 2048, 64), (640, 2432, 32), (672, 2432, 32),
                   (704, 2432, 32), (736, 2432, 32), (768, 1024, 512), (1280, 2112, 64), (1344, 2112, 64)]
        for i, (d0, s0, n) in enumerate(wkparts):
            S.dma("pool", lambda e, d0=d0, s0=s0, n=n: e.dma_start(out=wk[:, :, d0:d0 + n], in_=win3[:, :, s0:s0 + n]),
                  writes=[("wk", i)])
        WK = [("wk", i) for i in range(len(wkparts))]
        wqparts = [(0, 0, 512), (512, 1536, 512), (1024, 2176, 256), (1280, 2464, 8)]
        for i, (d0, s0, n) in enumerate(wqparts):
            S.dma("pool", lambda e, d0=d0, s0=s0, n=n: e.dma_start(out=wq[:, :, d0:d0 + n], in_=win3[:, :, s0:s0 + n]),
                  writes=[("wq", i)])
        WQ = [("wq", i) for i in range(len(wqparts))]
        load_w(wo, lambda i: ("wo", i), wout.rearrange("(c p) n -> p c n", p=128), 1024, 512)
        WO = [("wo", 0), ("wo", 1)]
        S.dma("sp", lambda e: e.dma_start(out=g0, in_=gb[0]), writes=["g0"])
        S.dma("sp", lambda e: e.dma_start(out=g1, in_=gb[1]), writes=["g1"])
        S.dma("sp", lambda e: e.dma_start(out=cAB[:, 0:8], in_=cA_d), writes=["cAB"])
        S.dma("sp", lambda e: e.dma_start(out=cAB[:, 8:16], in_=cB_d), writes=["cAB2"])
        for which, (src, dstb) in enumerate(((biasA_d, biasA), (biasB_d, biasB))):
            S.dma("sp", lambda e, src=src: e.dma_start(out=biasf, in_=src), writes=["biasf"])
            for m in range(8):
                S.op("dve", lambda e, m=m, dstb=dstb, which=which: e.tensor_scalar(
                    out=dstb[:, :, m, :], in0=biasf[:, :, m, :], scalar1=cAB[:, which * 8 + m:which * 8 + m + 1],
                    scalar2=None, op0=ALU.subtract),
                    reads=["biasf", "cAB", "cAB2"], writes=[("bias", which)])
        lam_init = 0.8 - 0.6 * math.exp(-0.3 * 0)
        S.dma("sp", lambda e: e.dma_start(out=lvb, in_=lvb_d), writes=["lvb"])
        S.dma("sp", lambda e: e.dma_start(out=smallc[:, 5:6], in_=sgn_d), writes=["sgraw"])
        e1, e1k = newstat()
        e2, e2k = newstat()
        S.op("dve", lambda e: e.tensor_tensor(out=lvj, in0=lvb[:, 0, :], in1=lvb[:, 1, :], op=ALU.mult),
             reads=["lvb"], writes=["lvj"])
        S.op("dve", lambda e: e.tensor_reduce(out=e1, in_=lvj, axis=AX.X, op=ALU.add), reads=["lvj"], writes=[e1k])
        S.op("dve", lambda e: e.tensor_tensor(out=lvj, in0=lvb[:, 2, :], in1=lvb[:, 3, :], op=ALU.mult),
             reads=["lvb", e1k], writes=["lvj"])
        S.op("dve", lambda e: e.tensor_reduce(out=e2, in_=lvj, axis=AX.X, op=ALU.add), reads=["lvj"], writes=[e2k])
        S.op("act", lambda e: e.activation(out=e1, in_=e1, func=AF.Exp), reads=[e1k], writes=[e1k])
        S.op("act", lambda e: e.activation(out=e2, in_=e2, func=AF.Exp), reads=[e2k], writes=[e2k])
        S.op("dve", lambda e: e.scalar_tensor_tensor(out=neglam, in0=e2, scalar=-lam_init, in1=e1,
                                                     op0=ALU.add, op1=ALU.subtract),
             reads=[e1k, e2k], writes=["neglam"])
        S.op("dve", lambda e: e.tensor_scalar(out=sgc, in0=sgc, scalar1=1.0 - lam_init, scalar2=None, op0=ALU.mult),
             reads=["sgraw"], writes=["sg"])
        S.op("pool", lambda e: e.memset(VB2[:, :, :], 0.0), writes=["VB2"])

        xts = [xt, xt2]
        for g in range(8):
            for i in range(4):
                t = 4 * g + i
                xa = xts[t % 2]
                xk = ("xt", t % 2)
                S.dma("sp", lambda e, t=t, xa=xa: e.dma_start(out=xa, in_=xloc[t * 128:(t + 1) * 128, :]), writes=[xk])
                rmsnorm_rows(xa, xk, g0, "g0", hb, "hb", junkb)
                transpose8(hb, "hb", hT4[:, :, i * 128:(i + 1) * 128], ("hT4", i), eng="act" if i % 2 == 0 else "dve")
            HT4 = [("hT4", i) for i in range(4)]
            for c in range(6):
                bk = c % 2
                for dc in range(8):
                    S.op("pe", lambda e, c=c, dc=dc, bk=bk: e.matmul(pb[bk][:], lhsT=wk[:, dc, c * 128:(c + 1) * 128],
                                                                    rhs=hT4[:, dc, :], start=(dc == 0), stop=(dc == 7)),
                         reads=HT4 + WK, writes=[pbk(bk)])
                if c < 4:
                    dst = KA[:, c, g * 512:(g + 1) * 512]
                elif c == 4:
                    dst = KB2[:, g * 512:(g + 1) * 512]
                else:
                    dst = KI4[:, g * 512:(g + 1) * 512]
                if c % 2 == 0:
                    S.op("act", lambda e, dst=dst, bk=bk: e.copy(out=dst, in_=pb[bk][:]), reads=[pbk(bk)], writes=[("K", c, g)])
                else:
                    S.op("dve", lambda e, dst=dst, bk=bk: e.tensor_copy(out=dst, in_=pb[bk][:]), reads=[pbk(bk)], writes=[("K", c, g)])
            for i in range(4):
                t = 4 * g + i
                for dc in range(8):
                    S.op("pe", lambda e, i=i, dc=dc: e.matmul(pb[2][:], lhsT=hT4[:, dc, i * 128:(i + 1) * 128],
                                                              rhs=wk[:, dc, 768:1280], start=(dc == 0), stop=(dc == 7)),
                         reads=HT4 + WK, writes=[pbk(2)])
                for dc in range(8):
                    S.op("pe", lambda e, i=i, dc=dc: e.matmul(pb[3][:, 0:128], lhsT=hT4[:, dc, i * 128:(i + 1) * 128],
                                                              rhs=wk[:, dc, 1280:1408], start=(dc == 0), stop=(dc == 7)),
                         reads=HT4 + WK, writes=[pbk(3)])
                S.op("act", lambda e, t=t: e.copy(out=VA[:, t, :], in_=pb[2][:]), reads=[pbk(2)], writes=[("VA", t)])
                S.op("dve", lambda e, t=t: e.tensor_copy(out=VB2[:, t, :], in_=pb[3][:, 0:128]),
                     reads=[pbk(3), "VB2"], writes=[("VB", t)])
        S.barrier()

        ALLK = [("K", c, g) for c in range(6) for g in range(8)]
        outs = []
        for qi in range(NQT):
            qt = QT0 + qi
            nk = qt + 1
            N = nk * 128
            xk = ("xt", 0)
            S.dma("sp", lambda e, qt=qt: e.dma_start(out=xt, in_=xloc[qt * 128:(qt + 1) * 128, :]), writes=[xk])
            rmsnorm_rows(xt, xk, g0, "g0", hb, "hb", junkb)
            transpose8(hb, "hb", hT, "hT")
            for c in range(10):
                bank, col = (4, c) if c < 4 else ((5, c - 4) if c < 8 else (6, c - 8))
                for dc in range(8):
                    S.op("pe", lambda e, c=c, dc=dc, bank=bank, col=col: e.matmul(
                        pb[bank][:, col * 128:(col + 1) * 128], lhsT=wq[:, dc, c * 128:(c + 1) * 128], rhs=hT[:, dc, :],
                        start=(dc == 0), stop=(dc == 7)), reads=["hT"] + WQ, writes=[pbk(bank)])
            for dc in range(8):
                S.op("pe", lambda e, dc=dc: e.matmul(pb[6][:, 256:264], lhsT=hT[:, dc, :], rhs=wq[:, dc, 1280:1288],
                                                     start=(dc == 0), stop=(dc == 7)), reads=["hT"] + WQ, writes=[pbk(6)])
            cnt = 0
            for (bank, dstq, nsub, sel) in ((4, qmA, 2, selA), (5, qmB, 2, selA), (6, qmI, 4, selI)):
                for c in range(dstq.shape[1]):
                    for s_ in range(nsub):
                        src = pb[bank][:, c * 128:(c + 1) * 128]
                        dst = dstq[:, c, s_, :]
                        sca = sel[:, s_:s_ + 1]
                        if cnt % 2 == 0:
                            S.op("act", lambda e, src=src, dst=dst, sca=sca: e.activation(out=dst, in_=src, func=AF.Copy, scale=sca),
                                 reads=[pbk(bank), "selA", "selI"], writes=[("qm", bank)])
                        else:
                            S.op("dve", lambda e, src=src, dst=dst, sca=sca: e.tensor_scalar(out=dst, in0=src, scalar1=sca, scalar2=None, op0=ALU.mult),
                                 reads=[pbk(bank), "selA", "selI"], writes=[("qm", bank)])
                        cnt += 1
            S.op("act", lambda e: e.mul(out=wis, in_=pb[6][:, 256:264], mul=(32.0 ** -0.5) * (8.0 ** -0.5)),
                 reads=[pbk(6)], writes=["wis"])
            nkb = (N + 511) // 512
            for kb in range(nkb):
                cols = min(512, N - kb * 512)
                for j in range(8):
                    bk = 4 + (j % 2)
                    S.op("pe", lambda e, j=j, bk=bk, kb=kb, cols=cols: e.matmul(
                        pb[bk][:, 0:cols], lhsT=qmI[:, j // 4, j % 4, :], rhs=KI4[:, kb * 512:kb * 512 + cols],
                        start=True, stop=True), reads=[("qm", 6)] + ALLK, writes=[pbk(bk)])
                    rb = rbuf[j % 2]
                    S.op("act", lambda e, bk=bk, rb=rb, cols=cols: e.activation(out=rb[:, 0:cols], in_=pb[bk][:, 0:cols], func=AF.Relu),
                         reads=[pbk(bk)], writes=[("rbuf", j % 2)])
                    scv = sc[:, kb * 512:kb * 512 + cols]
                    if j == 0:
                        S.op("dve", lambda e, rb=rb, scv=scv, cols=cols: e.tensor_scalar(
                            out=scv, in0=rb[:, 0:cols], scalar1=wis[:, 0:1], scalar2=None, op0=ALU.mult),
                            reads=[("rbuf", 0), "wis"], writes=[("sc", kb)])
                    else:
                        S.op("dve", lambda e, rb=rb, scv=scv, cols=cols, j=j: e.scalar_tensor_tensor(
                            out=scv, in0=rb[:, 0:cols], scalar=wis[:, j:j + 1], in1=scv, op0=ALU.mult, op1=ALU.add),
                            reads=[("rbuf", j % 2), "wis", ("sc", kb)], writes=[("sc", kb)])
            SCK = [("sc", kb) for kb in range(nkb)]
            mx, mn, Rr, lo, tth, cA_, cB_, gg = [bis[:, i:i + 1] for i in range(8)]
            S.op("dve", lambda e, N=N: e.tensor_reduce(out=mx, in_=sc[:, 0:N], axis=AX.X, op=ALU.max), reads=SCK, writes=["b_mx"])
            S.op("dve", lambda e, N=N: e.tensor_reduce(out=lo, in_=sc[:, 0:N], axis=AX.X, op=ALU.min), reads=SCK, writes=["b_lo"])
            S.op("dve", lambda e: e.tensor_tensor(out=Rr, in0=mx, in1=lo, op=ALU.subtract), reads=["b_mx", "b_lo"], writes=["b_R"])
            S.op("pool", lambda e, N=N: e.memset(sc[0:64, N - 64:N], NEG), reads=SCK + ["b_mx", "b_lo"], writes=SCK)
            npv = 15 * 128 if qt == 15 else 2048
            S.op("pool", lambda e, npv=npv: e.tensor_scalar(out=sc[:, 0:npv], in0=sc[:, 0:npv], scalar1=pnegc, scalar2=None, op0=ALU.add),
                 reads=SCK + ["flags"], writes=SCK)
            halves = [(0, N)] if N <= 2048 else [(0, 2048), (2048, N)]
            S.op("dve", lambda e: e.tensor_scalar(out=Rs, in0=stepc, scalar1=Rr, scalar2=None, op0=ALU.mult),
                 reads=["b_R", "stepc"], writes=["b_Rs"])

            def bis_iter(it):
                S.op("dve", lambda e: e.tensor_tensor(out=tth, in0=Rs[:, it:it + 1], in1=lo, op=ALU.add),
                     reads=["b_Rs", "b_lo"], writes=["b_t"])
                for hi_, (a0, a1) in enumerate(halves):
                    co = cA_ if hi_ == 0 else cB_
                    S.op("dve", lambda e, a0=a0, a1=a1, co=co: e.tensor_scalar(
                        out=junk[:, 0:a1 - a0], in0=sc[:, a0:a1], scalar1=tth, scalar2=None, op0=ALU.is_ge, op1=ALU.add, accum_out=co),
                        reads=SCK + ["b_t"], writes=["junk", ("b_c", hi_)])
                if len(halves) == 2:
                    S.op("dve", lambda e: e.tensor_tensor(out=cA_, in0=cA_, in1=cB_, op=ALU.add),
                         reads=[("b_c", 0), ("b_c", 1)], writes=[("b_c", 0)])
                S.op("dve", lambda e: e.tensor_scalar(out=gg, in0=cA_, scalar1=TOPK - 0.5, scalar2=None, op0=ALU.is_ge),
                     reads=[("b_c", 0)], writes=["b_g"])
                S.op("dve", lambda e: e.scalar_tensor_tensor(out=lo, in0=gg, scalar=Rs[:, it:it + 1], in1=lo, op0=ALU.mult, op1=ALU.add),
                     reads=["b_g", "b_lo", "b_Rs"], writes=["b_lo"])

            MK = [("maskT", k4) for k4 in range((nk + 3) // 4)]

            def emit_group(typ, gi):
                def qk_stage(kt):
                    sbk = kt % 2
                    band = kt >= qt - 1
                    if band:
                        bt = 0 if kt == qt else 1
                        bsrc = (biasA if typ == "A" else biasB)[:, bt, 4 * gi:4 * gi + 4, :]
                        S.op("pe", lambda e: e.matmul(pb[sbk][:].rearrange("p (a b) -> p a b", a=4), lhsT=identb, rhs=bsrc,
                                                      start=True, stop=False),
                             reads=["identb", ("bias", 0), ("bias", 1)], writes=[pbk(sbk)])
                    for mi in range(4):
                        if typ == "A":
                            h = 2 * gi + mi // 2
                            lhsT = KA[:, h, kt * 128:(kt + 1) * 128]
                            rhs = qmA[:, h, mi % 2, :]
                            qk = ("qm", 4)
                        else:
                            j = 4 * gi + mi
                            lhsT = KB2[:, kt * 128:(kt + 1) * 128]
                            rhs = qmB[:, j // 2, j % 2, :]
                            qk = ("qm", 5)
                        S.op("pe", lambda e, mi=mi, lhsT=lhsT, rhs=rhs: e.matmul(
                            pb[sbk][:, mi * 128:(mi + 1) * 128], lhsT=lhsT, rhs=rhs, start=(not band), stop=((not band) or mi == 3)),
                            reads=[qk] + ALLK, writes=[pbk(sbk)])
                    masked = (kt <= 14) or (kt == 15 and qt >= 16)
                    bias_ap = pnegc if masked else zeroc
                    pt = ptb[kt % 2]
                    S.op("act", lambda e: e.activation(out=pt, in_=pb[sbk][:], func=AF.Exp, bias=bias_ap, scale=1.0),
                         reads=[pbk(sbk), "flags", "zero"], writes=[("pt", kt % 2)])
                    if typ == "B":
                        pm = pmb[kt % 2]
                        S.op("dve", lambda e: e.tensor_tensor(
                            out=pm.rearrange("p (a b) -> p a b", a=4), in0=pt.rearrange("p (a b) -> p a b", a=4),
                            in1=maskT[:, kt, :].unsqueeze(1).to_broadcast([128, 4, 128]), op=ALU.mult),
                            reads=[("pt", kt % 2)] + MK, writes=[("pm", kt % 2)])

                def pv_stage(kt):
                    if typ == "B":
                        src, srck = pmb[kt % 2], ("pm", kt % 2)
                    else:
                        src, srck = ptb[kt % 2], ("pt", kt % 2)
                    if typ == "A":
                        for hl in range(2):
                            h = 2 * gi + hl
                            ob = 2 if hl == 0 else 4
                            S.op("pe", lambda e, hl=hl, h=h, ob=ob: e.matmul(
                                pb[ob][:, 0:256], lhsT=VA[:, kt, h * 128:(h + 1) * 128], rhs=src[:, hl * 256:(hl + 1) * 256],
                                start=(kt == 0), stop=(kt == nk - 1)), reads=[srck, ("VA", kt)], writes=[pbk(ob)])
                    else:
                        S.op("pe", lambda e: e.matmul(pb[2][:], lhsT=VB2[:, kt, :], rhs=src, start=(kt == 0), stop=(kt == nk - 1)),
                             reads=[srck, ("VB", kt)], writes=[pbk(2)])
                    S.op("pe", lambda e: e.matmul(pb[3][:], lhsT=onesb, rhs=src, start=(kt == 0), stop=(kt == nk - 1)),
                         reads=[srck, "onesb"], writes=[pbk(3)])

                qk_stage(0)
                for kt in range(nk):
                    if kt + 1 < nk:
                        qk_stage(kt + 1)
                    pv_stage(kt)
                S.op("dve", lambda e: e.reciprocal(out=rz, in_=pb[3][:]), reads=[pbk(3)], writes=["rz"])
                if typ == "A":
                    S.op("dve", lambda e: e.tensor_tensor(out=tt[:, 0:256], in0=pb[2][:, 0:256], in1=rz[:, 0:256], op=ALU.mult),
                         reads=[pbk(2), "rz"], writes=["tt"])
                    S.op("dve", lambda e: e.tensor_tensor(out=tt[:, 256:512], in0=pb[4][:, 0:256], in1=rz[:, 256:512], op=ALU.mult),
                         reads=[pbk(4), "rz", "tt"], writes=["tt"])
                    t4 = tt.rearrange("p (h m q) -> p h m q", h=2, m=2)
                    S.op("dve", lambda e: e.scalar_tensor_tensor(out=dd.rearrange("p (h q) -> p h q", h=2), in0=t4[:, :, 1, :], scalar=neglam,
                                                                 in1=t4[:, :, 0, :], op0=ALU.mult, op1=ALU.add),
                         reads=["tt", "neglam"], writes=["dd"])
                    S.op("pool", lambda e: e.tensor_tensor(out=sq, in0=dd, in1=dd, op=ALU.mult), reads=["dd"], writes=["sq"])
                    S.op("pe", lambda e: e.matmul(pb[6][:, 0:256], lhsT=onesf, rhs=sq, start=True, stop=True),
                         reads=["sq", "onesf"], writes=[pbk(6)])
                    S.op("act", lambda e: e.activation(out=rstd, in_=pb[6][:, 0:256], func=AF.Sqrt, scale=1.0 / 128, bias=epsc),
                         reads=[pbk(6), "eps"], writes=["rstd"])
                    S.op("dve", lambda e: e.reciprocal(out=rstd, in_=rstd), reads=["rstd"], writes=["rstd"])
                    S.op("dve", lambda e: e.scalar_tensor_tensor(out=yT[:, 2 * gi:2 * gi + 2, :], in0=dd.rearrange("p (h q) -> p h q", h=2),
                                                                 scalar=sgc, in1=rstd.rearrange("p (h q) -> p h q", h=2),
                                                                 op0=ALU.mult, op1=ALU.mult),
                         reads=["dd", "rstd", "sg"], writes=[("yT", gi)])
                else:
                    for cl in range(2):
                        ch = 4 + 2 * gi + cl
                        for hf in range(2):
                            jl = 2 * cl + hf
                            S.op("dve", lambda e, ch=ch, hf=hf, jl=jl: e.tensor_tensor(
                                out=yT[hf * 64:(hf + 1) * 64, ch, :], in0=pb[2][hf * 64:(hf + 1) * 64, jl * 128:(jl + 1) * 128],
                                in1=rz[hf * 64:(hf + 1) * 64, jl * 128:(jl + 1) * 128], op=ALU.mult),
                                reads=[pbk(2), "rz"], writes=[("yT", 2 + gi)])

            for it in range(NITER // 2):
                bis_iter(it)
            emit_group("A", 0)
            for it in range(NITER // 2, NITER):
                bis_iter(it)
            emit_group("A", 1)
            S.op("dve", lambda e, N=N: e.tensor_scalar(out=sc[:, 0:N], in0=sc[:, 0:N], scalar1=lo, scalar2=None, op0=ALU.is_ge),
                 reads=SCK + ["b_lo"], writes=SCK)
            for k4 in range((nk + 3) // 4):
                n4 = min(4, nk - 4 * k4)
                for i in range(n4):
                    kt = 4 * k4 + i
                    S.op("pe", lambda e, kt=kt, i=i: e.transpose(out=pb[6][:, i * 128:(i + 1) * 128], in_=sc[:, kt * 128:(kt + 1) * 128],
                                                                 identity=identf), reads=SCK + ["identf"], writes=[pbk(6)])
                S.op("act", lambda e, k4=k4, n4=n4: e.copy(out=maskT[:, 4 * k4:4 * k4 + n4, :],
                                                           in_=pb[6][:, 0:n4 * 128].rearrange("p (a b) -> p a b", a=n4)),
                     reads=[pbk(6)], writes=[("maskT", k4)])
            emit_group("B", 0)
            emit_group("B", 1)
            YT = [("yT", i) for i in range(4)]
            if debug:
                S.dma("pool", lambda e, qi=qi: e.dma_start(out=ydbg[qi], in_=yT), reads=YT, writes=["ydbg"])
            for hf in range(2):
                for c in range(8):
                    S.op("pe", lambda e, hf=hf, c=c: e.matmul(pb[hf][:], lhsT=yT[:, c, :], rhs=wo[:, c, hf * 512:(hf + 1) * 512],
                                                              start=(c == 0), stop=(c == 7)), reads=YT + WO, writes=[pbk(hf)])
            norm_residual(0, 1, g1, "g1", xt, xk, tmpn, junkf)
            outs.append(S.dma("sp", lambda e, qi=qi: e.dma_start(out=x1d[qi * 128:(qi + 1) * 128, :], in_=xt), reads=[xk], writes=["x1d"]))
        S.barrier()

        def mlp_phase(layer, ntiles, src_d, dst_d, gi0):
            A.off = P0
            Wup = A.alloc((8, 4096), BF16)
            Wdn = A.alloc((32, 1024), BF16)
            ga = A.alloc((1024,), F32)
            gb_ = A.alloc((1024,), F32)
            xtm = [A.alloc((1024,), F32) for _ in range(2)]
            hbm = A.alloc((1024,), BF16)
            jb = A.alloc((1024,), BF16)
            hTm = A.alloc((8, 256), BF16)
            uT = A.alloc((32, 256), BF16)
            rb2 = [A.alloc((2, 256), F32) for _ in range(2)]
            tmpm = A.alloc((1024,), F32)
            jf = A.alloc((512,), F32)
            assert A.off <= NW
            load_w(Wup, lambda i: ("Wup", i), wup[layer].rearrange("(c p) n -> p c n", p=128), 4096, 512)
            for p4 in range(4):
                S.dma("pool", lambda e, p4=p4: e.dma_start(out=Wdn[:, 8 * p4:8 * p4 + 8, :],
                                                          in_=wdn[layer].rearrange("(f p) n -> p f n", p=128)[:, 8 * p4:8 * p4 + 8, :]),
                      writes=[("Wdn", p4)])
            S.dma("sp", lambda e: e.dma_start(out=ga, in_=gb[gi0]), writes=["ga"])
            S.dma("sp", lambda e: e.dma_start(out=gb_, in_=gb[gi0 + 1]), writes=["gb"])
            res = []
            groups = [list(range(i, min(i + 2, ntiles))) for i in range(0, ntiles, 2)]
            for grp in groups:
                T = 128 * len(grp)
                for i, t in enumerate(grp):
                    xk = ("xtm", i)
                    S.dma("sp", lambda e, t=t, i=i: e.dma_start(out=xtm[i], in_=src_d[t * 128:(t + 1) * 128, :]), reads=["srcd"], writes=[xk])
                    rmsnorm_rows(xtm[i], xk, ga, "ga", hbm, "hbm", jb)
                    transpose8(hbm, "hbm", hTm[:, :, i * 128:(i + 1) * 128], ("hTm", i), eng="act" if i == 0 else "dve")
                HK = [("hTm", i) for i in range(len(grp))]
                for f2 in range(16):
                    bk = 4 + f2 % 2
                    for fl in range(2):
                        f = 2 * f2 + fl
                        for dc in range(8):
                            S.op("pe", lambda e, f=f, fl=fl, dc=dc, bk=bk, T=T: e.matmul(
                                pb[bk][:, fl * 256:fl * 256 + T], lhsT=Wup[:, dc, f * 128:(f + 1) * 128], rhs=hTm[:, dc, 0:T],
                                start=(dc == 0), stop=(dc == 7)), reads=HK + [("Wup", f // 4)], writes=[pbk(bk)])
                    rb = rb2[f2 % 2]
                    S.op("act", lambda e, bk=bk, rb=rb, T=T: e.activation(
                        out=rb[:, :, 0:T], in_=pb[bk][:].rearrange("p (a b) -> p a b", a=2)[:, :, 0:T], func=AF.Relu),
                        reads=[pbk(bk)], writes=[("rb2", f2 % 2)])
                    S.op("pool", lambda e, rb=rb, f2=f2, T=T: e.tensor_tensor(out=uT[:, 2 * f2:2 * f2 + 2, 0:T], in0=rb[:, :, 0:T],
                                                                           in1=rb[:, :, 0:T], op=ALU.mult),
                         reads=[("rb2", f2 % 2)], writes=[("uT", f2)])
                UK = [("uT", f2) for f2 in range(16)]
                for i, t in enumerate(grp):
                    for hf in range(2):
                        for f in range(32):
                            S.op("pe", lambda e, i=i, hf=hf, f=f: e.matmul(pb[hf][:], lhsT=uT[:, f, i * 128:(i + 1) * 128],
                                                                          rhs=Wdn[:, f, hf * 512:(hf + 1) * 512], start=(f == 0), stop=(f == 31)),
                                 reads=UK + [("Wdn", f // 8)], writes=[pbk(hf)])
                    norm_residual(0, 1, gb_, "gb", xtm[i], ("xtm", i), tmpm, jf)
                    res.append(S.dma("sp", lambda e, t=t, i=i: e.dma_start(out=dst_d[t * 128:(t + 1) * 128, :], in_=xtm[i]),
                                     reads=[("xtm", i)], writes=["dstd"]))
            S.barrier()
            return res

        mlp_phase(0, NQT, x1d, x2d, 2)

        A.off = P0
        wci = A.alloc((8, 2560), BF16)
        wco = A.alloc((8, 1024), BF16)
        dg = A.alloc((31, 4, 128), BF16)
        dwT = A.alloc((4, 31), F32)
        cvec = A.alloc((3, 4), F32)
        scw = A.alloc((4, 3), F32)
        g4 = A.alloc((1024,), F32)
        g5 = A.alloc((1024,), F32)
        xtc = [A.alloc((1024,), F32) for _ in range(2)]
        hbc = A.alloc((1024,), BF16)
        jbc = A.alloc((1024,), BF16)
        hTc = A.alloc((8, 256), BF16)
        uTb = A.alloc((4, 32 + 256), BF16)
        eTb = A.alloc((4, 2 + 256), F32)
        sgb = A.alloc((256,), F32)
        utmp = A.alloc((256,), F32)
        dhs = A.alloc((256,), F32)
        z1 = A.alloc((256,), F32)
        vv = A.alloc((4, 256), F32)
        sqv = A.alloc((4, 256), F32)
        mean = A.alloc((256,), F32)
        msq = A.alloc((256,), F32)
        rsd = A.alloc((256,), F32)
        tcb = A.alloc((256,), F32)
        ymT = A.alloc((8, 256), BF16)
        tmpc = A.alloc((1024,), F32)
        jfc = A.alloc((512,), F32)
        assert A.off <= NW
        load_w(wci, lambda i: ("wci", i), cwin.rearrange("(c p) n -> p c n", p=128), 2560, 512)
        WCI = [("wci", i) for i in range(5)]
        load_w(wco, lambda i: ("wco", i), cwout.rearrange("(c p) n -> p c n", p=128), 1024, 512)
        WCO = [("wco", 0), ("wco", 1)]
        S.dma("sp", lambda e: e.dma_start(out=dwT, in_=dwT_d), writes=["dwT"])
        S.dma("sp", lambda e: e.dma_start(out=cvec, in_=cvec_d), writes=["cvec"])
        S.dma("sp", lambda e: e.dma_start(out=scw, in_=scw_d), writes=["scw"])
        S.dma("sp", lambda e: e.dma_start(out=g4, in_=gb[4]), writes=["g4"])
        S.dma("sp", lambda e: e.dma_start(out=g5, in_=gb[5]), writes=["g5"])
        cnt = 0
        for j in range(31):
            for c in range(4):
                eng = "dve" if cnt % 2 == 0 else "pool"
                S.op(eng, lambda e, j=j, c=c: e.tensor_scalar(out=dg[:, j, c, :], in0=identf, scalar1=dwT[:, c, j:j + 1], scalar2=None, op0=ALU.mult),
                     reads=["identf", "dwT"], writes=[("dg", eng)])
                cnt += 1
        DG = [("dg", "dve"), ("dg", "pool")]
        S.op("pool", lambda e: e.memset(uTb[:, :, :], 0.0), writes=["uTb"])
        S.op("pool", lambda e: e.memset(eTb[:, :, :], 0.0), writes=["eTb"])
        cgroups = [[0]] + [[1 + 2 * i, 2 + 2 * i] for i in range(8)]
        for grp in cgroups:
            T = 128 * len(grp)
            halo = (grp == [0])
            for i, li in enumerate(grp):
                xk = ("xtc", i)
                S.dma("sp", lambda e, li=li, i=i: e.dma_start(out=xtc[i], in_=x2d[li * 128:(li + 1) * 128, :]), reads=["dstd"], writes=[xk])
                rmsnorm_rows(xtc[i], xk, g4, "g4", hbc, "hbc", jbc)
                transpose8(hbc, "hbc", hTc[:, :, i * 128:(i + 1) * 128], ("hTc", i), eng="act" if i == 0 else "dve")
            HK = [("hTc", i) for i in range(len(grp))]

            def proj(bank, col0, T=T, HK=HK):
                for dc in range(8):
                    S.op("pe", lambda e, dc=dc: e.matmul(pb[bank][:, 0:T], lhsT=wci[:, dc, col0:col0 + 128], rhs=hTc[:, dc, 0:T],
                                                         start=(dc == 0), stop=(dc == 7)), reads=HK + WCI, writes=[pbk(bank)])
            for c in range(4):
                proj(0, c * 128)
                proj(1, 512 + c * 128)
                S.op("act", lambda e, T=T: e.activation(out=sgb[:, 0:T], in_=pb[1][:, 0:T], func=AF.Sigmoid), reads=[pbk(1)], writes=["sgb"])
                if halo:
                    S.op("dve", lambda e, T=T: e.tensor_tensor(out=utmp[:, 0:T], in0=pb[0][:, 0:T], in1=sgb[:, 0:T], op=ALU.mult),
                         reads=[pbk(0), "sgb"], writes=["utmp"])
                    S.op("dve", lambda e, c=c, T=T: e.tensor_scalar(out=uTb[:, c, 32:32 + T], in0=utmp[:, 0:T], scalar1=flagc, scalar2=None, op0=ALU.mult),
                         reads=["utmp", "flags", "uTb"], writes=[("uT", c)])
                else:
                    S.op("dve", lambda e, c=c, T=T: e.tensor_tensor(out=uTb[:, c, 32:32 + T], in0=pb[0][:, 0:T], in1=sgb[:, 0:T], op=ALU.mult),
                         reads=[pbk(0), "sgb", "uTb"], writes=[("uT", c)])
            for c in range(4):
                proj(2, 1536 + c * 128)
                proj(3, 2048 + c * 128)
                S.op("act", lambda e, T=T: e.copy(out=dhs[:, 0:T], in_=pb[3][:, 0:T]), reads=[pbk(3)], writes=["dhs"])
                if halo:
                    S.op("dve", lambda e, T=T: e.tensor_tensor(out=utmp[:, 0:T], in0=pb[2][:, 0:T], in1=dhs[:, 0:T], op=ALU.mult),
                         reads=[pbk(2), "dhs"], writes=["utmp"])
                    S.op("dve", lambda e, c=c, T=T: e.tensor_scalar(out=eTb[:, c, 2:2 + T], in0=utmp[:, 0:T], scalar1=flagc, scalar2=None, op0=ALU.mult),
                         reads=["utmp", "flags", "eTb"], writes=[("eT", c)])
                else:
                    S.op("dve", lambda e, c=c, T=T: e.tensor_tensor(out=eTb[:, c, 2:2 + T], in0=pb[2][:, 0:T], in1=dhs[:, 0:T], op=ALU.mult),
                         reads=[pbk(2), "dhs", "eTb"], writes=[("eT", c)])
            if not halo:
                for c in range(4):
                    proj(4, 1024 + c * 128)
                    S.op("dve", lambda e, c=c, T=T: e.tensor_scalar(out=z1[:, 0:T], in0=eTb[:, c, 0:T], scalar1=scw[:, c, 0:1], scalar2=None, op0=ALU.mult),
                         reads=[("eT", c), "scw"], writes=["z1"])
                    for jj in (1, 2):
                        S.op("dve", lambda e, c=c, T=T, jj=jj: e.scalar_tensor_tensor(out=z1[:, 0:T], in0=eTb[:, c, jj:jj + T], scalar=scw[:, c, jj:jj + 1],
                                                                                    in1=z1[:, 0:T], op0=ALU.mult, op1=ALU.add),
                             reads=[("eT", c), "scw", "z1"], writes=["z1"])
                    S.op("dve", lambda e, c=c, T=T: e.tensor_tensor(out=ymT[:, 4 + c, 0:T], in0=pb[4][:, 0:T], in1=z1[:, 0:T], op=ALU.mult),
                         reads=[pbk(4), "z1"], writes=[("ymT", 4 + c)])
                for c in range(4):
                    for j in range(31):
                        S.op("pe", lambda e, c=c, j=j, T=T: e.matmul(pb[5][:, 0:T], lhsT=dg[:, j, c, :], rhs=uTb[:, c, 2 + j:2 + j + T],
                                                                     start=(j == 0), stop=(j == 30)),
                             reads=[("uT", c)] + DG, writes=[pbk(5)])
                    S.op("act", lambda e, c=c, T=T: e.activation(out=vv[:, c, 0:T], in_=pb[5][:, 0:T], func=AF.Identity, bias=cvec[:, 0, c:c + 1], scale=1.0),
                         reads=[pbk(5), "cvec"], writes=[("vv", c)])
                    S.op("act", lambda e, c=c, T=T: e.activation(out=sqv[:, c, 0:T], in_=vv[:, c, 0:T], func=AF.Square),
                         reads=[("vv", c)], writes=[("sqv", c)])
                for c in range(4):
                    S.op("pe", lambda e, c=c, T=T: e.matmul(pb[6][:, 0:T], lhsT=onesf, rhs=vv[:, c, 0:T], start=(c == 0), stop=(c == 3)),
                         reads=[("vv", c), "onesf"], writes=[pbk(6)])
                for c in range(4):
                    S.op("pe", lambda e, c=c, T=T: e.matmul(pb[6][:, 256:256 + T], lhsT=onesf, rhs=sqv[:, c, 0:T], start=(c == 0), stop=(c == 3)),
                         reads=[("sqv", c), "onesf"], writes=[pbk(6)])
                S.op("act", lambda e, T=T: e.mul(out=mean[:, 0:T], in_=pb[6][:, 0:T], mul=1.0 / 512), reads=[pbk(6)], writes=["mean"])
                S.op("pool", lambda e, T=T: e.tensor_tensor(out=msq[:, 0:T], in0=mean[:, 0:T], in1=mean[:, 0:T], op=ALU.mult), reads=["mean"], writes=["msq"])
                S.op("dve", lambda e, T=T: e.scalar_tensor_tensor(out=rsd[:, 0:T], in0=pb[6][:, 256:256 + T], scalar=1.0 / 512, in1=msq[:, 0:T],
                                                                op0=ALU.mult, op1=ALU.subtract), reads=[pbk(6), "msq"], writes=["rsd"])
                S.op("act", lambda e, T=T: e.activation(out=rsd[:, 0:T], in_=rsd[:, 0:T], func=AF.Sqrt, scale=1.0, bias=epsc), reads=["rsd", "eps"], writes=["rsd"])
                S.op("dve", lambda e, T=T: e.reciprocal(out=rsd[:, 0:T], in_=rsd[:, 0:T]), reads=["rsd"], writes=["rsd"])
                for c in range(4):
                    S.op("pool", lambda e, c=c, T=T: e.tensor_tensor(out=tcb[:, 0:T], in0=vv[:, c, 0:T], in1=mean[:, 0:T], op=ALU.subtract),
                         reads=[("vv", c), "mean"], writes=["tcb"])
                    S.op("pool", lambda e, T=T: e.tensor_tensor(out=tcb[:, 0:T], in0=tcb[:, 0:T], in1=rsd[:, 0:T], op=ALU.mult),
                         reads=["tcb", "rsd"], writes=["tcb"])
                    S.op("act", lambda e, c=c, T=T: e.activation(out=ymT[:, c, 0:T], in_=tcb[:, 0:T], func=AF.Silu, scale=cvec[:, 1, c:c + 1],
                                                                bias=cvec[:, 2, c:c + 1]), reads=["tcb", "cvec"], writes=[("ymT", c)])
                YM = [("ymT", c) for c in range(8)]
                for i, li in enumerate(grp):
                    for hf in range(2):
                        for c in range(8):
                            S.op("pe", lambda e, i=i, hf=hf, c=c: e.matmul(pb[hf][:], lhsT=ymT[:, c, i * 128:(i + 1) * 128],
                                                                          rhs=wco[:, c, hf * 512:(hf + 1) * 512], start=(c == 0), stop=(c == 7)),
                                 reads=YM + WCO, writes=[pbk(hf)])
                    norm_residual(0, 1, g5, "g5", xtc[i], ("xtc", i), tmpc, jfc)
                    S.dma("sp", lambda e, li=li, i=i: e.dma_start(out=x3d[(li - 1) * 128:li * 128, :], in_=xtc[i]),
                          reads=[("xtc", i)], writes=["x3d"])
            UT = [("uT", c) for c in range(4)]
            ET = [("eT", c) for c in range(4)]
            S.op("pool", lambda e, T=T: e.tensor_copy(out=uTb[:, :, 0:32], in_=uTb[:, :, T:T + 32]), reads=UT, writes=UT + ["uTb"])
            S.op("pool", lambda e, T=T: e.tensor_copy(out=eTb[:, :, 0:2], in_=eTb[:, :, T:T + 2]), reads=ET, writes=ET + ["eTb"])
        S.barrier()

        A.off = P0

        def final_src_fix():
            pass
        res = mlp_phase_final = None
        res = mlp_phase(1, 16, x3d, out_d, 6)
        S.emit(nc, final_wait_tokens=res)
    return nc


def _t5_bucket_np(rel):
    import jax
    import jax.numpy as jnp
    cpu = jax.devices("cpu")[0]
    with jax.default_device(cpu):
        rel = jnp.asarray(rel, dtype=jnp.int32)
        nb = 16
        ret = jnp.where(rel > 0, nb, 0)
        n = jnp.abs(rel)
        max_exact = nb // 2
        nf = jnp.maximum(n, 1).astype(jnp.float32)
        large = max_exact + (jnp.log(nf / max_exact) / math.log(128 / max_exact) * (nb - max_exact)).astype(jnp.int32)
        large = jnp.minimum(large, nb - 1)
        out = ret + jnp.where(n < max_exact, n, large)
        return np.asarray(out)


_PROG = {}


def _get_prog(debug=False):
    if debug not in _PROG:
        _PROG[debug] = build_program(debug)
    return _PROG[debug]


def make_in_maps(inputs):
    f = lambda a: np.ascontiguousarray(np.asarray(a, dtype=np.float32))
    x = f(inputs["x"])
    rel_bias = f(inputs["rel_bias"])
    norm_g = f(inputs["norm_g"])
    kk = np.arange(128)[:, None]
    qq = np.arange(128)[None, :]
    bD = _t5_bucket_np(kk - qq)
    bP = _t5_bucket_np(kk - 128 - qq)
    admiss = (kk // 64) <= (qq // 64)
    tabA = rel_bias[:, :4]
    tabB = rel_bias[:, 4:]
    biasA = np.zeros((128, 2, 8, 128), np.float32)
    biasB = np.zeros((128, 2, 8, 128), np.float32)
    for m in range(8):
        biasA[:, 0, m, :] = np.where(admiss, tabA[bD, m // 2], np.float32(NEG))
        biasA[:, 1, m, :] = tabA[bP, m // 2]
        biasB[:, 0, m, :] = np.where(admiss, tabB[bD, m], np.float32(NEG))
        biasB[:, 1, m, :] = tabB[bP, m]
    cA = np.ascontiguousarray(np.broadcast_to(np.repeat(tabA[15], 2)[None, :], (128, 8)))
    cB = np.ascontiguousarray(np.broadcast_to(tabB[15][None, :], (128, 8)))
    gbb = np.ascontiguousarray(np.broadcast_to(norm_g.reshape(8, 1, D), (8, 128, D)))
    lvb = np.ascontiguousarray(np.broadcast_to(f(inputs["diff_lambda"])[0][None], (128, 4, 64)))
    sgn = np.ascontiguousarray(f(inputs["diff_subln_g"])[0].reshape(128, 1))
    selA = np.zeros((128, 2), np.float32)
    selA[:64, 0] = 0.125
    selA[64:, 1] = 0.125
    selI = np.zeros((128, 4), np.float32)
    for i in range(4):
        selI[32 * i:32 * i + 32, i] = 1.0
    dwT = np.ascontiguousarray(f(inputs["conv_dw_w"])[0].reshape(31, 4, 128).transpose(2, 1, 0))
    cvec = np.ascontiguousarray(np.stack([f(inputs["conv_dw_b"])[0].reshape(4, 128).T,
                                          f(inputs["conv_ln_g"])[0].reshape(4, 128).T,
                                          f(inputs["conv_ln_b"])[0].reshape(4, 128).T], axis=1))
    scw = np.ascontiguousarray(f(inputs["sconv_w"])[0].reshape(3, 4, 128).transpose(2, 1, 0))
    common = dict(win=f(inputs["attn_w_in"])[0], wout=f(inputs["attn_w_out"])[0], wup=f(inputs["w_mlp_up"]),
                  wdn=f(inputs["w_mlp_down"]), cwin=f(inputs["conv_w_in"])[0], cwout=f(inputs["conv_w_out"])[0],
                  gb=gbb, biasA=biasA, biasB=biasB, cA=cA, cB=cB, lvb=lvb, sgn=sgn, selA=selA, selI=selI,
                  ident=np.eye(128, dtype=np.float32), dwT=dwT, cvec=cvec, scw=scw)
    in_maps = []
    for c in range(8):
        b, h = c // 2, c % 2
        if h == 1:
            xl = x[b]
        else:
            xl = np.concatenate([np.zeros((2048, D), np.float32), x[b, :2048]], axis=0)
        flags = np.zeros((128, 2), np.float32)
        flags[:, 0] = float(h)
        flags[:, 1] = 0.0 if h == 1 else NEG
        m = dict(common)
        m["xloc"] = np.ascontiguousarray(xl)
        m["flags"] = flags
        in_maps.append(m)
    return in_maps


def kernel(**inputs):
    nc = _get_prog(False)
    in_maps = make_in_maps(inputs)
    res = run_bass_kernel_spmd(nc, in_maps, core_ids=list(range(8)))
    out = np.zeros((4, 4096, D), np.float32)
    for c in range(8):
        b, h = c // 2, c % 2
        out[b, h * 2048:(h + 1) * 2048] = res.results[c]["out"]
    return out
```

```python
import math
import contextlib
import numpy as np
import concourse.bass as bass
import concourse.mybir as mybir
from concourse.bass_utils import run_bass_kernel_spmd

F32 = mybir.dt.float32
BF16 = mybir.dt.bfloat16
ALU = mybir.AluOpType
AF = mybir.ActivationFunctionType
AX = mybir.AxisListType

ENGS = ["pe", "act", "dve", "pool", "sp"]
NDMA_SLOTS = 8
NEG = -1.0e30
EPS = 1e-6
NITER = 16
TOPK = 256
D = 1024
QT0 = 15
NQT = 17


class Sched:
    def __init__(self):
        self.ops = {e: [] for e in ENGS}
        self.last_w = {}
        self.readers = {}
        self.dma_slot_rr = {e: 0 for e in ENGS}
        self.dma_slot_last = {}
        self.dma_slot_uses = {}
        self.pending = {e: set() for e in ENGS}
        self.last_tok = {}
        self.open_dma = set()

    def _deps_for(self, reads, writes):
        deps = set()
        for k in reads:
            deps.update(self.last_w.get(k, {}).values())
        for k in writes:
            deps.update(self.last_w.get(k, {}).values())
            deps.update(self.readers.get(k, {}).values())
        return deps

    def _commit(self, tok, track, reads, writes):
        for k in reads:
            self.readers.setdefault(k, {})[track] = tok
        for k in writes:
            self.last_w[k] = {track: tok}
            self.readers[k] = {}

    def op(self, eng, fn, reads=(), writes=()):
        idx = len(self.ops[eng])
        deps = self._deps_for(reads, writes)
        tok = ("c", eng, idx)
        raw = set()
        if eng != "pe":
            for k in reads:
                raw.update(self.last_w.get(k, {}).values())
        deps = {d for d in deps if not (d[0] == "c" and d[1] == eng) or d in raw}
        deps |= self.pending[eng]
        self.pending[eng] = set()
        self.ops[eng].append(dict(fn=fn, deps=deps, tok=tok, dma=False))
        self._commit(tok, eng, reads, writes)
        self.last_tok[eng] = tok
        return tok

    def dma(self, eng, fn, reads=(), writes=()):
        slot = self.dma_slot_rr[eng]
        self.dma_slot_rr[eng] = (slot + 1) % NDMA_SLOTS
        use = self.dma_slot_uses.get((eng, slot), 0) + 1
        self.dma_slot_uses[(eng, slot)] = use
        tok = ("d", eng, slot, use)
        deps = self._deps_for(reads, writes)
        prev = self.dma_slot_last.get((eng, slot))
        if prev is not None:
            deps.add(prev)
            self.open_dma.discard(prev)
        self.dma_slot_last[(eng, slot)] = tok
        deps |= self.pending[eng]
        self.pending[eng] = set()
        self.ops[eng].append(dict(fn=fn, deps=deps, tok=tok, dma=True))
        self._commit(tok, ("dma", eng, slot), reads, writes)
        self.open_dma.add(tok)
        return tok

    def barrier(self):
        toks = set(self.last_tok.values()) | set(self.open_dma)
        for e in ENGS:
            self.pending[e] |= {t for t in toks if not (t[0] == "c" and t[1] == e)}
        self.open_dma = set()

    def emit(self, nc, final_wait_tokens=()):
        needed = {e: set() for e in ENGS}
        for e in ENGS:
            for o in self.ops[e]:
                for d in o["deps"]:
                    if d[0] == "c":
                        needed[d[1]].add(d[2])
        semval = {e: {} for e in ENGS}
        for e in ENGS:
            c = 0
            for i in range(len(self.ops[e])):
                if i in needed[e]:
                    c += 1
                    semval[e][i] = c
        with contextlib.ExitStack() as st:
            csem = {e: st.enter_context(nc.semaphore("c_" + e)) for e in ENGS if e != "sp"}
            dsem = {}
            for e in ENGS:
                for s in range(NDMA_SLOTS):
                    if (e, s) in self.dma_slot_uses:
                        dsem[(e, s)] = st.enter_context(nc.semaphore("d_%s_%d" % (e, s)))
            block = st.enter_context(nc.Block())

            def resolve(tok):
                if tok[0] == "c":
                    return csem[tok[1]], semval[tok[1]][tok[2]], ("c", tok[1])
                return dsem[(tok[1], tok[2])], 16 * tok[3], ("d", tok[1], tok[2])

            def run(e, engine):
                waited = {}
                for i, o in enumerate(self.ops[e]):
                    ws = {}
                    for d in o["deps"]:
                        sem, val, sk = resolve(d)
                        if waited.get(sk, 0) >= val:
                            continue
                        if sk not in ws or ws[sk][1] < val:
                            ws[sk] = (sem, val)
                    for sk, (sem, val) in ws.items():
                        engine.wait_ge(sem, val)
                        waited[sk] = val
                    ins = o["fn"](engine)
                    if o["dma"]:
                        ins.then_inc(dsem[(o["tok"][1], o["tok"][2])], 16)
                    elif i in needed[e]:
                        ins.then_inc(csem[e], 1)
                if e == "sp":
                    for d in final_wait_tokens:
                        sem, val, sk = resolve(d)
                        engine.wait_ge(sem, val)

            block.tensor(lambda eng: run("pe", eng))
            block.scalar(lambda eng: run("act", eng))
            block.vector(lambda eng: run("dve", eng))
            block.gpsimd(lambda eng: run("pool", eng))
            block.sync(lambda eng: run("sp", eng))


class Arena:
    def __init__(self, ap, nwords):
        self.ap = ap
        self.n = nwords
        self.off = 0

    def alloc(self, free_shape, dt, at=None):
        nel = int(np.prod(free_shape))
        nbytes = nel * (2 if dt == BF16 else 4)
        nw = (nbytes + 3) // 4
        off = self.off if at is None else at
        assert off + nw <= self.n, ("arena overflow", off, nw, self.n)
        a = self.ap[:, off:off + nw]
        if at is None:
            self.off += nw
        if dt == BF16:
            a = a.bitcast(BF16)[:, :nel]
        if len(free_shape) == 2:
            a = a.rearrange("p (a b) -> p a b", a=free_shape[0])
        elif len(free_shape) == 3:
            a = a.rearrange("p (a b c) -> p a b c", a=free_shape[0], b=free_shape[1])
        return a


class Ctx:
    pass


def build_program(debug=False):
    nc = bass.Bass("TRN2", target_bir_lowering=False)
    S = Sched()
    C = Ctx()
    C.nc, C.S = nc, S
    din = lambda n, s: nc.dram_tensor(n, list(s), F32, kind="ExternalInput").ap()
    xloc = din("xloc", [4096, D])
    win = din("win", [D, 2472])
    wout = din("wout", [D, D])
    wup = din("wup", [2, D, 4096])
    wdn = din("wdn", [2, 4096, D])
    cwin = din("cwin", [D, 2560])
    cwout = din("cwout", [D, D])
    gb = din("gb", [8, 128, D])
    biasA_d = din("biasA", [128, 2, 8, 128])
    biasB_d = din("biasB", [128, 2, 8, 128])
    cA_d = din("cA", [128, 8])
    cB_d = din("cB", [128, 8])
    lvb_d = din("lvb", [128, 4, 64])
    sgn_d = din("sgn", [128, 1])
    flags_d = din("flags", [128, 2])
    selA_d = din("selA", [128, 2])
    selI_d = din("selI", [128, 4])
    ident_d = din("ident", [128, 128])
    dwT_d = din("dwT", [128, 4, 31])
    cvec_d = din("cvec", [128, 3, 4])
    scw_d = din("scw", [128, 4, 3])
    out_d = nc.dram_tensor("out", [2048, D], F32, kind="ExternalOutput").ap()
    kind = "ExternalOutput"
    x1d = nc.dram_tensor("x1d", [NQT * 128, D], F32, kind=kind).ap()
    x2d = nc.dram_tensor("x2d", [NQT * 128, D], F32, kind=kind).ap()
    x3d = nc.dram_tensor("x3d", [2048, D], F32, kind=kind).ap()
    ydbg = nc.dram_tensor("ydbg", [NQT, 128, 8, 128], F32, kind="ExternalOutput").ap() if debug else None

    NW = 53000
    with contextlib.ExitStack() as st:
        arena_t = st.enter_context(nc.sbuf_tensor("arena", [128, NW], F32))
        A = Arena(arena_t[:], NW)
        pb = [st.enter_context(nc.psum_tensor("pb%d" % i, [128, 512], F32)) for i in range(7)]
        pbt = st.enter_context(nc.psum_tensor("pbt", [128, 1024], BF16))
        pbk = lambda i: ("pb", i)

        identf = A.alloc((128,), F32)
        identb = A.alloc((128,), BF16)
        onesf = A.alloc((128,), F32)
        onesb = A.alloc((128,), BF16)
        smallc = A.alloc((16,), F32)
        selA = A.alloc((2,), F32)
        selI = A.alloc((4,), F32)
        stat = A.alloc((64,), F32)
        stepc = A.alloc((NITER,), F32)
        P0 = A.off
        epsc, zeroc, flagc, pnegc = smallc[:, 0:1], smallc[:, 1:2], smallc[:, 2:3], smallc[:, 3:4]
        neglam, sgc = smallc[:, 4:5], smallc[:, 5:6]
        C.statctr = 0

        def newstat():
            i = C.statctr % 64
            C.statctr += 1
            return stat[:, i:i + 1], ("stat", i)

        S.dma("sp", lambda e: e.dma_start(out=identf, in_=ident_d), writes=["identf"])
        S.dma("sp", lambda e: e.dma_start(out=smallc[:, 2:4], in_=flags_d), writes=["flags"])
        S.dma("sp", lambda e: e.dma_start(out=selA, in_=selA_d), writes=["selA"])
        S.dma("sp", lambda e: e.dma_start(out=selI, in_=selI_d), writes=["selI"])
        S.op("dve", lambda e: e.tensor_copy(out=identb, in_=identf), reads=["identf"], writes=["identb"])
        S.op("pool", lambda e: e.memset(onesf, 1.0), writes=["onesf"])
        S.op("pool", lambda e: e.memset(onesb, 1.0), writes=["onesb"])
        S.op("pool", lambda e: e.memset(smallc[:, 0:1], EPS), writes=["eps"])
        S.op("pool", lambda e: e.memset(smallc[:, 1:2], 0.0), writes=["zero"])
        for it in range(NITER):
            S.op("pool", lambda e, it=it: e.memset(stepc[:, it:it + 1], 2.0 ** -(it + 1)), writes=["stepc"])

        def rmsnorm_rows(x_ap, x_key, g_ap, g_key, hb_ap, hb_key, junkb):
            ss, ssk = newstat()
            rs, rsk = newstat()
            S.op("act", lambda e: e.activation(out=junkb, in_=x_ap, func=AF.Square, accum_out=ss),
                 reads=[x_key], writes=["junkb", ssk])
            S.op("act", lambda e: e.activation(out=rs, in_=ss, func=AF.Sqrt, scale=1.0 / D, bias=epsc),
                 reads=[ssk, "eps"], writes=[rsk])
            S.op("dve", lambda e: e.reciprocal(out=rs, in_=rs), reads=[rsk], writes=[rsk])
            S.op("dve", lambda e: e.scalar_tensor_tensor(out=hb_ap, in0=x_ap, scalar=rs, in1=g_ap,
                                                         op0=ALU.mult, op1=ALU.mult),
                 reads=[x_key, rsk, g_key], writes=[hb_key])

        def transpose8(hb_ap, hb_key, dst_ap, dst_key, eng="act"):
            pv = pbt[:, 0:1024].rearrange("p (c t) -> p c t", c=8)
            for c in range(8):
                S.op("pe", lambda e, c=c: e.transpose(out=pv[:, c, :], in_=hb_ap[:, c * 128:(c + 1) * 128],
                                                      identity=identb),
                     reads=[hb_key, "identb"], writes=["pbt"])
            if eng == "act":
                S.op("act", lambda e: e.copy(out=dst_ap, in_=pv), reads=["pbt"], writes=[dst_key])
            else:
                S.op("dve", lambda e: e.tensor_copy(out=dst_ap, in_=pv), reads=["pbt"], writes=[dst_key])

        def norm_residual(bank0, bank1, g_ap, g_key, x_ap, x_key, tmp_ap, junkf):
            s0, k0 = newstat()
            s1, k1 = newstat()
            rs, rk = newstat()
            S.op("act", lambda e: e.activation(out=junkf, in_=pb[bank0][:], func=AF.Square, accum_out=s0),
                 reads=[pbk(bank0)], writes=["junkf", k0])
            S.op("act", lambda e: e.activation(out=junkf, in_=pb[bank1][:], func=AF.Square, accum_out=s1),
                 reads=[pbk(bank1)], writes=["junkf", k1])
            S.op("dve", lambda e: e.tensor_tensor(out=rs, in0=s0, in1=s1, op=ALU.add), reads=[k0, k1], writes=[rk])
            S.op("act", lambda e: e.activation(out=rs, in_=rs, func=AF.Sqrt, scale=1.0 / D, bias=epsc),
                 reads=[rk, "eps"], writes=[rk])
            S.op("dve", lambda e: e.reciprocal(out=rs, in_=rs), reads=[rk], writes=[rk])
            for hf, bk in enumerate((bank0, bank1)):
                S.op("dve", lambda e, hf=hf, bk=bk: e.scalar_tensor_tensor(
                    out=tmp_ap[:, hf * 512:(hf + 1) * 512], in0=pb[bk][:], scalar=rs,
                    in1=g_ap[:, hf * 512:(hf + 1) * 512], op0=ALU.mult, op1=ALU.mult),
                    reads=[pbk(bk), rk, g_key], writes=[("tmpn", hf)])
            S.op("pool", lambda e: e.tensor_tensor(out=x_ap, in0=x_ap, in1=tmp_ap, op=ALU.add),
                 reads=[x_key, ("tmpn", 0), ("tmpn", 1)], writes=[x_key])

        def load_w(dst, dst_keyf, src3, ncols, piece):
            for i, c0 in enumerate(range(0, ncols, piece)):
                c1 = min(ncols, c0 + piece)
                S.dma("pool", lambda e, c0=c0, c1=c1: e.dma_start(out=dst[:, :, c0:c1], in_=src3[:, :, c0:c1]),
                      writes=[dst_keyf(i)])

        A.off = P0
        KA = A.alloc((4, 4096), BF16)
        KB2 = A.alloc((4096,), BF16)
        KI4 = A.alloc((4096,), BF16)
        VA = A.alloc((32, 512), BF16)
        VB2 = A.alloc((32, 128), BF16)
        wq = A.alloc((8, 1288), BF16)
        wo = A.alloc((8, 1024), BF16)
        g0 = A.alloc((1024,), F32)
        g1 = A.alloc((1024,), F32)
        biasA = A.alloc((2, 8, 128), BF16)
        biasB = A.alloc((2, 8, 128), BF16)
        xt = A.alloc((1024,), F32)
        hb = A.alloc((1024,), BF16)
        junkb = A.alloc((1024,), BF16)
        OV = A.off
        wk = A.alloc((8, 1408), BF16)
        hT4 = A.alloc((8, 512), BF16)
        xt2 = A.alloc((1024,), F32)
        biasf = A.alloc((2, 8, 128), F32)
        cAB = A.alloc((16,), F32)
        lvb = A.alloc((4, 64), F32)
        lvj = A.alloc((64,), F32)
        kv_end = A.off
        A.off = OV
        sc = A.alloc((4096,), F32)
        junk = A.alloc((2048,), BF16)
        maskT = A.alloc((32, 128), BF16)
        hT = A.alloc((8, 128), BF16)
        qmA = A.alloc((4, 2, 128), BF16)
        qmB = A.alloc((4, 2, 128), BF16)
        qmI = A.alloc((2, 4, 128), BF16)
        wis = A.alloc((8,), F32)
        rbuf = [A.alloc((512,), F32) for _ in range(2)]
        ptb = [A.alloc((512,), BF16) for _ in range(2)]
        pmb = [A.alloc((512,), BF16) for _ in range(2)]
        rz = A.alloc((512,), F32)
        tt = A.alloc((512,), F32)
        dd = A.alloc((256,), F32)
        sq = A.alloc((256,), F32)
        rstd = A.alloc((256,), F32)
        yT = A.alloc((8, 128), BF16)
        tmpn = sc[:, 0:1024]
        junkf = tt
        bis = A.alloc((8,), F32)
        Rs = A.alloc((NITER,), F32)
        q_end = A.off
        assert max(kv_end, q_end) <= NW

        win3 = win.rearrange("(c p) n -> p c n", p=128)
        wkparts = [(0, 512, 512), (512, 2048, 64), (576, 2048, 64), (640, 2432, 32), (672, 2432, 32),
                   (704, 2432, 32), (736, 2432, 32), (768, 1024, 512), (1280, 2112, 64), (1344, 2112, 64)]
        for i, (d0, s0, n) in enumerate(wkparts):
            S.dma("pool", lambda e, d0=d0, s0=s0, n=n: e.dma_start(out=wk[:, :, d0:d0 + n], in_=win3[:, :, s0:s0 + n]),
                  writes=[("wk", i)])
        WK = [("wk", i) for i in range(len(wkparts))]
        wqparts = [(0, 0, 512), (512, 1536, 512), (1024, 2176, 256), (1280, 2464, 8)]
        for i, (d0, s0, n) in enumerate(wqparts):
            S.dma("pool", lambda e, d0=d0, s0=s0, n=n: e.dma_start(out=wq[:, :, d0:d0 + n], in_=win3[:, :, s0:s0 + n]),
                  writes=[("wq", i)])
        WQ = [("wq", i) for i in range(len(wqparts))]
        load_w(wo, lambda i: ("wo", i), wout.rearrange("(c p) n -> p c n", p=128), 1024, 512)
        WO = [("wo", 0), ("wo", 1)]
        S.dma("sp", lambda e: e.dma_start(out=g0, in_=gb[0]), writes=["g0"])
        S.dma("sp", lambda e: e.dma_start(out=g1, in_=gb[1]), writes=["g1"])
        S.dma("sp", lambda e: e.dma_start(out=cAB[:, 0:8], in_=cA_d), writes=["cAB"])
        S.dma("sp", lambda e: e.dma_start(out=cAB[:, 8:16], in_=cB_d), writes=["cAB2"])
        for which, (src, dstb) in enumerate(((biasA_d, biasA), (biasB_d, biasB))):
            S.dma("sp", lambda e, src=src: e.dma_start(out=biasf, in_=src), writes=["biasf"])
            for m in range(8):
                S.op("dve", lambda e, m=m, dstb=dstb, which=which: e.tensor_scalar(
                    out=dstb[:, :, m, :], in0=biasf[:, :, m, :], scalar1=cAB[:, which * 8 + m:which * 8 + m + 1],
                    scalar2=None, op0=ALU.subtract),
                    reads=["biasf", "cAB", "cAB2"], writes=[("bias", which)])
        lam_init = 0.8 - 0.6 * math.exp(-0.3 * 0)
        S.dma("sp", lambda e: e.dma_start(out=lvb, in_=lvb_d), writes=["lvb"])
        S.dma("sp", lambda e: e.dma_start(out=smallc[:, 5:6], in_=sgn_d), writes=["sgraw"])
        e1, e1k = newstat()
        e2, e2k = newstat()
        S.op("dve", lambda e: e.tensor_tensor(out=lvj, in0=lvb[:, 0, :], in1=lvb[:, 1, :], op=ALU.mult),
             reads=["lvb"], writes=["lvj"])
        S.op("dve", lambda e: e.tensor_reduce(out=e1, in_=lvj, axis=AX.X, op=ALU.add), reads=["lvj"], writes=[e1k])
        S.op("dve", lambda e: e.tensor_tensor(out=lvj, in0=lvb[:, 2, :], in1=lvb[:, 3, :], op=ALU.mult),
             reads=["lvb", e1k], writes=["lvj"])
        S.op("dve", lambda e: e.tensor_reduce(out=e2, in_=lvj, axis=AX.X, op=ALU.add), reads=["lvj"], writes=[e2k])
        S.op("act", lambda e: e.activation(out=e1, in_=e1, func=AF.Exp), reads=[e1k], writes=[e1k])
        S.op("act", lambda e: e.activation(out=e2, in_=e2, func=AF.Exp), reads=[e2k], writes=[e2k])
        S.op("dve", lambda e: e.scalar_tensor_tensor(out=neglam, in0=e2, scalar=-lam_init, in1=e1,
                                                     op0=ALU.add, op1=ALU.subtract),
             reads=[e1k, e2k], writes=["neglam"])
        S.op("dve", lambda e: e.tensor_scalar(out=sgc, in0=sgc, scalar1=1.0 - lam_init, scalar2=None, op0=ALU.mult),
             reads=["sgraw"], writes=["sg"])
        S.op("pool", lambda e: e.memset(VB2[:, :, :], 0.0), writes=["VB2"])

        xts = [xt, xt2]
        for g in range(8):
            for i in range(4):
                t = 4 * g + i
                xa = xts[t % 2]
                xk = ("xt", t % 2)
                S.dma("sp", lambda e, t=t, xa=xa: e.dma_start(out=xa, in_=xloc[t * 128:(t + 1) * 128, :]), writes=[xk])
                rmsnorm_rows(xa, xk, g0, "g0", hb, "hb", junkb)
                transpose8(hb, "hb", hT4[:, :, i * 128:(i + 1) * 128], ("hT4", i), eng="act" if i % 2 == 0 else "dve")
            HT4 = [("hT4", i) for i in range(4)]
            for c in range(6):
                bk = c % 2
                for dc in range(8):
                    S.op("pe", lambda e, c=c, dc=dc, bk=bk: e.matmul(pb[bk][:], lhsT=wk[:, dc, c * 128:(c + 1) * 128],
                                                                    rhs=hT4[:, dc, :], start=(dc == 0), stop=(dc == 7)),
                         reads=HT4 + WK, writes=[pbk(bk)])
                if c < 4:
                    dst = KA[:, c, g * 512:(g + 1) * 512]
                elif c == 4:
                    dst = KB2[:, g * 512:(g + 1) * 512]
                else:
                    dst = KI4[:, g * 512:(g + 1) * 512]
                if c % 2 == 0:
                    S.op("act", lambda e, dst=dst, bk=bk: e.copy(out=dst, in_=pb[bk][:]), reads=[pbk(bk)], writes=[("K", c, g)])
                else:
                    S.op("dve", lambda e, dst=dst, bk=bk: e.tensor_copy(out=dst, in_=pb[bk][:]), reads=[pbk(bk)], writes=[("K", c, g)])
            for i in range(4):
                t = 4 * g + i
                for dc in range(8):
                    S.op("pe", lambda e, i=i, dc=dc: e.matmul(pb[2][:], lhsT=hT4[:, dc, i * 128:(i + 1) * 128],
                                                              rhs=wk[:, dc, 768:1280], start=(dc == 0), stop=(dc == 7)),
                         reads=HT4 + WK, writes=[pbk(2)])
                for dc in range(8):
                    S.op("pe", lambda e, i=i, dc=dc: e.matmul(pb[3][:, 0:128], lhsT=hT4[:, dc, i * 128:(i + 1) * 128],
                                                              rhs=wk[:, dc, 1280:1408], start=(dc == 0), stop=(dc == 7)),
                         reads=HT4 + WK, writes=[pbk(3)])
                S.op("act", lambda e, t=t: e.copy(out=VA[:, t, :], in_=pb[2][:]), reads=[pbk(2)], writes=[("VA", t)])
                S.op("dve", lambda e, t=t: e.tensor_copy(out=VB2[:, t, :], in_=pb[3][:, 0:128]),
                     reads=[pbk(3), "VB2"], writes=[("VB", t)])
        S.barrier()

        ALLK = [("K", c, g) for c in range(6) for g in range(8)]
        outs = []
        for qi in range(NQT):
            qt = QT0 + qi
            nk = qt + 1
            N = nk * 128
            xk = ("xt", 0)
            S.dma("sp", lambda e, qt=qt: e.dma_start(out=xt, in_=xloc[qt * 128:(qt + 1) * 128, :]), writes=[xk])
            rmsnorm_rows(xt, xk, g0, "g0", hb, "hb", junkb)
            transpose8(hb, "hb", hT, "hT")
            for c in range(10):
                bank, col = (4, c) if c < 4 else ((5, c - 4) if c < 8 else (6, c - 8))
                for dc in range(8):
                    S.op("pe", lambda e, c=c, dc=dc, bank=bank, col=col: e.matmul(
                        pb[bank][:, col * 128:(col + 1) * 128], lhsT=wq[:, dc, c * 128:(c + 1) * 128], rhs=hT[:, dc, :],
                        start=(dc == 0), stop=(dc == 7)), reads=["hT"] + WQ, writes=[pbk(bank)])
            for dc in range(8):
                S.op("pe", lambda e, dc=dc: e.matmul(pb[6][:, 256:264], lhsT=hT[:, dc, :], rhs=wq[:, dc, 1280:1288],
                                                     start=(dc == 0), stop=(dc == 7)), reads=["hT"] + WQ, writes=[pbk(6)])
            cnt = 0
            for (bank, dstq, nsub, sel) in ((4, qmA, 2, selA), (5, qmB, 2, selA), (6, qmI, 4, selI)):
                for c in range(dstq.shape[1]):
                    for s_ in range(nsub):
                        src = pb[bank][:, c * 128:(c + 1) * 128]
                        dst = dstq[:, c, s_, :]
                        sca = sel[:, s_:s_ + 1]
                        if cnt % 2 == 0:
                            S.op("act", lambda e, src=src, dst=dst, sca=sca: e.activation(out=dst, in_=src, func=AF.Copy, scale=sca),
                                 reads=[pbk(bank), "selA", "selI"], writes=[("qm", bank)])
                        else:
                            S.op("dve", lambda e, src=src, dst=dst, sca=sca: e.tensor_scalar(out=dst, in0=src, scalar1=sca, scalar2=None, op0=ALU.mult),
                                 reads=[pbk(bank), "selA", "selI"], writes=[("qm", bank)])
                        cnt += 1
            S.op("act", lambda e: e.mul(out=wis, in_=pb[6][:, 256:264], mul=(32.0 ** -0.5) * (8.0 ** -0.5)),
                 reads=[pbk(6)], writes=["wis"])
            nkb = (N + 511) // 512
            for kb in range(nkb):
                cols = min(512, N - kb * 512)
                for j in range(8):
                    bk = 4 + (j % 2)
                    S.op("pe", lambda e, j=j, bk=bk, kb=kb, cols=cols: e.matmul(
                        pb[bk][:, 0:cols], lhsT=qmI[:, j // 4, j % 4, :], rhs=KI4[:, kb * 512:kb * 512 + cols],
                        start=True, stop=True), reads=[("qm", 6)] + ALLK, writes=[pbk(bk)])
                    rb = rbuf[j % 2]
                    S.op("act", lambda e, bk=bk, rb=rb, cols=cols: e.activation(out=rb[:, 0:cols], in_=pb[bk][:, 0:cols], func=AF.Relu),
                         reads=[pbk(bk)], writes=[("rbuf", j % 2)])
                    scv = sc[:, kb * 512:kb * 512 + cols]
                    if j == 0:
                        S.op("dve", lambda e, rb=rb, scv=scv, cols=cols: e.tensor_scalar(
                            out=scv, in0=rb[:, 0:cols], scalar1=wis[:, 0:1], scalar2=None, op0=ALU.mult),
                            reads=[("rbuf", 0), "wis"], writes=[("sc", kb)])
                    else:
                        S.op("dve", lambda e, rb=rb, scv=scv, cols=cols, j=j: e.scalar_tensor_tensor(
                            out=scv, in0=rb[:, 0:cols], scalar=wis[:, j:j + 1], in1=scv, op0=ALU.mult, op1=ALU.add),
                            reads=[("rbuf", j % 2), "wis", ("sc", kb)], writes=[("sc", kb)])
            SCK = [("sc", kb) for kb in range(nkb)]
            mx, mn, Rr, lo, tth, cA_, cB_, gg = [bis[:, i:i + 1] for i in range(8)]
            S.op("dve", lambda e, N=N: e.tensor_reduce(out=mx, in_=sc[:, 0:N], axis=AX.X, op=ALU.max), reads=SCK, writes=["b_mx"])
            S.op("dve", lambda e, N=N: e.tensor_reduce(out=lo, in_=sc[:, 0:N], axis=AX.X, op=ALU.min), reads=SCK, writes=["b_lo"])
            S.op("dve", lambda e: e.tensor_tensor(out=Rr, in0=mx, in1=lo, op=ALU.subtract), reads=["b_mx", "b_lo"], writes=["b_R"])
            S.op("pool", lambda e, N=N: e.memset(sc[0:64, N - 64:N], NEG), reads=SCK + ["b_mx", "b_lo"], writes=SCK)
            npv = 15 * 128 if qt == 15 else 2048
            S.op("dve", lambda e, npv=npv: e.tensor_scalar(out=sc[:, 0:npv], in0=sc[:, 0:npv], scalar1=pnegc, scalar2=None, op0=ALU.add),
                 reads=SCK + ["flags"], writes=SCK)
            halves = [(0, N)] if N <= 2048 else [(0, 2048), (2048, N)]
            S.op("dve", lambda e: e.tensor_scalar(out=Rs, in0=stepc, scalar1=Rr, scalar2=None, op0=ALU.mult),
                 reads=["b_R", "stepc"], writes=["b_Rs"])

            def bis_iter(it):
                S.op("dve", lambda e: e.tensor_tensor(out=tth, in0=Rs[:, it:it + 1], in1=lo, op=ALU.add),
                     reads=["b_Rs", "b_lo"], writes=["b_t"])
                for hi_, (a0, a1) in enumerate(halves):
                    co = cA_ if hi_ == 0 else cB_
                    S.op("dve", lambda e, a0=a0, a1=a1, co=co: e.tensor_scalar(
                        out=junk[:, 0:a1 - a0], in0=sc[:, a0:a1], scalar1=tth, scalar2=None, op0=ALU.is_ge, op1=ALU.add, accum_out=co),
                        reads=SCK + ["b_t"], writes=["junk", ("b_c", hi_)])
                if len(halves) == 2:
                    S.op("dve", lambda e: e.tensor_tensor(out=cA_, in0=cA_, in1=cB_, op=ALU.add),
                         reads=[("b_c", 0), ("b_c", 1)], writes=[("b_c", 0)])
                S.op("dve", lambda e: e.tensor_scalar(out=gg, in0=cA_, scalar1=TOPK - 0.5, scalar2=None, op0=ALU.is_ge),
                     reads=[("b_c", 0)], writes=["b_g"])
                S.op("dve", lambda e: e.scalar_tensor_tensor(out=lo, in0=gg, scalar=Rs[:, it:it + 1], in1=lo, op0=ALU.mult, op1=ALU.add),
                     reads=["b_g", "b_lo", "b_Rs"], writes=["b_lo"])

            MK = [("maskT", k4) for k4 in range((nk + 3) // 4)]

            def emit_group(typ, gi):
                def qk_stage(kt):
                    sbk = kt % 2
                    band = kt >= qt - 1
                    if band:
                        bt = 0 if kt == qt else 1
                        bsrc = (biasA if typ == "A" else biasB)[:, bt, 4 * gi:4 * gi + 4, :]
                        S.op("pe", lambda e: e.matmul(pb[sbk][:].rearrange("p (a b) -> p a b", a=4), lhsT=identb, rhs=bsrc,
                                                      start=True, stop=False),
                             reads=["identb", ("bias", 0), ("bias", 1)], writes=[pbk(sbk)])
                    for mi in range(4):
                        if typ == "A":
                            h = 2 * gi + mi // 2
                            lhsT = KA[:, h, kt * 128:(kt + 1) * 128]
                            rhs = qmA[:, h, mi % 2, :]
                            qk = ("qm", 4)
                        else:
                            j = 4 * gi + mi
                            lhsT = KB2[:, kt * 128:(kt + 1) * 128]
                            rhs = qmB[:, j // 2, j % 2, :]
                            qk = ("qm", 5)
                        S.op("pe", lambda e, mi=mi, lhsT=lhsT, rhs=rhs: e.matmul(
                            pb[sbk][:, mi * 128:(mi + 1) * 128], lhsT=lhsT, rhs=rhs, start=(not band), stop=((not band) or mi == 3)),
                            reads=[qk] + ALLK, writes=[pbk(sbk)])
                    masked = (kt <= 14) or (kt == 15 and qt >= 16)
                    bias_ap = pnegc if masked else zeroc
                    pt = ptb[kt % 2]
                    S.op("act", lambda e: e.activation(out=pt, in_=pb[sbk][:], func=AF.Exp, bias=bias_ap, scale=1.0),
                         reads=[pbk(sbk), "flags", "zero"], writes=[("pt", kt % 2)])
                    if typ == "B":
                        pm = pmb[kt % 2]
                        S.op("dve", lambda e: e.tensor_tensor(
                            out=pm.rearrange("p (a b) -> p a b", a=4), in0=pt.rearrange("p (a b) -> p a b", a=4),
                            in1=maskT[:, kt, :].unsqueeze(1).to_broadcast([128, 4, 128]), op=ALU.mult),
                            reads=[("pt", kt % 2)] + MK, writes=[("pm", kt % 2)])

                def pv_stage(kt):
                    if typ == "B":
                        src, srck = pmb[kt % 2], ("pm", kt % 2)
                    else:
                        src, srck = ptb[kt % 2], ("pt", kt % 2)
                    if typ == "A":
                        for hl in range(2):
                            h = 2 * gi + hl
                            ob = 2 if hl == 0 else 4
                            S.op("pe", lambda e, hl=hl, h=h, ob=ob: e.matmul(
                                pb[ob][:, 0:256], lhsT=VA[:, kt, h * 128:(h + 1) * 128], rhs=src[:, hl * 256:(hl + 1) * 256],
                                start=(kt == 0), stop=(kt == nk - 1)), reads=[srck, ("VA", kt)], writes=[pbk(ob)])
                    else:
                        S.op("pe", lambda e: e.matmul(pb[2][:], lhsT=VB2[:, kt, :], rhs=src, start=(kt == 0), stop=(kt == nk - 1)),
                             reads=[srck, ("VB", kt)], writes=[pbk(2)])
                    S.op("pe", lambda e: e.matmul(pb[3][:], lhsT=onesb, rhs=src, start=(kt == 0), stop=(kt == nk - 1)),
                         reads=[srck, "onesb"], writes=[pbk(3)])

                qk_stage(0)
                for kt in range(nk):
                    if kt + 1 < nk:
                        qk_stage(kt + 1)
                    pv_stage(kt)
                S.op("dve", lambda e: e.reciprocal(out=rz, in_=pb[3][:]), reads=[pbk(3)], writes=["rz"])
                if typ == "A":
                    S.op("dve", lambda e: e.tensor_tensor(out=tt[:, 0:256], in0=pb[2][:, 0:256], in1=rz[:, 0:256], op=ALU.mult),
                         reads=[pbk(2), "rz"], writes=["tt"])
                    S.op("dve", lambda e: e.tensor_tensor(out=tt[:, 256:512], in0=pb[4][:, 0:256], in1=rz[:, 256:512], op=ALU.mult),
                         reads=[pbk(4), "rz", "tt"], writes=["tt"])
                    t4 = tt.rearrange("p (h m q) -> p h m q", h=2, m=2)
                    S.op("dve", lambda e: e.scalar_tensor_tensor(out=dd.rearrange("p (h q) -> p h q", h=2), in0=t4[:, :, 1, :], scalar=neglam,
                                                                 in1=t4[:, :, 0, :], op0=ALU.mult, op1=ALU.add),
                         reads=["tt", "neglam"], writes=["dd"])
                    S.op("pool", lambda e: e.tensor_tensor(out=sq, in0=dd, in1=dd, op=ALU.mult), reads=["dd"], writes=["sq"])
                    S.op("pe", lambda e: e.matmul(pb[6][:, 0:256], lhsT=onesf, rhs=sq, start=True, stop=True),
                         reads=["sq", "onesf"], writes=[pbk(6)])
                    S.op("act", lambda e: e.activation(out=rstd, in_=pb[6][:, 0:256], func=AF.Sqrt, scale=1.0 / 128, bias=epsc),
                         reads=[pbk(6), "eps"], writes=["rstd"])
                    S.op("dve", lambda e: e.reciprocal(out=rstd, in_=rstd), reads=["rstd"], writes=["rstd"])
                    S.op("dve", lambda e: e.scalar_tensor_tensor(out=yT[:, 2 * gi:2 * gi + 2, :], in0=dd.rearrange("p (h q) -> p h q", h=2),
                                                                 scalar=sgc, in1=rstd.rearrange("p (h q) -> p h q", h=2),
                                                                 op0=ALU.mult, op1=ALU.mult),
                         reads=["dd", "rstd", "sg"], writes=[("yT", gi)])
                else:
                    for cl in range(2):
                        ch = 4 + 2 * gi + cl
                        for hf in range(2):
                            jl = 2 * cl + hf
                            S.op("dve", lambda e, ch=ch, hf=hf, jl=jl: e.tensor_tensor(
                                out=yT[hf * 64:(hf + 1) * 64, ch, :], in0=pb[2][hf * 64:(hf + 1) * 64, jl * 128:(jl + 1) * 128],
                                in1=rz[hf * 64:(hf + 1) * 64, jl * 128:(jl + 1) * 128], op=ALU.mult),
                                reads=[pbk(2), "rz"], writes=[("yT", 2 + gi)])

            for it in range(NITER // 2):
                bis_iter(it)
            emit_group("A", 0)
            for it in range(NITER // 2, NITER):
                bis_iter(it)
            emit_group("A", 1)
            S.op("dve", lambda e, N=N: e.tensor_scalar(out=sc[:, 0:N], in0=sc[:, 0:N], scalar1=lo, scalar2=None, op0=ALU.is_ge),
                 reads=SCK + ["b_lo"], writes=SCK)
            for k4 in range((nk + 3) // 4):
                n4 = min(4, nk - 4 * k4)
                for i in range(n4):
                    kt = 4 * k4 + i
                    S.op("pe", lambda e, kt=kt, i=i: e.transpose(out=pb[6][:, i * 128:(i + 1) * 128], in_=sc[:, kt * 128:(kt + 1) * 128],
                                                                 identity=identf), reads=SCK + ["identf"], writes=[pbk(6)])
                S.op("act", lambda e, k4=k4, n4=n4: e.copy(out=maskT[:, 4 * k4:4 * k4 + n4, :],
                                                           in_=pb[6][:, 0:n4 * 128].rearrange("p (a b) -> p a b", a=n4)),
                     reads=[pbk(6)], writes=[("maskT", k4)])
            emit_group("B", 0)
            emit_group("B", 1)
            YT = [("yT", i) for i in range(4)]
            if debug:
                S.dma("pool", lambda e, qi=qi: e.dma_start(out=ydbg[qi], in_=yT), reads=YT, writes=["ydbg"])
            for hf in range(2):
                for c in range(8):
                    S.op("pe", lambda e, hf=hf, c=c: e.matmul(pb[hf][:], lhsT=yT[:, c, :], rhs=wo[:, c, hf * 512:(hf + 1) * 512],
                                                              start=(c == 0), stop=(c == 7)), reads=YT + WO, writes=[pbk(hf)])
            norm_residual(0, 1, g1, "g1", xt, xk, tmpn, junkf)
            outs.append(S.dma("sp", lambda e, qi=qi: e.dma_start(out=x1d[qi * 128:(qi + 1) * 128, :], in_=xt), reads=[xk], writes=["x1d"]))
        S.barrier()

        def mlp_phase(layer, ntiles, src_d, dst_d, gi0):
            A.off = P0
            Wup = A.alloc((8, 4096), BF16)
            Wdn = A.alloc((32, 1024), BF16)
            ga = A.alloc((1024,), F32)
            gb_ = A.alloc((1024,), F32)
            xtm = [A.alloc((1024,), F32) for _ in range(4)]
            hbm = A.alloc((1024,), BF16)
            jb = A.alloc((1024,), BF16)
            hTm = A.alloc((8, 256), BF16)
            uT = A.alloc((32, 256), BF16)
            rb2 = [A.alloc((2, 256), F32) for _ in range(2)]
            tmpm = A.alloc((1024,), F32)
            jf = A.alloc((512,), F32)
            assert A.off <= NW
            load_w(Wup, lambda i: ("Wup", i), wup[layer].rearrange("(c p) n -> p c n", p=128), 4096, 512)
            for p4 in range(4):
                S.dma("pool", lambda e, p4=p4: e.dma_start(out=Wdn[:, 8 * p4:8 * p4 + 8, :],
                                                          in_=wdn[layer].rearrange("(f p) n -> p f n", p=128)[:, 8 * p4:8 * p4 + 8, :]),
                      writes=[("Wdn", p4)])
            S.dma("sp", lambda e: e.dma_start(out=ga, in_=gb[gi0]), writes=["ga"])
            S.dma("sp", lambda e: e.dma_start(out=gb_, in_=gb[gi0 + 1]), writes=["gb"])
            res = []
            groups = [list(range(i, min(i + 2, ntiles))) for i in range(0, ntiles, 2)]
            for gidx, grp in enumerate(groups):
                T = 128 * len(grp)
                xb = 2 * (gidx % 2)
                for i, t in enumerate(grp):
                    xk = ("xtm", xb + i)
                    S.dma("sp", lambda e, t=t, i=i, xb=xb: e.dma_start(out=xtm[xb + i], in_=src_d[t * 128:(t + 1) * 128, :]), reads=["srcd"], writes=[xk])
                    rmsnorm_rows(xtm[xb + i], xk, ga, "ga", hbm, "hbm", jb)
                    transpose8(hbm, "hbm", hTm[:, :, i * 128:(i + 1) * 128], ("hTm", i), eng="act" if i == 0 else "dve")
                HK = [("hTm", i) for i in range(len(grp))]
                for f2 in range(16):
                    bk = 4 + f2 % 2
                    for fl in range(2):
                        f = 2 * f2 + fl
                        for dc in range(8):
                            S.op("pe", lambda e, f=f, fl=fl, dc=dc, bk=bk, T=T: e.matmul(
                                pb[bk][:, fl * 256:fl * 256 + T], lhsT=Wup[:, dc, f * 128:(f + 1) * 128], rhs=hTm[:, dc, 0:T],
                                start=(dc == 0), stop=(dc == 7)), reads=HK + [("Wup", f // 4)], writes=[pbk(bk)])
                    rb = rb2[f2 % 2]
                    S.op("act", lambda e, bk=bk, rb=rb, T=T: e.activation(
                        out=rb[:, :, 0:T], in_=pb[bk][:].rearrange("p (a b) -> p a b", a=2)[:, :, 0:T], func=AF.Relu),
                        reads=[pbk(bk)], writes=[("rb2", f2 % 2)])
                    S.op("pool", lambda e, rb=rb, f2=f2, T=T: e.tensor_tensor(out=uT[:, 2 * f2:2 * f2 + 2, 0:T], in0=rb[:, :, 0:T],
                                                                           in1=rb[:, :, 0:T], op=ALU.mult),
                         reads=[("rb2", f2 % 2)], writes=[("uT", f2)])
                UK = [("uT", f2) for f2 in range(16)]
                for i, t in enumerate(grp):
                    for hf in range(2):
                        for f in range(32):
                            S.op("pe", lambda e, i=i, hf=hf, f=f: e.matmul(pb[hf][:], lhsT=uT[:, f, i * 128:(i + 1) * 128],
                                                                          rhs=Wdn[:, f, hf * 512:(hf + 1) * 512], start=(f == 0), stop=(f == 31)),
                                 reads=UK + [("Wdn", f // 8)], writes=[pbk(hf)])
                    norm_residual(0, 1, gb_, "gb", xtm[xb + i], ("xtm", xb + i), tmpm, jf)
                    res.append(S.dma("sp", lambda e, t=t, i=i, xb=xb: e.dma_start(out=dst_d[t * 128:(t + 1) * 128, :], in_=xtm[xb + i]),
                                     reads=[("xtm", xb + i)], writes=["dstd"]))
            S.barrier()
            return res

        mlp_phase(0, NQT, x1d, x2d, 2)

        A.off = P0
        wci = A.alloc((8, 2560), BF16)
        wco = A.alloc((8, 1024), BF16)
        dg = A.alloc((31, 4, 128), BF16)
        dwT = A.alloc((4, 31), F32)
        cvec = A.alloc((3, 4), F32)
        scw = A.alloc((4, 3), F32)
        g4 = A.alloc((1024,), F32)
        g5 = A.alloc((1024,), F32)
        xtc = [A.alloc((1024,), F32) for _ in range(4)]
        hbc = A.alloc((1024,), BF16)
        jbc = A.alloc((1024,), BF16)
        hTc = A.alloc((8, 256), BF16)
        uTb = A.alloc((4, 32 + 256), BF16)
        eTb = A.alloc((4, 2 + 256), F32)
        sgb = A.alloc((256,), F32)
        utmp = A.alloc((256,), F32)
        dhs = A.alloc((256,), F32)
        z1 = A.alloc((256,), F32)
        vv = A.alloc((4, 256), F32)
        sqv = A.alloc((4, 256), F32)
        mean = A.alloc((256,), F32)
        msq = A.alloc((256,), F32)
        rsd = A.alloc((256,), F32)
        tcb = A.alloc((256,), F32)
        ymT = A.alloc((8, 256), BF16)
        tmpc = A.alloc((1024,), F32)
        jfc = A.alloc((512,), F32)
        assert A.off <= NW
        load_w(wci, lambda i: ("wci", i), cwin.rearrange("(c p) n -> p c n", p=128), 2560, 512)
        WCI = [("wci", i) for i in range(5)]
        load_w(wco, lambda i: ("wco", i), cwout.rearrange("(c p) n -> p c n", p=128), 1024, 512)
        WCO = [("wco", 0), ("wco", 1)]
        S.dma("sp", lambda e: e.dma_start(out=dwT, in_=dwT_d), writes=["dwT"])
        S.dma("sp", lambda e: e.dma_start(out=cvec, in_=cvec_d), writes=["cvec"])
        S.dma("sp", lambda e: e.dma_start(out=scw, in_=scw_d), writes=["scw"])
        S.dma("sp", lambda e: e.dma_start(out=g4, in_=gb[4]), writes=["g4"])
        S.dma("sp", lambda e: e.dma_start(out=g5, in_=gb[5]), writes=["g5"])
        cnt = 0
        for j in range(31):
            for c in range(4):
                eng = "dve" if cnt % 2 == 0 else "pool"
                S.op(eng, lambda e, j=j, c=c: e.tensor_scalar(out=dg[:, j, c, :], in0=identf, scalar1=dwT[:, c, j:j + 1], scalar2=None, op0=ALU.mult),
                     reads=["identf", "dwT"], writes=[("dg", eng)])
                cnt += 1
        DG = [("dg", "dve"), ("dg", "pool")]
        S.op("pool", lambda e: e.memset(uTb[:, :, :], 0.0), writes=["uTb"])
        S.op("pool", lambda e: e.memset(eTb[:, :, :], 0.0), writes=["eTb"])
        cgroups = [[0]] + [[1 + 2 * i, 2 + 2 * i] for i in range(8)]
        for gidx, grp in enumerate(cgroups):
            T = 128 * len(grp)
            halo = (grp == [0])
            xb = 2 * (gidx % 2)
            for i, li in enumerate(grp):
                xk = ("xtc", xb + i)
                S.dma("sp", lambda e, li=li, i=i, xb=xb: e.dma_start(out=xtc[xb + i], in_=x2d[li * 128:(li + 1) * 128, :]), reads=["dstd"], writes=[xk])
                rmsnorm_rows(xtc[xb + i], xk, g4, "g4", hbc, "hbc", jbc)
                transpose8(hbc, "hbc", hTc[:, :, i * 128:(i + 1) * 128], ("hTc", i), eng="act" if i == 0 else "dve")
            HK = [("hTc", i) for i in range(len(grp))]

            def proj(bank, col0, T=T, HK=HK):
                for dc in range(8):
                    S.op("pe", lambda e, dc=dc: e.matmul(pb[bank][:, 0:T], lhsT=wci[:, dc, col0:col0 + 128], rhs=hTc[:, dc, 0:T],
                                                         start=(dc == 0), stop=(dc == 7)), reads=HK + WCI, writes=[pbk(bank)])
            for c in range(4):
                proj(0, c * 128)
                proj(1, 512 + c * 128)
                S.op("act", lambda e, T=T: e.activation(out=sgb[:, 0:T], in_=pb[1][:, 0:T], func=AF.Sigmoid), reads=[pbk(1)], writes=["sgb"])
                if halo:
                    S.op("dve", lambda e, T=T: e.tensor_tensor(out=utmp[:, 0:T], in0=pb[0][:, 0:T], in1=sgb[:, 0:T], op=ALU.mult),
                         reads=[pbk(0), "sgb"], writes=["utmp"])
                    S.op("dve", lambda e, c=c, T=T: e.tensor_scalar(out=uTb[:, c, 32:32 + T], in0=utmp[:, 0:T], scalar1=flagc, scalar2=None, op0=ALU.mult),
                         reads=["utmp", "flags", "uTb"], writes=[("uT", c)])
                else:
                    S.op("dve", lambda e, c=c, T=T: e.tensor_tensor(out=uTb[:, c, 32:32 + T], in0=pb[0][:, 0:T], in1=sgb[:, 0:T], op=ALU.mult),
                         reads=[pbk(0), "sgb", "uTb"], writes=[("uT", c)])
            for c in range(4):
                proj(2, 1536 + c * 128)
                proj(3, 2048 + c * 128)
                S.op("act", lambda e, T=T: e.copy(out=dhs[:, 0:T], in_=pb[3][:, 0:T]), reads=[pbk(3)], writes=["dhs"])
                if halo:
                    S.op("dve", lambda e, T=T: e.tensor_tensor(out=utmp[:, 0:T], in0=pb[2][:, 0:T], in1=dhs[:, 0:T], op=ALU.mult),
                         reads=[pbk(2), "dhs"], writes=["utmp"])
                    S.op("dve", lambda e, c=c, T=T: e.tensor_scalar(out=eTb[:, c, 2:2 + T], in0=utmp[:, 0:T], scalar1=flagc, scalar2=None, op0=ALU.mult),
                         reads=["utmp", "flags", "eTb"], writes=[("eT", c)])
                else:
                    S.op("dve", lambda e, c=c, T=T: e.tensor_tensor(out=eTb[:, c, 2:2 + T], in0=pb[2][:, 0:T], in1=dhs[:, 0:T], op=ALU.mult),
                         reads=[pbk(2), "dhs", "eTb"], writes=[("eT", c)])
            if not halo:
                for c in range(4):
                    proj(4, 1024 + c * 128)
                    S.op("dve", lambda e, c=c, T=T: e.tensor_scalar(out=z1[:, 0:T], in0=eTb[:, c, 0:T], scalar1=scw[:, c, 0:1], scalar2=None, op0=ALU.mult),
                         reads=[("eT", c), "scw"], writes=["z1"])
                    for jj in (1, 2):
                        S.op("dve", lambda e, c=c, T=T, jj=jj: e.scalar_tensor_tensor(out=z1[:, 0:T], in0=eTb[:, c, jj:jj + T], scalar=scw[:, c, jj:jj + 1],
                                                                                    in1=z1[:, 0:T], op0=ALU.mult, op1=ALU.add),
                             reads=[("eT", c), "scw", "z1"], writes=["z1"])
                    S.op("dve", lambda e, c=c, T=T: e.tensor_tensor(out=ymT[:, 4 + c, 0:T], in0=pb[4][:, 0:T], in1=z1[:, 0:T], op=ALU.mult),
                         reads=[pbk(4), "z1"], writes=[("ymT", 4 + c)])
                for c in range(4):
                    for j in range(31):
                        S.op("pe", lambda e, c=c, j=j, T=T: e.matmul(pb[5][:, 0:T], lhsT=dg[:, j, c, :], rhs=uTb[:, c, 2 + j:2 + j + T],
                                                                     start=(j == 0), stop=(j == 30)),
                             reads=[("uT", c)] + DG, writes=[pbk(5)])
                    S.op("act", lambda e, c=c, T=T: e.activation(out=vv[:, c, 0:T], in_=pb[5][:, 0:T], func=AF.Identity, bias=cvec[:, 0, c:c + 1], scale=1.0),
                         reads=[pbk(5), "cvec"], writes=[("vv", c)])
                    S.op("act", lambda e, c=c, T=T: e.activation(out=sqv[:, c, 0:T], in_=vv[:, c, 0:T], func=AF.Square),
                         reads=[("vv", c)], writes=[("sqv", c)])
                for c in range(4):
                    S.op("pe", lambda e, c=c, T=T: e.matmul(pb[6][:, 0:T], lhsT=onesf, rhs=vv[:, c, 0:T], start=(c == 0), stop=(c == 3)),
                         reads=[("vv", c), "onesf"], writes=[pbk(6)])
                for c in range(4):
                    S.op("pe", lambda e, c=c, T=T: e.matmul(pb[6][:, 256:256 + T], lhsT=onesf, rhs=sqv[:, c, 0:T], start=(c == 0), stop=(c == 3)),
                         reads=[("sqv", c), "onesf"], writes=[pbk(6)])
                S.op("act", lambda e, T=T: e.mul(out=mean[:, 0:T], in_=pb[6][:, 0:T], mul=1.0 / 512), reads=[pbk(6)], writes=["mean"])
                S.op("pool", lambda e, T=T: e.tensor_tensor(out=msq[:, 0:T], in0=mean[:, 0:T], in1=mean[:, 0:T], op=ALU.mult), reads=["mean"], writes=["msq"])
                S.op("dve", lambda e, T=T: e.scalar_tensor_tensor(out=rsd[:, 0:T], in0=pb[6][:, 256:256 + T], scalar=1.0 / 512, in1=msq[:, 0:T],
                                                                op0=ALU.mult, op1=ALU.subtract), reads=[pbk(6), "msq"], writes=["rsd"])
                S.op("act", lambda e, T=T: e.activation(out=rsd[:, 0:T], in_=rsd[:, 0:T], func=AF.Sqrt, scale=1.0, bias=epsc), reads=["rsd", "eps"], writes=["rsd"])
                S.op("dve", lambda e, T=T: e.reciprocal(out=rsd[:, 0:T], in_=rsd[:, 0:T]), reads=["rsd"], writes=["rsd"])
                for c in range(4):
                    S.op("pool", lambda e, c=c, T=T: e.tensor_tensor(out=tcb[:, 0:T], in0=vv[:, c, 0:T], in1=mean[:, 0:T], op=ALU.subtract),
                         reads=[("vv", c), "mean"], writes=["tcb"])
                    S.op("pool", lambda e, T=T: e.tensor_tensor(out=tcb[:, 0:T], in0=tcb[:, 0:T], in1=rsd[:, 0:T], op=ALU.mult),
                         reads=["tcb", "rsd"], writes=["tcb"])
                    S.op("act", lambda e, c=c, T=T: e.activation(out=ymT[:, c, 0:T], in_=tcb[:, 0:T], func=AF.Silu, scale=cvec[:, 1, c:c + 1],
                                                                bias=cvec[:, 2, c:c + 1]), reads=["tcb", "cvec"], writes=[("ymT", c)])
                YM = [("ymT", c) for c in range(8)]
                for i, li in enumerate(grp):
                    for hf in range(2):
                        for c in range(8):
                            S.op("pe", lambda e, i=i, hf=hf, c=c: e.matmul(pb[hf][:], lhsT=ymT[:, c, i * 128:(i + 1) * 128],
                                                                          rhs=wco[:, c, hf * 512:(hf + 1) * 512], start=(c == 0), stop=(c == 7)),
                                 reads=YM + WCO, writes=[pbk(hf)])
                    norm_residual(0, 1, g5, "g5", xtc[xb + i], ("xtc", xb + i), tmpc, jfc)
                    S.dma("sp", lambda e, li=li, i=i, xb=xb: e.dma_start(out=x3d[(li - 1) * 128:li * 128, :], in_=xtc[xb + i]),
                          reads=[("xtc", xb + i)], writes=["x3d"])
            UT = [("uT", c) for c in range(4)]
            ET = [("eT", c) for c in range(4)]
            S.op("pool", lambda e, T=T: e.tensor_copy(out=uTb[:, :, 0:32], in_=uTb[:, :, T:T + 32]), reads=UT, writes=UT + ["uTb"])
            S.op("pool", lambda e, T=T: e.tensor_copy(out=eTb[:, :, 0:2], in_=eTb[:, :, T:T + 2]), reads=ET, writes=ET + ["eTb"])
        S.barrier()

        A.off = P0

        def final_src_fix():
            pass
        res = mlp_phase_final = None
        res = mlp_phase(1, 16, x3d, out_d, 6)
        S.emit(nc, final_wait_tokens=res)
    return nc


def _t5_bucket_np(rel):
    import jax
    import jax.numpy as jnp
    cpu = jax.devices("cpu")[0]
    with jax.default_device(cpu):
        rel = jnp.asarray(rel, dtype=jnp.int32)
        nb = 16
        ret = jnp.where(rel > 0, nb, 0)
        n = jnp.abs(rel)
        max_exact = nb // 2
        nf = jnp.maximum(n, 1).astype(jnp.float32)
        large = max_exact + (jnp.log(nf / max_exact) / math.log(128 / max_exact) * (nb - max_exact)).astype(jnp.int32)
        large = jnp.minimum(large, nb - 1)
        out = ret + jnp.where(n < max_exact, n, large)
        return np.asarray(out)


_PROG = {}


def _get_prog(debug=False):
    if debug not in _PROG:
        _PROG[debug] = build_program(debug)
    return _PROG[debug]


def make_in_maps(inputs):
    f = lambda a: np.ascontiguousarray(np.asarray(a, dtype=np.float32))
    x = f(inputs["x"])
    rel_bias = f(inputs["rel_bias"])
    norm_g = f(inputs["norm_g"])
    kk = np.arange(128)[:, None]
    qq = np.arange(128)[None, :]
    bD = _t5_bucket_np(kk - qq)
    bP = _t5_bucket_np(kk - 128 - qq)
    admiss = (kk // 64) <= (qq // 64)
    tabA = rel_bias[:, :4]
    tabB = rel_bias[:, 4:]
    biasA = np.zeros((128, 2, 8, 128), np.float32)
    biasB = np.zeros((128, 2, 8, 128), np.float32)
    for m in range(8):
        biasA[:, 0, m, :] = np.where(admiss, tabA[bD, m // 2], np.float32(NEG))
        biasA[:, 1, m, :] = tabA[bP, m // 2]
        biasB[:, 0, m, :] = np.where(admiss, tabB[bD, m], np.float32(NEG))
        biasB[:, 1, m, :] = tabB[bP, m]
    cA = np.ascontiguousarray(np.broadcast_to(np.repeat(tabA[15], 2)[None, :], (128, 8)))
    cB = np.ascontiguousarray(np.broadcast_to(tabB[15][None, :], (128, 8)))
    gbb = np.ascontiguousarray(np.broadcast_to(norm_g.reshape(8, 1, D), (8, 128, D)))
    lvb = np.ascontiguousarray(np.broadcast_to(f(inputs["diff_lambda"])[0][None], (128, 4, 64)))
    sgn = np.ascontiguousarray(f(inputs["diff_subln_g"])[0].reshape(128, 1))
    selA = np.zeros((128, 2), np.float32)
    selA[:64, 0] = 0.125
    selA[64:, 1] = 0.125
    selI = np.zeros((128, 4), np.float32)
    for i in range(4):
        selI[32 * i:32 * i + 32, i] = 1.0
    dwT = np.ascontiguousarray(f(inputs["conv_dw_w"])[0].reshape(31, 4, 128).transpose(2, 1, 0))
    cvec = np.ascontiguousarray(np.stack([f(inputs["conv_dw_b"])[0].reshape(4, 128).T,
                                          f(inputs["conv_ln_g"])[0].reshape(4, 128).T,
                                          f(inputs["conv_ln_b"])[0].reshape(4, 128).T], axis=1))
    scw = np.ascontiguousarray(f(inputs["sconv_w"])[0].reshape(3, 4, 128).transpose(2, 1, 0))
    common = dict(win=f(inputs["attn_w_in"])[0], wout=f(inputs["attn_w_out"])[0], wup=f(inputs["w_mlp_up"]),
                  wdn=f(inputs["w_mlp_down"]), cwin=f(inputs["conv_w_in"])[0], cwout=f(inputs["conv_w_out"])[0],
                  gb=gbb, biasA=biasA, biasB=biasB, cA=cA, cB=cB, lvb=lvb, sgn=sgn, selA=selA, selI=selI,
                  ident=np.eye(128, dtype=np.float32), dwT=dwT, cvec=cvec, scw=scw)
    in_maps = []
    for c in range(8):
        b, h = c // 2, c % 2
        if h == 1:
            xl = x[b]
        else:
            xl = np.concatenate([np.zeros((2048, D), np.float32), x[b, :2048]], axis=0)
        flags = np.zeros((128, 2), np.float32)
        flags[:, 0] = float(h)
        flags[:, 1] = 0.0 if h == 1 else NEG
        m = dict(common)
        m["xloc"] = np.ascontiguousarray(xl)
        m["flags"] = flags
        in_maps.append(m)
    return in_maps


def kernel(**inputs):
    nc = _get_prog(False)
    in_maps = make_in_maps(inputs)
    res = run_bass_kernel_spmd(nc, in_maps, core_ids=list(range(8)))
    out = np.zeros((4, 4096, D), np.float32)
    for c in range(8):
        b, h = c // 2, c % 2
        out[b, h * 2048:(h + 1) * 2048] = res.results[c]["out"]
    return out
```

```python
import math
import contextlib
import numpy as np
import concourse.bass as bass
import concourse.mybir as mybir
from concourse.bass_utils import run_bass_kernel_spmd

F32 = mybir.dt.float32
BF16 = mybir.dt.bfloat16
ALU = mybir.AluOpType
AF = mybir.ActivationFunctionType
AX = mybir.AxisListType

ENGS = ["pe", "act", "dve", "pool", "sp"]
NDMA_SLOTS = 8
NEG = -1.0e30
EPS = 1e-6
NITER = 16
TOPK = 256
D = 1024
QT0 = 15
NQT = 17


class Sched:
    def __init__(self):
        self.ops = {e: [] for e in ENGS}
        self.last_w = {}
        self.readers = {}
        self.dma_slot_rr = {e: 0 for e in ENGS}
        self.dma_slot_last = {}
        self.dma_slot_uses = {}
        self.pending = {e: set() for e in ENGS}
        self.last_tok = {}
        self.open_dma = set()

    def _deps_for(self, reads, writes):
        deps = set()
        for k in reads:
            deps.update(self.last_w.get(k, {}).values())
        for k in writes:
            deps.update(self.last_w.get(k, {}).values())
            deps.update(self.readers.get(k, {}).values())
        return deps

    def _commit(self, tok, track, reads, writes):
        for k in reads:
            self.readers.setdefault(k, {})[track] = tok
        for k in writes:
            self.last_w[k] = {track: tok}
            self.readers[k] = {}

    def op(self, eng, fn, reads=(), writes=()):
        idx = len(self.ops[eng])
        deps = self._deps_for(reads, writes)
        tok = ("c", eng, idx)
        raw = set()
        if eng != "pe":
            for k in reads:
                raw.update(self.last_w.get(k, {}).values())
        deps = {d for d in deps if not (d[0] == "c" and d[1] == eng) or d in raw}
        deps |= self.pending[eng]
        self.pending[eng] = set()
        self.ops[eng].append(dict(fn=fn, deps=deps, tok=tok, dma=False))
        self._commit(tok, eng, reads, writes)
        self.last_tok[eng] = tok
        return tok

    def dma(self, eng, fn, reads=(), writes=()):
        slot = self.dma_slot_rr[eng]
        self.dma_slot_rr[eng] = (slot + 1) % NDMA_SLOTS
        use = self.dma_slot_uses.get((eng, slot), 0) + 1
        self.dma_slot_uses[(eng, slot)] = use
        tok = ("d", eng, slot, use)
        deps = self._deps_for(reads, writes)
        prev = self.dma_slot_last.get((eng, slot))
        if prev is not None:
            deps.add(prev)
            self.open_dma.discard(prev)
        self.dma_slot_last[(eng, slot)] = tok
        deps |= self.pending[eng]
        self.pending[eng] = set()
        self.ops[eng].append(dict(fn=fn, deps=deps, tok=tok, dma=True))
        self._commit(tok, ("dma", eng, slot), reads, writes)
        self.open_dma.add(tok)
        return tok

    def barrier(self):
        toks = set(self.last_tok.values()) | set(self.open_dma)
        for e in ENGS:
            self.pending[e] |= {t for t in toks if not (t[0] == "c" and t[1] == e)}
        self.open_dma = set()

    def emit(self, nc, final_wait_tokens=()):
        needed = {e: set() for e in ENGS}
        for e in ENGS:
            for o in self.ops[e]:
                for d in o["deps"]:
                    if d[0] == "c":
                        needed[d[1]].add(d[2])
        semval = {e: {} for e in ENGS}
        for e in ENGS:
            c = 0
            for i in range(len(self.ops[e])):
                if i in needed[e]:
                    c += 1
                    semval[e][i] = c
        with contextlib.ExitStack() as st:
            csem = {e: st.enter_context(nc.semaphore("c_" + e)) for e in ENGS if e != "sp"}
            dsem = {}
            for e in ENGS:
                for s in range(NDMA_SLOTS):
                    if (e, s) in self.dma_slot_uses:
                        dsem[(e, s)] = st.enter_context(nc.semaphore("d_%s_%d" % (e, s)))
            block = st.enter_context(nc.Block())

            def resolve(tok):
                if tok[0] == "c":
                    return csem[tok[1]], semval[tok[1]][tok[2]], ("c", tok[1])
                return dsem[(tok[1], tok[2])], 16 * tok[3], ("d", tok[1], tok[2])

            def run(e, engine):
                waited = {}
                for i, o in enumerate(self.ops[e]):
                    ws = {}
                    for d in o["deps"]:
                        sem, val, sk = resolve(d)
                        if waited.get(sk, 0) >= val:
                            continue
                        if sk not in ws or ws[sk][1] < val:
                            ws[sk] = (sem, val)
                    for sk, (sem, val) in ws.items():
                        engine.wait_ge(sem, val)
                        waited[sk] = val
                    ins = o["fn"](engine)
                    if o["dma"]:
                        ins.then_inc(dsem[(o["tok"][1], o["tok"][2])], 16)
                    elif i in needed[e]:
                        ins.then_inc(csem[e], 1)
                if e == "sp":
                    for d in final_wait_tokens:
                        sem, val, sk = resolve(d)
                        engine.wait_ge(sem, val)

            block.tensor(lambda eng: run("pe", eng))
            block.scalar(lambda eng: run("act", eng))
            block.vector(lambda eng: run("dve", eng))
            block.gpsimd(lambda eng: run("pool", eng))
            block.sync(lambda eng: run("sp", eng))


class Arena:
    def __init__(self, ap, nwords):
        self.ap = ap
        self.n = nwords
        self.off = 0

    def alloc(self, free_shape, dt, at=None):
        nel = int(np.prod(free_shape))
        nbytes = nel * (2 if dt == BF16 else 4)
        nw = (nbytes + 3) // 4
        off = self.off if at is None else at
        assert off + nw <= self.n, ("arena overflow", off, nw, self.n)
        a = self.ap[:, off:off + nw]
        if at is None:
            self.off += nw
        if dt == BF16:
            a = a.bitcast(BF16)[:, :nel]
        if len(free_shape) == 2:
            a = a.rearrange("p (a b) -> p a b", a=free_shape[0])
        elif len(free_shape) == 3:
            a = a.rearrange("p (a b c) -> p a b c", a=free_shape[0], b=free_shape[1])
        return a


class Ctx:
    pass


def build_program(debug=False):
    nc = bass.Bass("TRN2", target_bir_lowering=False)
    S = Sched()
    C = Ctx()
    C.nc, C.S = nc, S
    din = lambda n, s: nc.dram_tensor(n, list(s), F32, kind="ExternalInput").ap()
    xloc = din("xloc", [4096, D])
    win = din("win", [D, 2472])
    wout = din("wout", [D, D])
    wup = din("wup", [2, D, 4096])
    wdn = din("wdn", [2, 4096, D])
    cwin = din("cwin", [D, 2560])
    cwout = din("cwout", [D, D])
    gb = din("gb", [8, 128, D])
    biasA_d = din("biasA", [128, 2, 8, 128])
    biasB_d = din("biasB", [128, 2, 8, 128])
    cA_d = din("cA", [128, 8])
    cB_d = din("cB", [128, 8])
    lvb_d = din("lvb", [128, 4, 64])
    sgn_d = din("sgn", [128, 1])
    flags_d = din("flags", [128, 2])
    selA_d = din("selA", [128, 2])
    selI_d = din("selI", [128, 4])
    ident_d = din("ident", [128, 128])
    dwT_d = din("dwT", [128, 4, 31])
    cvec_d = din("cvec", [128, 3, 4])
    scw_d = din("scw", [128, 4, 3])
    out_d = nc.dram_tensor("out", [2048, D], F32, kind="ExternalOutput").ap()
    kind = "ExternalOutput"
    x1d = nc.dram_tensor("x1d", [NQT * 128, D], F32, kind=kind).ap()
    x2d = nc.dram_tensor("x2d", [NQT * 128, D], F32, kind=kind).ap()
    x3d = nc.dram_tensor("x3d", [2048, D], F32, kind=kind).ap()
    ydbg = nc.dram_tensor("ydbg", [NQT, 128, 8, 128], F32, kind="ExternalOutput").ap() if debug else None

    NW = 53200
    with contextlib.ExitStack() as st:
        arena_t = st.enter_context(nc.sbuf_tensor("arena", [128, NW], F32))
        A = Arena(arena_t[:], NW)
        pb = [st.enter_context(nc.psum_tensor("pb%d" % i, [128, 512], F32)) for i in range(7)]
        pbt = st.enter_context(nc.psum_tensor("pbt", [128, 1024], BF16))
        pbk = lambda i: ("pb", i)

        identf = A.alloc((128,), F32)
        identb = A.alloc((128,), BF16)
        onesf = A.alloc((128,), F32)
        onesb = A.alloc((128,), BF16)
        smallc = A.alloc((16,), F32)
        selA = A.alloc((2,), F32)
        selI = A.alloc((4,), F32)
        stat = A.alloc((64,), F32)
        stepc = A.alloc((NITER,), F32)
        P0 = A.off
        epsc, zeroc, flagc, pnegc = smallc[:, 0:1], smallc[:, 1:2], smallc[:, 2:3], smallc[:, 3:4]
        neglam, sgc = smallc[:, 4:5], smallc[:, 5:6]
        C.statctr = 0

        def newstat():
            i = C.statctr % 64
            C.statctr += 1
            return stat[:, i:i + 1], ("stat", i)

        S.dma("sp", lambda e: e.dma_start(out=identf, in_=ident_d), writes=["identf"])
        S.dma("sp", lambda e: e.dma_start(out=smallc[:, 2:4], in_=flags_d), writes=["flags"])
        S.dma("sp", lambda e: e.dma_start(out=selA, in_=selA_d), writes=["selA"])
        S.dma("sp", lambda e: e.dma_start(out=selI, in_=selI_d), writes=["selI"])
        S.op("dve", lambda e: e.tensor_copy(out=identb, in_=identf), reads=["identf"], writes=["identb"])
        S.op("pool", lambda e: e.memset(onesf, 1.0), writes=["onesf"])
        S.op("pool", lambda e: e.memset(onesb, 1.0), writes=["onesb"])
        S.op("pool", lambda e: e.memset(smallc[:, 0:1], EPS), writes=["eps"])
        S.op("pool", lambda e: e.memset(smallc[:, 1:2], 0.0), writes=["zero"])
        for it in range(NITER):
            S.op("pool", lambda e, it=it: e.memset(stepc[:, it:it + 1], 2.0 ** -(it + 1)), writes=["stepc"])

        def rmsnorm_rows(x_ap, x_key, g_ap, g_key, hb_ap, hb_key, junkb):
            ss, ssk = newstat()
            rs, rsk = newstat()
            S.op("act", lambda e: e.activation(out=junkb, in_=x_ap, func=AF.Square, accum_out=ss),
                 reads=[x_key], writes=["junkb", ssk])
            S.op("act", lambda e: e.activation(out=rs, in_=ss, func=AF.Sqrt, scale=1.0 / D, bias=epsc),
                 reads=[ssk, "eps"], writes=[rsk])
            S.op("dve", lambda e: e.reciprocal(out=rs, in_=rs), reads=[rsk], writes=[rsk])
            S.op("dve", lambda e: e.scalar_tensor_tensor(out=hb_ap, in0=x_ap, scalar=rs, in1=g_ap,
                                                         op0=ALU.mult, op1=ALU.mult),
                 reads=[x_key, rsk, g_key], writes=[hb_key])

        def transpose8(hb_ap, hb_key, dst_ap, dst_key, eng="act"):
            pv = pbt[:, 0:1024].rearrange("p (c t) -> p c t", c=8)
            for c in range(8):
                S.op("pe", lambda e, c=c: e.transpose(out=pv[:, c, :], in_=hb_ap[:, c * 128:(c + 1) * 128],
                                                      identity=identb),
                     reads=[hb_key, "identb"], writes=["pbt"])
            if eng == "act":
                S.op("act", lambda e: e.copy(out=dst_ap, in_=pv), reads=["pbt"], writes=[dst_key])
            else:
                S.op("dve", lambda e: e.tensor_copy(out=dst_ap, in_=pv), reads=["pbt"], writes=[dst_key])

        def norm_residual(bank0, bank1, g_ap, g_key, x_ap, x_key, tmp_ap, junkf):
            s0, k0 = newstat()
            s1, k1 = newstat()
            rs, rk = newstat()
            S.op("act", lambda e: e.activation(out=junkf, in_=pb[bank0][:], func=AF.Square, accum_out=s0),
                 reads=[pbk(bank0)], writes=["junkf", k0])
            S.op("act", lambda e: e.activation(out=junkf, in_=pb[bank1][:], func=AF.Square, accum_out=s1),
                 reads=[pbk(bank1)], writes=["junkf", k1])
            S.op("dve", lambda e: e.tensor_tensor(out=rs, in0=s0, in1=s1, op=ALU.add), reads=[k0, k1], writes=[rk])
            S.op("act", lambda e: e.activation(out=rs, in_=rs, func=AF.Sqrt, scale=1.0 / D, bias=epsc),
                 reads=[rk, "eps"], writes=[rk])
            S.op("dve", lambda e: e.reciprocal(out=rs, in_=rs), reads=[rk], writes=[rk])
            for hf, bk in enumerate((bank0, bank1)):
                S.op("dve", lambda e, hf=hf, bk=bk: e.scalar_tensor_tensor(
                    out=tmp_ap[:, hf * 512:(hf + 1) * 512], in0=pb[bk][:], scalar=rs,
                    in1=g_ap[:, hf * 512:(hf + 1) * 512], op0=ALU.mult, op1=ALU.mult),
                    reads=[pbk(bk), rk, g_key], writes=[("tmpn", hf)])
            S.op("pool", lambda e: e.tensor_tensor(out=x_ap, in0=x_ap, in1=tmp_ap, op=ALU.add),
                 reads=[x_key, ("tmpn", 0), ("tmpn", 1)], writes=[x_key])

        def load_w(dst, dst_keyf, src3, ncols, piece):
            for i, c0 in enumerate(range(0, ncols, piece)):
                c1 = min(ncols, c0 + piece)
                S.dma("pool", lambda e, c0=c0, c1=c1: e.dma_start(out=dst[:, :, c0:c1], in_=src3[:, :, c0:c1]),
                      writes=[dst_keyf(i)])

        A.off = P0
        KA = A.alloc((4, 4096), BF16)
        KB2 = A.alloc((4096,), BF16)
        KI4 = A.alloc((4096,), BF16)
        VA = A.alloc((32, 512), BF16)
        VB2 = A.alloc((32, 128), BF16)
        wq = A.alloc((8, 1288), BF16)
        wo = A.alloc((8, 1024), BF16)
        g0 = A.alloc((1024,), F32)
        g1 = A.alloc((1024,), F32)
        biasA = A.alloc((2, 8, 128), BF16)
        biasB = A.alloc((2, 8, 128), BF16)
        xt = A.alloc((1024,), F32)
        hb = A.alloc((1024,), BF16)
        junkb = A.alloc((1024,), BF16)
        OV = A.off
        wk = A.alloc((8, 1408), BF16)
        hT4 = A.alloc((8, 512), BF16)
        xt2 = A.alloc((1024,), F32)
        biasf = A.alloc((2, 8, 128), F32)
        cAB = A.alloc((16,), F32)
        lvb = A.alloc((4, 64), F32)
        lvj = A.alloc((64,), F32)
        kv_end = A.off
        A.off = OV
        sc = A.alloc((4096,), F32)
        junk = A.alloc((2048,), BF16)
        maskT = A.alloc((32, 128), BF16)
        hT = A.alloc((8, 128), BF16)
        qmA = A.alloc((4, 2, 128), BF16)
        qmB = A.alloc((4, 2, 128), BF16)
        qmI = A.alloc((2, 4, 128), BF16)
        wis = A.alloc((8,), F32)
        rbuf = [A.alloc((512,), F32) for _ in range(2)]
        ptb = [A.alloc((512,), BF16) for _ in range(2)]
        pmb = [A.alloc((512,), BF16) for _ in range(2)]
        rz = A.alloc((512,), F32)
        tt = A.alloc((512,), F32)
        dd = A.alloc((256,), F32)
        sq = A.alloc((256,), F32)
        rstd = A.alloc((256,), F32)
        yT = A.alloc((8, 128), BF16)
        tmpn = sc[:, 0:1024]
        junkf = tt
        junk2 = A.alloc((2048,), BF16)
        bis = A.alloc((8,), F32)
        Rs = A.alloc((NITER,), F32)
        q_end = A.off
        assert max(kv_end, q_end) <= NW

        win3 = win.rearrange("(c p) n -> p c n", p=128)
        wkparts = [(0, 512, 512), (512, 2048, 64), (576, 2048, 64), (640, 2432, 32), (672, 2432, 32),
                   (704, 2432, 32), (736, 2432, 32), (768, 1024, 512), (1280, 2112, 64), (1344, 2112, 64)]
        for i, (d0, s0, n) in enumerate(wkparts):
            S.dma("pool", lambda e, d0=d0, s0=s0, n=n: e.dma_start(out=wk[:, :, d0:d0 + n], in_=win3[:, :, s0:s0 + n]),
                  writes=[("wk", i)])
        WK = [("wk", i) for i in range(len(wkparts))]
        wqparts = [(0, 0, 512), (512, 1536, 512), (1024, 2176, 256), (1280, 2464, 8)]
        for i, (d0, s0, n) in enumerate(wqparts):
            S.dma("pool", lambda e, d0=d0, s0=s0, n=n: e.dma_start(out=wq[:, :, d0:d0 + n], in_=win3[:, :, s0:s0 + n]),
                  writes=[("wq", i)])
        WQ = [("wq", i) for i in range(len(wqparts))]
        load_w(wo, lambda i: ("wo", i), wout.rearrange("(c p) n -> p c n", p=128), 1024, 512)
        WO = [("wo", 0), ("wo", 1)]
        S.dma("sp", lambda e: e.dma_start(out=g0, in_=gb[0]), writes=["g0"])
        S.dma("sp", lambda e: e.dma_start(out=g1, in_=gb[1]), writes=["g1"])
        S.dma("sp", lambda e: e.dma_start(out=cAB[:, 0:8], in_=cA_d), writes=["cAB"])
        S.dma("sp", lambda e: e.dma_start(out=cAB[:, 8:16], in_=cB_d), writes=["cAB2"])
        for which, (src, dstb) in enumerate(((biasA_d, biasA), (biasB_d, biasB))):
            S.dma("sp", lambda e, src=src: e.dma_start(out=biasf, in_=src), writes=["biasf"])
            for m in range(8):
                S.op("dve", lambda e, m=m, dstb=dstb, which=which: e.tensor_scalar(
                    out=dstb[:, :, m, :], in0=biasf[:, :, m, :], scalar1=cAB[:, which * 8 + m:which * 8 + m + 1],
                    scalar2=None, op0=ALU.subtract),
                    reads=["biasf", "cAB", "cAB2"], writes=[("bias", which)])
        lam_init = 0.8 - 0.6 * math.exp(-0.3 * 0)
        S.dma("sp", lambda e: e.dma_start(out=lvb, in_=lvb_d), writes=["lvb"])
        S.dma("sp", lambda e: e.dma_start(out=smallc[:, 5:6], in_=sgn_d), writes=["sgraw"])
        e1, e1k = newstat()
        e2, e2k = newstat()
        S.op("dve", lambda e: e.tensor_tensor(out=lvj, in0=lvb[:, 0, :], in1=lvb[:, 1, :], op=ALU.mult),
             reads=["lvb"], writes=["lvj"])
        S.op("dve", lambda e: e.tensor_reduce(out=e1, in_=lvj, axis=AX.X, op=ALU.add), reads=["lvj"], writes=[e1k])
        S.op("dve", lambda e: e.tensor_tensor(out=lvj, in0=lvb[:, 2, :], in1=lvb[:, 3, :], op=ALU.mult),
             reads=["lvb", e1k], writes=["lvj"])
        S.op("dve", lambda e: e.tensor_reduce(out=e2, in_=lvj, axis=AX.X, op=ALU.add), reads=["lvj"], writes=[e2k])
        S.op("act", lambda e: e.activation(out=e1, in_=e1, func=AF.Exp), reads=[e1k], writes=[e1k])
        S.op("act", lambda e: e.activation(out=e2, in_=e2, func=AF.Exp), reads=[e2k], writes=[e2k])
        S.op("dve", lambda e: e.scalar_tensor_tensor(out=neglam, in0=e2, scalar=-lam_init, in1=e1,
                                                     op0=ALU.add, op1=ALU.subtract),
             reads=[e1k, e2k], writes=["neglam"])
        S.op("dve", lambda e: e.tensor_scalar(out=sgc, in0=sgc, scalar1=1.0 - lam_init, scalar2=None, op0=ALU.mult),
             reads=["sgraw"], writes=["sg"])
        S.op("pool", lambda e: e.memset(VB2[:, :, :], 0.0), writes=["VB2"])

        xts = [xt, xt2]
        for g in range(8):
            for i in range(4):
                t = 4 * g + i
                xa = xts[t % 2]
                xk = ("xt", t % 2)
                S.dma("sp", lambda e, t=t, xa=xa: e.dma_start(out=xa, in_=xloc[t * 128:(t + 1) * 128, :]), writes=[xk])
                rmsnorm_rows(xa, xk, g0, "g0", hb, "hb", junkb)
                transpose8(hb, "hb", hT4[:, :, i * 128:(i + 1) * 128], ("hT4", i), eng="act" if i % 2 == 0 else "dve")
            HT4 = [("hT4", i) for i in range(4)]
            for c in range(6):
                bk = c % 2
                for dc in range(8):
                    S.op("pe", lambda e, c=c, dc=dc, bk=bk: e.matmul(pb[bk][:], lhsT=wk[:, dc, c * 128:(c + 1) * 128],
                                                                    rhs=hT4[:, dc, :], start=(dc == 0), stop=(dc == 7)),
                         reads=HT4 + WK, writes=[pbk(bk)])
                if c < 4:
                    dst = KA[:, c, g * 512:(g + 1) * 512]
                elif c == 4:
                    dst = KB2[:, g * 512:(g + 1) * 512]
                else:
                    dst = KI4[:, g * 512:(g + 1) * 512]
                if c % 2 == 0:
                    S.op("act", lambda e, dst=dst, bk=bk: e.copy(out=dst, in_=pb[bk][:]), reads=[pbk(bk)], writes=[("K", c, g)])
                else:
                    S.op("dve", lambda e, dst=dst, bk=bk: e.tensor_copy(out=dst, in_=pb[bk][:]), reads=[pbk(bk)], writes=[("K", c, g)])
            for i in range(4):
                t = 4 * g + i
                for dc in range(8):
                    S.op("pe", lambda e, i=i, dc=dc: e.matmul(pb[2][:], lhsT=hT4[:, dc, i * 128:(i + 1) * 128],
                                                              rhs=wk[:, dc, 768:1280], start=(dc == 0), stop=(dc == 7)),
                         reads=HT4 + WK, writes=[pbk(2)])
                for dc in range(8):
                    S.op("pe", lambda e, i=i, dc=dc: e.matmul(pb[3][:, 0:128], lhsT=hT4[:, dc, i * 128:(i + 1) * 128],
                                                              rhs=wk[:, dc, 1280:1408], start=(dc == 0), stop=(dc == 7)),
                         reads=HT4 + WK, writes=[pbk(3)])
                S.op("act", lambda e, t=t: e.copy(out=VA[:, t, :], in_=pb[2][:]), reads=[pbk(2)], writes=[("VA", t)])
                S.op("dve", lambda e, t=t: e.tensor_copy(out=VB2[:, t, :], in_=pb[3][:, 0:128]),
                     reads=[pbk(3), "VB2"], writes=[("VB", t)])
        S.barrier()

        ALLK = [("K", c, g) for c in range(6) for g in range(8)]
        outs = []
        for qi in range(NQT):
            qt = QT0 + qi
            nk = qt + 1
            N = nk * 128
            xk = ("xt", 0)
            S.dma("sp", lambda e, qt=qt: e.dma_start(out=xt, in_=xloc[qt * 128:(qt + 1) * 128, :]), writes=[xk])
            rmsnorm_rows(xt, xk, g0, "g0", hb, "hb", junkb)
            transpose8(hb, "hb", hT, "hT")
            for c in range(10):
                bank, col = (4, c) if c < 4 else ((5, c - 4) if c < 8 else (6, c - 8))
                for dc in range(8):
                    S.op("pe", lambda e, c=c, dc=dc, bank=bank, col=col: e.matmul(
                        pb[bank][:, col * 128:(col + 1) * 128], lhsT=wq[:, dc, c * 128:(c + 1) * 128], rhs=hT[:, dc, :],
                        start=(dc == 0), stop=(dc == 7)), reads=["hT"] + WQ, writes=[pbk(bank)])
            for dc in range(8):
                S.op("pe", lambda e, dc=dc: e.matmul(pb[6][:, 256:264], lhsT=hT[:, dc, :], rhs=wq[:, dc, 1280:1288],
                                                     start=(dc == 0), stop=(dc == 7)), reads=["hT"] + WQ, writes=[pbk(6)])
            cnt = 0
            for (bank, dstq, nsub, sel) in ((4, qmA, 2, selA), (5, qmB, 2, selA), (6, qmI, 4, selI)):
                for c in range(dstq.shape[1]):
                    for s_ in range(nsub):
                        src = pb[bank][:, c * 128:(c + 1) * 128]
                        dst = dstq[:, c, s_, :]
                        sca = sel[:, s_:s_ + 1]
                        if cnt % 2 == 0:
                            S.op("act", lambda e, src=src, dst=dst, sca=sca: e.activation(out=dst, in_=src, func=AF.Copy, scale=sca),
                                 reads=[pbk(bank), "selA", "selI"], writes=[("qm", bank)])
                        else:
                            S.op("dve", lambda e, src=src, dst=dst, sca=sca: e.tensor_scalar(out=dst, in0=src, scalar1=sca, scalar2=None, op0=ALU.mult),
                                 reads=[pbk(bank), "selA", "selI"], writes=[("qm", bank)])
                        cnt += 1
            S.op("act", lambda e: e.mul(out=wis, in_=pb[6][:, 256:264], mul=(32.0 ** -0.5) * (8.0 ** -0.5)),
                 reads=[pbk(6)], writes=["wis"])
            nkb = (N + 511) // 512
            for kb in range(nkb):
                cols = min(512, N - kb * 512)
                for j in range(8):
                    bk = 4 + (j % 2)
                    S.op("pe", lambda e, j=j, bk=bk, kb=kb, cols=cols: e.matmul(
                        pb[bk][:, 0:cols], lhsT=qmI[:, j // 4, j % 4, :], rhs=KI4[:, kb * 512:kb * 512 + cols],
                        start=True, stop=True), reads=[("qm", 6)] + ALLK, writes=[pbk(bk)])
                    rb = rbuf[j % 2]
                    S.op("act", lambda e, bk=bk, rb=rb, cols=cols: e.activation(out=rb[:, 0:cols], in_=pb[bk][:, 0:cols], func=AF.Relu),
                         reads=[pbk(bk)], writes=[("rbuf", j % 2)])
                    scv = sc[:, kb * 512:kb * 512 + cols]
                    if j == 0:
                        S.op("dve", lambda e, rb=rb, scv=scv, cols=cols: e.tensor_scalar(
                            out=scv, in0=rb[:, 0:cols], scalar1=wis[:, 0:1], scalar2=None, op0=ALU.mult),
                            reads=[("rbuf", 0), "wis"], writes=[("sc", kb)])
                    else:
                        S.op("dve", lambda e, rb=rb, scv=scv, cols=cols, j=j: e.scalar_tensor_tensor(
                            out=scv, in0=rb[:, 0:cols], scalar=wis[:, j:j + 1], in1=scv, op0=ALU.mult, op1=ALU.add),
                            reads=[("rbuf", j % 2), "wis", ("sc", kb)], writes=[("sc", kb)])
            SCK = [("sc", kb) for kb in range(nkb)]
            mx, mn, Rr, lo, tth, cA_, cB_, gg = [bis[:, i:i + 1] for i in range(8)]
            S.op("dve", lambda e, N=N: e.tensor_reduce(out=mx, in_=sc[:, 0:N], axis=AX.X, op=ALU.max), reads=SCK, writes=["b_mx"])
            S.op("dve", lambda e, N=N: e.tensor_reduce(out=lo, in_=sc[:, 0:N], axis=AX.X, op=ALU.min), reads=SCK, writes=["b_lo"])
            S.op("dve", lambda e: e.tensor_tensor(out=Rr, in0=mx, in1=lo, op=ALU.subtract), reads=["b_mx", "b_lo"], writes=["b_R"])
            S.op("pool", lambda e, N=N: e.memset(sc[0:64, N - 64:N], NEG), reads=SCK + ["b_mx", "b_lo"], writes=SCK)
            npv = 15 * 128 if qt == 15 else 2048
            S.op("dve", lambda e, npv=npv: e.tensor_scalar(out=sc[:, 0:npv], in0=sc[:, 0:npv], scalar1=pnegc, scalar2=None, op0=ALU.add),
                 reads=SCK + ["flags"], writes=SCK)
            halves = [(0, N)] if N <= 2048 else [(0, 2048), (2048, N)]
            S.op("dve", lambda e: e.tensor_scalar(out=Rs, in0=stepc, scalar1=Rr, scalar2=None, op0=ALU.mult),
                 reads=["b_R", "stepc"], writes=["b_Rs"])

            def bis_iter(it):
                two = len(halves) == 2
                S.op("dve", lambda e: e.tensor_tensor(out=tth, in0=Rs[:, it:it + 1], in1=lo, op=ALU.add),
                     reads=["b_Rs", "b_lo"], writes=["b_t"])
                if two:
                    S.op("dve", lambda e: e.scalar_tensor_tensor(out=mn, in0=Rs[:, it:it + 1], scalar=-1.0, in1=lo, op0=ALU.mult, op1=ALU.subtract),
                         reads=["b_Rs", "b_lo"], writes=["b_nt"])
                a0, a1 = halves[0]
                S.op("dve", lambda e: e.tensor_scalar(
                    out=junk[:, 0:a1 - a0], in0=sc[:, a0:a1], scalar1=tth, scalar2=None, op0=ALU.is_ge, op1=ALU.add, accum_out=cA_),
                    reads=SCK + ["b_t"], writes=["junk", ("b_c", 0)])
                thr = TOPK - 0.5
                if two:
                    b0, b1 = halves[1]
                    S.op("act", lambda e: e.activation(out=junk2[:, 0:b1 - b0], in_=sc[:, b0:b1], func=AF.Sign, bias=mn, scale=1.0, accum_out=cB_),
                         reads=SCK + ["b_nt"], writes=["junk2", ("b_c", 1)])
                    S.op("dve", lambda e: e.scalar_tensor_tensor(out=cA_, in0=cB_, scalar=0.5, in1=cA_, op0=ALU.mult, op1=ALU.add),
                         reads=[("b_c", 0), ("b_c", 1)], writes=[("b_c", 0)])
                    thr = TOPK - 0.5 - 0.5 * (b1 - b0)
                S.op("dve", lambda e: e.tensor_scalar(out=gg, in0=cA_, scalar1=thr, scalar2=None, op0=ALU.is_ge),
                     reads=[("b_c", 0)], writes=["b_g"])
                S.op("dve", lambda e: e.scalar_tensor_tensor(out=lo, in0=gg, scalar=Rs[:, it:it + 1], in1=lo, op0=ALU.mult, op1=ALU.add),
                     reads=["b_g", "b_lo", "b_Rs"], writes=["b_lo"])

            MK = [("maskT", k4) for k4 in range((nk + 3) // 4)]

            def emit_group(typ, gi, hook=None):
                def qk_stage(kt):
                    sbk = kt % 2
                    band = kt >= qt - 1
                    if band:
                        bt = 0 if kt == qt else 1
                        bsrc = (biasA if typ == "A" else biasB)[:, bt, 4 * gi:4 * gi + 4, :]
                        S.op("pe", lambda e: e.matmul(pb[sbk][:].rearrange("p (a b) -> p a b", a=4), lhsT=identb, rhs=bsrc,
                                                      start=True, stop=False),
                             reads=["identb", ("bias", 0), ("bias", 1)], writes=[pbk(sbk)])
                    for mi in range(4):
                        if typ == "A":
                            h = 2 * gi + mi // 2
                            lhsT = KA[:, h, kt * 128:(kt + 1) * 128]
                            rhs = qmA[:, h, mi % 2, :]
                            qk = ("qm", 4)
                        else:
                            j = 4 * gi + mi
                            lhsT = KB2[:, kt * 128:(kt + 1) * 128]
                            rhs = qmB[:, j // 2, j % 2, :]
                            qk = ("qm", 5)
                        S.op("pe", lambda e, mi=mi, lhsT=lhsT, rhs=rhs: e.matmul(
                            pb[sbk][:, mi * 128:(mi + 1) * 128], lhsT=lhsT, rhs=rhs, start=(not band), stop=((not band) or mi == 3)),
                            reads=[qk] + ALLK, writes=[pbk(sbk)])
                    masked = (kt <= 14) or (kt == 15 and qt >= 16)
                    bias_ap = pnegc if masked else zeroc
                    pt = ptb[kt % 2]
                    S.op("act", lambda e: e.activation(out=pt, in_=pb[sbk][:], func=AF.Exp, bias=bias_ap, scale=1.0),
                         reads=[pbk(sbk), "flags", "zero"], writes=[("pt", kt % 2)])
                    if typ == "B":
                        pm = pmb[kt % 2]
                        S.op("dve", lambda e: e.tensor_tensor(
                            out=pm.rearrange("p (a b) -> p a b", a=4), in0=pt.rearrange("p (a b) -> p a b", a=4),
                            in1=maskT[:, kt, :].unsqueeze(1).to_broadcast([128, 4, 128]), op=ALU.mult),
                            reads=[("pt", kt % 2)] + MK, writes=[("pm", kt % 2)])

                def pv_stage(kt):
                    if typ == "B":
                        src, srck = pmb[kt % 2], ("pm", kt % 2)
                    else:
                        src, srck = ptb[kt % 2], ("pt", kt % 2)
                    if typ == "A":
                        for hl in range(2):
                            h = 2 * gi + hl
                            ob = 2 if hl == 0 else 4
                            S.op("pe", lambda e, hl=hl, h=h, ob=ob: e.matmul(
                                pb[ob][:, 0:256], lhsT=VA[:, kt, h * 128:(h + 1) * 128], rhs=src[:, hl * 256:(hl + 1) * 256],
                                start=(kt == 0), stop=(kt == nk - 1)), reads=[srck, ("VA", kt)], writes=[pbk(ob)])
                    else:
                        S.op("pe", lambda e: e.matmul(pb[2][:], lhsT=VB2[:, kt, :], rhs=src, start=(kt == 0), stop=(kt == nk - 1)),
                             reads=[srck, ("VB", kt)], writes=[pbk(2)])
                    S.op("pe", lambda e: e.matmul(pb[3][:], lhsT=onesb, rhs=src, start=(kt == 0), stop=(kt == nk - 1)),
                         reads=[srck, "onesb"], writes=[pbk(3)])

                qk_stage(0)
                for kt in range(nk):
                    if kt + 1 < nk:
                        qk_stage(kt + 1)
                    pv_stage(kt)
                    if hook is not None:
                        hook(kt)
                S.op("dve", lambda e: e.reciprocal(out=rz, in_=pb[3][:]), reads=[pbk(3)], writes=["rz"])
                if typ == "A":
                    S.op("dve", lambda e: e.tensor_tensor(out=tt[:, 0:256], in0=pb[2][:, 0:256], in1=rz[:, 0:256], op=ALU.mult),
                         reads=[pbk(2), "rz"], writes=["tt"])
                    S.op("dve", lambda e: e.tensor_tensor(out=tt[:, 256:512], in0=pb[4][:, 0:256], in1=rz[:, 256:512], op=ALU.mult),
                         reads=[pbk(4), "rz", "tt"], writes=["tt"])
                    t4 = tt.rearrange("p (h m q) -> p h m q", h=2, m=2)
                    S.op("dve", lambda e: e.scalar_tensor_tensor(out=dd.rearrange("p (h q) -> p h q", h=2), in0=t4[:, :, 1, :], scalar=neglam,
                                                                 in1=t4[:, :, 0, :], op0=ALU.mult, op1=ALU.add),
                         reads=["tt", "neglam"], writes=["dd"])
                    S.op("pool", lambda e: e.tensor_tensor(out=sq, in0=dd, in1=dd, op=ALU.mult), reads=["dd"], writes=["sq"])
                    S.op("pe", lambda e: e.matmul(pb[6][:, 0:256], lhsT=onesf, rhs=sq, start=True, stop=True),
                         reads=["sq", "onesf"], writes=[pbk(6)])
                    S.op("act", lambda e: e.activation(out=rstd, in_=pb[6][:, 0:256], func=AF.Sqrt, scale=1.0 / 128, bias=epsc),
                         reads=[pbk(6), "eps"], writes=["rstd"])
                    S.op("dve", lambda e: e.reciprocal(out=rstd, in_=rstd), reads=["rstd"], writes=["rstd"])
                    S.op("dve", lambda e: e.scalar_tensor_tensor(out=yT[:, 2 * gi:2 * gi + 2, :], in0=dd.rearrange("p (h q) -> p h q", h=2),
                                                                 scalar=sgc, in1=rstd.rearrange("p (h q) -> p h q", h=2),
                                                                 op0=ALU.mult, op1=ALU.mult),
                         reads=["dd", "rstd", "sg"], writes=[("yT", gi)])
                else:
                    for cl in range(2):
                        ch = 4 + 2 * gi + cl
                        for hf in range(2):
                            jl = 2 * cl + hf
                            S.op("dve", lambda e, ch=ch, hf=hf, jl=jl: e.tensor_tensor(
                                out=yT[hf * 64:(hf + 1) * 64, ch, :], in0=pb[2][hf * 64:(hf + 1) * 64, jl * 128:(jl + 1) * 128],
                                in1=rz[hf * 64:(hf + 1) * 64, jl * 128:(jl + 1) * 128], op=ALU.mult),
                                reads=[pbk(2), "rz"], writes=[("yT", 2 + gi)])

            def make_hook(it0, it1):
                st_ = {"next": it0}

                def hook(kt):
                    want = it0 + ((kt + 1) * (it1 - it0)) // nk
                    while st_["next"] < want:
                        bis_iter(st_["next"])
                        st_["next"] += 1
                return hook
            emit_group("A", 0, make_hook(0, NITER // 2))
            emit_group("A", 1, make_hook(NITER // 2, NITER))
            S.op("dve", lambda e, N=N: e.tensor_scalar(out=sc[:, 0:N], in0=sc[:, 0:N], scalar1=lo, scalar2=None, op0=ALU.is_ge),
                 reads=SCK + ["b_lo"], writes=SCK)
            for k4 in range((nk + 3) // 4):
                n4 = min(4, nk - 4 * k4)
                for i in range(n4):
                    kt = 4 * k4 + i
                    S.op("pe", lambda e, kt=kt, i=i: e.transpose(out=pb[6][:, i * 128:(i + 1) * 128], in_=sc[:, kt * 128:(kt + 1) * 128],
                                                                 identity=identf), reads=SCK + ["identf"], writes=[pbk(6)])
                S.op("act", lambda e, k4=k4, n4=n4: e.copy(out=maskT[:, 4 * k4:4 * k4 + n4, :],
                                                           in_=pb[6][:, 0:n4 * 128].rearrange("p (a b) -> p a b", a=n4)),
                     reads=[pbk(6)], writes=[("maskT", k4)])
            emit_group("B", 0)
            emit_group("B", 1)
            YT = [("yT", i) for i in range(4)]
            if debug:
                S.dma("pool", lambda e, qi=qi: e.dma_start(out=ydbg[qi], in_=yT), reads=YT, writes=["ydbg"])
            for hf in range(2):
                for c in range(8):
                    S.op("pe", lambda e, hf=hf, c=c: e.matmul(pb[hf][:], lhsT=yT[:, c, :], rhs=wo[:, c, hf * 512:(hf + 1) * 512],
                                                              start=(c == 0), stop=(c == 7)), reads=YT + WO, writes=[pbk(hf)])
            norm_residual(0, 1, g1, "g1", xt, xk, tmpn, junkf)
            outs.append(S.dma("sp", lambda e, qi=qi: e.dma_start(out=x1d[qi * 128:(qi + 1) * 128, :], in_=xt), reads=[xk], writes=["x1d"]))
        S.barrier()

        def mlp_phase(layer, ntiles, src_d, dst_d, gi0):
            A.off = P0
            Wup = A.alloc((8, 4096), BF16)
            Wdn = A.alloc((32, 1024), BF16)
            ga = A.alloc((1024,), F32)
            gb_ = A.alloc((1024,), F32)
            xtm = [A.alloc((1024,), F32) for _ in range(4)]
            hbm = A.alloc((1024,), BF16)
            jb = A.alloc((1024,), BF16)
            hTm = A.alloc((8, 256), BF16)
            uT = A.alloc((32, 256), BF16)
            rb2 = [A.alloc((2, 256), F32) for _ in range(2)]
            tmpm = A.alloc((1024,), F32)
            jf = A.alloc((512,), F32)
            assert A.off <= NW
            load_w(Wup, lambda i: ("Wup", i), wup[layer].rearrange("(c p) n -> p c n", p=128), 4096, 512)
            for p4 in range(4):
                S.dma("pool", lambda e, p4=p4: e.dma_start(out=Wdn[:, 8 * p4:8 * p4 + 8, :],
                                                          in_=wdn[layer].rearrange("(f p) n -> p f n", p=128)[:, 8 * p4:8 * p4 + 8, :]),
                      writes=[("Wdn", p4)])
            S.dma("sp", lambda e: e.dma_start(out=ga, in_=gb[gi0]), writes=["ga"])
            S.dma("sp", lambda e: e.dma_start(out=gb_, in_=gb[gi0 + 1]), writes=["gb"])
            res = []
            groups = [list(range(i, min(i + 2, ntiles))) for i in range(0, ntiles, 2)]
            for gidx, grp in enumerate(groups):
                T = 128 * len(grp)
                xb = 2 * (gidx % 2)
                for i, t in enumerate(grp):
                    xk = ("xtm", xb + i)
                    S.dma("sp", lambda e, t=t, i=i, xb=xb: e.dma_start(out=xtm[xb + i], in_=src_d[t * 128:(t + 1) * 128, :]), reads=["srcd"], writes=[xk])
                    rmsnorm_rows(xtm[xb + i], xk, ga, "ga", hbm, "hbm", jb)
                    transpose8(hbm, "hbm", hTm[:, :, i * 128:(i + 1) * 128], ("hTm", i), eng="act" if i == 0 else "dve")
                HK = [("hTm", i) for i in range(len(grp))]
                for f2 in range(16):
                    bk = 4 + f2 % 2
                    for fl in range(2):
                        f = 2 * f2 + fl
                        for dc in range(8):
                            S.op("pe", lambda e, f=f, fl=fl, dc=dc, bk=bk, T=T: e.matmul(
                                pb[bk][:, fl * 256:fl * 256 + T], lhsT=Wup[:, dc, f * 128:(f + 1) * 128], rhs=hTm[:, dc, 0:T],
                                start=(dc == 0), stop=(dc == 7)), reads=HK + [("Wup", f // 4)], writes=[pbk(bk)])
                    rb = rb2[f2 % 2]
                    S.op("act", lambda e, bk=bk, rb=rb, T=T: e.activation(
                        out=rb[:, :, 0:T], in_=pb[bk][:].rearrange("p (a b) -> p a b", a=2)[:, :, 0:T], func=AF.Relu),
                        reads=[pbk(bk)], writes=[("rb2", f2 % 2)])
                    S.op("pool", lambda e, rb=rb, f2=f2, T=T: e.tensor_tensor(out=uT[:, 2 * f2:2 * f2 + 2, 0:T], in0=rb[:, :, 0:T],
                                                                           in1=rb[:, :, 0:T], op=ALU.mult),
                         reads=[("rb2", f2 % 2)], writes=[("uT", f2)])
                UK = [("uT", f2) for f2 in range(16)]
                for i, t in enumerate(grp):
                    for hf in range(2):
                        for f in range(32):
                            S.op("pe", lambda e, i=i, hf=hf, f=f: e.matmul(pb[hf][:], lhsT=uT[:, f, i * 128:(i + 1) * 128],
                                                                          rhs=Wdn[:, f, hf * 512:(hf + 1) * 512], start=(f == 0), stop=(f == 31)),
                                 reads=UK + [("Wdn", f // 8)], writes=[pbk(hf)])
                    norm_residual(0, 1, gb_, "gb", xtm[xb + i], ("xtm", xb + i), tmpm, jf)
                    res.append(S.dma("sp", lambda e, t=t, i=i, xb=xb: e.dma_start(out=dst_d[t * 128:(t + 1) * 128, :], in_=xtm[xb + i]),
                                     reads=[("xtm", xb + i)], writes=["dstd"]))
            S.barrier()
            return res

        mlp_phase(0, NQT, x1d, x2d, 2)

        A.off = P0
        wci = A.alloc((8, 2560), BF16)
        wco = A.alloc((8, 1024), BF16)
        dg = A.alloc((31, 4, 128), BF16)
        dwT = A.alloc((4, 31), F32)
        cvec = A.alloc((3, 4), F32)
        scw = A.alloc((4, 3), F32)
        g4 = A.alloc((1024,), F32)
        g5 = A.alloc((1024,), F32)
        xtc = [A.alloc((1024,), F32) for _ in range(4)]
        hbc = A.alloc((1024,), BF16)
        jbc = A.alloc((1024,), BF16)
        hTc = A.alloc((8, 256), BF16)
        uTb = A.alloc((4, 32 + 256), BF16)
        eTb = A.alloc((4, 2 + 256), F32)
        sgb = A.alloc((256,), F32)
        utmp = A.alloc((256,), F32)
        dhs = A.alloc((256,), F32)
        z1 = A.alloc((256,), F32)
        vv = A.alloc((4, 256), F32)
        sqv = A.alloc((4, 256), F32)
        mean = A.alloc((256,), F32)
        msq = A.alloc((256,), F32)
        rsd = A.alloc((256,), F32)
        tcb = A.alloc((256,), F32)
        ymT = A.alloc((8, 256), BF16)
        tmpc = A.alloc((1024,), F32)
        jfc = A.alloc((512,), F32)
        assert A.off <= NW
        load_w(wci, lambda i: ("wci", i), cwin.rearrange("(c p) n -> p c n", p=128), 2560, 512)
        WCI = [("wci", i) for i in range(5)]
        load_w(wco, lambda i: ("wco", i), cwout.rearrange("(c p) n -> p c n", p=128), 1024, 512)
        WCO = [("wco", 0), ("wco", 1)]
        S.dma("sp", lambda e: e.dma_start(out=dwT, in_=dwT_d), writes=["dwT"])
        S.dma("sp", lambda e: e.dma_start(out=cvec, in_=cvec_d), writes=["cvec"])
        S.dma("sp", lambda e: e.dma_start(out=scw, in_=scw_d), writes=["scw"])
        S.dma("sp", lambda e: e.dma_start(out=g4, in_=gb[4]), writes=["g4"])
        S.dma("sp", lambda e: e.dma_start(out=g5, in_=gb[5]), writes=["g5"])
        cnt = 0
        for j in range(31):
            for c in range(4):
                eng = "dve" if cnt % 2 == 0 else "pool"
                S.op(eng, lambda e, j=j, c=c: e.tensor_scalar(out=dg[:, j, c, :], in0=identf, scalar1=dwT[:, c, j:j + 1], scalar2=None, op0=ALU.mult),
                     reads=["identf", "dwT"], writes=[("dg", eng)])
                cnt += 1
        DG = [("dg", "dve"), ("dg", "pool")]
        S.op("pool", lambda e: e.memset(uTb[:, :, :], 0.0), writes=["uTb"])
        S.op("pool", lambda e: e.memset(eTb[:, :, :], 0.0), writes=["eTb"])
        cgroups = [[0]] + [[1 + 2 * i, 2 + 2 * i] for i in range(8)]
        for gidx, grp in enumerate(cgroups):
            T = 128 * len(grp)
            halo = (grp == [0])
            xb = 2 * (gidx % 2)
            for i, li in enumerate(grp):
                xk = ("xtc", xb + i)
                S.dma("sp", lambda e, li=li, i=i, xb=xb: e.dma_start(out=xtc[xb + i], in_=x2d[li * 128:(li + 1) * 128, :]), reads=["dstd"], writes=[xk])
                rmsnorm_rows(xtc[xb + i], xk, g4, "g4", hbc, "hbc", jbc)
                transpose8(hbc, "hbc", hTc[:, :, i * 128:(i + 1) * 128], ("hTc", i), eng="act" if i == 0 else "dve")
            HK = [("hTc", i) for i in range(len(grp))]

            def proj(bank, col0, T=T, HK=HK):
                for dc in range(8):
                    S.op("pe", lambda e, dc=dc: e.matmul(pb[bank][:, 0:T], lhsT=wci[:, dc, col0:col0 + 128], rhs=hTc[:, dc, 0:T],
                                                         start=(dc == 0), stop=(dc == 7)), reads=HK + WCI, writes=[pbk(bank)])
            for c in range(4):
                proj(0, c * 128)
                proj(1, 512 + c * 128)
                S.op("act", lambda e, T=T: e.activation(out=sgb[:, 0:T], in_=pb[1][:, 0:T], func=AF.Sigmoid), reads=[pbk(1)], writes=["sgb"])
                if halo:
                    S.op("dve", lambda e, T=T: e.tensor_tensor(out=utmp[:, 0:T], in0=pb[0][:, 0:T], in1=sgb[:, 0:T], op=ALU.mult),
                         reads=[pbk(0), "sgb"], writes=["utmp"])
                    S.op("dve", lambda e, c=c, T=T: e.tensor_scalar(out=uTb[:, c, 32:32 + T], in0=utmp[:, 0:T], scalar1=flagc, scalar2=None, op0=ALU.mult),
                         reads=["utmp", "flags", "uTb"], writes=[("uT", c)])
                else:
                    S.op("dve", lambda e, c=c, T=T: e.tensor_tensor(out=uTb[:, c, 32:32 + T], in0=pb[0][:, 0:T], in1=sgb[:, 0:T], op=ALU.mult),
                         reads=[pbk(0), "sgb", "uTb"], writes=[("uT", c)])
            for c in range(4):
                proj(2, 1536 + c * 128)
                proj(3, 2048 + c * 128)
                S.op("act", lambda e, T=T: e.copy(out=dhs[:, 0:T], in_=pb[3][:, 0:T]), reads=[pbk(3)], writes=["dhs"])
                if halo:
                    S.op("dve", lambda e, T=T: e.tensor_tensor(out=utmp[:, 0:T], in0=pb[2][:, 0:T], in1=dhs[:, 0:T], op=ALU.mult),
                         reads=[pbk(2), "dhs"], writes=["utmp"])
                    S.op("dve", lambda e, c=c, T=T: e.tensor_scalar(out=eTb[:, c, 2:2 + T], in0=utmp[:, 0:T], scalar1=flagc, scalar2=None, op0=ALU.mult),
                         reads=["utmp", "flags", "eTb"], writes=[("eT", c)])
                else:
                    S.op("dve", lambda e, c=c, T=T: e.tensor_tensor(out=eTb[:, c, 2:2 + T], in0=pb[2][:, 0:T], in1=dhs[:, 0:T], op=ALU.mult),
                         reads=[pbk(2), "dhs", "eTb"], writes=[("eT", c)])
            if not halo:
                for c in range(4):
                    proj(4, 1024 + c * 128)
                    S.op("dve", lambda e, c=c, T=T: e.tensor_scalar(out=z1[:, 0:T], in0=eTb[:, c, 0:T], scalar1=scw[:, c, 0:1], scalar2=None, op0=ALU.mult),
                         reads=[("eT", c), "scw"], writes=["z1"])
                    for jj in (1, 2):
                        S.op("dve", lambda e, c=c, T=T, jj=jj: e.scalar_tensor_tensor(out=z1[:, 0:T], in0=eTb[:, c, jj:jj + T], scalar=scw[:, c, jj:jj + 1],
                                                                                    in1=z1[:, 0:T], op0=ALU.mult, op1=ALU.add),
                             reads=[("eT", c), "scw", "z1"], writes=["z1"])
                    S.op("dve", lambda e, c=c, T=T: e.tensor_tensor(out=ymT[:, 4 + c, 0:T], in0=pb[4][:, 0:T], in1=z1[:, 0:T], op=ALU.mult),
                         reads=[pbk(4), "z1"], writes=[("ymT", 4 + c)])
                for c in range(4):
                    for j in range(31):
                        S.op("pe", lambda e, c=c, j=j, T=T: e.matmul(pb[5][:, 0:T], lhsT=dg[:, j, c, :], rhs=uTb[:, c, 2 + j:2 + j + T],
                                                                     start=(j == 0), stop=(j == 30)),
                             reads=[("uT", c)] + DG, writes=[pbk(5)])
                    S.op("act", lambda e, c=c, T=T: e.activation(out=vv[:, c, 0:T], in_=pb[5][:, 0:T], func=AF.Identity, bias=cvec[:, 0, c:c + 1], scale=1.0),
                         reads=[pbk(5), "cvec"], writes=[("vv", c)])
                    S.op("act", lambda e, c=c, T=T: e.activation(out=sqv[:, c, 0:T], in_=vv[:, c, 0:T], func=AF.Square),
                         reads=[("vv", c)], writes=[("sqv", c)])
                for c in range(4):
                    S.op("pe", lambda e, c=c, T=T: e.matmul(pb[6][:, 0:T], lhsT=onesf, rhs=vv[:, c, 0:T], start=(c == 0), stop=(c == 3)),
                         reads=[("vv", c), "onesf"], writes=[pbk(6)])
                for c in range(4):
                    S.op("pe", lambda e, c=c, T=T: e.matmul(pb[6][:, 256:256 + T], lhsT=onesf, rhs=sqv[:, c, 0:T], start=(c == 0), stop=(c == 3)),
                         reads=[("sqv", c), "onesf"], writes=[pbk(6)])
                S.op("act", lambda e, T=T: e.mul(out=mean[:, 0:T], in_=pb[6][:, 0:T], mul=1.0 / 512), reads=[pbk(6)], writes=["mean"])
                S.op("pool", lambda e, T=T: e.tensor_tensor(out=msq[:, 0:T], in0=mean[:, 0:T], in1=mean[:, 0:T], op=ALU.mult), reads=["mean"], writes=["msq"])
                S.op("dve", lambda e, T=T: e.scalar_tensor_tensor(out=rsd[:, 0:T], in0=pb[6][:, 256:256 + T], scalar=1.0 / 512, in1=msq[:, 0:T],
                                                                op0=ALU.mult, op1=ALU.subtract), reads=[pbk(6), "msq"], writes=["rsd"])
                S.op("act", lambda e, T=T: e.activation(out=rsd[:, 0:T], in_=rsd[:, 0:T], func=AF.Sqrt, scale=1.0, bias=epsc), reads=["rsd", "eps"], writes=["rsd"])
                S.op("dve", lambda e, T=T: e.reciprocal(out=rsd[:, 0:T], in_=rsd[:, 0:T]), reads=["rsd"], writes=["rsd"])
                for c in range(4):
                    S.op("pool", lambda e, c=c, T=T: e.tensor_tensor(out=tcb[:, 0:T], in0=vv[:, c, 0:T], in1=mean[:, 0:T], op=ALU.subtract),
                         reads=[("vv", c), "mean"], writes=["tcb"])
                    S.op("pool", lambda e, T=T: e.tensor_tensor(out=tcb[:, 0:T], in0=tcb[:, 0:T], in1=rsd[:, 0:T], op=ALU.mult),
                         reads=["tcb", "rsd"], writes=["tcb"])
                    S.op("act", lambda e, c=c, T=T: e.activation(out=ymT[:, c, 0:T], in_=tcb[:, 0:T], func=AF.Silu, scale=cvec[:, 1, c:c + 1],
                                                                bias=cvec[:, 2, c:c + 1]), reads=["tcb", "cvec"], writes=[("ymT", c)])
                YM = [("ymT", c) for c in range(8)]
                for i, li in enumerate(grp):
                    for hf in range(2):
                        for c in range(8):
                            S.op("pe", lambda e, i=i, hf=hf, c=c: e.matmul(pb[hf][:], lhsT=ymT[:, c, i * 128:(i + 1) * 128],
                                                                          rhs=wco[:, c, hf * 512:(hf + 1) * 512], start=(c == 0), stop=(c == 7)),
                                 reads=YM + WCO, writes=[pbk(hf)])
                    norm_residual(0, 1, g5, "g5", xtc[xb + i], ("xtc", xb + i), tmpc, jfc)
                    S.dma("sp", lambda e, li=li, i=i, xb=xb: e.dma_start(out=x3d[(li - 1) * 128:li * 128, :], in_=xtc[xb + i]),
                          reads=[("xtc", xb + i)], writes=["x3d"])
            UT = [("uT", c) for c in range(4)]
            ET = [("eT", c) for c in range(4)]
            S.op("pool", lambda e, T=T: e.tensor_copy(out=uTb[:, :, 0:32], in_=uTb[:, :, T:T + 32]), reads=UT, writes=UT + ["uTb"])
            S.op("pool", lambda e, T=T: e.tensor_copy(out=eTb[:, :, 0:2], in_=eTb[:, :, T:T + 2]), reads=ET, writes=ET + ["eTb"])
        S.barrier()

        A.off = P0

        def final_src_fix():
            pass
        res = mlp_phase_final = None
        res = mlp_phase(1, 16, x3d, out_d, 6)
        S.emit(nc, final_wait_tokens=res)
    return nc


def _t5_bucket_np(rel):
    import jax
    import jax.numpy as jnp
    cpu = jax.devices("cpu")[0]
    with jax.default_device(cpu):
        rel = jnp.asarray(rel, dtype=jnp.int32)
        nb = 16
        ret = jnp.where(rel > 0, nb, 0)
        n = jnp.abs(rel)
        max_exact = nb // 2
        nf = jnp.maximum(n, 1).astype(jnp.float32)
        large = max_exact + (jnp.log(nf / max_exact) / math.log(128 / max_exact) * (nb - max_exact)).astype(jnp.int32)
        large = jnp.minimum(large, nb - 1)
        out = ret + jnp.where(n < max_exact, n, large)
        return np.asarray(out)


_PROG = {}


def _get_prog(debug=False):
    if debug not in _PROG:
        _PROG[debug] = build_program(debug)
    return _PROG[debug]


def make_in_maps(inputs):
    f = lambda a: np.ascontiguousarray(np.asarray(a, dtype=np.float32))
    x = f(inputs["x"])
    rel_bias = f(inputs["rel_bias"])
    norm_g = f(inputs["norm_g"])
    kk = np.arange(128)[:, None]
    qq = np.arange(128)[None, :]
    bD = _t5_bucket_np(kk - qq)
    bP = _t5_bucket_np(kk - 128 - qq)
    admiss = (kk // 64) <= (qq // 64)
    tabA = rel_bias[:, :4]
    tabB = rel_bias[:, 4:]
    biasA = np.zeros((128, 2, 8, 128), np.float32)
    biasB = np.zeros((128, 2, 8, 128), np.float32)
    for m in range(8):
        biasA[:, 0, m, :] = np.where(admiss, tabA[bD, m // 2], np.float32(NEG))
        biasA[:, 1, m, :] = tabA[bP, m // 2]
        biasB[:, 0, m, :] = np.where(admiss, tabB[bD, m], np.float32(NEG))
        biasB[:, 1, m, :] = tabB[bP, m]
    cA = np.ascontiguousarray(np.broadcast_to(np.repeat(tabA[15], 2)[None, :], (128, 8)))
    cB = np.ascontiguousarray(np.broadcast_to(tabB[15][None, :], (128, 8)))
    gbb = np.ascontiguousarray(np.broadcast_to(norm_g.reshape(8, 1, D), (8, 128, D)))
    lvb = np.ascontiguousarray(np.broadcast_to(f(inputs["diff_lambda"])[0][None], (128, 4, 64)))
    sgn = np.ascontiguousarray(f(inputs["diff_subln_g"])[0].reshape(128, 1))
    selA = np.zeros((128, 2), np.float32)
    selA[:64, 0] = 0.125
    selA[64:, 1] = 0.125
    selI = np.zeros((128, 4), np.float32)
    for i in range(4):
        selI[32 * i:32 * i + 32, i] = 1.0
    dwT = np.ascontiguousarray(f(inputs["conv_dw_w"])[0].reshape(31, 4, 128).transpose(2, 1, 0))
    cvec = np.ascontiguousarray(np.stack([f(inputs["conv_dw_b"])[0].reshape(4, 128).T,
                                          f(inputs["conv_ln_g"])[0].reshape(4, 128).T,
                                          f(inputs["conv_ln_b"])[0].reshape(4, 128).T], axis=1))
    scw = np.ascontiguousarray(f(inputs["sconv_w"])[0].reshape(3, 4, 128).transpose(2, 1, 0))
    common = dict(win=f(inputs["attn_w_in"])[0], wout=f(inputs["attn_w_out"])[0], wup=f(inputs["w_mlp_up"]),
                  wdn=f(inputs["w_mlp_down"]), cwin=f(inputs["conv_w_in"])[0], cwout=f(inputs["conv_w_out"])[0],
                  gb=gbb, biasA=biasA, biasB=biasB, cA=cA, cB=cB, lvb=lvb, sgn=sgn, selA=selA, selI=selI,
                  ident=np.eye(128, dtype=np.float32), dwT=dwT, cvec=cvec, scw=scw)
    in_maps = []
    for c in range(8):
        b, h = c // 2, c % 2
        if h == 1:
            xl = x[b]
        else:
            xl = np.concatenate([np.zeros((2048, D), np.float32), x[b, :2048]], axis=0)
        flags = np.zeros((128, 2), np.float32)
        flags[:, 0] = float(h)
        flags[:, 1] = 0.0 if h == 1 else NEG
        m = dict(common)
        m["xloc"] = np.ascontiguousarray(xl)
        m["flags"] = flags
        in_maps.append(m)
    return in_maps


def kernel(**inputs):
    nc = _get_prog(False)
    in_maps = make_in_maps(inputs)
    res = run_bass_kernel_spmd(nc, in_maps, core_ids=list(range(8)))
    out = np.zeros((4, 4096, D), np.float32)
    for c in range(8):
        b, h = c // 2, c % 2
        out[b, h * 2048:(h + 1) * 2048] = res.results[c]["out"]
    return out
```

```python
import math
import contextlib
import numpy as np
import concourse.bass as bass
import concourse.mybir as mybir
from concourse.bass_utils import run_bass_kernel_spmd

F32 = mybir.dt.float32
BF16 = mybir.dt.bfloat16
ALU = mybir.AluOpType
AF = mybir.ActivationFunctionType
AX = mybir.AxisListType

ENGS = ["pe", "act", "dve", "pool", "sp"]
NDMA_SLOTS = 8
NEG = -1.0e30
EPS = 1e-6
NITER = 16
TOPK = 256
D = 1024
QT0 = 15
NQT = 17


class Sched:
    def __init__(self):
        self.ops = {e: [] for e in ENGS}
        self.last_w = {}
        self.readers = {}
        self.dma_slot_rr = {e: 0 for e in ENGS}
        self.dma_slot_last = {}
        self.dma_slot_uses = {}
        self.pending = {e: set() for e in ENGS}
        self.last_tok = {}
        self.open_dma = set()

    def _deps_for(self, reads, writes):
        deps = set()
        for k in reads:
            deps.update(self.last_w.get(k, {}).values())
        for k in writes:
            deps.update(self.last_w.get(k, {}).values())
            deps.update(self.readers.get(k, {}).values())
        return deps

    def _commit(self, tok, track, reads, writes):
        for k in reads:
            self.readers.setdefault(k, {})[track] = tok
        for k in writes:
            self.last_w[k] = {track: tok}
            self.readers[k] = {}

    def op(self, eng, fn, reads=(), writes=()):
        idx = len(self.ops[eng])
        deps = self._deps_for(reads, writes)
        tok = ("c", eng, idx)
        raw = set()
        if eng != "pe":
            for k in reads:
                raw.update(self.last_w.get(k, {}).values())
        deps = {d for d in deps if not (d[0] == "c" and d[1] == eng) or d in raw}
        deps |= self.pending[eng]
        self.pending[eng] = set()
        self.ops[eng].append(dict(fn=fn, deps=deps, tok=tok, dma=False))
        self._commit(tok, eng, reads, writes)
        self.last_tok[eng] = tok
        return tok

    def dma(self, eng, fn, reads=(), writes=()):
        slot = self.dma_slot_rr[eng]
        self.dma_slot_rr[eng] = (slot + 1) % NDMA_SLOTS
        use = self.dma_slot_uses.get((eng, slot), 0) + 1
        self.dma_slot_uses[(eng, slot)] = use
        tok = ("d", eng, slot, use)
        deps = self._deps_for(reads, writes)
        prev = self.dma_slot_last.get((eng, slot))
        if prev is not None:
            deps.add(prev)
            self.open_dma.discard(prev)
        self.dma_slot_last[(eng, slot)] = tok
        deps |= self.pending[eng]
        self.pending[eng] = set()
        self.ops[eng].append(dict(fn=fn, deps=deps, tok=tok, dma=True))
        self._commit(tok, ("dma", eng, slot), reads, writes)
        self.open_dma.add(tok)
        return tok

    def barrier(self):
        toks = set(self.last_tok.values()) | set(self.open_dma)
        for e in ENGS:
            self.pending[e] |= {t for t in toks if not (t[0] == "c" and t[1] == e)}
        self.open_dma = set()

    def emit(self, nc, final_wait_tokens=()):
        needed = {e: set() for e in ENGS}
        for e in ENGS:
            for o in self.ops[e]:
                for d in o["deps"]:
                    if d[0] == "c":
                        needed[d[1]].add(d[2])
        semval = {e: {} for e in ENGS}
        for e in ENGS:
            c = 0
            for i in range(len(self.ops[e])):
                if i in needed[e]:
                    c += 1
                    semval[e][i] = c
        with contextlib.ExitStack() as st:
            csem = {e: st.enter_context(nc.semaphore("c_" + e)) for e in ENGS if e != "sp"}
            dsem = {}
            for e in ENGS:
                for s in range(NDMA_SLOTS):
                    if (e, s) in self.dma_slot_uses:
                        dsem[(e, s)] = st.enter_context(nc.semaphore("d_%s_%d" % (e, s)))
            block = st.enter_context(nc.Block())

            def resolve(tok):
                if tok[0] == "c":
                    return csem[tok[1]], semval[tok[1]][tok[2]], ("c", tok[1])
                return dsem[(tok[1], tok[2])], 16 * tok[3], ("d", tok[1], tok[2])

            def run(e, engine):
                waited = {}
                for i, o in enumerate(self.ops[e]):
                    ws = {}
                    for d in o["deps"]:
                        sem, val, sk = resolve(d)
                        if waited.get(sk, 0) >= val:
                            continue
                        if sk not in ws or ws[sk][1] < val:
                            ws[sk] = (sem, val)
                    for sk, (sem, val) in ws.items():
                        engine.wait_ge(sem, val)
                        waited[sk] = val
                    ins = o["fn"](engine)
                    if o["dma"]:
                        ins.then_inc(dsem[(o["tok"][1], o["tok"][2])], 16)
                    elif i in needed[e]:
                        ins.then_inc(csem[e], 1)
                if e == "sp":
                    for d in final_wait_tokens:
                        sem, val, sk = resolve(d)
                        engine.wait_ge(sem, val)

            block.tensor(lambda eng: run("pe", eng))
            block.scalar(lambda eng: run("act", eng))
            block.vector(lambda eng: run("dve", eng))
            block.gpsimd(lambda eng: run("pool", eng))
            block.sync(lambda eng: run("sp", eng))


class Arena:
    def __init__(self, ap, nwords):
        self.ap = ap
        self.n = nwords
        self.off = 0

    def alloc(self, free_shape, dt, at=None):
        nel = int(np.prod(free_shape))
        nbytes = nel * (2 if dt == BF16 else 4)
        nw = (nbytes + 3) // 4
        off = self.off if at is None else at
        assert off + nw <= self.n, ("arena overflow", off, nw, self.n)
        a = self.ap[:, off:off + nw]
        if at is None:
            self.off += nw
        if dt == BF16:
            a = a.bitcast(BF16)[:, :nel]
        if len(free_shape) == 2:
            a = a.rearrange("p (a b) -> p a b", a=free_shape[0])
        elif len(free_shape) == 3:
            a = a.rearrange("p (a b c) -> p a b c", a=free_shape[0], b=free_shape[1])
        return a


class Ctx:
    pass


def build_program(debug=False):
    nc = bass.Bass("TRN2", target_bir_lowering=False)
    S = Sched()
    C = Ctx()
    C.nc, C.S = nc, S
    din = lambda n, s: nc.dram_tensor(n, list(s), F32, kind="ExternalInput").ap()
    xloc = din("xloc", [4096, D])
    win = din("win", [D, 2472])
    wout = din("wout", [D, D])
    wup = din("wup", [2, D, 4096])
    wdn = din("wdn", [2, 4096, D])
    cwin = din("cwin", [D, 2560])
    cwout = din("cwout", [D, D])
    gb = din("gb", [8, 128, D])
    biasA_d = din("biasA", [128, 2, 8, 128])
    biasB_d = din("biasB", [128, 2, 8, 128])
    cA_d = din("cA", [128, 8])
    cB_d = din("cB", [128, 8])
    lvb_d = din("lvb", [128, 4, 64])
    sgn_d = din("sgn", [128, 1])
    flags_d = din("flags", [128, 2])
    selA_d = din("selA", [128, 2])
    selI_d = din("selI", [128, 4])
    ident_d = din("ident", [128, 128])
    dwT_d = din("dwT", [128, 4, 31])
    cvec_d = din("cvec", [128, 3, 4])
    scw_d = din("scw", [128, 4, 3])
    out_d = nc.dram_tensor("out", [2048, D], F32, kind="ExternalOutput").ap()
    kind = "ExternalOutput"
    x1d = nc.dram_tensor("x1d", [NQT * 128, D], F32, kind=kind).ap()
    x2d = nc.dram_tensor("x2d", [NQT * 128, D], F32, kind=kind).ap()
    x3d = nc.dram_tensor("x3d", [2048, D], F32, kind=kind).ap()
    ydbg = nc.dram_tensor("ydbg", [NQT, 128, 8, 128], F32, kind="ExternalOutput").ap() if debug else None

    NW = 53200
    with contextlib.ExitStack() as st:
        arena_t = st.enter_context(nc.sbuf_tensor("arena", [128, NW], F32))
        A = Arena(arena_t[:], NW)
        pb = [st.enter_context(nc.psum_tensor("pb%d" % i, [128, 512], F32)) for i in range(7)]
        pbt = st.enter_context(nc.psum_tensor("pbt", [128, 1024], BF16))
        pbk = lambda i: ("pb", i)

        identf = A.alloc((128,), F32)
        identb = A.alloc((128,), BF16)
        onesf = A.alloc((128,), F32)
        onesb = A.alloc((128,), BF16)
        smallc = A.alloc((16,), F32)
        selA = A.alloc((2,), F32)
        selI = A.alloc((4,), F32)
        stat = A.alloc((64,), F32)
        stepc = A.alloc((NITER,), F32)
        P0 = A.off
        epsc, zeroc, flagc, pnegc = smallc[:, 0:1], smallc[:, 1:2], smallc[:, 2:3], smallc[:, 3:4]
        neglam, sgc = smallc[:, 4:5], smallc[:, 5:6]
        C.statctr = 0

        def newstat():
            i = C.statctr % 64
            C.statctr += 1
            return stat[:, i:i + 1], ("stat", i)

        S.dma("sp", lambda e: e.dma_start(out=identf, in_=ident_d), writes=["identf"])
        S.dma("sp", lambda e: e.dma_start(out=smallc[:, 2:4], in_=flags_d), writes=["flags"])
        S.dma("sp", lambda e: e.dma_start(out=selA, in_=selA_d), writes=["selA"])
        S.dma("sp", lambda e: e.dma_start(out=selI, in_=selI_d), writes=["selI"])
        S.op("dve", lambda e: e.tensor_copy(out=identb, in_=identf), reads=["identf"], writes=["identb"])
        S.op("pool", lambda e: e.memset(onesf, 1.0), writes=["onesf"])
        S.op("pool", lambda e: e.memset(onesb, 1.0), writes=["onesb"])
        S.op("pool", lambda e: e.memset(smallc[:, 0:1], EPS), writes=["eps"])
        S.op("pool", lambda e: e.memset(smallc[:, 1:2], 0.0), writes=["zero"])
        for it in range(NITER):
            S.op("pool", lambda e, it=it: e.memset(stepc[:, it:it + 1], 2.0 ** -(it + 1)), writes=["stepc"])

        def rmsnorm_rows(x_ap, x_key, g_ap, g_key, hb_ap, hb_key, junkb):
            ss, ssk = newstat()
            rs, rsk = newstat()
            S.op("act", lambda e: e.activation(out=junkb, in_=x_ap, func=AF.Square, accum_out=ss),
                 reads=[x_key], writes=["junkb", ssk])
            S.op("act", lambda e: e.activation(out=rs, in_=ss, func=AF.Sqrt, scale=1.0 / D, bias=epsc),
                 reads=[ssk, "eps"], writes=[rsk])
            S.op("dve", lambda e: e.reciprocal(out=rs, in_=rs), reads=[rsk], writes=[rsk])
            S.op("dve", lambda e: e.scalar_tensor_tensor(out=hb_ap, in0=x_ap, scalar=rs, in1=g_ap,
                                                         op0=ALU.mult, op1=ALU.mult),
                 reads=[x_key, rsk, g_key], writes=[hb_key])

        def transpose8(hb_ap, hb_key, dst_ap, dst_key, eng="act"):
            pv = pbt[:, 0:1024].rearrange("p (c t) -> p c t", c=8)
            for c in range(8):
                S.op("pe", lambda e, c=c: e.transpose(out=pv[:, c, :], in_=hb_ap[:, c * 128:(c + 1) * 128],
                                                      identity=identb),
                     reads=[hb_key, "identb"], writes=["pbt"])
            if eng == "act":
                S.op("act", lambda e: e.copy(out=dst_ap, in_=pv), reads=["pbt"], writes=[dst_key])
            else:
                S.op("dve", lambda e: e.tensor_copy(out=dst_ap, in_=pv), reads=["pbt"], writes=[dst_key])

        def norm_residual(bank0, bank1, g_ap, g_key, x_ap, x_key, tmp_ap, junkf):
            s0, k0 = newstat()
            s1, k1 = newstat()
            rs, rk = newstat()
            S.op("act", lambda e: e.activation(out=junkf, in_=pb[bank0][:], func=AF.Square, accum_out=s0),
                 reads=[pbk(bank0)], writes=["junkf", k0])
            S.op("act", lambda e: e.activation(out=junkf, in_=pb[bank1][:], func=AF.Square, accum_out=s1),
                 reads=[pbk(bank1)], writes=["junkf", k1])
            S.op("dve", lambda e: e.tensor_tensor(out=rs, in0=s0, in1=s1, op=ALU.add), reads=[k0, k1], writes=[rk])
            S.op("act", lambda e: e.activation(out=rs, in_=rs, func=AF.Sqrt, scale=1.0 / D, bias=epsc),
                 reads=[rk, "eps"], writes=[rk])
            S.op("dve", lambda e: e.reciprocal(out=rs, in_=rs), reads=[rk], writes=[rk])
            for hf, bk in enumerate((bank0, bank1)):
                S.op("dve", lambda e, hf=hf, bk=bk: e.scalar_tensor_tensor(
                    out=tmp_ap[:, hf * 512:(hf + 1) * 512], in0=pb[bk][:], scalar=rs,
                    in1=g_ap[:, hf * 512:(hf + 1) * 512], op0=ALU.mult, op1=ALU.mult),
                    reads=[pbk(bk), rk, g_key], writes=[("tmpn", hf)])
            S.op("pool", lambda e: e.tensor_tensor(out=x_ap, in0=x_ap, in1=tmp_ap, op=ALU.add),
                 reads=[x_key, ("tmpn", 0), ("tmpn", 1)], writes=[x_key])

        def load_w(dst, dst_keyf, src3, ncols, piece):
            for i, c0 in enumerate(range(0, ncols, piece)):
                c1 = min(ncols, c0 + piece)
                S.dma("pool", lambda e, c0=c0, c1=c1: e.dma_start(out=dst[:, :, c0:c1], in_=src3[:, :, c0:c1]),
                      writes=[dst_keyf(i)])

        A.off = P0
        KA = A.alloc((4, 4096), BF16)
        KB2 = A.alloc((4096,), BF16)
        KI4 = A.alloc((4096,), BF16)
        VA = A.alloc((32, 512), BF16)
        VB2 = A.alloc((32, 128), BF16)
        wq = A.alloc((8, 1288), BF16)
        wo = A.alloc((8, 1024), BF16)
        g0 = A.alloc((1024,), F32)
        g1 = A.alloc((1024,), F32)
        biasA = A.alloc((2, 8, 128), BF16)
        biasB = A.alloc((2, 8, 128), BF16)
        xt = A.alloc((1024,), F32)
        hb = A.alloc((1024,), BF16)
        junkb = A.alloc((1024,), BF16)
        OV = A.off
        wk = A.alloc((8, 1408), BF16)
        hT4 = A.alloc((8, 512), BF16)
        xt2 = A.alloc((1024,), F32)
        biasf = A.alloc((2, 8, 128), F32)
        cAB = A.alloc((16,), F32)
        lvb = A.alloc((4, 64), F32)
        lvj = A.alloc((64,), F32)
        kv_end = A.off
        A.off = OV
        sc = A.alloc((4096,), F32)
        junk = A.alloc((2048,), BF16)
        maskT = A.alloc((32, 128), BF16)
        hT = A.alloc((8, 128), BF16)
        qmA = A.alloc((4, 2, 128), BF16)
        qmB = A.alloc((4, 2, 128), BF16)
        qmI = A.alloc((2, 4, 128), BF16)
        wis = A.alloc((8,), F32)
        rbuf = [A.alloc((512,), F32) for _ in range(2)]
        ptb = [A.alloc((512,), BF16) for _ in range(2)]
        pmb = [A.alloc((512,), BF16) for _ in range(2)]
        rz = A.alloc((512,), F32)
        tt = A.alloc((512,), F32)
        dd = A.alloc((256,), F32)
        sq = A.alloc((256,), F32)
        rstd = A.alloc((256,), F32)
        yT = A.alloc((8, 128), BF16)
        tmpn = sc[:, 0:1024]
        junkf = tt
        junk2 = A.alloc((2048,), BF16)
        bis = A.alloc((8,), F32)
        Rs = A.alloc((NITER,), F32)
        q_end = A.off
        assert max(kv_end, q_end) <= NW

        win3 = win.rearrange("(c p) n -> p c n", p=128)
        wkparts = [(0, 512, 512), (512, 2048, 64), (576, 2048, 64), (640, 2432, 32), (672, 2432, 32),
                   (704, 2432, 32), (736, 2432, 32), (768, 1024, 512), (1280, 2112, 64), (1344, 2112, 64)]
        for i, (d0, s0, n) in enumerate(wkparts):
            S.dma("pool", lambda e, d0=d0, s0=s0, n=n: e.dma_start(out=wk[:, :, d0:d0 + n], in_=win3[:, :, s0:s0 + n]),
                  writes=[("wk", i)])
        WK = [("wk", i) for i in range(len(wkparts))]
        wqparts = [(0, 0, 512), (512, 1536, 512), (1024, 2176, 256), (1280, 2464, 8)]
        for i, (d0, s0, n) in enumerate(wqparts):
            S.dma("pool", lambda e, d0=d0, s0=s0, n=n: e.dma_start(out=wq[:, :, d0:d0 + n], in_=win3[:, :, s0:s0 + n]),
                  writes=[("wq", i)])
        WQ = [("wq", i) for i in range(len(wqparts))]
        load_w(wo, lambda i: ("wo", i), wout.rearrange("(c p) n -> p c n", p=128), 1024, 512)
        WO = [("wo", 0), ("wo", 1)]
        S.dma("sp", lambda e: e.dma_start(out=g0, in_=gb[0]), writes=["g0"])
        S.dma("sp", lambda e: e.dma_start(out=g1, in_=gb[1]), writes=["g1"])
        S.dma("sp", lambda e: e.dma_start(out=cAB[:, 0:8], in_=cA_d), writes=["cAB"])
        S.dma("sp", lambda e: e.dma_start(out=cAB[:, 8:16], in_=cB_d), writes=["cAB2"])
        for which, (src, dstb) in enumerate(((biasA_d, biasA), (biasB_d, biasB))):
            S.dma("sp", lambda e, src=src: e.dma_start(out=biasf, in_=src), writes=["biasf"])
            for m in range(8):
                S.op("dve", lambda e, m=m, dstb=dstb, which=which: e.tensor_scalar(
                    out=dstb[:, :, m, :], in0=biasf[:, :, m, :], scalar1=cAB[:, which * 8 + m:which * 8 + m + 1],
                    scalar2=None, op0=ALU.subtract),
                    reads=["biasf", "cAB", "cAB2"], writes=[("bias", which)])
        lam_init = 0.8 - 0.6 * math.exp(-0.3 * 0)
        S.dma("sp", lambda e: e.dma_start(out=lvb, in_=lvb_d), writes=["lvb"])
        S.dma("sp", lambda e: e.dma_start(out=smallc[:, 5:6], in_=sgn_d), writes=["sgraw"])
        e1, e1k = newstat()
        e2, e2k = newstat()
        S.op("dve", lambda e: e.tensor_tensor(out=lvj, in0=lvb[:, 0, :], in1=lvb[:, 1, :], op=ALU.mult),
             reads=["lvb"], writes=["lvj"])
        S.op("dve", lambda e: e.tensor_reduce(out=e1, in_=lvj, axis=AX.X, op=ALU.add), reads=["lvj"], writes=[e1k])
        S.op("dve", lambda e: e.tensor_tensor(out=lvj, in0=lvb[:, 2, :], in1=lvb[:, 3, :], op=ALU.mult),
             reads=["lvb", e1k], writes=["lvj"])
        S.op("dve", lambda e: e.tensor_reduce(out=e2, in_=lvj, axis=AX.X, op=ALU.add), reads=["lvj"], writes=[e2k])
        S.op("act", lambda e: e.activation(out=e1, in_=e1, func=AF.Exp), reads=[e1k], writes=[e1k])
        S.op("act", lambda e: e.activation(out=e2, in_=e2, func=AF.Exp), reads=[e2k], writes=[e2k])
        S.op("dve", lambda e: e.scalar_tensor_tensor(out=neglam, in0=e2, scalar=-lam_init, in1=e1,
                                                     op0=ALU.add, op1=ALU.subtract),
             reads=[e1k, e2k], writes=["neglam"])
        S.op("dve", lambda e: e.tensor_scalar(out=sgc, in0=sgc, scalar1=1.0 - lam_init, scalar2=None, op0=ALU.mult),
             reads=["sgraw"], writes=["sg"])
        S.op("pool", lambda e: e.memset(VB2[:, :, :], 0.0), writes=["VB2"])

        xts = [xt, xt2]
        for g in range(8):
            for i in range(4):
                t = 4 * g + i
                xa = xts[t % 2]
                xk = ("xt", t % 2)
                S.dma("sp", lambda e, t=t, xa=xa: e.dma_start(out=xa, in_=xloc[t * 128:(t + 1) * 128, :]), writes=[xk])
                rmsnorm_rows(xa, xk, g0, "g0", hb, "hb", junkb)
                transpose8(hb, "hb", hT4[:, :, i * 128:(i + 1) * 128], ("hT4", i), eng="act" if i % 2 == 0 else "dve")
            HT4 = [("hT4", i) for i in range(4)]
            for c in range(6):
                bk = c % 2
                for dc in range(8):
                    S.op("pe", lambda e, c=c, dc=dc, bk=bk: e.matmul(pb[bk][:], lhsT=wk[:, dc, c * 128:(c + 1) * 128],
                                                                    rhs=hT4[:, dc, :], start=(dc == 0), stop=(dc == 7)),
                         reads=HT4 + WK, writes=[pbk(bk)])
                if c < 4:
                    dst = KA[:, c, g * 512:(g + 1) * 512]
                elif c == 4:
                    dst = KB2[:, g * 512:(g + 1) * 512]
                else:
                    dst = KI4[:, g * 512:(g + 1) * 512]
                if c % 2 == 0:
                    S.op("act", lambda e, dst=dst, bk=bk: e.copy(out=dst, in_=pb[bk][:]), reads=[pbk(bk)], writes=[("K", c, g)])
                else:
                    S.op("dve", lambda e, dst=dst, bk=bk: e.tensor_copy(out=dst, in_=pb[bk][:]), reads=[pbk(bk)], writes=[("K", c, g)])
            for i in range(4):
                t = 4 * g + i
                for dc in range(8):
                    S.op("pe", lambda e, i=i, dc=dc: e.matmul(pb[2][:], lhsT=hT4[:, dc, i * 128:(i + 1) * 128],
                                                              rhs=wk[:, dc, 768:1280], start=(dc == 0), stop=(dc == 7)),
                         reads=HT4 + WK, writes=[pbk(2)])
                for dc in range(8):
                    S.op("pe", lambda e, i=i, dc=dc: e.matmul(pb[3][:, 0:128], lhsT=hT4[:, dc, i * 128:(i + 1) * 128],
                                                              rhs=wk[:, dc, 1280:1408], start=(dc == 0), stop=(dc == 7)),
                         reads=HT4 + WK, writes=[pbk(3)])
                S.op("act", lambda e, t=t: e.copy(out=VA[:, t, :], in_=pb[2][:]), reads=[pbk(2)], writes=[("VA", t)])
                S.op("dve", lambda e, t=t: e.tensor_copy(out=VB2[:, t, :], in_=pb[3][:, 0:128]),
                     reads=[pbk(3), "VB2"], writes=[("VB", t)])
        S.barrier()

        ALLK = [("K", c, g) for c in range(6) for g in range(8)]
        outs = []
        for qi in range(NQT):
            qt = QT0 + qi
            nk = qt + 1
            N = nk * 128
            xk = ("xt", 0)
            S.dma("sp", lambda e, qt=qt: e.dma_start(out=xt, in_=xloc[qt * 128:(qt + 1) * 128, :]), writes=[xk])
            rmsnorm_rows(xt, xk, g0, "g0", hb, "hb", junkb)
            transpose8(hb, "hb", hT, "hT")
            for c in range(10):
                bank, col = (4, c) if c < 4 else ((5, c - 4) if c < 8 else (6, c - 8))
                for dc in range(8):
                    S.op("pe", lambda e, c=c, dc=dc, bank=bank, col=col: e.matmul(
                        pb[bank][:, col * 128:(col + 1) * 128], lhsT=wq[:, dc, c * 128:(c + 1) * 128], rhs=hT[:, dc, :],
                        start=(dc == 0), stop=(dc == 7)), reads=["hT"] + WQ, writes=[pbk(bank)])
            for dc in range(8):
                S.op("pe", lambda e, dc=dc: e.matmul(pb[6][:, 256:264], lhsT=hT[:, dc, :], rhs=wq[:, dc, 1280:1288],
                                                     start=(dc == 0), stop=(dc == 7)), reads=["hT"] + WQ, writes=[pbk(6)])
            cnt = 0
            for (bank, dstq, nsub, sel) in ((4, qmA, 2, selA), (5, qmB, 2, selA), (6, qmI, 4, selI)):
                for c in range(dstq.shape[1]):
                    for s_ in range(nsub):
                        src = pb[bank][:, c * 128:(c + 1) * 128]
                        dst = dstq[:, c, s_, :]
                        sca = sel[:, s_:s_ + 1]
                        if cnt % 2 == 0:
                            S.op("act", lambda e, src=src, dst=dst, sca=sca: e.activation(out=dst, in_=src, func=AF.Copy, scale=sca),
                                 reads=[pbk(bank), "selA", "selI"], writes=[("qm", bank)])
                        else:
                            S.op("dve", lambda e, src=src, dst=dst, sca=sca: e.tensor_scalar(out=dst, in0=src, scalar1=sca, scalar2=None, op0=ALU.mult),
                                 reads=[pbk(bank), "selA", "selI"], writes=[("qm", bank)])
                        cnt += 1
            S.op("act", lambda e: e.mul(out=wis, in_=pb[6][:, 256:264], mul=(32.0 ** -0.5) * (8.0 ** -0.5)),
                 reads=[pbk(6)], writes=["wis"])
            nkb = (N + 511) // 512
            for kb in range(nkb):
                cols = min(512, N - kb * 512)
                for j in range(8):
                    bk = 4 + (j % 2)
                    S.op("pe", lambda e, j=j, bk=bk, kb=kb, cols=cols: e.matmul(
                        pb[bk][:, 0:cols], lhsT=qmI[:, j // 4, j % 4, :], rhs=KI4[:, kb * 512:kb * 512 + cols],
                        start=True, stop=True), reads=[("qm", 6)] + ALLK, writes=[pbk(bk)])
                    rb = rbuf[j % 2]
                    S.op("act", lambda e, bk=bk, rb=rb, cols=cols: e.activation(out=rb[:, 0:cols], in_=pb[bk][:, 0:cols], func=AF.Relu),
                         reads=[pbk(bk)], writes=[("rbuf", j % 2)])
                    scv = sc[:, kb * 512:kb * 512 + cols]
                    if j == 0:
                        S.op("dve", lambda e, rb=rb, scv=scv, cols=cols: e.tensor_scalar(
                            out=scv, in0=rb[:, 0:cols], scalar1=wis[:, 0:1], scalar2=None, op0=ALU.mult),
                            reads=[("rbuf", 0), "wis"], writes=[("sc", kb)])
                    else:
                        S.op("dve", lambda e, rb=rb, scv=scv, cols=cols, j=j: e.scalar_tensor_tensor(
                            out=scv, in0=rb[:, 0:cols], scalar=wis[:, j:j + 1], in1=scv, op0=ALU.mult, op1=ALU.add),
                            reads=[("rbuf", j % 2), "wis", ("sc", kb)], writes=[("sc", kb)])
            SCK = [("sc", kb) for kb in range(nkb)]
            mx, mn, Rr, lo, tth, cA_, cB_, gg = [bis[:, i:i + 1] for i in range(8)]
            S.op("dve", lambda e, N=N: e.tensor_reduce(out=mx, in_=sc[:, 0:N], axis=AX.X, op=ALU.max), reads=SCK, writes=["b_mx"])
            S.op("dve", lambda e, N=N: e.tensor_reduce(out=lo, in_=sc[:, 0:N], axis=AX.X, op=ALU.min), reads=SCK, writes=["b_lo"])
            S.op("dve", lambda e: e.tensor_tensor(out=Rr, in0=mx, in1=lo, op=ALU.subtract), reads=["b_mx", "b_lo"], writes=["b_R"])
            S.op("pool", lambda e, N=N: e.memset(sc[0:64, N - 64:N], NEG), reads=SCK + ["b_mx", "b_lo"], writes=SCK)
            npv = 15 * 128 if qt == 15 else 2048
            S.op("dve", lambda e, npv=npv: e.tensor_scalar(out=sc[:, 0:npv], in0=sc[:, 0:npv], scalar1=pnegc, scalar2=None, op0=ALU.add),
                 reads=SCK + ["flags"], writes=SCK)
            halves = [(0, N)] if N <= 2048 else [(0, 2048), (2048, N)]
            S.op("dve", lambda e: e.tensor_scalar(out=Rs, in0=stepc, scalar1=Rr, scalar2=None, op0=ALU.mult),
                 reads=["b_R", "stepc"], writes=["b_Rs"])

            def bis_iter(it):
                two = len(halves) == 2
                S.op("dve", lambda e: e.tensor_tensor(out=tth, in0=Rs[:, it:it + 1], in1=lo, op=ALU.add),
                     reads=["b_Rs", "b_lo"], writes=["b_t"])
                if two:
                    S.op("dve", lambda e: e.scalar_tensor_tensor(out=mn, in0=Rs[:, it:it + 1], scalar=-1.0, in1=lo, op0=ALU.mult, op1=ALU.subtract),
                         reads=["b_Rs", "b_lo"], writes=["b_nt"])
                a0, a1 = halves[0]
                S.op("dve", lambda e: e.tensor_scalar(
                    out=junk[:, 0:a1 - a0], in0=sc[:, a0:a1], scalar1=tth, scalar2=None, op0=ALU.is_ge, op1=ALU.add, accum_out=cA_),
                    reads=SCK + ["b_t"], writes=["junk", ("b_c", 0)])
                thr = TOPK - 0.5
                if two:
                    b0, b1 = halves[1]
                    S.op("act", lambda e: e.activation(out=junk2[:, 0:b1 - b0], in_=sc[:, b0:b1], func=AF.Sign, bias=mn, scale=1.0, accum_out=cB_),
                         reads=SCK + ["b_nt"], writes=["junk2", ("b_c", 1)])
                    S.op("dve", lambda e: e.scalar_tensor_tensor(out=cA_, in0=cB_, scalar=0.5, in1=cA_, op0=ALU.mult, op1=ALU.add),
                         reads=[("b_c", 0), ("b_c", 1)], writes=[("b_c", 0)])
                    thr = TOPK - 0.5 - 0.5 * (b1 - b0)
                S.op("dve", lambda e: e.tensor_scalar(out=gg, in0=cA_, scalar1=thr, scalar2=None, op0=ALU.is_ge),
                     reads=[("b_c", 0)], writes=["b_g"])
                S.op("dve", lambda e: e.scalar_tensor_tensor(out=lo, in0=gg, scalar=Rs[:, it:it + 1], in1=lo, op0=ALU.mult, op1=ALU.add),
                     reads=["b_g", "b_lo", "b_Rs"], writes=["b_lo"])

            MK = [("maskT", k4) for k4 in range((nk + 3) // 4)]

            def emit_group(typ, gi, hook=None):
                def qk_stage(kt):
                    sbk = kt % 2
                    band = kt >= qt - 1
                    if band:
                        bt = 0 if kt == qt else 1
                        bsrc = (biasA if typ == "A" else biasB)[:, bt, 4 * gi:4 * gi + 4, :]
                        S.op("pe", lambda e: e.matmul(pb[sbk][:].rearrange("p (a b) -> p a b", a=4), lhsT=identb, rhs=bsrc,
                                                      start=True, stop=False),
                             reads=["identb", ("bias", 0), ("bias", 1)], writes=[pbk(sbk)])
                    for mi in range(4):
                        if typ == "A":
                            h = 2 * gi + mi // 2
                            lhsT = KA[:, h, kt * 128:(kt + 1) * 128]
                            rhs = qmA[:, h, mi % 2, :]
                            qk = ("qm", 4)
                        else:
                            j = 4 * gi + mi
                            lhsT = KB2[:, kt * 128:(kt + 1) * 128]
                            rhs = qmB[:, j // 2, j % 2, :]
                            qk = ("qm", 5)
                        S.op("pe", lambda e, mi=mi, lhsT=lhsT, rhs=rhs: e.matmul(
                            pb[sbk][:, mi * 128:(mi + 1) * 128], lhsT=lhsT, rhs=rhs, start=(not band), stop=((not band) or mi == 3)),
                            reads=[qk] + ALLK, writes=[pbk(sbk)])
                    masked = (kt <= 14) or (kt == 15 and qt >= 16)
                    bias_ap = pnegc if masked else zeroc
                    pt = ptb[kt % 2]
                    S.op("act", lambda e: e.activation(out=pt, in_=pb[sbk][:], func=AF.Exp, bias=bias_ap, scale=1.0),
                         reads=[pbk(sbk), "flags", "zero"], writes=[("pt", kt % 2)])
                    if typ == "B":
                        pm = pmb[kt % 2]
                        S.op("dve", lambda e: e.tensor_tensor(
                            out=pm.rearrange("p (a b) -> p a b", a=4), in0=pt.rearrange("p (a b) -> p a b", a=4),
                            in1=maskT[:, kt, :].unsqueeze(1).to_broadcast([128, 4, 128]), op=ALU.mult),
                            reads=[("pt", kt % 2)] + MK, writes=[("pm", kt % 2)])

                def pv_stage(kt):
                    if typ == "B":
                        src, srck = pmb[kt % 2], ("pm", kt % 2)
                    else:
                        src, srck = ptb[kt % 2], ("pt", kt % 2)
                    if typ == "A":
                        for hl in range(2):
                            h = 2 * gi + hl
                            ob = 2 if hl == 0 else 4
                            S.op("pe", lambda e, hl=hl, h=h, ob=ob: e.matmul(
                                pb[ob][:, 0:256], lhsT=VA[:, kt, h * 128:(h + 1) * 128], rhs=src[:, hl * 256:(hl + 1) * 256],
                                start=(kt == 0), stop=(kt == nk - 1)), reads=[srck, ("VA", kt)], writes=[pbk(ob)])
                    else:
                        S.op("pe", lambda e: e.matmul(pb[2][:], lhsT=VB2[:, kt, :], rhs=src, start=(kt == 0), stop=(kt == nk - 1)),
                             reads=[srck, ("VB", kt)], writes=[pbk(2)])
                    S.op("pe", lambda e: e.matmul(pb[3][:], lhsT=onesb, rhs=src, start=(kt == 0), stop=(kt == nk - 1)),
                         reads=[srck, "onesb"], writes=[pbk(3)])

                qk_stage(0)
                for kt in range(nk):
                    if kt + 1 < nk:
                        qk_stage(kt + 1)
                    pv_stage(kt)
                    if hook is not None:
                        hook(kt)
                S.op("dve", lambda e: e.reciprocal(out=rz, in_=pb[3][:]), reads=[pbk(3)], writes=["rz"])
                if typ == "A":
                    S.op("dve", lambda e: e.tensor_tensor(out=tt[:, 0:256], in0=pb[2][:, 0:256], in1=rz[:, 0:256], op=ALU.mult),
                         reads=[pbk(2), "rz"], writes=["tt"])
                    S.op("dve", lambda e: e.tensor_tensor(out=tt[:, 256:512], in0=pb[4][:, 0:256], in1=rz[:, 256:512], op=ALU.mult),
                         reads=[pbk(4), "rz", "tt"], writes=["tt"])
                    t4 = tt.rearrange("p (h m q) -> p h m q", h=2, m=2)
                    S.op("dve", lambda e: e.scalar_tensor_tensor(out=dd.rearrange("p (h q) -> p h q", h=2), in0=t4[:, :, 1, :], scalar=neglam,
                                                                 in1=t4[:, :, 0, :], op0=ALU.mult, op1=ALU.add),
                         reads=["tt", "neglam"], writes=["dd"])
                    S.op("pool", lambda e: e.tensor_tensor(out=sq, in0=dd, in1=dd, op=ALU.mult), reads=["dd"], writes=["sq"])
                    S.op("pe", lambda e: e.matmul(pb[6][:, 0:256], lhsT=onesf, rhs=sq, start=True, stop=True),
                         reads=["sq", "onesf"], writes=[pbk(6)])
                    S.op("act", lambda e: e.activation(out=rstd, in_=pb[6][:, 0:256], func=AF.Sqrt, scale=1.0 / 128, bias=epsc),
                         reads=[pbk(6), "eps"], writes=["rstd"])
                    S.op("dve", lambda e: e.reciprocal(out=rstd, in_=rstd), reads=["rstd"], writes=["rstd"])
                    S.op("dve", lambda e: e.scalar_tensor_tensor(out=yT[:, 2 * gi:2 * gi + 2, :], in0=dd.rearrange("p (h q) -> p h q", h=2),
                                                                 scalar=sgc, in1=rstd.rearrange("p (h q) -> p h q", h=2),
                                                                 op0=ALU.mult, op1=ALU.mult),
                         reads=["dd", "rstd", "sg"], writes=[("yT", gi)])
                else:
                    for cl in range(2):
                        ch = 4 + 2 * gi + cl
                        for hf in range(2):
                            jl = 2 * cl + hf
                            S.op("dve", lambda e, ch=ch, hf=hf, jl=jl: e.tensor_tensor(
                                out=yT[hf * 64:(hf + 1) * 64, ch, :], in0=pb[2][hf * 64:(hf + 1) * 64, jl * 128:(jl + 1) * 128],
                                in1=rz[hf * 64:(hf + 1) * 64, jl * 128:(jl + 1) * 128], op=ALU.mult),
                                reads=[pbk(2), "rz"], writes=[("yT", 2 + gi)])

            def make_hook(it0, it1):
                st_ = {"next": it0}

                def hook(kt):
                    want = it0 + ((kt + 1) * (it1 - it0)) // nk
                    while st_["next"] < want:
                        bis_iter(st_["next"])
                        st_["next"] += 1
                return hook
            emit_group("A", 0, make_hook(0, NITER // 2))
            emit_group("A", 1, make_hook(NITER // 2, NITER))
            S.op("dve", lambda e, N=N: e.tensor_scalar(out=sc[:, 0:N], in0=sc[:, 0:N], scalar1=lo, scalar2=None, op0=ALU.is_ge),
                 reads=SCK + ["b_lo"], writes=SCK)
            for k4 in range((nk + 3) // 4):
                n4 = min(4, nk - 4 * k4)
                for i in range(n4):
                    kt = 4 * k4 + i
                    S.op("pe", lambda e, kt=kt, i=i: e.transpose(out=pb[6][:, i * 128:(i + 1) * 128], in_=sc[:, kt * 128:(kt + 1) * 128],
                                                                 identity=identf), reads=SCK + ["identf"], writes=[pbk(6)])
                S.op("act", lambda e, k4=k4, n4=n4: e.copy(out=maskT[:, 4 * k4:4 * k4 + n4, :],
                                                           in_=pb[6][:, 0:n4 * 128].rearrange("p (a b) -> p a b", a=n4)),
                     reads=[pbk(6)], writes=[("maskT", k4)])
            emit_group("B", 0)
            emit_group("B", 1)
            YT = [("yT", i) for i in range(4)]
            if debug:
                S.dma("pool", lambda e, qi=qi: e.dma_start(out=ydbg[qi], in_=yT), reads=YT, writes=["ydbg"])
            for hf in range(2):
                for c in range(8):
                    S.op("pe", lambda e, hf=hf, c=c: e.matmul(pb[hf][:], lhsT=yT[:, c, :], rhs=wo[:, c, hf * 512:(hf + 1) * 512],
                                                              start=(c == 0), stop=(c == 7)), reads=YT + WO, writes=[pbk(hf)])
            norm_residual(0, 1, g1, "g1", xt, xk, tmpn, junkf)
            outs.append(S.dma("sp", lambda e, qi=qi: e.dma_start(out=x1d[qi * 128:(qi + 1) * 128, :], in_=xt), reads=[xk], writes=["x1d"]))
        S.barrier()

        def mlp_phase(layer, ntiles, src_d, dst_d, gi0):
            A.off = P0
            Wup = A.alloc((8, 4096), BF16)
            Wdn = A.alloc((32, 1024), BF16)
            ga = A.alloc((1024,), F32)
            gb_ = A.alloc((1024,), F32)
            xtm = [A.alloc((1024,), F32) for _ in range(4)]
            hbm = A.alloc((1024,), BF16)
            jb = A.alloc((1024,), BF16)
            hTm = A.alloc((8, 256), BF16)
            uT = A.alloc((32, 256), BF16)
            rb2 = [A.alloc((2, 256), F32) for _ in range(2)]
            tmpm = A.alloc((1024,), F32)
            jf = A.alloc((512,), F32)
            assert A.off <= NW
            load_w(Wup, lambda i: ("Wup", i), wup[layer].rearrange("(c p) n -> p c n", p=128), 4096, 512)
            for p4 in range(4):
                S.dma("pool", lambda e, p4=p4: e.dma_start(out=Wdn[:, 8 * p4:8 * p4 + 8, :],
                                                          in_=wdn[layer].rearrange("(f p) n -> p f n", p=128)[:, 8 * p4:8 * p4 + 8, :]),
                      writes=[("Wdn", p4)])
            S.dma("sp", lambda e: e.dma_start(out=ga, in_=gb[gi0]), writes=["ga"])
            S.dma("sp", lambda e: e.dma_start(out=gb_, in_=gb[gi0 + 1]), writes=["gb"])
            res = []
            groups = [list(range(i, min(i + 2, ntiles))) for i in range(0, ntiles, 2)]
            def front(gidx):
                grp = groups[gidx]
                xb = 2 * (gidx % 2)
                for i, t in enumerate(grp):
                    xk = ("xtm", xb + i)
                    S.dma("sp", lambda e, t=t, i=i: e.dma_start(out=xtm[xb + i], in_=src_d[t * 128:(t + 1) * 128, :]), reads=["srcd"], writes=[xk])
                    rmsnorm_rows(xtm[xb + i], xk, ga, "ga", hbm, "hbm", jb)
                    transpose8(hbm, "hbm", hTm[:, :, i * 128:(i + 1) * 128], ("hTm", i), eng="act" if i == 0 else "dve")

            def up(gidx):
                grp = groups[gidx]
                T = 128 * len(grp)
                HK = [("hTm", i) for i in range(len(grp))]
                for f2 in range(16):
                    bk = 4 + f2 % 2
                    for fl in range(2):
                        f = 2 * f2 + fl
                        for dc in range(8):
                            S.op("pe", lambda e, f=f, fl=fl, dc=dc, bk=bk: e.matmul(
                                pb[bk][:, fl * 256:fl * 256 + T], lhsT=Wup[:, dc, f * 128:(f + 1) * 128], rhs=hTm[:, dc, 0:T],
                                start=(dc == 0), stop=(dc == 7)), reads=HK + [("Wup", f // 4)], writes=[pbk(bk)])
                    rb = rb2[f2 % 2]
                    S.op("act", lambda e, bk=bk, rb=rb: e.activation(
                        out=rb[:, :, 0:T], in_=pb[bk][:].rearrange("p (a b) -> p a b", a=2)[:, :, 0:T], func=AF.Relu),
                        reads=[pbk(bk)], writes=[("rb2", f2 % 2)])
                    S.op("pool", lambda e, rb=rb, f2=f2: e.tensor_tensor(out=uT[:, 2 * f2:2 * f2 + 2, 0:T], in0=rb[:, :, 0:T],
                                                                    in1=rb[:, :, 0:T], op=ALU.mult),
                         reads=[("rb2", f2 % 2)], writes=[("uT", f2)])

            def down(gidx):
                grp = groups[gidx]
                xb = 2 * (gidx % 2)
                UK = [("uT", f2) for f2 in range(16)]
                for i, t in enumerate(grp):
                    b0 = 0 if i == 0 else 2
                    for hf in range(2):
                        for f in range(32):
                            S.op("pe", lambda e, i=i, hf=hf, f=f, b0=b0: e.matmul(pb[b0 + hf][:], lhsT=uT[:, f, i * 128:(i + 1) * 128],
                                                                                 rhs=Wdn[:, f, hf * 512:(hf + 1) * 512], start=(f == 0), stop=(f == 31)),
                                 reads=UK + [("Wdn", f // 8)], writes=[pbk(b0 + hf)])
                    norm_residual(b0, b0 + 1, gb_, "gb", xtm[xb + i], ("xtm", xb + i), tmpm, jf)
                    res.append(S.dma("sp", lambda e, t=t, i=i: e.dma_start(out=dst_d[t * 128:(t + 1) * 128, :], in_=xtm[xb + i]),
                                     reads=[("xtm", xb + i)], writes=["dstd"]))

            front(0)
            for gidx in range(len(groups)):
                up(gidx)
                if gidx + 1 < len(groups):
                    front(gidx + 1)
                down(gidx)
            S.barrier()
            return res

        mlp_phase(0, NQT, x1d, x2d, 2)

        A.off = P0
        wci = A.alloc((8, 2560), BF16)
        wco = A.alloc((8, 1024), BF16)
        dg = A.alloc((31, 4, 128), BF16)
        dwT = A.alloc((4, 31), F32)
        cvec = A.alloc((3, 4), F32)
        scw = A.alloc((4, 3), F32)
        g4 = A.alloc((1024,), F32)
        g5 = A.alloc((1024,), F32)
        xtc = [A.alloc((1024,), F32) for _ in range(4)]
        hbc = A.alloc((1024,), BF16)
        jbc = A.alloc((1024,), BF16)
        hTc = A.alloc((8, 256), BF16)
        uTb = A.alloc((4, 32 + 256), BF16)
        eTb = A.alloc((4, 2 + 256), F32)
        sgb = A.alloc((256,), F32)
        utmp = A.alloc((256,), F32)
        dhs = A.alloc((256,), F32)
        z1 = A.alloc((256,), F32)
        vv = A.alloc((4, 256), F32)
        sqv = A.alloc((4, 256), F32)
        mean = A.alloc((256,), F32)
        msq = A.alloc((256,), F32)
        rsd = A.alloc((256,), F32)
        tcb = A.alloc((256,), F32)
        ymT = A.alloc((8, 256), BF16)
        tmpc = A.alloc((1024,), F32)
        jfc = A.alloc((512,), F32)
        assert A.off <= NW
        load_w(wci, lambda i: ("wci", i), cwin.rearrange("(c p) n -> p c n", p=128), 2560, 512)
        WCI = [("wci", i) for i in range(5)]
        load_w(wco, lambda i: ("wco", i), cwout.rearrange("(c p) n -> p c n", p=128), 1024, 512)
        WCO = [("wco", 0), ("wco", 1)]
        S.dma("sp", lambda e: e.dma_start(out=dwT, in_=dwT_d), writes=["dwT"])
        S.dma("sp", lambda e: e.dma_start(out=cvec, in_=cvec_d), writes=["cvec"])
        S.dma("sp", lambda e: e.dma_start(out=scw, in_=scw_d), writes=["scw"])
        S.dma("sp", lambda e: e.dma_start(out=g4, in_=gb[4]), writes=["g4"])
        S.dma("sp", lambda e: e.dma_start(out=g5, in_=gb[5]), writes=["g5"])
        cnt = 0
        for j in range(31):
            for c in range(4):
                eng = "dve" if cnt % 2 == 0 else "pool"
                S.op(eng, lambda e, j=j, c=c: e.tensor_scalar(out=dg[:, j, c, :], in0=identf, scalar1=dwT[:, c, j:j + 1], scalar2=None, op0=ALU.mult),
                     reads=["identf", "dwT"], writes=[("dg", eng)])
                cnt += 1
        DG = [("dg", "dve"), ("dg", "pool")]
        S.op("pool", lambda e: e.memset(uTb[:, :, :], 0.0), writes=["uTb"])
        S.op("pool", lambda e: e.memset(eTb[:, :, :], 0.0), writes=["eTb"])
        cgroups = [[0]] + [[1 + 2 * i, 2 + 2 * i] for i in range(8)]
        for gidx, grp in enumerate(cgroups):
            T = 128 * len(grp)
            halo = (grp == [0])
            xb = 2 * (gidx % 2)
            for i, li in enumerate(grp):
                xk = ("xtc", xb + i)
                S.dma("sp", lambda e, li=li, i=i, xb=xb: e.dma_start(out=xtc[xb + i], in_=x2d[li * 128:(li + 1) * 128, :]), reads=["dstd"], writes=[xk])
                rmsnorm_rows(xtc[xb + i], xk, g4, "g4", hbc, "hbc", jbc)
                transpose8(hbc, "hbc", hTc[:, :, i * 128:(i + 1) * 128], ("hTc", i), eng="act" if i == 0 else "dve")
            HK = [("hTc", i) for i in range(len(grp))]

            def proj(bank, col0, T=T, HK=HK):
                for dc in range(8):
                    S.op("pe", lambda e, dc=dc: e.matmul(pb[bank][:, 0:T], lhsT=wci[:, dc, col0:col0 + 128], rhs=hTc[:, dc, 0:T],
                                                         start=(dc == 0), stop=(dc == 7)), reads=HK + WCI, writes=[pbk(bank)])
            for c in range(4):
                proj(0, c * 128)
                proj(1, 512 + c * 128)
                S.op("act", lambda e, T=T: e.activation(out=sgb[:, 0:T], in_=pb[1][:, 0:T], func=AF.Sigmoid), reads=[pbk(1)], writes=["sgb"])
                if halo:
                    S.op("dve", lambda e, T=T: e.tensor_tensor(out=utmp[:, 0:T], in0=pb[0][:, 0:T], in1=sgb[:, 0:T], op=ALU.mult),
                         reads=[pbk(0), "sgb"], writes=["utmp"])
                    S.op("dve", lambda e, c=c, T=T: e.tensor_scalar(out=uTb[:, c, 32:32 + T], in0=utmp[:, 0:T], scalar1=flagc, scalar2=None, op0=ALU.mult),
                         reads=["utmp", "flags", "uTb"], writes=[("uT", c)])
                else:
                    S.op("dve", lambda e, c=c, T=T: e.tensor_tensor(out=uTb[:, c, 32:32 + T], in0=pb[0][:, 0:T], in1=sgb[:, 0:T], op=ALU.mult),
                         reads=[pbk(0), "sgb", "uTb"], writes=[("uT", c)])
            for c in range(4):
                proj(2, 1536 + c * 128)
                proj(3, 2048 + c * 128)
                S.op("act", lambda e, T=T: e.copy(out=dhs[:, 0:T], in_=pb[3][:, 0:T]), reads=[pbk(3)], writes=["dhs"])
                if halo:
                    S.op("dve", lambda e, T=T: e.tensor_tensor(out=utmp[:, 0:T], in0=pb[2][:, 0:T], in1=dhs[:, 0:T], op=ALU.mult),
                         reads=[pbk(2), "dhs"], writes=["utmp"])
                    S.op("dve", lambda e, c=c, T=T: e.tensor_scalar(out=eTb[:, c, 2:2 + T], in0=utmp[:, 0:T], scalar1=flagc, scalar2=None, op0=ALU.mult),
                         reads=["utmp", "flags", "eTb"], writes=[("eT", c)])
                else:
                    S.op("dve", lambda e, c=c, T=T: e.tensor_tensor(out=eTb[:, c, 2:2 + T], in0=pb[2][:, 0:T], in1=dhs[:, 0:T], op=ALU.mult),
                         reads=[pbk(2), "dhs", "eTb"], writes=[("eT", c)])
            if not halo:
                for c in range(4):
                    proj(4, 1024 + c * 128)
                    S.op("dve", lambda e, c=c, T=T: e.tensor_scalar(out=z1[:, 0:T], in0=eTb[:, c, 0:T], scalar1=scw[:, c, 0:1], scalar2=None, op0=ALU.mult),
                         reads=[("eT", c), "scw"], writes=["z1"])
                    for jj in (1, 2):
                        S.op("dve", lambda e, c=c, T=T, jj=jj: e.scalar_tensor_tensor(out=z1[:, 0:T], in0=eTb[:, c, jj:jj + T], scalar=scw[:, c, jj:jj + 1],
                                                                                    in1=z1[:, 0:T], op0=ALU.mult, op1=ALU.add),
                             reads=[("eT", c), "scw", "z1"], writes=["z1"])
                    S.op("dve", lambda e, c=c, T=T: e.tensor_tensor(out=ymT[:, 4 + c, 0:T], in0=pb[4][:, 0:T], in1=z1[:, 0:T], op=ALU.mult),
                         reads=[pbk(4), "z1"], writes=[("ymT", 4 + c)])
                for c in range(4):
                    for j in range(31):
                        S.op("pe", lambda e, c=c, j=j, T=T: e.matmul(pb[5][:, 0:T], lhsT=dg[:, j, c, :], rhs=uTb[:, c, 2 + j:2 + j + T],
                                                                     start=(j == 0), stop=(j == 30)),
                             reads=[("uT", c)] + DG, writes=[pbk(5)])
                    S.op("act", lambda e, c=c, T=T: e.activation(out=vv[:, c, 0:T], in_=pb[5][:, 0:T], func=AF.Identity, bias=cvec[:, 0, c:c + 1], scale=1.0),
                         reads=[pbk(5), "cvec"], writes=[("vv", c)])
                    S.op("act", lambda e, c=c, T=T: e.activation(out=sqv[:, c, 0:T], in_=vv[:, c, 0:T], func=AF.Square),
                         reads=[("vv", c)], writes=[("sqv", c)])
                for c in range(4):
                    S.op("pe", lambda e, c=c, T=T: e.matmul(pb[6][:, 0:T], lhsT=onesf, rhs=vv[:, c, 0:T], start=(c == 0), stop=(c == 3)),
                         reads=[("vv", c), "onesf"], writes=[pbk(6)])
                for c in range(4):
                    S.op("pe", lambda e, c=c, T=T: e.matmul(pb[6][:, 256:256 + T], lhsT=onesf, rhs=sqv[:, c, 0:T], start=(c == 0), stop=(c == 3)),
                         reads=[("sqv", c), "onesf"], writes=[pbk(6)])
                S.op("act", lambda e, T=T: e.mul(out=mean[:, 0:T], in_=pb[6][:, 0:T], mul=1.0 / 512), reads=[pbk(6)], writes=["mean"])
                S.op("pool", lambda e, T=T: e.tensor_tensor(out=msq[:, 0:T], in0=mean[:, 0:T], in1=mean[:, 0:T], op=ALU.mult), reads=["mean"], writes=["msq"])
                S.op("dve", lambda e, T=T: e.scalar_tensor_tensor(out=rsd[:, 0:T], in0=pb[6][:, 256:256 + T], scalar=1.0 / 512, in1=msq[:, 0:T],
                                                                op0=ALU.mult, op1=ALU.subtract), reads=[pbk(6), "msq"], writes=["rsd"])
                S.op("act", lambda e, T=T: e.activation(out=rsd[:, 0:T], in_=rsd[:, 0:T], func=AF.Sqrt, scale=1.0, bias=epsc), reads=["rsd", "eps"], writes=["rsd"])
                S.op("dve", lambda e, T=T: e.reciprocal(out=rsd[:, 0:T], in_=rsd[:, 0:T]), reads=["rsd"], writes=["rsd"])
                for c in range(4):
                    S.op("pool", lambda e, c=c, T=T: e.tensor_tensor(out=tcb[:, 0:T], in0=vv[:, c, 0:T], in1=mean[:, 0:T], op=ALU.subtract),
                         reads=[("vv", c), "mean"], writes=["tcb"])
                    S.op("pool", lambda e, T=T: e.tensor_tensor(out=tcb[:, 0:T], in0=tcb[:, 0:T], in1=rsd[:, 0:T], op=ALU.mult),
                         reads=["tcb", "rsd"], writes=["tcb"])
                    S.op("act", lambda e, c=c, T=T: e.activation(out=ymT[:, c, 0:T], in_=tcb[:, 0:T], func=AF.Silu, scale=cvec[:, 1, c:c + 1],
                                                                bias=cvec[:, 2, c:c + 1]), reads=["tcb", "cvec"], writes=[("ymT", c)])
                YM = [("ymT", c) for c in range(8)]
                for i, li in enumerate(grp):
                    for hf in range(2):
                        for c in range(8):
                            S.op("pe", lambda e, i=i, hf=hf, c=c: e.matmul(pb[hf][:], lhsT=ymT[:, c, i * 128:(i + 1) * 128],
                                                                          rhs=wco[:, c, hf * 512:(hf + 1) * 512], start=(c == 0), stop=(c == 7)),
                                 reads=YM + WCO, writes=[pbk(hf)])
                    norm_residual(0, 1, g5, "g5", xtc[xb + i], ("xtc", xb + i), tmpc, jfc)
                    S.dma("sp", lambda e, li=li, i=i, xb=xb: e.dma_start(out=x3d[(li - 1) * 128:li * 128, :], in_=xtc[xb + i]),
                          reads=[("xtc", xb + i)], writes=["x3d"])
            UT = [("uT", c) for c in range(4)]
            ET = [("eT", c) for c in range(4)]
            S.op("pool", lambda e, T=T: e.tensor_copy(out=uTb[:, :, 0:32], in_=uTb[:, :, T:T + 32]), reads=UT, writes=UT + ["uTb"])
            S.op("pool", lambda e, T=T: e.tensor_copy(out=eTb[:, :, 0:2], in_=eTb[:, :, T:T + 2]), reads=ET, writes=ET + ["eTb"])
        S.barrier()

        A.off = P0

        def final_src_fix():
            pass
        res = mlp_phase_final = None
        res = mlp_phase(1, 16, x3d, out_d, 6)
        S.emit(nc, final_wait_tokens=res)
    return nc


def _t5_bucket_np(rel):
    import jax
    import jax.numpy as jnp
    cpu = jax.devices("cpu")[0]
    with jax.default_device(cpu):
        rel = jnp.asarray(rel, dtype=jnp.int32)
        nb = 16
        ret = jnp.where(rel > 0, nb, 0)
        n = jnp.abs(rel)
        max_exact = nb // 2
        nf = jnp.maximum(n, 1).astype(jnp.float32)
        large = max_exact + (jnp.log(nf / max_exact) / math.log(128 / max_exact) * (nb - max_exact)).astype(jnp.int32)
        large = jnp.minimum(large, nb - 1)
        out = ret + jnp.where(n < max_exact, n, large)
        return np.asarray(out)


_PROG = {}


def _get_prog(debug=False):
    if debug not in _PROG:
        _PROG[debug] = build_program(debug)
    return _PROG[debug]


def make_in_maps(inputs):
    f = lambda a: np.ascontiguousarray(np.asarray(a, dtype=np.float32))
    x = f(inputs["x"])
    rel_bias = f(inputs["rel_bias"])
    norm_g = f(inputs["norm_g"])
    kk = np.arange(128)[:, None]
    qq = np.arange(128)[None, :]
    bD = _t5_bucket_np(kk - qq)
    bP = _t5_bucket_np(kk - 128 - qq)
    admiss = (kk // 64) <= (qq // 64)
    tabA = rel_bias[:, :4]
    tabB = rel_bias[:, 4:]
    biasA = np.zeros((128, 2, 8, 128), np.float32)
    biasB = np.zeros((128, 2, 8, 128), np.float32)
    for m in range(8):
        biasA[:, 0, m, :] = np.where(admiss, tabA[bD, m // 2], np.float32(NEG))
        biasA[:, 1, m, :] = tabA[bP, m // 2]
        biasB[:, 0, m, :] = np.where(admiss, tabB[bD, m], np.float32(NEG))
        biasB[:, 1, m, :] = tabB[bP, m]
    cA = np.ascontiguousarray(np.broadcast_to(np.repeat(tabA[15], 2)[None, :], (128, 8)))
    cB = np.ascontiguousarray(np.broadcast_to(tabB[15][None, :], (128, 8)))
    gbb = np.ascontiguousarray(np.broadcast_to(norm_g.reshape(8, 1, D), (8, 128, D)))
    lvb = np.ascontiguousarray(np.broadcast_to(f(inputs["diff_lambda"])[0][None], (128, 4, 64)))
    sgn = np.ascontiguousarray(f(inputs["diff_subln_g"])[0].reshape(128, 1))
    selA = np.zeros((128, 2), np.float32)
    selA[:64, 0] = 0.125
    selA[64:, 1] = 0.125
    selI = np.zeros((128, 4), np.float32)
    for i in range(4):
        selI[32 * i:32 * i + 32, i] = 1.0
    dwT = np.ascontiguousarray(f(inputs["conv_dw_w"])[0].reshape(31, 4, 128).transpose(2, 1, 0))
    cvec = np.ascontiguousarray(np.stack([f(inputs["conv_dw_b"])[0].reshape(4, 128).T,
                                          f(inputs["conv_ln_g"])[0].reshape(4, 128).T,
                                          f(inputs["conv_ln_b"])[0].reshape(4, 128).T], axis=1))
    scw = np.ascontiguousarray(f(inputs["sconv_w"])[0].reshape(3, 4, 128).transpose(2, 1, 0))
    common = dict(win=f(inputs["attn_w_in"])[0], wout=f(inputs["attn_w_out"])[0], wup=f(inputs["w_mlp_up"]),
                  wdn=f(inputs["w_mlp_down"]), cwin=f(inputs["conv_w_in"])[0], cwout=f(inputs["conv_w_out"])[0],
                  gb=gbb, biasA=biasA, biasB=biasB, cA=cA, cB=cB, lvb=lvb, sgn=sgn, selA=selA, selI=selI,
                  ident=np.eye(128, dtype=np.float32), dwT=dwT, cvec=cvec, scw=scw)
    in_maps = []
    for c in range(8):
        b, h = c // 2, c % 2
        if h == 1:
            xl = x[b]
        else:
            xl = np.concatenate([np.zeros((2048, D), np.float32), x[b, :2048]], axis=0)
        flags = np.zeros((128, 2), np.float32)
        flags[:, 0] = float(h)
        flags[:, 1] = 0.0 if h == 1 else NEG
        m = dict(common)
        m["xloc"] = np.ascontiguousarray(xl)
        m["flags"] = flags
        in_maps.append(m)
    return in_maps


def kernel(**inputs):
    nc = _get_prog(False)
    in_maps = make_in_maps(inputs)
    res = run_bass_kernel_spmd(nc, in_maps, core_ids=list(range(8)))
    out = np.zeros((4, 4096, D), np.float32)
    for c in range(8):
        b, h = c // 2, c % 2
        out[b, h * 2048:(h + 1) * 2048] = res.results[c]["out"]
    return out
```

```python
import math
import contextlib
import numpy as np
import concourse.bass as bass
import concourse.mybir as mybir
from concourse.bass_utils import run_bass_kernel_spmd

F32 = mybir.dt.float32
BF16 = mybir.dt.bfloat16
ALU = mybir.AluOpType
AF = mybir.ActivationFunctionType
AX = mybir.AxisListType

ENGS = ["pe", "act", "dve", "pool", "sp"]
NDMA_SLOTS = 8
NEG = -1.0e30
EPS = 1e-6
NITER = 16
TOPK = 256
D = 1024
QT0 = 15
NQT = 17


class Sched:
    def __init__(self):
        self.ops = {e: [] for e in ENGS}
        self.last_w = {}
        self.readers = {}
        self.dma_slot_rr = {e: 0 for e in ENGS}
        self.dma_slot_last = {}
        self.dma_slot_uses = {}
        self.pending = {e: set() for e in ENGS}
        self.last_tok = {}
        self.open_dma = set()

    def _deps_for(self, reads, writes):
        deps = set()
        for k in reads:
            deps.update(self.last_w.get(k, {}).values())
        for k in writes:
            deps.update(self.last_w.get(k, {}).values())
            deps.update(self.readers.get(k, {}).values())
        return deps

    def _commit(self, tok, track, reads, writes):
        for k in reads:
            self.readers.setdefault(k, {})[track] = tok
        for k in writes:
            self.last_w[k] = {track: tok}
            self.readers[k] = {}

    def op(self, eng, fn, reads=(), writes=()):
        idx = len(self.ops[eng])
        deps = self._deps_for(reads, writes)
        tok = ("c", eng, idx)
        raw = set()
        if eng != "pe":
            for k in reads:
                raw.update(self.last_w.get(k, {}).values())
        deps = {d for d in deps if not (d[0] == "c" and d[1] == eng) or d in raw}
        deps |= self.pending[eng]
        self.pending[eng] = set()
        self.ops[eng].append(dict(fn=fn, deps=deps, tok=tok, dma=False))
        self._commit(tok, eng, reads, writes)
        self.last_tok[eng] = tok
        return tok

    def dma(self, eng, fn, reads=(), writes=()):
        slot = self.dma_slot_rr[eng]
        self.dma_slot_rr[eng] = (slot + 1) % NDMA_SLOTS
        use = self.dma_slot_uses.get((eng, slot), 0) + 1
        self.dma_slot_uses[(eng, slot)] = use
        tok = ("d", eng, slot, use)
        deps = self._deps_for(reads, writes)
        prev = self.dma_slot_last.get((eng, slot))
        if prev is not None:
            deps.add(prev)
            self.open_dma.discard(prev)
        self.dma_slot_last[(eng, slot)] = tok
        deps |= self.pending[eng]
        self.pending[eng] = set()
        self.ops[eng].append(dict(fn=fn, deps=deps, tok=tok, dma=True))
        self._commit(tok, ("dma", eng, slot), reads, writes)
        self.open_dma.add(tok)
        return tok

    def barrier(self):
        toks = set(self.last_tok.values()) | set(self.open_dma)
        for e in ENGS:
            self.pending[e] |= {t for t in toks if not (t[0] == "c" and t[1] == e)}
        self.open_dma = set()

    def emit(self, nc, final_wait_tokens=()):
        needed = {e: set() for e in ENGS}
        for e in ENGS:
            for o in self.ops[e]:
                for d in o["deps"]:
                    if d[0] == "c":
                        needed[d[1]].add(d[2])
        semval = {e: {} for e in ENGS}
        for e in ENGS:
            c = 0
            for i in range(len(self.ops[e])):
                if i in needed[e]:
                    c += 1
                    semval[e][i] = c
        with contextlib.ExitStack() as st:
            csem = {e: st.enter_context(nc.semaphore("c_" + e)) for e in ENGS if e != "sp"}
            dsem = {}
            for e in ENGS:
                for s in range(NDMA_SLOTS):
                    if (e, s) in self.dma_slot_uses:
                        dsem[(e, s)] = st.enter_context(nc.semaphore("d_%s_%d" % (e, s)))
            block = st.enter_context(nc.Block())

            def resolve(tok):
                if tok[0] == "c":
                    return csem[tok[1]], semval[tok[1]][tok[2]], ("c", tok[1])
                return dsem[(tok[1], tok[2])], 16 * tok[3], ("d", tok[1], tok[2])

            def run(e, engine):
                waited = {}
                for i, o in enumerate(self.ops[e]):
                    ws = {}
                    for d in o["deps"]:
                        sem, val, sk = resolve(d)
                        if waited.get(sk, 0) >= val:
                            continue
                        if sk not in ws or ws[sk][1] < val:
                            ws[sk] = (sem, val)
                    for sk, (sem, val) in ws.items():
                        engine.wait_ge(sem, val)
                        waited[sk] = val
                    ins = o["fn"](engine)
                    if o["dma"]:
                        ins.then_inc(dsem[(o["tok"][1], o["tok"][2])], 16)
                    elif i in needed[e]:
                        ins.then_inc(csem[e], 1)
                if e == "sp":
                    for d in final_wait_tokens:
                        sem, val, sk = resolve(d)
                        engine.wait_ge(sem, val)

            block.tensor(lambda eng: run("pe", eng))
            block.scalar(lambda eng: run("act", eng))
            block.vector(lambda eng: run("dve", eng))
            block.gpsimd(lambda eng: run("pool", eng))
            block.sync(lambda eng: run("sp", eng))


class Arena:
    def __init__(self, ap, nwords):
        self.ap = ap
        self.n = nwords
        self.off = 0

    def alloc(self, free_shape, dt, at=None):
        nel = int(np.prod(free_shape))
        nbytes = nel * (2 if dt == BF16 else 4)
        nw = (nbytes + 3) // 4
        off = self.off if at is None else at
        assert off + nw <= self.n, ("arena overflow", off, nw, self.n)
        a = self.ap[:, off:off + nw]
        if at is None:
            self.off += nw
        if dt == BF16:
            a = a.bitcast(BF16)[:, :nel]
        if len(free_shape) == 2:
            a = a.rearrange("p (a b) -> p a b", a=free_shape[0])
        elif len(free_shape) == 3:
            a = a.rearrange("p (a b c) -> p a b c", a=free_shape[0], b=free_shape[1])
        return a


class Ctx:
    pass


def build_program(debug=False):
    nc = bass.Bass("TRN2", target_bir_lowering=False)
    S = Sched()
    C = Ctx()
    C.nc, C.S = nc, S
    din = lambda n, s: nc.dram_tensor(n, list(s), F32, kind="ExternalInput").ap()
    xloc = din("xloc", [4096, D])
    win = din("win", [D, 2472])
    wout = din("wout", [D, D])
    wup = din("wup", [2, D, 4096])
    wdn = din("wdn", [2, 4096, D])
    cwin = din("cwin", [D, 2560])
    cwout = din("cwout", [D, D])
    gb = din("gb", [8, 128, D])
    biasA_d = din("biasA", [128, 2, 8, 128])
    biasB_d = din("biasB", [128, 2, 8, 128])
    cA_d = din("cA", [128, 8])
    cB_d = din("cB", [128, 8])
    lvb_d = din("lvb", [128, 4, 64])
    sgn_d = din("sgn", [128, 1])
    flags_d = din("flags", [128, 2])
    selA_d = din("selA", [128, 2])
    selI_d = din("selI", [128, 4])
    ident_d = din("ident", [128, 128])
    dwT_d = din("dwT", [128, 4, 31])
    cvec_d = din("cvec", [128, 3, 4])
    scw_d = din("scw", [128, 4, 3])
    out_d = nc.dram_tensor("out", [2048, D], F32, kind="ExternalOutput").ap()
    kind = "ExternalOutput"
    x1d = nc.dram_tensor("x1d", [NQT * 128, D], F32, kind=kind).ap()
    x2d = nc.dram_tensor("x2d", [NQT * 128, D], F32, kind=kind).ap()
    x3d = nc.dram_tensor("x3d", [2048, D], F32, kind=kind).ap()
    ydbg = nc.dram_tensor("ydbg", [NQT, 128, 8, 128], F32, kind="ExternalOutput").ap() if debug else None

    NW = 53200
    with contextlib.ExitStack() as st:
        arena_t = st.enter_context(nc.sbuf_tensor("arena", [128, NW], F32))
        A = Arena(arena_t[:], NW)
        pb = [st.enter_context(nc.psum_tensor("pb%d" % i, [128, 512], F32)) for i in range(7)]
        pbt = st.enter_context(nc.psum_tensor("pbt", [128, 1024], BF16))
        pbk = lambda i: ("pb", i)

        identf = A.alloc((128,), F32)
        identb = A.alloc((128,), BF16)
        onesf = A.alloc((128,), F32)
        onesb = A.alloc((128,), BF16)
        smallc = A.alloc((16,), F32)
        selA = A.alloc((2,), F32)
        selI = A.alloc((4,), F32)
        stat = A.alloc((64,), F32)
        stepc = A.alloc((NITER,), F32)
        P0 = A.off
        epsc, zeroc, flagc, pnegc = smallc[:, 0:1], smallc[:, 1:2], smallc[:, 2:3], smallc[:, 3:4]
        neglam, sgc = smallc[:, 4:5], smallc[:, 5:6]
        C.statctr = 0

        def newstat():
            i = C.statctr % 64
            C.statctr += 1
            return stat[:, i:i + 1], ("stat", i)

        S.dma("sp", lambda e: e.dma_start(out=identf, in_=ident_d), writes=["identf"])
        S.dma("sp", lambda e: e.dma_start(out=smallc[:, 2:4], in_=flags_d), writes=["flags"])
        S.dma("sp", lambda e: e.dma_start(out=selA, in_=selA_d), writes=["selA"])
        S.dma("sp", lambda e: e.dma_start(out=selI, in_=selI_d), writes=["selI"])
        S.op("dve", lambda e: e.tensor_copy(out=identb, in_=identf), reads=["identf"], writes=["identb"])
        S.op("pool", lambda e: e.memset(onesf, 1.0), writes=["onesf"])
        S.op("pool", lambda e: e.memset(onesb, 1.0), writes=["onesb"])
        S.op("pool", lambda e: e.memset(smallc[:, 0:1], EPS), writes=["eps"])
        S.op("pool", lambda e: e.memset(smallc[:, 1:2], 0.0), writes=["zero"])
        for it in range(NITER):
            S.op("pool", lambda e, it=it: e.memset(stepc[:, it:it + 1], 2.0 ** -(it + 1)), writes=["stepc"])

        def rmsnorm_rows(x_ap, x_key, g_ap, g_key, hb_ap, hb_key, junkb):
            ss, ssk = newstat()
            rs, rsk = newstat()
            S.op("act", lambda e: e.activation(out=junkb, in_=x_ap, func=AF.Square, accum_out=ss),
                 reads=[x_key], writes=["junkb", ssk])
            S.op("act", lambda e: e.activation(out=rs, in_=ss, func=AF.Sqrt, scale=1.0 / D, bias=epsc),
                 reads=[ssk, "eps"], writes=[rsk])
            S.op("dve", lambda e: e.reciprocal(out=rs, in_=rs), reads=[rsk], writes=[rsk])
            S.op("dve", lambda e: e.scalar_tensor_tensor(out=hb_ap, in0=x_ap, scalar=rs, in1=g_ap,
                                                         op0=ALU.mult, op1=ALU.mult),
                 reads=[x_key, rsk, g_key], writes=[hb_key])

        def transpose8(hb_ap, hb_key, dst_ap, dst_key, eng="act"):
            pv = pbt[:, 0:1024].rearrange("p (c t) -> p c t", c=8)
            for c in range(8):
                S.op("pe", lambda e, c=c: e.transpose(out=pv[:, c, :], in_=hb_ap[:, c * 128:(c + 1) * 128],
                                                      identity=identb),
                     reads=[hb_key, "identb"], writes=["pbt"])
            if eng == "act":
                S.op("act", lambda e: e.copy(out=dst_ap, in_=pv), reads=["pbt"], writes=[dst_key])
            else:
                S.op("dve", lambda e: e.tensor_copy(out=dst_ap, in_=pv), reads=["pbt"], writes=[dst_key])

        def norm_residual(bank0, bank1, g_ap, g_key, x_ap, x_key, tmp_ap, junkf):
            s0, k0 = newstat()
            s1, k1 = newstat()
            rs, rk = newstat()
            S.op("act", lambda e: e.activation(out=junkf, in_=pb[bank0][:], func=AF.Square, accum_out=s0),
                 reads=[pbk(bank0)], writes=["junkf", k0])
            S.op("act", lambda e: e.activation(out=junkf, in_=pb[bank1][:], func=AF.Square, accum_out=s1),
                 reads=[pbk(bank1)], writes=["junkf", k1])
            S.op("dve", lambda e: e.tensor_tensor(out=rs, in0=s0, in1=s1, op=ALU.add), reads=[k0, k1], writes=[rk])
            S.op("act", lambda e: e.activation(out=rs, in_=rs, func=AF.Sqrt, scale=1.0 / D, bias=epsc),
                 reads=[rk, "eps"], writes=[rk])
            S.op("dve", lambda e: e.reciprocal(out=rs, in_=rs), reads=[rk], writes=[rk])
            for hf, bk in enumerate((bank0, bank1)):
                S.op("dve", lambda e, hf=hf, bk=bk: e.scalar_tensor_tensor(
                    out=tmp_ap[:, hf * 512:(hf + 1) * 512], in0=pb[bk][:], scalar=rs,
                    in1=g_ap[:, hf * 512:(hf + 1) * 512], op0=ALU.mult, op1=ALU.mult),
                    reads=[pbk(bk), rk, g_key], writes=[("tmpn", hf)])
            S.op("pool", lambda e: e.tensor_tensor(out=x_ap, in0=x_ap, in1=tmp_ap, op=ALU.add),
                 reads=[x_key, ("tmpn", 0), ("tmpn", 1)], writes=[x_key])

        def load_w(dst, dst_keyf, src3, ncols, piece):
            for i, c0 in enumerate(range(0, ncols, piece)):
                c1 = min(ncols, c0 + piece)
                S.dma("pool", lambda e, c0=c0, c1=c1: e.dma_start(out=dst[:, :, c0:c1], in_=src3[:, :, c0:c1]),
                      writes=[dst_keyf(i)])

        A.off = P0
        KA = A.alloc((4, 4096), BF16)
        KB2 = A.alloc((4096,), BF16)
        KI4 = A.alloc((4096,), BF16)
        VA = A.alloc((32, 512), BF16)
        VB2 = A.alloc((32, 128), BF16)
        wq = A.alloc((8, 1288), BF16)
        wo = A.alloc((8, 1024), BF16)
        g0 = A.alloc((1024,), F32)
        g1 = A.alloc((1024,), F32)
        biasA = A.alloc((2, 8, 128), BF16)
        biasB = A.alloc((2, 8, 128), BF16)
        xt = A.alloc((1024,), F32)
        hb = A.alloc((1024,), BF16)
        junkb = A.alloc((1024,), BF16)
        OV = A.off
        wk = A.alloc((8, 1408), BF16)
        hT4 = A.alloc((8, 512), BF16)
        xt2 = A.alloc((1024,), F32)
        biasf = A.alloc((2, 8, 128), F32)
        cAB = A.alloc((16,), F32)
        lvb = A.alloc((4, 64), F32)
        lvj = A.alloc((64,), F32)
        kv_end = A.off
        A.off = OV
        sc = A.alloc((4096,), F32)
        junk = A.alloc((2048,), BF16)
        maskT = A.alloc((32, 128), BF16)
        hT = A.alloc((8, 128), BF16)
        qmA = A.alloc((4, 2, 128), BF16)
        qmB = A.alloc((4, 2, 128), BF16)
        qmI = A.alloc((2, 4, 128), BF16)
        wis = A.alloc((8,), F32)
        rbuf = [A.alloc((512,), F32) for _ in range(2)]
        ptb = [A.alloc((512,), BF16) for _ in range(2)]
        pmb = [A.alloc((512,), BF16) for _ in range(2)]
        rz = A.alloc((512,), F32)
        tt = A.alloc((512,), F32)
        dd = A.alloc((256,), F32)
        sq = tt[:, 0:256]
        rstd = tt[:, 256:512]
        yT = A.alloc((8, 128), BF16)
        tmpn = sc[:, 0:1024]
        junkf = tt
        junk2 = junk
        xtq = [xt, A.alloc((1024,), F32)]
        qmBs = [qmB, A.alloc((4, 2, 128), BF16)]
        bis = A.alloc((8,), F32)
        Rs = A.alloc((NITER,), F32)
        q_end = A.off
        assert max(kv_end, q_end) <= NW

        win3 = win.rearrange("(c p) n -> p c n", p=128)
        wkparts = [(0, 512, 512), (512, 2048, 64), (576, 2048, 64), (640, 2432, 32), (672, 2432, 32),
                   (704, 2432, 32), (736, 2432, 32), (768, 1024, 512), (1280, 2112, 64), (1344, 2112, 64)]
        for i, (d0, s0, n) in enumerate(wkparts):
            S.dma("pool", lambda e, d0=d0, s0=s0, n=n: e.dma_start(out=wk[:, :, d0:d0 + n], in_=win3[:, :, s0:s0 + n]),
                  writes=[("wk", i)])
        WK = [("wk", i) for i in range(len(wkparts))]
        wqparts = [(0, 0, 512), (512, 1536, 512), (1024, 2176, 256), (1280, 2464, 8)]
        for i, (d0, s0, n) in enumerate(wqparts):
            S.dma("pool", lambda e, d0=d0, s0=s0, n=n: e.dma_start(out=wq[:, :, d0:d0 + n], in_=win3[:, :, s0:s0 + n]),
                  writes=[("wq", i)])
        WQ = [("wq", i) for i in range(len(wqparts))]
        load_w(wo, lambda i: ("wo", i), wout.rearrange("(c p) n -> p c n", p=128), 1024, 512)
        WO = [("wo", 0), ("wo", 1)]
        S.dma("sp", lambda e: e.dma_start(out=g0, in_=gb[0]), writes=["g0"])
        S.dma("sp", lambda e: e.dma_start(out=g1, in_=gb[1]), writes=["g1"])
        S.dma("sp", lambda e: e.dma_start(out=cAB[:, 0:8], in_=cA_d), writes=["cAB"])
        S.dma("sp", lambda e: e.dma_start(out=cAB[:, 8:16], in_=cB_d), writes=["cAB2"])
        for which, (src, dstb) in enumerate(((biasA_d, biasA), (biasB_d, biasB))):
            S.dma("sp", lambda e, src=src: e.dma_start(out=biasf, in_=src), writes=["biasf"])
            for m in range(8):
                S.op("dve", lambda e, m=m, dstb=dstb, which=which: e.tensor_scalar(
                    out=dstb[:, :, m, :], in0=biasf[:, :, m, :], scalar1=cAB[:, which * 8 + m:which * 8 + m + 1],
                    scalar2=None, op0=ALU.subtract),
                    reads=["biasf", "cAB", "cAB2"], writes=[("bias", which)])
        lam_init = 0.8 - 0.6 * math.exp(-0.3 * 0)
        S.dma("sp", lambda e: e.dma_start(out=lvb, in_=lvb_d), writes=["lvb"])
        S.dma("sp", lambda e: e.dma_start(out=smallc[:, 5:6], in_=sgn_d), writes=["sgraw"])
        e1, e1k = newstat()
        e2, e2k = newstat()
        S.op("dve", lambda e: e.tensor_tensor(out=lvj, in0=lvb[:, 0, :], in1=lvb[:, 1, :], op=ALU.mult),
             reads=["lvb"], writes=["lvj"])
        S.op("dve", lambda e: e.tensor_reduce(out=e1, in_=lvj, axis=AX.X, op=ALU.add), reads=["lvj"], writes=[e1k])
        S.op("dve", lambda e: e.tensor_tensor(out=lvj, in0=lvb[:, 2, :], in1=lvb[:, 3, :], op=ALU.mult),
             reads=["lvb", e1k], writes=["lvj"])
        S.op("dve", lambda e: e.tensor_reduce(out=e2, in_=lvj, axis=AX.X, op=ALU.add), reads=["lvj"], writes=[e2k])
        S.op("act", lambda e: e.activation(out=e1, in_=e1, func=AF.Exp), reads=[e1k], writes=[e1k])
        S.op("act", lambda e: e.activation(out=e2, in_=e2, func=AF.Exp), reads=[e2k], writes=[e2k])
        S.op("dve", lambda e: e.scalar_tensor_tensor(out=neglam, in0=e2, scalar=-lam_init, in1=e1,
                                                     op0=ALU.add, op1=ALU.subtract),
             reads=[e1k, e2k], writes=["neglam"])
        S.op("dve", lambda e: e.tensor_scalar(out=sgc, in0=sgc, scalar1=1.0 - lam_init, scalar2=None, op0=ALU.mult),
             reads=["sgraw"], writes=["sg"])
        S.op("pool", lambda e: e.memset(VB2[:, :, :], 0.0), writes=["VB2"])

        xts = [xt, xt2]
        for g in range(8):
            for i in range(4):
                t = 4 * g + i
                xa = xts[t % 2]
                xk = ("xt", t % 2)
                S.dma("sp", lambda e, t=t, xa=xa: e.dma_start(out=xa, in_=xloc[t * 128:(t + 1) * 128, :]), writes=[xk])
                rmsnorm_rows(xa, xk, g0, "g0", hb, "hb", junkb)
                transpose8(hb, "hb", hT4[:, :, i * 128:(i + 1) * 128], ("hT4", i), eng="act" if i % 2 == 0 else "dve")
            HT4 = [("hT4", i) for i in range(4)]
            for c in range(6):
                bk = c % 2
                for dc in range(8):
                    S.op("pe", lambda e, c=c, dc=dc, bk=bk: e.matmul(pb[bk][:], lhsT=wk[:, dc, c * 128:(c + 1) * 128],
                                                                    rhs=hT4[:, dc, :], start=(dc == 0), stop=(dc == 7)),
                         reads=HT4 + WK, writes=[pbk(bk)])
                if c < 4:
                    dst = KA[:, c, g * 512:(g + 1) * 512]
                elif c == 4:
                    dst = KB2[:, g * 512:(g + 1) * 512]
                else:
                    dst = KI4[:, g * 512:(g + 1) * 512]
                if c % 2 == 0:
                    S.op("act", lambda e, dst=dst, bk=bk: e.copy(out=dst, in_=pb[bk][:]), reads=[pbk(bk)], writes=[("K", c, g)])
                else:
                    S.op("dve", lambda e, dst=dst, bk=bk: e.tensor_copy(out=dst, in_=pb[bk][:]), reads=[pbk(bk)], writes=[("K", c, g)])
            for i in range(4):
                t = 4 * g + i
                for dc in range(8):
                    S.op("pe", lambda e, i=i, dc=dc: e.matmul(pb[2][:], lhsT=hT4[:, dc, i * 128:(i + 1) * 128],
                                                              rhs=wk[:, dc, 768:1280], start=(dc == 0), stop=(dc == 7)),
                         reads=HT4 + WK, writes=[pbk(2)])
                for dc in range(8):
                    S.op("pe", lambda e, i=i, dc=dc: e.matmul(pb[3][:, 0:128], lhsT=hT4[:, dc, i * 128:(i + 1) * 128],
                                                              rhs=wk[:, dc, 1280:1408], start=(dc == 0), stop=(dc == 7)),
                         reads=HT4 + WK, writes=[pbk(3)])
                S.op("act", lambda e, t=t: e.copy(out=VA[:, t, :], in_=pb[2][:]), reads=[pbk(2)], writes=[("VA", t)])
                S.op("dve", lambda e, t=t: e.tensor_copy(out=VB2[:, t, :], in_=pb[3][:, 0:128]),
                     reads=[pbk(3), "VB2"], writes=[("VB", t)])
        S.barrier()

        ALLK = [("K", c, g) for c in range(6) for g in range(8)]
        outs = []
        def q_front(qi):
            qt = QT0 + qi
            xc = xtq[qi % 2]
            xk = ("xtq", qi % 2)
            qmBc = qmBs[qi % 2]
            S.dma("sp", lambda e, qt=qt: e.dma_start(out=xc, in_=xloc[qt * 128:(qt + 1) * 128, :]), writes=[xk])
            rmsnorm_rows(xc, xk, g0, "g0", hb, "hb", junkb)
            transpose8(hb, "hb", hT, "hT")
            for c in range(10):
                bank, col = (4, c) if c < 4 else ((5, c - 4) if c < 8 else (6, c - 8))
                for dc in range(8):
                    S.op("pe", lambda e, c=c, dc=dc, bank=bank, col=col: e.matmul(
                        pb[bank][:, col * 128:(col + 1) * 128], lhsT=wq[:, dc, c * 128:(c + 1) * 128], rhs=hT[:, dc, :],
                        start=(dc == 0), stop=(dc == 7)), reads=["hT"] + WQ, writes=[pbk(bank)])
            for dc in range(8):
                S.op("pe", lambda e, dc=dc: e.matmul(pb[6][:, 256:264], lhsT=hT[:, dc, :], rhs=wq[:, dc, 1280:1288],
                                                     start=(dc == 0), stop=(dc == 7)), reads=["hT"] + WQ, writes=[pbk(6)])
            cnt = 0
            for (bank, dstq, nsub, sel) in ((4, qmA, 2, selA), (5, qmBc, 2, selA), (6, qmI, 4, selI)):
                for c in range(dstq.shape[1]):
                    for s_ in range(nsub):
                        src = pb[bank][:, c * 128:(c + 1) * 128]
                        dst = dstq[:, c, s_, :]
                        sca = sel[:, s_:s_ + 1]
                        if cnt % 2 == 0:
                            S.op("act", lambda e, src=src, dst=dst, sca=sca: e.activation(out=dst, in_=src, func=AF.Copy, scale=sca),
                                 reads=[pbk(bank), "selA", "selI"], writes=[("qm", bank) if bank != 5 else ("qmB", qi % 2)])
                        else:
                            S.op("dve", lambda e, src=src, dst=dst, sca=sca: e.tensor_scalar(out=dst, in0=src, scalar1=sca, scalar2=None, op0=ALU.mult),
                                 reads=[pbk(bank), "selA", "selI"], writes=[("qm", bank) if bank != 5 else ("qmB", qi % 2)])
                        cnt += 1
            S.op("act", lambda e: e.mul(out=wis, in_=pb[6][:, 256:264], mul=(32.0 ** -0.5) * (8.0 ** -0.5)),
                 reads=[pbk(6)], writes=["wis"])
        q_front(0)
        for qi in range(NQT):
            qt = QT0 + qi
            nk = qt + 1
            N = nk * 128
            xk = ("xtq", qi % 2)
            xc = xtq[qi % 2]
            qmBc = qmBs[qi % 2]
            nkb = (N + 511) // 512
            for kb in range(nkb):
                cols = min(512, N - kb * 512)
                for j in range(8):
                    bk = 4 + (j % 2)
                    S.op("pe", lambda e, j=j, bk=bk, kb=kb, cols=cols: e.matmul(
                        pb[bk][:, 0:cols], lhsT=qmI[:, j // 4, j % 4, :], rhs=KI4[:, kb * 512:kb * 512 + cols],
                        start=True, stop=True), reads=[("qm", 6)] + ALLK, writes=[pbk(bk)])
                    rb = rbuf[j % 2]
                    S.op("act", lambda e, bk=bk, rb=rb, cols=cols: e.activation(out=rb[:, 0:cols], in_=pb[bk][:, 0:cols], func=AF.Relu),
                         reads=[pbk(bk)], writes=[("rbuf", j % 2)])
                    scv = sc[:, kb * 512:kb * 512 + cols]
                    if j == 0:
                        S.op("dve", lambda e, rb=rb, scv=scv, cols=cols: e.tensor_scalar(
                            out=scv, in0=rb[:, 0:cols], scalar1=wis[:, 0:1], scalar2=None, op0=ALU.mult),
                            reads=[("rbuf", 0), "wis"], writes=[("sc", kb)] + ([("tmpn", 0), ("tmpn", 1)] if kb < 2 else []))
                    else:
                        S.op("dve", lambda e, rb=rb, scv=scv, cols=cols, j=j: e.scalar_tensor_tensor(
                            out=scv, in0=rb[:, 0:cols], scalar=wis[:, j:j + 1], in1=scv, op0=ALU.mult, op1=ALU.add),
                            reads=[("rbuf", j % 2), "wis", ("sc", kb)], writes=[("sc", kb)])
            SCK = [("sc", kb) for kb in range(nkb)]
            mx, mn, Rr, lo, tth, cA_, cB_, gg = [bis[:, i:i + 1] for i in range(8)]
            S.op("dve", lambda e, N=N: e.tensor_reduce(out=mx, in_=sc[:, 0:N], axis=AX.X, op=ALU.max), reads=SCK, writes=["b_mx"])
            S.op("dve", lambda e, N=N: e.tensor_reduce(out=lo, in_=sc[:, 0:N], axis=AX.X, op=ALU.min), reads=SCK, writes=["b_lo"])
            S.op("dve", lambda e: e.tensor_tensor(out=Rr, in0=mx, in1=lo, op=ALU.subtract), reads=["b_mx", "b_lo"], writes=["b_R"])
            S.op("pool", lambda e, N=N: e.memset(sc[0:64, N - 64:N], NEG), reads=SCK + ["b_mx", "b_lo"], writes=SCK)
            npv = 15 * 128 if qt == 15 else 2048
            S.op("dve", lambda e, npv=npv: e.tensor_scalar(out=sc[:, 0:npv], in0=sc[:, 0:npv], scalar1=pnegc, scalar2=None, op0=ALU.add),
                 reads=SCK + ["flags"], writes=SCK)
            halves = [(0, N)] if N <= 2048 else [(0, 2048), (2048, N)]
            S.op("dve", lambda e: e.tensor_scalar(out=Rs, in0=stepc, scalar1=Rr, scalar2=None, op0=ALU.mult),
                 reads=["b_R", "stepc"], writes=["b_Rs"])

            def bis_iter(it):
                two = len(halves) == 2
                S.op("dve", lambda e: e.tensor_tensor(out=tth, in0=Rs[:, it:it + 1], in1=lo, op=ALU.add),
                     reads=["b_Rs", "b_lo"], writes=["b_t"])
                if two:
                    S.op("dve", lambda e: e.scalar_tensor_tensor(out=mn, in0=Rs[:, it:it + 1], scalar=-1.0, in1=lo, op0=ALU.mult, op1=ALU.subtract),
                         reads=["b_Rs", "b_lo"], writes=["b_nt"])
                a0, a1 = halves[0]
                S.op("dve", lambda e: e.tensor_scalar(
                    out=junk[:, 0:a1 - a0], in0=sc[:, a0:a1], scalar1=tth, scalar2=None, op0=ALU.is_ge, op1=ALU.add, accum_out=cA_),
                    reads=SCK + ["b_t"], writes=["junk", ("b_c", 0)])
                thr = TOPK - 0.5
                if two:
                    b0, b1 = halves[1]
                    S.op("act", lambda e: e.activation(out=junk2[:, 0:b1 - b0], in_=sc[:, b0:b1], func=AF.Sign, bias=mn, scale=1.0, accum_out=cB_),
                         reads=SCK + ["b_nt"], writes=["junk2", ("b_c", 1)])
                    S.op("dve", lambda e: e.scalar_tensor_tensor(out=cA_, in0=cB_, scalar=0.5, in1=cA_, op0=ALU.mult, op1=ALU.add),
                         reads=[("b_c", 0), ("b_c", 1)], writes=[("b_c", 0)])
                    thr = TOPK - 0.5 - 0.5 * (b1 - b0)
                S.op("dve", lambda e: e.tensor_scalar(out=gg, in0=cA_, scalar1=thr, scalar2=None, op0=ALU.is_ge),
                     reads=[("b_c", 0)], writes=["b_g"])
                S.op("dve", lambda e: e.scalar_tensor_tensor(out=lo, in0=gg, scalar=Rs[:, it:it + 1], in1=lo, op0=ALU.mult, op1=ALU.add),
                     reads=["b_g", "b_lo", "b_Rs"], writes=["b_lo"])

            MK = [("maskT", k4) for k4 in range((nk + 3) // 4)]

            def emit_group(typ, gi, hook=None):
                def qk_stage(kt):
                    sbk = kt % 2
                    band = kt >= qt - 1
                    if band:
                        bt = 0 if kt == qt else 1
                        bsrc = (biasA if typ == "A" else biasB)[:, bt, 4 * gi:4 * gi + 4, :]
                        S.op("pe", lambda e: e.matmul(pb[sbk][:].rearrange("p (a b) -> p a b", a=4), lhsT=identb, rhs=bsrc,
                                                      start=True, stop=False),
                             reads=["identb", ("bias", 0), ("bias", 1)], writes=[pbk(sbk)])
                    for mi in range(4):
                        if typ == "A":
                            h = 2 * gi + mi // 2
                            lhsT = KA[:, h, kt * 128:(kt + 1) * 128]
                            rhs = qmA[:, h, mi % 2, :]
                            qk = ("qm", 4)
                        else:
                            j = 4 * gi + mi
                            lhsT = KB2[:, kt * 128:(kt + 1) * 128]
                            rhs = qmBc[:, j // 2, j % 2, :]
                            qk = ("qmB", qi % 2)
                        S.op("pe", lambda e, mi=mi, lhsT=lhsT, rhs=rhs: e.matmul(
                            pb[sbk][:, mi * 128:(mi + 1) * 128], lhsT=lhsT, rhs=rhs, start=(not band), stop=((not band) or mi == 3)),
                            reads=[qk] + ALLK, writes=[pbk(sbk)])
                    masked = (kt <= 14) or (kt == 15 and qt >= 16)
                    bias_ap = pnegc if masked else zeroc
                    pt = ptb[kt % 2]
                    S.op("act", lambda e: e.activation(out=pt, in_=pb[sbk][:], func=AF.Exp, bias=bias_ap, scale=1.0),
                         reads=[pbk(sbk), "flags", "zero"], writes=[("pt", kt % 2)])
                    if typ == "B":
                        pm = pmb[kt % 2]
                        S.op("dve", lambda e: e.tensor_tensor(
                            out=pm.rearrange("p (a b) -> p a b", a=4), in0=pt.rearrange("p (a b) -> p a b", a=4),
                            in1=maskT[:, kt, :].unsqueeze(1).to_broadcast([128, 4, 128]), op=ALU.mult),
                            reads=[("pt", kt % 2)] + MK, writes=[("pm", kt % 2)])

                def pv_stage(kt):
                    if typ == "B":
                        src, srck = pmb[kt % 2], ("pm", kt % 2)
                    else:
                        src, srck = ptb[kt % 2], ("pt", kt % 2)
                    if typ == "A":
                        for hl in range(2):
                            h = 2 * gi + hl
                            ob = 2 if hl == 0 else 4
                            S.op("pe", lambda e, hl=hl, h=h, ob=ob: e.matmul(
                                pb[ob][:, 0:256], lhsT=VA[:, kt, h * 128:(h + 1) * 128], rhs=src[:, hl * 256:(hl + 1) * 256],
                                start=(kt == 0), stop=(kt == nk - 1)), reads=[srck, ("VA", kt)], writes=[pbk(ob)])
                    else:
                        S.op("pe", lambda e: e.matmul(pb[2][:], lhsT=VB2[:, kt, :], rhs=src, start=(kt == 0), stop=(kt == nk - 1)),
                             reads=[srck, ("VB", kt)], writes=[pbk(2)])
                    S.op("pe", lambda e: e.matmul(pb[3][:], lhsT=onesb, rhs=src, start=(kt == 0), stop=(kt == nk - 1)),
                         reads=[srck, "onesb"], writes=[pbk(3)])

                qk_stage(0)
                for kt in range(nk):
                    if kt + 1 < nk:
                        qk_stage(kt + 1)
                    pv_stage(kt)
                    if hook is not None:
                        hook(kt)
                S.op("dve", lambda e: e.reciprocal(out=rz, in_=pb[3][:]), reads=[pbk(3)], writes=["rz"])
                if typ == "A":
                    S.op("dve", lambda e: e.tensor_tensor(out=tt[:, 0:256], in0=pb[2][:, 0:256], in1=rz[:, 0:256], op=ALU.mult),
                         reads=[pbk(2), "rz"], writes=["tt"])
                    S.op("dve", lambda e: e.tensor_tensor(out=tt[:, 256:512], in0=pb[4][:, 0:256], in1=rz[:, 256:512], op=ALU.mult),
                         reads=[pbk(4), "rz", "tt"], writes=["tt"])
                    t4 = tt.rearrange("p (h m q) -> p h m q", h=2, m=2)
                    S.op("dve", lambda e: e.scalar_tensor_tensor(out=dd.rearrange("p (h q) -> p h q", h=2), in0=t4[:, :, 1, :], scalar=neglam,
                                                                 in1=t4[:, :, 0, :], op0=ALU.mult, op1=ALU.add),
                         reads=["tt", "neglam"], writes=["dd"])
                    S.op("pool", lambda e: e.tensor_tensor(out=sq, in0=dd, in1=dd, op=ALU.mult), reads=["dd"], writes=["sq"])
                    S.op("pe", lambda e: e.matmul(pb[6][:, 0:256], lhsT=onesf, rhs=sq, start=True, stop=True),
                         reads=["sq", "onesf"], writes=[pbk(6)])
                    S.op("act", lambda e: e.activation(out=rstd, in_=pb[6][:, 0:256], func=AF.Sqrt, scale=1.0 / 128, bias=epsc),
                         reads=[pbk(6), "eps"], writes=["rstd"])
                    S.op("dve", lambda e: e.reciprocal(out=rstd, in_=rstd), reads=["rstd"], writes=["rstd"])
                    S.op("dve", lambda e: e.scalar_tensor_tensor(out=yT[:, 2 * gi:2 * gi + 2, :], in0=dd.rearrange("p (h q) -> p h q", h=2),
                                                                 scalar=sgc, in1=rstd.rearrange("p (h q) -> p h q", h=2),
                                                                 op0=ALU.mult, op1=ALU.mult),
                         reads=["dd", "rstd", "sg"], writes=[("yT", gi)])
                else:
                    for cl in range(2):
                        ch = 4 + 2 * gi + cl
                        for hf in range(2):
                            jl = 2 * cl + hf
                            S.op("dve", lambda e, ch=ch, hf=hf, jl=jl: e.tensor_tensor(
                                out=yT[hf * 64:(hf + 1) * 64, ch, :], in0=pb[2][hf * 64:(hf + 1) * 64, jl * 128:(jl + 1) * 128],
                                in1=rz[hf * 64:(hf + 1) * 64, jl * 128:(jl + 1) * 128], op=ALU.mult),
                                reads=[pbk(2), "rz"], writes=[("yT", 2 + gi)])

            def make_hook(it0, it1):
                st_ = {"next": it0}

                def hook(kt):
                    want = it0 + ((kt + 1) * (it1 - it0)) // nk
                    while st_["next"] < want:
                        bis_iter(st_["next"])
                        st_["next"] += 1
                return hook
            emit_group("A", 0, make_hook(0, NITER // 2))
            emit_group("A", 1, make_hook(NITER // 2, NITER))
            S.op("dve", lambda e, N=N: e.tensor_scalar(out=sc[:, 0:N], in0=sc[:, 0:N], scalar1=lo, scalar2=None, op0=ALU.is_ge),
                 reads=SCK + ["b_lo"], writes=SCK)
            for k4 in range((nk + 3) // 4):
                n4 = min(4, nk - 4 * k4)
                for i in range(n4):
                    kt = 4 * k4 + i
                    S.op("pe", lambda e, kt=kt, i=i: e.transpose(out=pb[6][:, i * 128:(i + 1) * 128], in_=sc[:, kt * 128:(kt + 1) * 128],
                                                                 identity=identf), reads=SCK + ["identf"], writes=[pbk(6)])
                S.op("act", lambda e, k4=k4, n4=n4: e.copy(out=maskT[:, 4 * k4:4 * k4 + n4, :],
                                                           in_=pb[6][:, 0:n4 * 128].rearrange("p (a b) -> p a b", a=n4)),
                     reads=[pbk(6)], writes=[("maskT", k4)])
            if qi + 1 < NQT:
                q_front(qi + 1)
            emit_group("B", 0)
            emit_group("B", 1)
            YT = [("yT", i) for i in range(4)]
            if debug:
                S.dma("pool", lambda e, qi=qi: e.dma_start(out=ydbg[qi], in_=yT), reads=YT, writes=["ydbg"])
            for hf in range(2):
                for c in range(8):
                    S.op("pe", lambda e, hf=hf, c=c: e.matmul(pb[hf][:], lhsT=yT[:, c, :], rhs=wo[:, c, hf * 512:(hf + 1) * 512],
                                                              start=(c == 0), stop=(c == 7)), reads=YT + WO, writes=[pbk(hf)])
            norm_residual(0, 1, g1, "g1", xc, xk, tmpn, junkf)
            outs.append(S.dma("sp", lambda e, qi=qi, xc=xc: e.dma_start(out=x1d[qi * 128:(qi + 1) * 128, :], in_=xc), reads=[xk], writes=["x1d"]))
        S.barrier()

        def mlp_phase(layer, ntiles, src_d, dst_d, gi0):
            A.off = P0
            Wup = A.alloc((8, 4096), BF16)
            Wdn = A.alloc((32, 1024), BF16)
            ga = A.alloc((1024,), F32)
            gb_ = A.alloc((1024,), F32)
            xtm = [A.alloc((1024,), F32) for _ in range(4)]
            hbm = A.alloc((1024,), BF16)
            jb = A.alloc((1024,), BF16)
            hTm = A.alloc((8, 256), BF16)
            uT = A.alloc((32, 256), BF16)
            rb2 = [A.alloc((2, 256), F32) for _ in range(2)]
            tmpm = A.alloc((1024,), F32)
            jf = A.alloc((512,), F32)
            assert A.off <= NW
            load_w(Wup, lambda i: ("Wup", i), wup[layer].rearrange("(c p) n -> p c n", p=128), 4096, 512)
            for p4 in range(4):
                S.dma("pool", lambda e, p4=p4: e.dma_start(out=Wdn[:, 8 * p4:8 * p4 + 8, :],
                                                          in_=wdn[layer].rearrange("(f p) n -> p f n", p=128)[:, 8 * p4:8 * p4 + 8, :]),
                      writes=[("Wdn", p4)])
            S.dma("sp", lambda e: e.dma_start(out=ga, in_=gb[gi0]), writes=["ga"])
            S.dma("sp", lambda e: e.dma_start(out=gb_, in_=gb[gi0 + 1]), writes=["gb"])
            res = []
            groups = [list(range(i, min(i + 2, ntiles))) for i in range(0, ntiles, 2)]
            def front(gidx):
                grp = groups[gidx]
                xb = 2 * (gidx % 2)
                for i, t in enumerate(grp):
                    xk = ("xtm", xb + i)
                    S.dma("sp", lambda e, t=t, i=i: e.dma_start(out=xtm[xb + i], in_=src_d[t * 128:(t + 1) * 128, :]), reads=["srcd"], writes=[xk])
                    rmsnorm_rows(xtm[xb + i], xk, ga, "ga", hbm, "hbm", jb)
                    transpose8(hbm, "hbm", hTm[:, :, i * 128:(i + 1) * 128], ("hTm", i), eng="act" if i == 0 else "dve")

            def up(gidx):
                grp = groups[gidx]
                T = 128 * len(grp)
                HK = [("hTm", i) for i in range(len(grp))]
                for f2 in range(16):
                    bk = 4 + f2 % 2
                    for fl in range(2):
                        f = 2 * f2 + fl
                        for dc in range(8):
                            S.op("pe", lambda e, f=f, fl=fl, dc=dc, bk=bk: e.matmul(
                                pb[bk][:, fl * 256:fl * 256 + T], lhsT=Wup[:, dc, f * 128:(f + 1) * 128], rhs=hTm[:, dc, 0:T],
                                start=(dc == 0), stop=(dc == 7)), reads=HK + [("Wup", f // 4)], writes=[pbk(bk)])
                    rb = rb2[f2 % 2]
                    S.op("act", lambda e, bk=bk, rb=rb: e.activation(
                        out=rb[:, :, 0:T], in_=pb[bk][:].rearrange("p (a b) -> p a b", a=2)[:, :, 0:T], func=AF.Relu),
                        reads=[pbk(bk)], writes=[("rb2", f2 % 2)])
                    S.op("pool", lambda e, rb=rb, f2=f2: e.tensor_tensor(out=uT[:, 2 * f2:2 * f2 + 2, 0:T], in0=rb[:, :, 0:T],
                                                                    in1=rb[:, :, 0:T], op=ALU.mult),
                         reads=[("rb2", f2 % 2)], writes=[("uT", f2)])

            def down(gidx):
                grp = groups[gidx]
                xb = 2 * (gidx % 2)
                UK = [("uT", f2) for f2 in range(16)]
                for i, t in enumerate(grp):
                    b0 = 0 if i == 0 else 2
                    for hf in range(2):
                        for f in range(32):
                            S.op("pe", lambda e, i=i, hf=hf, f=f, b0=b0: e.matmul(pb[b0 + hf][:], lhsT=uT[:, f, i * 128:(i + 1) * 128],
                                                                                 rhs=Wdn[:, f, hf * 512:(hf + 1) * 512], start=(f == 0), stop=(f == 31)),
                                 reads=UK + [("Wdn", f // 8)], writes=[pbk(b0 + hf)])
                    norm_residual(b0, b0 + 1, gb_, "gb", xtm[xb + i], ("xtm", xb + i), tmpm, jf)
                    res.append(S.dma("sp", lambda e, t=t, i=i: e.dma_start(out=dst_d[t * 128:(t + 1) * 128, :], in_=xtm[xb + i]),
                                     reads=[("xtm", xb + i)], writes=["dstd"]))

            front(0)
            for gidx in range(len(groups)):
                up(gidx)
                if gidx + 1 < len(groups):
                    front(gidx + 1)
                down(gidx)
            S.barrier()
            return res

        mlp_phase(0, NQT, x1d, x2d, 2)

        A.off = P0
        wci = A.alloc((8, 2560), BF16)
        wco = A.alloc((8, 1024), BF16)
        dg = A.alloc((31, 4, 128), BF16)
        dwT = A.alloc((4, 31), F32)
        cvec = A.alloc((3, 4), F32)
        scw = A.alloc((4, 3), F32)
        g4 = A.alloc((1024,), F32)
        g5 = A.alloc((1024,), F32)
        xtc = [A.alloc((1024,), F32) for _ in range(4)]
        hbc = A.alloc((1024,), BF16)
        jbc = A.alloc((1024,), BF16)
        hTc = A.alloc((8, 256), BF16)
        uTb = A.alloc((4, 32 + 256), BF16)
        eTb = A.alloc((4, 2 + 256), F32)
        sgb = A.alloc((256,), F32)
        utmp = A.alloc((256,), F32)
        dhs = A.alloc((256,), F32)
        z1 = A.alloc((256,), F32)
        vv = A.alloc((4, 256), F32)
        sqv = A.alloc((4, 256), F32)
        mean = A.alloc((256,), F32)
        msq = A.alloc((256,), F32)
        rsd = A.alloc((256,), F32)
        tcb = A.alloc((256,), F32)
        ymT = A.alloc((8, 256), BF16)
        tmpc = A.alloc((1024,), F32)
        jfc = A.alloc((512,), F32)
        assert A.off <= NW
        load_w(wci, lambda i: ("wci", i), cwin.rearrange("(c p) n -> p c n", p=128), 2560, 512)
        WCI = [("wci", i) for i in range(5)]
        load_w(wco, lambda i: ("wco", i), cwout.rearrange("(c p) n -> p c n", p=128), 1024, 512)
        WCO = [("wco", 0), ("wco", 1)]
        S.dma("sp", lambda e: e.dma_start(out=dwT, in_=dwT_d), writes=["dwT"])
        S.dma("sp", lambda e: e.dma_start(out=cvec, in_=cvec_d), writes=["cvec"])
        S.dma("sp", lambda e: e.dma_start(out=scw, in_=scw_d), writes=["scw"])
        S.dma("sp", lambda e: e.dma_start(out=g4, in_=gb[4]), writes=["g4"])
        S.dma("sp", lambda e: e.dma_start(out=g5, in_=gb[5]), writes=["g5"])
        cnt = 0
        for j in range(31):
            for c in range(4):
                eng = "dve" if cnt % 2 == 0 else "pool"
                S.op(eng, lambda e, j=j, c=c: e.tensor_scalar(out=dg[:, j, c, :], in0=identf, scalar1=dwT[:, c, j:j + 1], scalar2=None, op0=ALU.mult),
                     reads=["identf", "dwT"], writes=[("dg", eng)])
                cnt += 1
        DG = [("dg", "dve"), ("dg", "pool")]
        S.op("pool", lambda e: e.memset(uTb[:, :, :], 0.0), writes=["uTb"])
        S.op("pool", lambda e: e.memset(eTb[:, :, :], 0.0), writes=["eTb"])
        cgroups = [[0]] + [[1 + 2 * i, 2 + 2 * i] for i in range(8)]
        for gidx, grp in enumerate(cgroups):
            T = 128 * len(grp)
            halo = (grp == [0])
            xb = 2 * (gidx % 2)
            for i, li in enumerate(grp):
                xk = ("xtc", xb + i)
                S.dma("sp", lambda e, li=li, i=i, xb=xb: e.dma_start(out=xtc[xb + i], in_=x2d[li * 128:(li + 1) * 128, :]), reads=["dstd"], writes=[xk])
                rmsnorm_rows(xtc[xb + i], xk, g4, "g4", hbc, "hbc", jbc)
                transpose8(hbc, "hbc", hTc[:, :, i * 128:(i + 1) * 128], ("hTc", i), eng="act" if i == 0 else "dve")
            HK = [("hTc", i) for i in range(len(grp))]

            def proj(bank, col0, T=T, HK=HK):
                for dc in range(8):
                    S.op("pe", lambda e, dc=dc: e.matmul(pb[bank][:, 0:T], lhsT=wci[:, dc, col0:col0 + 128], rhs=hTc[:, dc, 0:T],
                                                         start=(dc == 0), stop=(dc == 7)), reads=HK + WCI, writes=[pbk(bank)])
            for c in range(4):
                proj(0, c * 128)
                proj(1, 512 + c * 128)
                S.op("act", lambda e, T=T: e.activation(out=sgb[:, 0:T], in_=pb[1][:, 0:T], func=AF.Sigmoid), reads=[pbk(1)], writes=["sgb"])
                if halo:
                    S.op("dve", lambda e, T=T: e.tensor_tensor(out=utmp[:, 0:T], in0=pb[0][:, 0:T], in1=sgb[:, 0:T], op=ALU.mult),
                         reads=[pbk(0), "sgb"], writes=["utmp"])
                    S.op("dve", lambda e, c=c, T=T: e.tensor_scalar(out=uTb[:, c, 32:32 + T], in0=utmp[:, 0:T], scalar1=flagc, scalar2=None, op0=ALU.mult),
                         reads=["utmp", "flags", "uTb"], writes=[("uT", c)])
                else:
                    S.op("dve", lambda e, c=c, T=T: e.tensor_tensor(out=uTb[:, c, 32:32 + T], in0=pb[0][:, 0:T], in1=sgb[:, 0:T], op=ALU.mult),
                         reads=[pbk(0), "sgb", "uTb"], writes=[("uT", c)])
            for c in range(4):
                proj(2, 1536 + c * 128)
                proj(3, 2048 + c * 128)
                S.op("act", lambda e, T=T: e.copy(out=dhs[:, 0:T], in_=pb[3][:, 0:T]), reads=[pbk(3)], writes=["dhs"])
                if halo:
                    S.op("dve", lambda e, T=T: e.tensor_tensor(out=utmp[:, 0:T], in0=pb[2][:, 0:T], in1=dhs[:, 0:T], op=ALU.mult),
                         reads=[pbk(2), "dhs"], writes=["utmp"])
                    S.op("dve", lambda e, c=c, T=T: e.tensor_scalar(out=eTb[:, c, 2:2 + T], in0=utmp[:, 0:T], scalar1=flagc, scalar2=None, op0=ALU.mult),
                         reads=["utmp", "flags", "eTb"], writes=[("eT", c)])
                else:
                    S.op("dve", lambda e, c=c, T=T: e.tensor_tensor(out=eTb[:, c, 2:2 + T], in0=pb[2][:, 0:T], in1=dhs[:, 0:T], op=ALU.mult),
                         reads=[pbk(2), "dhs", "eTb"], writes=[("eT", c)])
            if not halo:
                for c in range(4):
                    proj(4, 1024 + c * 128)
                    S.op("dve", lambda e, c=c, T=T: e.tensor_scalar(out=z1[:, 0:T], in0=eTb[:, c, 0:T], scalar1=scw[:, c, 0:1], scalar2=None, op0=ALU.mult),
                         reads=[("eT", c), "scw"], writes=["z1"])
                    for jj in (1, 2):
                        S.op("dve", lambda e, c=c, T=T, jj=jj: e.scalar_tensor_tensor(out=z1[:, 0:T], in0=eTb[:, c, jj:jj + T], scalar=scw[:, c, jj:jj + 1],
                                                                                    in1=z1[:, 0:T], op0=ALU.mult, op1=ALU.add),
                             reads=[("eT", c), "scw", "z1"], writes=["z1"])
                    S.op("dve", lambda e, c=c, T=T: e.tensor_tensor(out=ymT[:, 4 + c, 0:T], in0=pb[4][:, 0:T], in1=z1[:, 0:T], op=ALU.mult),
                         reads=[pbk(4), "z1"], writes=[("ymT", 4 + c)])
                for c in range(4):
                    for j in range(31):
                        S.op("pe", lambda e, c=c, j=j, T=T: e.matmul(pb[5][:, 0:T], lhsT=dg[:, j, c, :], rhs=uTb[:, c, 2 + j:2 + j + T],
                                                                     start=(j == 0), stop=(j == 30)),
                             reads=[("uT", c)] + DG, writes=[pbk(5)])
                    S.op("act", lambda e, c=c, T=T: e.activation(out=vv[:, c, 0:T], in_=pb[5][:, 0:T], func=AF.Identity, bias=cvec[:, 0, c:c + 1], scale=1.0),
                         reads=[pbk(5), "cvec"], writes=[("vv", c)])
                    S.op("act", lambda e, c=c, T=T: e.activation(out=sqv[:, c, 0:T], in_=vv[:, c, 0:T], func=AF.Square),
                         reads=[("vv", c)], writes=[("sqv", c)])
                for c in range(4):
                    S.op("pe", lambda e, c=c, T=T: e.matmul(pb[6][:, 0:T], lhsT=onesf, rhs=vv[:, c, 0:T], start=(c == 0), stop=(c == 3)),
                         reads=[("vv", c), "onesf"], writes=[pbk(6)])
                for c in range(4):
                    S.op("pe", lambda e, c=c, T=T: e.matmul(pb[6][:, 256:256 + T], lhsT=onesf, rhs=sqv[:, c, 0:T], start=(c == 0), stop=(c == 3)),
                         reads=[("sqv", c), "onesf"], writes=[pbk(6)])
                S.op("act", lambda e, T=T: e.mul(out=mean[:, 0:T], in_=pb[6][:, 0:T], mul=1.0 / 512), reads=[pbk(6)], writes=["mean"])
                S.op("pool", lambda e, T=T: e.tensor_tensor(out=msq[:, 0:T], in0=mean[:, 0:T], in1=mean[:, 0:T], op=ALU.mult), reads=["mean"], writes=["msq"])
                S.op("dve", lambda e, T=T: e.scalar_tensor_tensor(out=rsd[:, 0:T], in0=pb[6][:, 256:256 + T], scalar=1.0 / 512, in1=msq[:, 0:T],
                                                                op0=ALU.mult, op1=ALU.subtract), reads=[pbk(6), "msq"], writes=["rsd"])
                S.op("act", lambda e, T=T: e.activation(out=rsd[:, 0:T], in_=rsd[:, 0:T], func=AF.Sqrt, scale=1.0, bias=epsc), reads=["rsd", "eps"], writes=["rsd"])
                S.op("dve", lambda e, T=T: e.reciprocal(out=rsd[:, 0:T], in_=rsd[:, 0:T]), reads=["rsd"], writes=["rsd"])
                for c in range(4):
                    S.op("pool", lambda e, c=c, T=T: e.tensor_tensor(out=tcb[:, 0:T], in0=vv[:, c, 0:T], in1=mean[:, 0:T], op=ALU.subtract),
                         reads=[("vv", c), "mean"], writes=["tcb"])
                    S.op("pool", lambda e, T=T: e.tensor_tensor(out=tcb[:, 0:T], in0=tcb[:, 0:T], in1=rsd[:, 0:T], op=ALU.mult),
                         reads=["tcb", "rsd"], writes=["tcb"])
                    S.op("act", lambda e, c=c, T=T: e.activation(out=ymT[:, c, 0:T], in_=tcb[:, 0:T], func=AF.Silu, scale=cvec[:, 1, c:c + 1],
                                                                bias=cvec[:, 2, c:c + 1]), reads=["tcb", "cvec"], writes=[("ymT", c)])
                YM = [("ymT", c) for c in range(8)]
                for i, li in enumerate(grp):
                    for hf in range(2):
                        for c in range(8):
                            S.op("pe", lambda e, i=i, hf=hf, c=c: e.matmul(pb[hf][:], lhsT=ymT[:, c, i * 128:(i + 1) * 128],
                                                                          rhs=wco[:, c, hf * 512:(hf + 1) * 512], start=(c == 0), stop=(c == 7)),
                                 reads=YM + WCO, writes=[pbk(hf)])
                    norm_residual(0, 1, g5, "g5", xtc[xb + i], ("xtc", xb + i), tmpc, jfc)
                    S.dma("sp", lambda e, li=li, i=i, xb=xb: e.dma_start(out=x3d[(li - 1) * 128:li * 128, :], in_=xtc[xb + i]),
                          reads=[("xtc", xb + i)], writes=["x3d"])
            UT = [("uT", c) for c in range(4)]
            ET = [("eT", c) for c in range(4)]
            S.op("pool", lambda e, T=T: e.tensor_copy(out=uTb[:, :, 0:32], in_=uTb[:, :, T:T + 32]), reads=UT, writes=UT + ["uTb"])
            S.op("pool", lambda e, T=T: e.tensor_copy(out=eTb[:, :, 0:2], in_=eTb[:, :, T:T + 2]), reads=ET, writes=ET + ["eTb"])
        S.barrier()

        A.off = P0

        def final_src_fix():
            pass
        res = mlp_phase_final = None
        res = mlp_phase(1, 16, x3d, out_d, 6)
        S.emit(nc, final_wait_tokens=res)
    return nc


def _t5_bucket_np(rel):
    import jax
    import jax.numpy as jnp
    cpu = jax.devices("cpu")[0]
    with jax.default_device(cpu):
        rel = jnp.asarray(rel, dtype=jnp.int32)
        nb = 16
        ret = jnp.where(rel > 0, nb, 0)
        n = jnp.abs(rel)
        max_exact = nb // 2
        nf = jnp.maximum(n, 1).astype(jnp.float32)
        large = max_exact + (jnp.log(nf / max_exact) / math.log(128 / max_exact) * (nb - max_exact)).astype(jnp.int32)
        large = jnp.minimum(large, nb - 1)
        out = ret + jnp.where(n < max_exact, n, large)
        return np.asarray(out)


_PROG = {}


def _get_prog(debug=False):
    if debug not in _PROG:
        _PROG[debug] = build_program(debug)
    return _PROG[debug]


def make_in_maps(inputs):
    f = lambda a: np.ascontiguousarray(np.asarray(a, dtype=np.float32))
    x = f(inputs["x"])
    rel_bias = f(inputs["rel_bias"])
    norm_g = f(inputs["norm_g"])
    kk = np.arange(128)[:, None]
    qq = np.arange(128)[None, :]
    bD = _t5_bucket_np(kk - qq)
    bP = _t5_bucket_np(kk - 128 - qq)
    admiss = (kk // 64) <= (qq // 64)
    tabA = rel_bias[:, :4]
    tabB = rel_bias[:, 4:]
    biasA = np.zeros((128, 2, 8, 128), np.float32)
    biasB = np.zeros((128, 2, 8, 128), np.float32)
    for m in range(8):
        biasA[:, 0, m, :] = np.where(admiss, tabA[bD, m // 2], np.float32(NEG))
        biasA[:, 1, m, :] = tabA[bP, m // 2]
        biasB[:, 0, m, :] = np.where(admiss, tabB[bD, m], np.float32(NEG))
        biasB[:, 1, m, :] = tabB[bP, m]
    cA = np.ascontiguousarray(np.broadcast_to(np.repeat(tabA[15], 2)[None, :], (128, 8)))
    cB = np.ascontiguousarray(np.broadcast_to(tabB[15][None, :], (128, 8)))
    gbb = np.ascontiguousarray(np.broadcast_to(norm_g.reshape(8, 1, D), (8, 128, D)))
    lvb = np.ascontiguousarray(np.broadcast_to(f(inputs["diff_lambda"])[0][None], (128, 4, 64)))
    sgn = np.ascontiguousarray(f(inputs["diff_subln_g"])[0].reshape(128, 1))
    selA = np.zeros((128, 2), np.float32)
    selA[:64, 0] = 0.125
    selA[64:, 1] = 0.125
    selI = np.zeros((128, 4), np.float32)
    for i in range(4):
        selI[32 * i:32 * i + 32, i] = 1.0
    dwT = np.ascontiguousarray(f(inputs["conv_dw_w"])[0].reshape(31, 4, 128).transpose(2, 1, 0))
    cvec = np.ascontiguousarray(np.stack([f(inputs["conv_dw_b"])[0].reshape(4, 128).T,
                                          f(inputs["conv_ln_g"])[0].reshape(4, 128).T,
                                          f(inputs["conv_ln_b"])[0].reshape(4, 128).T], axis=1))
    scw = np.ascontiguousarray(f(inputs["sconv_w"])[0].reshape(3, 4, 128).transpose(2, 1, 0))
    common = dict(win=f(inputs["attn_w_in"])[0], wout=f(inputs["attn_w_out"])[0], wup=f(inputs["w_mlp_up"]),
                  wdn=f(inputs["w_mlp_down"]), cwin=f(inputs["conv_w_in"])[0], cwout=f(inputs["conv_w_out"])[0],
                  gb=gbb, biasA=biasA, biasB=biasB, cA=cA, cB=cB, lvb=lvb, sgn=sgn, selA=selA, selI=selI,
                  ident=np.eye(128, dtype=np.float32), dwT=dwT, cvec=cvec, scw=scw)
    in_maps = []
    for c in range(8):
        b, h = c // 2, c % 2
        if h == 1:
            xl = x[b]
        else:
            xl = np.concatenate([np.zeros((2048, D), np.float32), x[b, :2048]], axis=0)
        flags = np.zeros((128, 2), np.float32)
        flags[:, 0] = float(h)
        flags[:, 1] = 0.0 if h == 1 else NEG
        m = dict(common)
        m["xloc"] = np.ascontiguousarray(xl)
        m["flags"] = flags
        in_maps.append(m)
    return in_maps


def kernel(**inputs):
    nc = _get_prog(False)
    in_maps = make_in_maps(inputs)
    res = run_bass_kernel_spmd(nc, in_maps, core_ids=list(range(8)))
    out = np.zeros((4, 4096, D), np.float32)
    for c in range(8):
        b, h = c // 2, c % 2
        out[b, h * 2048:(h + 1) * 2048] = res.results[c]["out"]
    return out
```

```python
import math
import contextlib
import numpy as np
import concourse.bass as bass
import concourse.mybir as mybir
from concourse.bass_utils import run_bass_kernel_spmd

F32 = mybir.dt.float32
BF16 = mybir.dt.bfloat16
ALU = mybir.AluOpType
AF = mybir.ActivationFunctionType
AX = mybir.AxisListType

ENGS = ["pe", "act", "dve", "pool", "sp"]
NDMA_SLOTS = 8
NEG = -1.0e30
EPS = 1e-6
NITER = 16
TOPK = 256
D = 1024
QT0 = 15
NQT = 17


class Sched:
    def __init__(self):
        self.ops = {e: [] for e in ENGS}
        self.last_w = {}
        self.readers = {}
        self.dma_slot_rr = {e: 0 for e in ENGS}
        self.dma_slot_last = {}
        self.dma_slot_uses = {}
        self.pending = {e: set() for e in ENGS}
        self.last_tok = {}
        self.open_dma = set()

    def _deps_for(self, reads, writes):
        deps = set()
        for k in reads:
            deps.update(self.last_w.get(k, {}).values())
        for k in writes:
            deps.update(self.last_w.get(k, {}).values())
            deps.update(self.readers.get(k, {}).values())
        return deps

    def _commit(self, tok, track, reads, writes):
        for k in reads:
            self.readers.setdefault(k, {})[track] = tok
        for k in writes:
            self.last_w[k] = {track: tok}
            self.readers[k] = {}

    def op(self, eng, fn, reads=(), writes=()):
        idx = len(self.ops[eng])
        deps = self._deps_for(reads, writes)
        tok = ("c", eng, idx)
        raw = set()
        if eng != "pe":
            for k in reads:
                raw.update(self.last_w.get(k, {}).values())
        deps = {d for d in deps if not (d[0] == "c" and d[1] == eng) or d in raw}
        deps |= self.pending[eng]
        self.pending[eng] = set()
        self.ops[eng].append(dict(fn=fn, deps=deps, tok=tok, dma=False))
        self._commit(tok, eng, reads, writes)
        self.last_tok[eng] = tok
        return tok

    def dma(self, eng, fn, reads=(), writes=()):
        slot = self.dma_slot_rr[eng]
        self.dma_slot_rr[eng] = (slot + 1) % NDMA_SLOTS
        use = self.dma_slot_uses.get((eng, slot), 0) + 1
        self.dma_slot_uses[(eng, slot)] = use
        tok = ("d", eng, slot, use)
        deps = self._deps_for(reads, writes)
        prev = self.dma_slot_last.get((eng, slot))
        if prev is not None:
            deps.add(prev)
            self.open_dma.discard(prev)
        self.dma_slot_last[(eng, slot)] = tok
        deps |= self.pending[eng]
        self.pending[eng] = set()
        self.ops[eng].append(dict(fn=fn, deps=deps, tok=tok, dma=True))
        self._commit(tok, ("dma", eng, slot), reads, writes)
        self.open_dma.add(tok)
        return tok

    def barrier(self):
        toks = set(self.last_tok.values()) | set(self.open_dma)
        for e in ENGS:
            self.pending[e] |= {t for t in toks if not (t[0] == "c" and t[1] == e)}
        self.open_dma = set()

    def emit(self, nc, final_wait_tokens=()):
        needed = {e: set() for e in ENGS}
        for e in ENGS:
            for o in self.ops[e]:
                for d in o["deps"]:
                    if d[0] == "c":
                        needed[d[1]].add(d[2])
        semval = {e: {} for e in ENGS}
        for e in ENGS:
            c = 0
            for i in range(len(self.ops[e])):
                if i in needed[e]:
                    c += 1
                    semval[e][i] = c
        with contextlib.ExitStack() as st:
            csem = {e: st.enter_context(nc.semaphore("c_" + e)) for e in ENGS if e != "sp"}
            dsem = {}
            for e in ENGS:
                for s in range(NDMA_SLOTS):
                    if (e, s) in self.dma_slot_uses:
                        dsem[(e, s)] = st.enter_context(nc.semaphore("d_%s_%d" % (e, s)))
            block = st.enter_context(nc.Block())

            def resolve(tok):
                if tok[0] == "c":
                    return csem[tok[1]], semval[tok[1]][tok[2]], ("c", tok[1])
                return dsem[(tok[1], tok[2])], 16 * tok[3], ("d", tok[1], tok[2])

            def run(e, engine):
                waited = {}
                for i, o in enumerate(self.ops[e]):
                    ws = {}
                    for d in o["deps"]:
                        sem, val, sk = resolve(d)
                        if waited.get(sk, 0) >= val:
                            continue
                        if sk not in ws or ws[sk][1] < val:
                            ws[sk] = (sem, val)
                    for sk, (sem, val) in ws.items():
                        engine.wait_ge(sem, val)
                        waited[sk] = val
                    ins = o["fn"](engine)
                    if o["dma"]:
                        ins.then_inc(dsem[(o["tok"][1], o["tok"][2])], 16)
                    elif i in needed[e]:
                        ins.then_inc(csem[e], 1)
                if e == "sp":
                    for d in final_wait_tokens:
                        sem, val, sk = resolve(d)
                        engine.wait_ge(sem, val)

            block.tensor(lambda eng: run("pe", eng))
            block.scalar(lambda eng: run("act", eng))
            block.vector(lambda eng: run("dve", eng))
            block.gpsimd(lambda eng: run("pool", eng))
            block.sync(lambda eng: run("sp", eng))


class Arena:
    def __init__(self, ap, nwords):
        self.ap = ap
        self.n = nwords
        self.off = 0

    def alloc(self, free_shape, dt, at=None):
        nel = int(np.prod(free_shape))
        nbytes = nel * (2 if dt == BF16 else 4)
        nw = (nbytes + 3) // 4
        off = self.off if at is None else at
        assert off + nw <= self.n, ("arena overflow", off, nw, self.n)
        a = self.ap[:, off:off + nw]
        if at is None:
            self.off += nw
        if dt == BF16:
            a = a.bitcast(BF16)[:, :nel]
        if len(free_shape) == 2:
            a = a.rearrange("p (a b) -> p a b", a=free_shape[0])
        elif len(free_shape) == 3:
            a = a.rearrange("p (a b c) -> p a b c", a=free_shape[0], b=free_shape[1])
        return a


class Ctx:
    pass


def build_program(debug=False):
    nc = bass.Bass("TRN2", target_bir_lowering=False)
    S = Sched()
    C = Ctx()
    C.nc, C.S = nc, S
    din = lambda n, s: nc.dram_tensor(n, list(s), F32, kind="ExternalInput").ap()
    xloc = din("xloc", [4096, D])
    win = din("win", [D, 2472])
    wout = din("wout", [D, D])
    wup = din("wup", [2, D, 4096])
    wdn = din("wdn", [2, 4096, D])
    cwin = din("cwin", [D, 2560])
    cwout = din("cwout", [D, D])
    gb = din("gb", [8, 128, D])
    biasA_d = din("biasA", [128, 2, 8, 128])
    biasB_d = din("biasB", [128, 2, 8, 128])
    cA_d = din("cA", [128, 8])
    cB_d = din("cB", [128, 8])
    lvb_d = din("lvb", [128, 4, 64])
    sgn_d = din("sgn", [128, 1])
    flags_d = din("flags", [128, 2])
    selA_d = din("selA", [128, 2])
    selI_d = din("selI", [128, 4])
    ident_d = din("ident", [128, 128])
    dwT_d = din("dwT", [128, 4, 31])
    cvec_d = din("cvec", [128, 3, 4])
    scw_d = din("scw", [128, 4, 3])
    out_d = nc.dram_tensor("out", [2048, D], F32, kind="ExternalOutput").ap()
    kind = "ExternalOutput"
    x1d = nc.dram_tensor("x1d", [NQT * 128, D], F32, kind=kind).ap()
    x2d = nc.dram_tensor("x2d", [NQT * 128, D], F32, kind=kind).ap()
    x3d = nc.dram_tensor("x3d", [2048, D], F32, kind=kind).ap()
    ydbg = nc.dram_tensor("ydbg", [NQT, 128, 8, 128], F32, kind="ExternalOutput").ap() if debug else None

    NW = 53200
    with contextlib.ExitStack() as st:
        arena_t = st.enter_context(nc.sbuf_tensor("arena", [128, NW], F32))
        A = Arena(arena_t[:], NW)
        pb = [st.enter_context(nc.psum_tensor("pb%d" % i, [128, 512], F32)) for i in range(7)]
        pbt = st.enter_context(nc.psum_tensor("pbt", [128, 1024], BF16))
        pbk = lambda i: ("pb", i)

        identf = A.alloc((128,), F32)
        identb = A.alloc((128,), BF16)
        onesf = A.alloc((128,), F32)
        onesb = A.alloc((128,), BF16)
        smallc = A.alloc((16,), F32)
        selA = A.alloc((2,), F32)
        selI = A.alloc((4,), F32)
        stat = A.alloc((64,), F32)
        stepc = A.alloc((NITER,), F32)
        P0 = A.off
        epsc, zeroc, flagc, pnegc = smallc[:, 0:1], smallc[:, 1:2], smallc[:, 2:3], smallc[:, 3:4]
        neglam, sgc = smallc[:, 4:5], smallc[:, 5:6]
        C.statctr = 0

        def newstat():
            i = C.statctr % 64
            C.statctr += 1
            return stat[:, i:i + 1], ("stat", i)

        S.dma("sp", lambda e: e.dma_start(out=identf, in_=ident_d), writes=["identf"])
        S.dma("sp", lambda e: e.dma_start(out=smallc[:, 2:4], in_=flags_d), writes=["flags"])
        S.dma("sp", lambda e: e.dma_start(out=selA, in_=selA_d), writes=["selA"])
        S.dma("sp", lambda e: e.dma_start(out=selI, in_=selI_d), writes=["selI"])
        S.op("dve", lambda e: e.tensor_copy(out=identb, in_=identf), reads=["identf"], writes=["identb"])
        S.op("pool", lambda e: e.memset(onesf, 1.0), writes=["onesf"])
        S.op("pool", lambda e: e.memset(onesb, 1.0), writes=["onesb"])
        S.op("pool", lambda e: e.memset(smallc[:, 0:1], EPS), writes=["eps"])
        S.op("pool", lambda e: e.memset(smallc[:, 1:2], 0.0), writes=["zero"])
        for it in range(NITER):
            S.op("pool", lambda e, it=it: e.memset(stepc[:, it:it + 1], 2.0 ** -(it + 1)), writes=["stepc"])

        def rmsnorm_rows(x_ap, x_key, g_ap, g_key, hb_ap, hb_key, junkb):
            ss, ssk = newstat()
            rs, rsk = newstat()
            S.op("act", lambda e: e.activation(out=junkb, in_=x_ap, func=AF.Square, accum_out=ss),
                 reads=[x_key], writes=["junkb", ssk])
            S.op("act", lambda e: e.activation(out=rs, in_=ss, func=AF.Sqrt, scale=1.0 / D, bias=epsc),
                 reads=[ssk, "eps"], writes=[rsk])
            S.op("dve", lambda e: e.reciprocal(out=rs, in_=rs), reads=[rsk], writes=[rsk])
            S.op("dve", lambda e: e.scalar_tensor_tensor(out=hb_ap, in0=x_ap, scalar=rs, in1=g_ap,
                                                         op0=ALU.mult, op1=ALU.mult),
                 reads=[x_key, rsk, g_key], writes=[hb_key])

        def transpose8(hb_ap, hb_key, dst_ap, dst_key, eng="act"):
            pv = pbt[:, 0:1024].rearrange("p (c t) -> p c t", c=8)
            for c in range(8):
                S.op("pe", lambda e, c=c: e.transpose(out=pv[:, c, :], in_=hb_ap[:, c * 128:(c + 1) * 128],
                                                      identity=identb),
                     reads=[hb_key, "identb"], writes=["pbt"])
            if eng == "act":
                S.op("act", lambda e: e.copy(out=dst_ap, in_=pv), reads=["pbt"], writes=[dst_key])
            else:
                S.op("dve", lambda e: e.tensor_copy(out=dst_ap, in_=pv), reads=["pbt"], writes=[dst_key])

        def norm_residual(bank0, bank1, g_ap, g_key, x_ap, x_key, tmp_ap, junkf):
            s0, k0 = newstat()
            s1, k1 = newstat()
            rs, rk = newstat()
            S.op("act", lambda e: e.activation(out=junkf, in_=pb[bank0][:], func=AF.Square, accum_out=s0),
                 reads=[pbk(bank0)], writes=["junkf", k0])
            S.op("act", lambda e: e.activation(out=junkf, in_=pb[bank1][:], func=AF.Square, accum_out=s1),
                 reads=[pbk(bank1)], writes=["junkf", k1])
            S.op("dve", lambda e: e.tensor_tensor(out=rs, in0=s0, in1=s1, op=ALU.add), reads=[k0, k1], writes=[rk])
            S.op("act", lambda e: e.activation(out=rs, in_=rs, func=AF.Sqrt, scale=1.0 / D, bias=epsc),
                 reads=[rk, "eps"], writes=[rk])
            S.op("dve", lambda e: e.reciprocal(out=rs, in_=rs), reads=[rk], writes=[rk])
            for hf, bk in enumerate((bank0, bank1)):
                S.op("dve", lambda e, hf=hf, bk=bk: e.scalar_tensor_tensor(
                    out=tmp_ap[:, hf * 512:(hf + 1) * 512], in0=pb[bk][:], scalar=rs,
                    in1=g_ap[:, hf * 512:(hf + 1) * 512], op0=ALU.mult, op1=ALU.mult),
                    reads=[pbk(bk), rk, g_key], writes=[("tmpn", hf)])
            S.op("pool", lambda e: e.tensor_tensor(out=x_ap, in0=x_ap, in1=tmp_ap, op=ALU.add),
                 reads=[x_key, ("tmpn", 0), ("tmpn", 1)], writes=[x_key])

        def load_w(dst, dst_keyf, src3, ncols, piece):
            for i, c0 in enumerate(range(0, ncols, piece)):
                c1 = min(ncols, c0 + piece)
                S.dma("pool", lambda e, c0=c0, c1=c1: e.dma_start(out=dst[:, :, c0:c1], in_=src3[:, :, c0:c1]),
                      writes=[dst_keyf(i)])

        A.off = P0
        KA = A.alloc((4, 4096), BF16)
        KB2 = A.alloc((4096,), BF16)
        KI4 = A.alloc((4096,), BF16)
        VA = A.alloc((32, 512), BF16)
        VB2 = A.alloc((32, 128), BF16)
        wq = A.alloc((8, 1288), BF16)
        wo = A.alloc((8, 1024), BF16)
        g0 = A.alloc((1024,), F32)
        g1 = A.alloc((1024,), F32)
        biasA = A.alloc((2, 8, 128), BF16)
        biasB = A.alloc((2, 8, 128), BF16)
        xt = A.alloc((1024,), F32)
        hb = A.alloc((1024,), BF16)
        junkb = A.alloc((1024,), BF16)
        OV = A.off
        wk = A.alloc((8, 1408), BF16)
        hT4 = A.alloc((8, 512), BF16)
        xt2 = A.alloc((1024,), F32)
        biasf = A.alloc((2, 8, 128), F32)
        cAB = A.alloc((16,), F32)
        lvb = A.alloc((4, 64), F32)
        lvj = A.alloc((64,), F32)
        kv_end = A.off
        A.off = OV
        sc = A.alloc((4096,), F32)
        junk = A.alloc((2048,), BF16)
        maskT = A.alloc((32, 128), BF16)
        hT = A.alloc((8, 128), BF16)
        qmA = A.alloc((4, 2, 128), BF16)
        qmB = A.alloc((4, 2, 128), BF16)
        qmI = A.alloc((2, 4, 128), BF16)
        wis = A.alloc((8,), F32)
        rbuf = [A.alloc((512,), F32) for _ in range(2)]
        ptb = [A.alloc((512,), BF16) for _ in range(2)]
        pmb = [A.alloc((512,), BF16) for _ in range(2)]
        rz = A.alloc((512,), F32)
        tt = A.alloc((512,), F32)
        dd = A.alloc((256,), F32)
        sq = tt[:, 0:256]
        rstd = tt[:, 256:512]
        yT = A.alloc((8, 128), BF16)
        tmpn = sc[:, 0:1024]
        junkf = tt
        junk2 = junk
        xtq = [xt, A.alloc((1024,), F32)]
        qmBs = [qmB, A.alloc((4, 2, 128), BF16)]
        bis = A.alloc((8,), F32)
        Rs = A.alloc((NITER,), F32)
        q_end = A.off
        assert max(kv_end, q_end) <= NW

        win3 = win.rearrange("(c p) n -> p c n", p=128)
        wkparts = [(0, 512, 512), (512, 2048, 64), (576, 2048, 64), (640, 2432, 32), (672, 2432, 32),
                   (704, 2432, 32), (736, 2432, 32), (768, 1024, 512), (1280, 2112, 64), (1344, 2112, 64)]
        for i, (d0, s0, n) in enumerate(wkparts):
            S.dma("pool", lambda e, d0=d0, s0=s0, n=n: e.dma_start(out=wk[:, :, d0:d0 + n], in_=win3[:, :, s0:s0 + n]),
                  writes=[("wk", i)])
        WK = [("wk", i) for i in range(len(wkparts))]
        wqparts = [(0, 0, 512), (512, 1536, 512), (1024, 2176, 256), (1280, 2464, 8)]
        for i, (d0, s0, n) in enumerate(wqparts):
            S.dma("pool", lambda e, d0=d0, s0=s0, n=n: e.dma_start(out=wq[:, :, d0:d0 + n], in_=win3[:, :, s0:s0 + n]),
                  writes=[("wq", i)])
        WQ = [("wq", i) for i in range(len(wqparts))]
        load_w(wo, lambda i: ("wo", i), wout.rearrange("(c p) n -> p c n", p=128), 1024, 512)
        WO = [("wo", 0), ("wo", 1)]
        S.dma("sp", lambda e: e.dma_start(out=g0, in_=gb[0]), writes=["g0"])
        S.dma("sp", lambda e: e.dma_start(out=g1, in_=gb[1]), writes=["g1"])
        S.dma("sp", lambda e: e.dma_start(out=cAB[:, 0:8], in_=cA_d), writes=["cAB"])
        S.dma("sp", lambda e: e.dma_start(out=cAB[:, 8:16], in_=cB_d), writes=["cAB2"])
        for which, (src, dstb) in enumerate(((biasA_d, biasA), (biasB_d, biasB))):
            S.dma("sp", lambda e, src=src: e.dma_start(out=biasf, in_=src), writes=["biasf"])
            for m in range(8):
                S.op("dve", lambda e, m=m, dstb=dstb, which=which: e.tensor_scalar(
                    out=dstb[:, :, m, :], in0=biasf[:, :, m, :], scalar1=cAB[:, which * 8 + m:which * 8 + m + 1],
                    scalar2=None, op0=ALU.subtract),
                    reads=["biasf", "cAB", "cAB2"], writes=[("bias", which)])
        lam_init = 0.8 - 0.6 * math.exp(-0.3 * 0)
        S.dma("sp", lambda e: e.dma_start(out=lvb, in_=lvb_d), writes=["lvb"])
        S.dma("sp", lambda e: e.dma_start(out=smallc[:, 5:6], in_=sgn_d), writes=["sgraw"])
        e1, e1k = newstat()
        e2, e2k = newstat()
        S.op("dve", lambda e: e.tensor_tensor(out=lvj, in0=lvb[:, 0, :], in1=lvb[:, 1, :], op=ALU.mult),
             reads=["lvb"], writes=["lvj"])
        S.op("dve", lambda e: e.tensor_reduce(out=e1, in_=lvj, axis=AX.X, op=ALU.add), reads=["lvj"], writes=[e1k])
        S.op("dve", lambda e: e.tensor_tensor(out=lvj, in0=lvb[:, 2, :], in1=lvb[:, 3, :], op=ALU.mult),
             reads=["lvb", e1k], writes=["lvj"])
        S.op("dve", lambda e: e.tensor_reduce(out=e2, in_=lvj, axis=AX.X, op=ALU.add), reads=["lvj"], writes=[e2k])
        S.op("act", lambda e: e.activation(out=e1, in_=e1, func=AF.Exp), reads=[e1k], writes=[e1k])
        S.op("act", lambda e: e.activation(out=e2, in_=e2, func=AF.Exp), reads=[e2k], writes=[e2k])
        S.op("dve", lambda e: e.scalar_tensor_tensor(out=neglam, in0=e2, scalar=-lam_init, in1=e1,
                                                     op0=ALU.add, op1=ALU.subtract),
             reads=[e1k, e2k], writes=["neglam"])
        S.op("dve", lambda e: e.tensor_scalar(out=sgc, in0=sgc, scalar1=1.0 - lam_init, scalar2=None, op0=ALU.mult),
             reads=["sgraw"], writes=["sg"])
        S.op("pool", lambda e: e.memset(VB2[:, :, :], 0.0), writes=["VB2"])

        xts = [xt, xt2]
        for g in range(8):
            for i in range(4):
                t = 4 * g + i
                xa = xts[t % 2]
                xk = ("xt", t % 2)
                S.dma("sp", lambda e, t=t, xa=xa: e.dma_start(out=xa, in_=xloc[t * 128:(t + 1) * 128, :]), writes=[xk])
                rmsnorm_rows(xa, xk, g0, "g0", hb, "hb", junkb)
                transpose8(hb, "hb", hT4[:, :, i * 128:(i + 1) * 128], ("hT4", i), eng="act" if i % 2 == 0 else "dve")
            HT4 = [("hT4", i) for i in range(4)]
            for c in range(6):
                bk = c % 2
                for dc in range(8):
                    S.op("pe", lambda e, c=c, dc=dc, bk=bk: e.matmul(pb[bk][:], lhsT=wk[:, dc, c * 128:(c + 1) * 128],
                                                                    rhs=hT4[:, dc, :], start=(dc == 0), stop=(dc == 7)),
                         reads=HT4 + WK, writes=[pbk(bk)])
                if c < 4:
                    dst = KA[:, c, g * 512:(g + 1) * 512]
                elif c == 4:
                    dst = KB2[:, g * 512:(g + 1) * 512]
                else:
                    dst = KI4[:, g * 512:(g + 1) * 512]
                if c % 2 == 0:
                    S.op("act", lambda e, dst=dst, bk=bk: e.copy(out=dst, in_=pb[bk][:]), reads=[pbk(bk)], writes=[("K", c, g)])
                else:
                    S.op("dve", lambda e, dst=dst, bk=bk: e.tensor_copy(out=dst, in_=pb[bk][:]), reads=[pbk(bk)], writes=[("K", c, g)])
            for i in range(4):
                t = 4 * g + i
                for dc in range(8):
                    S.op("pe", lambda e, i=i, dc=dc: e.matmul(pb[2][:], lhsT=hT4[:, dc, i * 128:(i + 1) * 128],
                                                              rhs=wk[:, dc, 768:1280], start=(dc == 0), stop=(dc == 7)),
                         reads=HT4 + WK, writes=[pbk(2)])
                for dc in range(8):
                    S.op("pe", lambda e, i=i, dc=dc: e.matmul(pb[3][:, 0:128], lhsT=hT4[:, dc, i * 128:(i + 1) * 128],
                                                              rhs=wk[:, dc, 1280:1408], start=(dc == 0), stop=(dc == 7)),
                         reads=HT4 + WK, writes=[pbk(3)])
                S.op("act", lambda e, t=t: e.copy(out=VA[:, t, :], in_=pb[2][:]), reads=[pbk(2)], writes=[("VA", t)])
                S.op("dve", lambda e, t=t: e.tensor_copy(out=VB2[:, t, :], in_=pb[3][:, 0:128]),
                     reads=[pbk(3), "VB2"], writes=[("VB", t)])
        S.barrier()

        ALLK = [("K", c, g) for c in range(6) for g in range(8)]
        outs = []
        def q_front(qi):
            qt = QT0 + qi
            xc = xtq[qi % 2]
            xk = ("xtq", qi % 2)
            qmBc = qmBs[qi % 2]
            S.dma("sp", lambda e, qt=qt: e.dma_start(out=xc, in_=xloc[qt * 128:(qt + 1) * 128, :]), writes=[xk])
            rmsnorm_rows(xc, xk, g0, "g0", hb, "hb", junkb)
            transpose8(hb, "hb", hT, "hT")
            for c in range(10):
                bank, col = (4, c) if c < 4 else ((5, c - 4) if c < 8 else (6, c - 8))
                for dc in range(8):
                    S.op("pe", lambda e, c=c, dc=dc, bank=bank, col=col: e.matmul(
                        pb[bank][:, col * 128:(col + 1) * 128], lhsT=wq[:, dc, c * 128:(c + 1) * 128], rhs=hT[:, dc, :],
                        start=(dc == 0), stop=(dc == 7)), reads=["hT"] + WQ, writes=[pbk(bank)])
            for dc in range(8):
                S.op("pe", lambda e, dc=dc: e.matmul(pb[6][:, 256:264], lhsT=hT[:, dc, :], rhs=wq[:, dc, 1280:1288],
                                                     start=(dc == 0), stop=(dc == 7)), reads=["hT"] + WQ, writes=[pbk(6)])
            cnt = 0
            for (bank, dstq, nsub, sel) in ((4, qmA, 2, selA), (5, qmBc, 2, selA), (6, qmI, 4, selI)):
                for c in range(dstq.shape[1]):
                    for s_ in range(nsub):
                        src = pb[bank][:, c * 128:(c + 1) * 128]
                        dst = dstq[:, c, s_, :]
                        sca = sel[:, s_:s_ + 1]
                        if cnt % 2 == 0:
                            S.op("act", lambda e, src=src, dst=dst, sca=sca: e.activation(out=dst, in_=src, func=AF.Copy, scale=sca),
                                 reads=[pbk(bank), "selA", "selI"], writes=[("qm", bank) if bank != 5 else ("qmB", qi % 2)])
                        else:
                            S.op("dve", lambda e, src=src, dst=dst, sca=sca: e.tensor_scalar(out=dst, in0=src, scalar1=sca, scalar2=None, op0=ALU.mult),
                                 reads=[pbk(bank), "selA", "selI"], writes=[("qm", bank) if bank != 5 else ("qmB", qi % 2)])
                        cnt += 1
            S.op("act", lambda e: e.mul(out=wis, in_=pb[6][:, 256:264], mul=(32.0 ** -0.5) * (8.0 ** -0.5)),
                 reads=[pbk(6)], writes=["wis"])
        q_front(0)
        for qi in range(NQT):
            qt = QT0 + qi
            nk = qt + 1
            N = nk * 128
            xk = ("xtq", qi % 2)
            xc = xtq[qi % 2]
            qmBc = qmBs[qi % 2]
            nkb = (N + 511) // 512
            for kb in range(nkb):
                cols = min(512, N - kb * 512)
                for j in range(8):
                    bk = 4 + (j % 2)
                    S.op("pe", lambda e, j=j, bk=bk, kb=kb, cols=cols: e.matmul(
                        pb[bk][:, 0:cols], lhsT=qmI[:, j // 4, j % 4, :], rhs=KI4[:, kb * 512:kb * 512 + cols],
                        start=True, stop=True), reads=[("qm", 6)] + ALLK, writes=[pbk(bk)])
                    rb = rbuf[j % 2]
                    S.op("act", lambda e, bk=bk, rb=rb, cols=cols: e.activation(out=rb[:, 0:cols], in_=pb[bk][:, 0:cols], func=AF.Relu),
                         reads=[pbk(bk)], writes=[("rbuf", j % 2)])
                    scv = sc[:, kb * 512:kb * 512 + cols]
                    if j == 0:
                        S.op("dve", lambda e, rb=rb, scv=scv, cols=cols: e.tensor_scalar(
                            out=scv, in0=rb[:, 0:cols], scalar1=wis[:, 0:1], scalar2=None, op0=ALU.mult),
                            reads=[("rbuf", 0), "wis"], writes=[("sc", kb)] + ([("tmpn", 0), ("tmpn", 1)] if kb < 2 else []))
                    else:
                        S.op("dve", lambda e, rb=rb, scv=scv, cols=cols, j=j: e.scalar_tensor_tensor(
                            out=scv, in0=rb[:, 0:cols], scalar=wis[:, j:j + 1], in1=scv, op0=ALU.mult, op1=ALU.add),
                            reads=[("rbuf", j % 2), "wis", ("sc", kb)], writes=[("sc", kb)])
            SCK = [("sc", kb) for kb in range(nkb)]
            mx, mn, Rr, lo, tth, cA_, cB_, gg = [bis[:, i:i + 1] for i in range(8)]
            S.op("dve", lambda e, N=N: e.tensor_reduce(out=mx, in_=sc[:, 0:N], axis=AX.X, op=ALU.max), reads=SCK, writes=["b_mx"])
            S.op("dve", lambda e, N=N: e.tensor_reduce(out=lo, in_=sc[:, 0:N], axis=AX.X, op=ALU.min), reads=SCK, writes=["b_lo"])
            S.op("dve", lambda e: e.tensor_tensor(out=Rr, in0=mx, in1=lo, op=ALU.subtract), reads=["b_mx", "b_lo"], writes=["b_R"])
            S.op("pool", lambda e, N=N: e.memset(sc[0:64, N - 64:N], NEG), reads=SCK + ["b_mx", "b_lo"], writes=SCK)
            npv = 15 * 128 if qt == 15 else 2048
            S.op("dve", lambda e, npv=npv: e.tensor_scalar(out=sc[:, 0:npv], in0=sc[:, 0:npv], scalar1=pnegc, scalar2=None, op0=ALU.add),
                 reads=SCK + ["flags"], writes=SCK)
            halves = [(0, N)] if N <= 2048 else [(0, 2048), (2048, N)]
            S.op("dve", lambda e: e.tensor_scalar(out=Rs, in0=stepc, scalar1=Rr, scalar2=None, op0=ALU.mult),
                 reads=["b_R", "stepc"], writes=["b_Rs"])

            def bis_iter(it):
                two = len(halves) == 2
                S.op("dve", lambda e: e.tensor_tensor(out=tth, in0=Rs[:, it:it + 1], in1=lo, op=ALU.add),
                     reads=["b_Rs", "b_lo"], writes=["b_t"])
                if two:
                    S.op("dve", lambda e: e.scalar_tensor_tensor(out=mn, in0=Rs[:, it:it + 1], scalar=-1.0, in1=lo, op0=ALU.mult, op1=ALU.subtract),
                         reads=["b_Rs", "b_lo"], writes=["b_nt"])
                a0, a1 = halves[0]
                S.op("dve", lambda e: e.tensor_scalar(
                    out=junk[:, 0:a1 - a0], in0=sc[:, a0:a1], scalar1=tth, scalar2=None, op0=ALU.is_ge, op1=ALU.add, accum_out=cA_),
                    reads=SCK + ["b_t"], writes=["junk", ("b_c", 0)])
                thr = TOPK - 0.5
                if two:
                    b0, b1 = halves[1]
                    S.op("act", lambda e: e.activation(out=junk2[:, 0:b1 - b0], in_=sc[:, b0:b1], func=AF.Sign, bias=mn, scale=1.0, accum_out=cB_),
                         reads=SCK + ["b_nt"], writes=["junk2", ("b_c", 1)])
                    S.op("dve", lambda e: e.scalar_tensor_tensor(out=cA_, in0=cB_, scalar=0.5, in1=cA_, op0=ALU.mult, op1=ALU.add),
                         reads=[("b_c", 0), ("b_c", 1)], writes=[("b_c", 0)])
                    thr = TOPK - 0.5 - 0.5 * (b1 - b0)
                S.op("dve", lambda e: e.tensor_scalar(out=gg, in0=cA_, scalar1=thr, scalar2=None, op0=ALU.is_ge),
                     reads=[("b_c", 0)], writes=["b_g"])
                S.op("dve", lambda e: e.scalar_tensor_tensor(out=lo, in0=gg, scalar=Rs[:, it:it + 1], in1=lo, op0=ALU.mult, op1=ALU.add),
                     reads=["b_g", "b_lo", "b_Rs"], writes=["b_lo"])

            MK = [("maskT", k4) for k4 in range((nk + 3) // 4)]

            def emit_group(typ, gi, hook=None):
                def qk_stage(kt):
                    sbk = kt % 2
                    band = kt >= qt - 1
                    if band:
                        bt = 0 if kt == qt else 1
                        bsrc = (biasA if typ == "A" else biasB)[:, bt, 4 * gi:4 * gi + 4, :]
                        S.op("pe", lambda e: e.matmul(pb[sbk][:].rearrange("p (a b) -> p a b", a=4), lhsT=identb, rhs=bsrc,
                                                      start=True, stop=False),
                             reads=["identb", ("bias", 0), ("bias", 1)], writes=[pbk(sbk)])
                    for mi in range(4):
                        if typ == "A":
                            h = 2 * gi + mi // 2
                            lhsT = KA[:, h, kt * 128:(kt + 1) * 128]
                            rhs = qmA[:, h, mi % 2, :]
                            qk = ("qm", 4)
                        else:
                            j = 4 * gi + mi
                            lhsT = KB2[:, kt * 128:(kt + 1) * 128]
                            rhs = qmBc[:, j // 2, j % 2, :]
                            qk = ("qmB", qi % 2)
                        S.op("pe", lambda e, mi=mi, lhsT=lhsT, rhs=rhs: e.matmul(
                            pb[sbk][:, mi * 128:(mi + 1) * 128], lhsT=lhsT, rhs=rhs, start=(not band), stop=((not band) or mi == 3)),
                            reads=[qk] + ALLK, writes=[pbk(sbk)])
                    masked = (kt <= 14) or (kt == 15 and qt >= 16)
                    bias_ap = pnegc if masked else zeroc
                    pt = ptb[kt % 2]
                    S.op("act", lambda e: e.activation(out=pt, in_=pb[sbk][:], func=AF.Exp, bias=bias_ap, scale=1.0),
                         reads=[pbk(sbk), "flags", "zero"], writes=[("pt", kt % 2)])
                    if typ == "B":
                        pm = pmb[kt % 2]
                        S.op("dve", lambda e: e.tensor_tensor(
                            out=pm.rearrange("p (a b) -> p a b", a=4), in0=pt.rearrange("p (a b) -> p a b", a=4),
                            in1=maskT[:, kt, :].unsqueeze(1).to_broadcast([128, 4, 128]), op=ALU.mult),
                            reads=[("pt", kt % 2)] + MK, writes=[("pm", kt % 2)])

                def pv_stage(kt):
                    first, last = (kt == 0), (kt == nk - 1)
                    if typ == "B":
                        src, srck = pmb[kt % 2], ("pm", kt % 2)
                    else:
                        src, srck = ptb[kt % 2], ("pt", kt % 2)
                    if typ == "A":
                        for hl in range(2):
                            h = 2 * gi + hl
                            ob = 2 if hl == 0 else 4
                            S.op("pe", lambda e, hl=hl, h=h, ob=ob: e.matmul(
                                pb[ob][:, 0:256], lhsT=VA[:, kt, h * 128:(h + 1) * 128], rhs=src[:, hl * 256:(hl + 1) * 256],
                                start=first, stop=last), reads=[srck, ("VA", kt)], writes=[pbk(ob)])
                    else:
                        S.op("pe", lambda e: e.matmul(pb[2][:], lhsT=VB2[:, kt, :], rhs=src, start=first, stop=last),
                             reads=[srck, ("VB", kt)], writes=[pbk(2)])
                    S.op("pe", lambda e: e.matmul(pb[3][:], lhsT=onesb, rhs=src, start=first, stop=last),
                         reads=[srck, "onesb"], writes=[pbk(3)])

                qk_stage(0)
                for kt in range(nk):
                    if kt + 1 < nk:
                        qk_stage(kt + 1)
                    pv_stage(kt)
                    if hook is not None:
                        hook(kt)
                S.op("dve", lambda e: e.reciprocal(out=rz, in_=pb[3][:]), reads=[pbk(3)], writes=["rz"])
                if typ == "A":
                    S.op("dve", lambda e: e.tensor_tensor(out=tt[:, 0:256], in0=pb[2][:, 0:256], in1=rz[:, 0:256], op=ALU.mult),
                         reads=[pbk(2), "rz"], writes=["tt"])
                    S.op("dve", lambda e: e.tensor_tensor(out=tt[:, 256:512], in0=pb[4][:, 0:256], in1=rz[:, 256:512], op=ALU.mult),
                         reads=[pbk(4), "rz", "tt"], writes=["tt"])
                    t4 = tt.rearrange("p (h m q) -> p h m q", h=2, m=2)
                    S.op("dve", lambda e: e.scalar_tensor_tensor(out=dd.rearrange("p (h q) -> p h q", h=2), in0=t4[:, :, 1, :], scalar=neglam,
                                                                 in1=t4[:, :, 0, :], op0=ALU.mult, op1=ALU.add),
                         reads=["tt", "neglam"], writes=["dd"])
                    S.op("pool", lambda e: e.tensor_tensor(out=sq, in0=dd, in1=dd, op=ALU.mult), reads=["dd"], writes=["sq"])
                    S.op("pe", lambda e: e.matmul(pb[6][:, 0:256], lhsT=onesf, rhs=sq, start=True, stop=True),
                         reads=["sq", "onesf"], writes=[pbk(6)])
                    S.op("act", lambda e: e.activation(out=rstd, in_=pb[6][:, 0:256], func=AF.Sqrt, scale=1.0 / 128, bias=epsc),
                         reads=[pbk(6), "eps"], writes=["rstd"])
                    S.op("dve", lambda e: e.reciprocal(out=rstd, in_=rstd), reads=["rstd"], writes=["rstd"])
                    S.op("dve", lambda e: e.scalar_tensor_tensor(out=yT[:, 2 * gi:2 * gi + 2, :], in0=dd.rearrange("p (h q) -> p h q", h=2),
                                                                 scalar=sgc, in1=rstd.rearrange("p (h q) -> p h q", h=2),
                                                                 op0=ALU.mult, op1=ALU.mult),
                         reads=["dd", "rstd", "sg"], writes=[("yT", gi)])
                else:
                    for cl in range(2):
                        ch = 4 + 2 * gi + cl
                        for hf in range(2):
                            jl = 2 * cl + hf
                            S.op("dve", lambda e, ch=ch, hf=hf, jl=jl: e.tensor_tensor(
                                out=yT[hf * 64:(hf + 1) * 64, ch, :], in0=pb[2][hf * 64:(hf + 1) * 64, jl * 128:(jl + 1) * 128],
                                in1=rz[hf * 64:(hf + 1) * 64, jl * 128:(jl + 1) * 128], op=ALU.mult),
                                reads=[pbk(2), "rz"], writes=[("yT", 2 + gi)])

            def make_hook(it0, it1):
                st_ = {"next": it0}

                def hook(kt):
                    want = it0 + ((kt + 1) * (it1 - it0)) // nk
                    while st_["next"] < want:
                        bis_iter(st_["next"])
                        st_["next"] += 1
                return hook
            emit_group("A", 0, make_hook(0, NITER // 2))
            emit_group("A", 1, make_hook(NITER // 2, NITER))
            S.op("dve", lambda e, N=N: e.tensor_scalar(out=sc[:, 0:N], in0=sc[:, 0:N], scalar1=lo, scalar2=None, op0=ALU.is_ge),
                 reads=SCK + ["b_lo"], writes=SCK)
            for k4 in range((nk + 3) // 4):
                n4 = min(4, nk - 4 * k4)
                for i in range(n4):
                    kt = 4 * k4 + i
                    S.op("pe", lambda e, kt=kt, i=i: e.transpose(out=pb[6][:, i * 128:(i + 1) * 128], in_=sc[:, kt * 128:(kt + 1) * 128],
                                                                 identity=identf), reads=SCK + ["identf"], writes=[pbk(6)])
                S.op("act", lambda e, k4=k4, n4=n4: e.copy(out=maskT[:, 4 * k4:4 * k4 + n4, :],
                                                           in_=pb[6][:, 0:n4 * 128].rearrange("p (a b) -> p a b", a=n4)),
                     reads=[pbk(6)], writes=[("maskT", k4)])
            if qi + 1 < NQT:
                q_front(qi + 1)
            emit_group("B", 0)
            emit_group("B", 1)
            YT = [("yT", i) for i in range(4)]
            if debug:
                S.dma("pool", lambda e, qi=qi: e.dma_start(out=ydbg[qi], in_=yT), reads=YT, writes=["ydbg"])
            for hf in range(2):
                for c in range(8):
                    S.op("pe", lambda e, hf=hf, c=c: e.matmul(pb[hf][:], lhsT=yT[:, c, :], rhs=wo[:, c, hf * 512:(hf + 1) * 512],
                                                              start=(c == 0), stop=(c == 7)), reads=YT + WO, writes=[pbk(hf)])
            norm_residual(0, 1, g1, "g1", xc, xk, tmpn, junkf)
            outs.append(S.dma("sp", lambda e, qi=qi, xc=xc: e.dma_start(out=x1d[qi * 128:(qi + 1) * 128, :], in_=xc), reads=[xk], writes=["x1d"]))
        S.barrier()

        def mlp_phase(layer, ntiles, src_d, dst_d, gi0):
            A.off = P0
            Wup = A.alloc((8, 4096), BF16)
            Wdn = A.alloc((32, 1024), BF16)
            ga = A.alloc((1024,), F32)
            gb_ = A.alloc((1024,), F32)
            xtm = [A.alloc((1024,), F32) for _ in range(4)]
            hbm = A.alloc((1024,), BF16)
            jb = A.alloc((1024,), BF16)
            hTm = A.alloc((8, 256), BF16)
            uT = A.alloc((32, 256), BF16)
            rb2 = [A.alloc((2, 256), F32) for _ in range(2)]
            tmpm = A.alloc((1024,), F32)
            jf = A.alloc((512,), F32)
            assert A.off <= NW
            load_w(Wup, lambda i: ("Wup", i), wup[layer].rearrange("(c p) n -> p c n", p=128), 4096, 512)
            for p4 in range(4):
                S.dma("pool", lambda e, p4=p4: e.dma_start(out=Wdn[:, 8 * p4:8 * p4 + 8, :],
                                                          in_=wdn[layer].rearrange("(f p) n -> p f n", p=128)[:, 8 * p4:8 * p4 + 8, :]),
                      writes=[("Wdn", p4)])
            S.dma("sp", lambda e: e.dma_start(out=ga, in_=gb[gi0]), writes=["ga"])
            S.dma("sp", lambda e: e.dma_start(out=gb_, in_=gb[gi0 + 1]), writes=["gb"])
            res = []
            groups = [list(range(i, min(i + 2, ntiles))) for i in range(0, ntiles, 2)]
            def front(gidx):
                grp = groups[gidx]
                xb = 2 * (gidx % 2)
                for i, t in enumerate(grp):
                    xk = ("xtm", xb + i)
                    S.dma("sp", lambda e, t=t, i=i: e.dma_start(out=xtm[xb + i], in_=src_d[t * 128:(t + 1) * 128, :]), reads=["srcd"], writes=[xk])
                    rmsnorm_rows(xtm[xb + i], xk, ga, "ga", hbm, "hbm", jb)
                    transpose8(hbm, "hbm", hTm[:, :, i * 128:(i + 1) * 128], ("hTm", i), eng="act" if i == 0 else "dve")

            def up(gidx):
                grp = groups[gidx]
                T = 128 * len(grp)
                HK = [("hTm", i) for i in range(len(grp))]
                for f2 in range(16):
                    bk = 4 + f2 % 2
                    for fl in range(2):
                        f = 2 * f2 + fl
                        for dc in range(8):
                            S.op("pe", lambda e, f=f, fl=fl, dc=dc, bk=bk: e.matmul(
                                pb[bk][:, fl * 256:fl * 256 + T], lhsT=Wup[:, dc, f * 128:(f + 1) * 128], rhs=hTm[:, dc, 0:T],
                                start=(dc == 0), stop=(dc == 7)), reads=HK + [("Wup", f // 4)], writes=[pbk(bk)])
                    rb = rb2[f2 % 2]
                    S.op("act", lambda e, bk=bk, rb=rb: e.activation(
                        out=rb[:, :, 0:T], in_=pb[bk][:].rearrange("p (a b) -> p a b", a=2)[:, :, 0:T], func=AF.Relu),
                        reads=[pbk(bk)], writes=[("rb2", f2 % 2)])
                    S.op("pool", lambda e, rb=rb, f2=f2: e.tensor_tensor(out=uT[:, 2 * f2:2 * f2 + 2, 0:T], in0=rb[:, :, 0:T],
                                                                    in1=rb[:, :, 0:T], op=ALU.mult),
                         reads=[("rb2", f2 % 2)], writes=[("uT", f2)])

            def down(gidx):
                grp = groups[gidx]
                xb = 2 * (gidx % 2)
                UK = [("uT", f2) for f2 in range(16)]
                for i, t in enumerate(grp):
                    b0 = 0 if i == 0 else 2
                    for hf in range(2):
                        for f in range(32):
                            S.op("pe", lambda e, i=i, hf=hf, f=f, b0=b0: e.matmul(pb[b0 + hf][:], lhsT=uT[:, f, i * 128:(i + 1) * 128],
                                                                                 rhs=Wdn[:, f, hf * 512:(hf + 1) * 512], start=(f == 0), stop=(f == 31)),
                                 reads=UK + [("Wdn", f // 8)], writes=[pbk(b0 + hf)])
                    norm_residual(b0, b0 + 1, gb_, "gb", xtm[xb + i], ("xtm", xb + i), tmpm, jf)
                    res.append(S.dma("sp", lambda e, t=t, i=i: e.dma_start(out=dst_d[t * 128:(t + 1) * 128, :], in_=xtm[xb + i]),
                                     reads=[("xtm", xb + i)], writes=["dstd"]))

            front(0)
            for gidx in range(len(groups)):
                up(gidx)
                if gidx + 1 < len(groups):
                    front(gidx + 1)
                down(gidx)
            S.barrier()
            return res

        mlp_phase(0, NQT, x1d, x2d, 2)

        A.off = P0
        wci = A.alloc((8, 2560), BF16)
        wco = A.alloc((8, 1024), BF16)
        dg = A.alloc((31, 4, 128), BF16)
        dwT = A.alloc((4, 31), F32)
        cvec = A.alloc((3, 4), F32)
        scw = A.alloc((4, 3), F32)
        g4 = A.alloc((1024,), F32)
        g5 = A.alloc((1024,), F32)
        xtc = [A.alloc((1024,), F32) for _ in range(4)]
        hbc = A.alloc((1024,), BF16)
        jbc = A.alloc((1024,), BF16)
        hTc = A.alloc((8, 256), BF16)
        uTb = A.alloc((4, 32 + 256), BF16)
        eTb = A.alloc((4, 2 + 256), F32)
        sgb = A.alloc((256,), F32)
        utmp = A.alloc((256,), F32)
        dhs = A.alloc((256,), F32)
        z1 = A.alloc((256,), F32)
        vv = A.alloc((4, 256), F32)
        sqv = A.alloc((4, 256), F32)
        mean = A.alloc((256,), F32)
        msq = A.alloc((256,), F32)
        rsd = A.alloc((256,), F32)
        tcb = A.alloc((256,), F32)
        ymT = A.alloc((8, 256), BF16)
        tmpc = A.alloc((1024,), F32)
        jfc = A.alloc((512,), F32)
        assert A.off <= NW
        load_w(wci, lambda i: ("wci", i), cwin.rearrange("(c p) n -> p c n", p=128), 2560, 512)
        WCI = [("wci", i) for i in range(5)]
        load_w(wco, lambda i: ("wco", i), cwout.rearrange("(c p) n -> p c n", p=128), 1024, 512)
        WCO = [("wco", 0), ("wco", 1)]
        S.dma("sp", lambda e: e.dma_start(out=dwT, in_=dwT_d), writes=["dwT"])
        S.dma("sp", lambda e: e.dma_start(out=cvec, in_=cvec_d), writes=["cvec"])
        S.dma("sp", lambda e: e.dma_start(out=scw, in_=scw_d), writes=["scw"])
        S.dma("sp", lambda e: e.dma_start(out=g4, in_=gb[4]), writes=["g4"])
        S.dma("sp", lambda e: e.dma_start(out=g5, in_=gb[5]), writes=["g5"])
        cnt = 0
        for j in range(31):
            for c in range(4):
                eng = "dve" if cnt % 2 == 0 else "pool"
                S.op(eng, lambda e, j=j, c=c: e.tensor_scalar(out=dg[:, j, c, :], in0=identf, scalar1=dwT[:, c, j:j + 1], scalar2=None, op0=ALU.mult),
                     reads=["identf", "dwT"], writes=[("dg", eng)])
                cnt += 1
        DG = [("dg", "dve"), ("dg", "pool")]
        S.op("pool", lambda e: e.memset(uTb[:, :, :], 0.0), writes=["uTb"])
        S.op("pool", lambda e: e.memset(eTb[:, :, :], 0.0), writes=["eTb"])
        cgroups = [[0]] + [[1 + 2 * i, 2 + 2 * i] for i in range(8)]
        for gidx, grp in enumerate(cgroups):
            T = 128 * len(grp)
            halo = (grp == [0])
            xb = 2 * (gidx % 2)
            for i, li in enumerate(grp):
                xk = ("xtc", xb + i)
                S.dma("sp", lambda e, li=li, i=i, xb=xb: e.dma_start(out=xtc[xb + i], in_=x2d[li * 128:(li + 1) * 128, :]), reads=["dstd"], writes=[xk])
                rmsnorm_rows(xtc[xb + i], xk, g4, "g4", hbc, "hbc", jbc)
                transpose8(hbc, "hbc", hTc[:, :, i * 128:(i + 1) * 128], ("hTc", i), eng="act" if i == 0 else "dve")
            HK = [("hTc", i) for i in range(len(grp))]

            def proj(bank, col0, T=T, HK=HK):
                for dc in range(8):
                    S.op("pe", lambda e, dc=dc: e.matmul(pb[bank][:, 0:T], lhsT=wci[:, dc, col0:col0 + 128], rhs=hTc[:, dc, 0:T],
                                                         start=(dc == 0), stop=(dc == 7)), reads=HK + WCI, writes=[pbk(bank)])
            for c in range(4):
                proj(0, c * 128)
                proj(1, 512 + c * 128)
                S.op("act", lambda e, T=T: e.activation(out=sgb[:, 0:T], in_=pb[1][:, 0:T], func=AF.Sigmoid), reads=[pbk(1)], writes=["sgb"])
                if halo:
                    S.op("dve", lambda e, T=T: e.tensor_tensor(out=utmp[:, 0:T], in0=pb[0][:, 0:T], in1=sgb[:, 0:T], op=ALU.mult),
                         reads=[pbk(0), "sgb"], writes=["utmp"])
                    S.op("dve", lambda e, c=c, T=T: e.tensor_scalar(out=uTb[:, c, 32:32 + T], in0=utmp[:, 0:T], scalar1=flagc, scalar2=None, op0=ALU.mult),
                         reads=["utmp", "flags", "uTb"], writes=[("uT", c)])
                else:
                    S.op("dve", lambda e, c=c, T=T: e.tensor_tensor(out=uTb[:, c, 32:32 + T], in0=pb[0][:, 0:T], in1=sgb[:, 0:T], op=ALU.mult),
                         reads=[pbk(0), "sgb", "uTb"], writes=[("uT", c)])
            for c in range(4):
                proj(2, 1536 + c * 128)
                proj(3, 2048 + c * 128)
                S.op("act", lambda e, T=T: e.copy(out=dhs[:, 0:T], in_=pb[3][:, 0:T]), reads=[pbk(3)], writes=["dhs"])
                if halo:
                    S.op("dve", lambda e, T=T: e.tensor_tensor(out=utmp[:, 0:T], in0=pb[2][:, 0:T], in1=dhs[:, 0:T], op=ALU.mult),
                         reads=[pbk(2), "dhs"], writes=["utmp"])
                    S.op("dve", lambda e, c=c, T=T: e.tensor_scalar(out=eTb[:, c, 2:2 + T], in0=utmp[:, 0:T], scalar1=flagc, scalar2=None, op0=ALU.mult),
                         reads=["utmp", "flags", "eTb"], writes=[("eT", c)])
                else:
                    S.op("dve", lambda e, c=c, T=T: e.tensor_tensor(out=eTb[:, c, 2:2 + T], in0=pb[2][:, 0:T], in1=dhs[:, 0:T], op=ALU.mult),
                         reads=[pbk(2), "dhs", "eTb"], writes=[("eT", c)])
            if not halo:
                for c in range(4):
                    proj(4, 1024 + c * 128)
                    S.op("dve", lambda e, c=c, T=T: e.tensor_scalar(out=z1[:, 0:T], in0=eTb[:, c, 0:T], scalar1=scw[:, c, 0:1], scalar2=None, op0=ALU.mult),
                         reads=[("eT", c), "scw"], writes=["z1"])
                    for jj in (1, 2):
                        S.op("dve", lambda e, c=c, T=T, jj=jj: e.scalar_tensor_tensor(out=z1[:, 0:T], in0=eTb[:, c, jj:jj + T], scalar=scw[:, c, jj:jj + 1],
                                                                                    in1=z1[:, 0:T], op0=ALU.mult, op1=ALU.add),
                             reads=[("eT", c), "scw", "z1"], writes=["z1"])
                    S.op("dve", lambda e, c=c, T=T: e.tensor_tensor(out=ymT[:, 4 + c, 0:T], in0=pb[4][:, 0:T], in1=z1[:, 0:T], op=ALU.mult),
                         reads=[pbk(4), "z1"], writes=[("ymT", 4 + c)])
                for c in range(4):
                    for j in range(31):
                        S.op("pe", lambda e, c=c, j=j, T=T: e.matmul(pb[5][:, 0:T], lhsT=dg[:, j, c, :], rhs=uTb[:, c, 2 + j:2 + j + T],
                                                                     start=(j == 0), stop=(j == 30)),
                             reads=[("uT", c)] + DG, writes=[pbk(5)])
                    S.op("act", lambda e, c=c, T=T: e.activation(out=vv[:, c, 0:T], in_=pb[5][:, 0:T], func=AF.Identity, bias=cvec[:, 0, c:c + 1], scale=1.0),
                         reads=[pbk(5), "cvec"], writes=[("vv", c)])
                    S.op("act", lambda e, c=c, T=T: e.activation(out=sqv[:, c, 0:T], in_=vv[:, c, 0:T], func=AF.Square),
                         reads=[("vv", c)], writes=[("sqv", c)])
                for c in range(4):
                    S.op("pe", lambda e, c=c, T=T: e.matmul(pb[6][:, 0:T], lhsT=onesf, rhs=vv[:, c, 0:T], start=(c == 0), stop=(c == 3)),
                         reads=[("vv", c), "onesf"], writes=[pbk(6)])
                for c in range(4):
                    S.op("pe", lambda e, c=c, T=T: e.matmul(pb[6][:, 256:256 + T], lhsT=onesf, rhs=sqv[:, c, 0:T], start=(c == 0), stop=(c == 3)),
                         reads=[("sqv", c), "onesf"], writes=[pbk(6)])
                S.op("act", lambda e, T=T: e.mul(out=mean[:, 0:T], in_=pb[6][:, 0:T], mul=1.0 / 512), reads=[pbk(6)], writes=["mean"])
                S.op("pool", lambda e, T=T: e.tensor_tensor(out=msq[:, 0:T], in0=mean[:, 0:T], in1=mean[:, 0:T], op=ALU.mult), reads=["mean"], writes=["msq"])
                S.op("dve", lambda e, T=T: e.scalar_tensor_tensor(out=rsd[:, 0:T], in0=pb[6][:, 256:256 + T], scalar=1.0 / 512, in1=msq[:, 0:T],
                                                                op0=ALU.mult, op1=ALU.subtract), reads=[pbk(6), "msq"], writes=["rsd"])
                S.op("act", lambda e, T=T: e.activation(out=rsd[:, 0:T], in_=rsd[:, 0:T], func=AF.Sqrt, scale=1.0, bias=epsc), reads=["rsd", "eps"], writes=["rsd"])
                S.op("dve", lambda e, T=T: e.reciprocal(out=rsd[:, 0:T], in_=rsd[:, 0:T]), reads=["rsd"], writes=["rsd"])
                for c in range(4):
                    S.op("pool", lambda e, c=c, T=T: e.tensor_tensor(out=tcb[:, 0:T], in0=vv[:, c, 0:T], in1=mean[:, 0:T], op=ALU.subtract),
                         reads=[("vv", c), "mean"], writes=["tcb"])
                    S.op("pool", lambda e, T=T: e.tensor_tensor(out=tcb[:, 0:T], in0=tcb[:, 0:T], in1=rsd[:, 0:T], op=ALU.mult),
                         reads=["tcb", "rsd"], writes=["tcb"])
                    S.op("act", lambda e, c=c, T=T: e.activation(out=ymT[:, c, 0:T], in_=tcb[:, 0:T], func=AF.Silu, scale=cvec[:, 1, c:c + 1],
                                                                bias=cvec[:, 2, c:c + 1]), reads=["tcb", "cvec"], writes=[("ymT", c)])
                YM = [("ymT", c) for c in range(8)]
                for i, li in enumerate(grp):
                    for hf in range(2):
                        for c in range(8):
                            S.op("pe", lambda e, i=i, hf=hf, c=c: e.matmul(pb[hf][:], lhsT=ymT[:, c, i * 128:(i + 1) * 128],
                                                                          rhs=wco[:, c, hf * 512:(hf + 1) * 512], start=(c == 0), stop=(c == 7)),
                                 reads=YM + WCO, writes=[pbk(hf)])
                    norm_residual(0, 1, g5, "g5", xtc[xb + i], ("xtc", xb + i), tmpc, jfc)
                    S.dma("sp", lambda e, li=li, i=i, xb=xb: e.dma_start(out=x3d[(li - 1) * 128:li * 128, :], in_=xtc[xb + i]),
                          reads=[("xtc", xb + i)], writes=["x3d"])
            UT = [("uT", c) for c in range(4)]
            ET = [("eT", c) for c in range(4)]
            S.op("pool", lambda e, T=T: e.tensor_copy(out=uTb[:, :, 0:32], in_=uTb[:, :, T:T + 32]), reads=UT, writes=UT + ["uTb"])
            S.op("pool", lambda e, T=T: e.tensor_copy(out=eTb[:, :, 0:2], in_=eTb[:, :, T:T + 2]), reads=ET, writes=ET + ["eTb"])
        S.barrier()

        A.off = P0

        def final_src_fix():
            pass
        res = mlp_phase_final = None
        res = mlp_phase(1, 16, x3d, out_d, 6)
        S.emit(nc, final_wait_tokens=res)
    return nc


def _t5_bucket_np(rel):
    import jax
    import jax.numpy as jnp
    cpu = jax.devices("cpu")[0]
    with jax.default_device(cpu):
        rel = jnp.asarray(rel, dtype=jnp.int32)
        nb = 16
        ret = jnp.where(rel > 0, nb, 0)
        n = jnp.abs(rel)
        max_exact = nb // 2
        nf = jnp.maximum(n, 1).astype(jnp.float32)
        large = max_exact + (jnp.log(nf / max_exact) / math.log(128 / max_exact) * (nb - max_exact)).astype(jnp.int32)
        large = jnp.minimum(large, nb - 1)
        out = ret + jnp.where(n < max_exact, n, large)
        return np.asarray(out)


_PROG = {}


def _get_prog(debug=False):
    if debug not in _PROG:
        _PROG[debug] = build_program(debug)
    return _PROG[debug]


def make_in_maps(inputs):
    f = lambda a: np.ascontiguousarray(np.asarray(a, dtype=np.float32))
    x = f(inputs["x"])
    rel_bias = f(inputs["rel_bias"])
    norm_g = f(inputs["norm_g"])
    kk = np.arange(128)[:, None]
    qq = np.arange(128)[None, :]
    bD = _t5_bucket_np(kk - qq)
    bP = _t5_bucket_np(kk - 128 - qq)
    admiss = (kk // 64) <= (qq // 64)
    tabA = rel_bias[:, :4]
    tabB = rel_bias[:, 4:]
    biasA = np.zeros((128, 2, 8, 128), np.float32)
    biasB = np.zeros((128, 2, 8, 128), np.float32)
    for m in range(8):
        biasA[:, 0, m, :] = np.where(admiss, tabA[bD, m // 2], np.float32(NEG))
        biasA[:, 1, m, :] = tabA[bP, m // 2]
        biasB[:, 0, m, :] = np.where(admiss, tabB[bD, m], np.float32(NEG))
        biasB[:, 1, m, :] = tabB[bP, m]
    cA = np.ascontiguousarray(np.broadcast_to(np.repeat(tabA[15], 2)[None, :], (128, 8)))
    cB = np.ascontiguousarray(np.broadcast_to(tabB[15][None, :], (128, 8)))
    gbb = np.ascontiguousarray(np.broadcast_to(norm_g.reshape(8, 1, D), (8, 128, D)))
    lvb = np.ascontiguousarray(np.broadcast_to(f(inputs["diff_lambda"])[0][None], (128, 4, 64)))
    sgn = np.ascontiguousarray(f(inputs["diff_subln_g"])[0].reshape(128, 1))
    selA = np.zeros((128, 2), np.float32)
    selA[:64, 0] = 0.125
    selA[64:, 1] = 0.125
    selI = np.zeros((128, 4), np.float32)
    for i in range(4):
        selI[32 * i:32 * i + 32, i] = 1.0
    dwT = np.ascontiguousarray(f(inputs["conv_dw_w"])[0].reshape(31, 4, 128).transpose(2, 1, 0))
    cvec = np.ascontiguousarray(np.stack([f(inputs["conv_dw_b"])[0].reshape(4, 128).T,
                                          f(inputs["conv_ln_g"])[0].reshape(4, 128).T,
                                          f(inputs["conv_ln_b"])[0].reshape(4, 128).T], axis=1))
    scw = np.ascontiguousarray(f(inputs["sconv_w"])[0].reshape(3, 4, 128).transpose(2, 1, 0))
    common = dict(win=f(inputs["attn_w_in"])[0], wout=f(inputs["attn_w_out"])[0], wup=f(inputs["w_mlp_up"]),
                  wdn=f(inputs["w_mlp_down"]), cwin=f(inputs["conv_w_in"])[0], cwout=f(inputs["conv_w_out"])[0],
                  gb=gbb, biasA=biasA, biasB=biasB, cA=cA, cB=cB, lvb=lvb, sgn=sgn, selA=selA, selI=selI,
                  ident=np.eye(128, dtype=np.float32), dwT=dwT, cvec=cvec, scw=scw)
    in_maps = []
    for c in range(8):
        b, h = c // 2, c % 2
        if h == 1:
            xl = x[b]
        else:
            xl = np.concatenate([np.zeros((2048, D), np.float32), x[b, :2048]], axis=0)
        flags = np.zeros((128, 2), np.float32)
        flags[:, 0] = float(h)
        flags[:, 1] = 0.0 if h == 1 else NEG
        m = dict(common)
        m["xloc"] = np.ascontiguousarray(xl)
        m["flags"] = flags
        in_maps.append(m)
    return in_maps


def kernel(**inputs):
    nc = _get_prog(False)
    in_maps = make_in_maps(inputs)
    res = run_bass_kernel_spmd(nc, in_maps, core_ids=list(range(8)))
    out = np.zeros((4, 4096, D), np.float32)
    for c in range(8):
        b, h = c // 2, c % 2
        out[b, h * 2048:(h + 1) * 2048] = res.results[c]["out"]
    return out
```

```python
import math
import contextlib
import numpy as np
import concourse.bass as bass
import concourse.mybir as mybir
from concourse.bass_utils import run_bass_kernel_spmd

F32 = mybir.dt.float32
BF16 = mybir.dt.bfloat16
ALU = mybir.AluOpType
AF = mybir.ActivationFunctionType
AX = mybir.AxisListType

ENGS = ["pe", "act", "dve", "pool", "sp"]
NDMA_SLOTS = 8
NEG = -1.0e30
EPS = 1e-6
NITER = 16
TOPK = 256
D = 1024
QT0 = 15
NQT = 17


class Sched:
    def __init__(self):
        self.ops = {e: [] for e in ENGS}
        self.last_w = {}
        self.readers = {}
        self.dma_slot_rr = {e: 0 for e in ENGS}
        self.dma_slot_last = {}
        self.dma_slot_uses = {}
        self.pending = {e: set() for e in ENGS}
        self.last_tok = {}
        self.open_dma = set()

    def _deps_for(self, reads, writes):
        deps = set()
        for k in reads:
            deps.update(self.last_w.get(k, {}).values())
        for k in writes:
            deps.update(self.last_w.get(k, {}).values())
            deps.update(self.readers.get(k, {}).values())
        return deps

    def _commit(self, tok, track, reads, writes):
        for k in reads:
            self.readers.setdefault(k, {})[track] = tok
        for k in writes:
            self.last_w[k] = {track: tok}
            self.readers[k] = {}

    def op(self, eng, fn, reads=(), writes=()):
        idx = len(self.ops[eng])
        deps = self._deps_for(reads, writes)
        tok = ("c", eng, idx)
        raw = set()
        if eng != "pe":
            for k in reads:
                raw.update(self.last_w.get(k, {}).values())
        deps = {d for d in deps if not (d[0] == "c" and d[1] == eng) or d in raw}
        deps |= self.pending[eng]
        self.pending[eng] = set()
        self.ops[eng].append(dict(fn=fn, deps=deps, tok=tok, dma=False))
        self._commit(tok, eng, reads, writes)
        self.last_tok[eng] = tok
        return tok

    def dma(self, eng, fn, reads=(), writes=()):
        slot = self.dma_slot_rr[eng]
        self.dma_slot_rr[eng] = (slot + 1) % NDMA_SLOTS
        use = self.dma_slot_uses.get((eng, slot), 0) + 1
        self.dma_slot_uses[(eng, slot)] = use
        tok = ("d", eng, slot, use)
        deps = self._deps_for(reads, writes)
        prev = self.dma_slot_last.get((eng, slot))
        if prev is not None:
            deps.add(prev)
            self.open_dma.discard(prev)
        self.dma_slot_last[(eng, slot)] = tok
        deps |= self.pending[eng]
        self.pending[eng] = set()
        self.ops[eng].append(dict(fn=fn, deps=deps, tok=tok, dma=True))
        self._commit(tok, ("dma", eng, slot), reads, writes)
        self.open_dma.add(tok)
        return tok

    def barrier(self):
        toks = set(self.last_tok.values()) | set(self.open_dma)
        for e in ENGS:
            self.pending[e] |= {t for t in toks if not (t[0] == "c" and t[1] == e)}
        self.open_dma = set()

    def emit(self, nc, final_wait_tokens=()):
        needed = {e: set() for e in ENGS}
        for e in ENGS:
            for o in self.ops[e]:
                for d in o["deps"]:
                    if d[0] == "c":
                        needed[d[1]].add(d[2])
        semval = {e: {} for e in ENGS}
        for e in ENGS:
            c = 0
            for i in range(len(self.ops[e])):
                if i in needed[e]:
                    c += 1
                    semval[e][i] = c
        with contextlib.ExitStack() as st:
            csem = {e: st.enter_context(nc.semaphore("c_" + e)) for e in ENGS if e != "sp"}
            dsem = {}
            for e in ENGS:
                for s in range(NDMA_SLOTS):
                    if (e, s) in self.dma_slot_uses:
                        dsem[(e, s)] = st.enter_context(nc.semaphore("d_%s_%d" % (e, s)))
            block = st.enter_context(nc.Block())

            def resolve(tok):
                if tok[0] == "c":
                    return csem[tok[1]], semval[tok[1]][tok[2]], ("c", tok[1])
                return dsem[(tok[1], tok[2])], 16 * tok[3], ("d", tok[1], tok[2])

            def run(e, engine):
                waited = {}
                for i, o in enumerate(self.ops[e]):
                    ws = {}
                    for d in o["deps"]:
                        sem, val, sk = resolve(d)
                        if waited.get(sk, 0) >= val:
                            continue
                        if sk not in ws or ws[sk][1] < val:
                            ws[sk] = (sem, val)
                    for sk, (sem, val) in ws.items():
                        engine.wait_ge(sem, val)
                        waited[sk] = val
                    ins = o["fn"](engine)
                    if o["dma"]:
                        ins.then_inc(dsem[(o["tok"][1], o["tok"][2])], 16)
                    elif i in needed[e]:
                        ins.then_inc(csem[e], 1)
                if e == "sp":
                    for d in final_wait_tokens:
                        sem, val, sk = resolve(d)
                        engine.wait_ge(sem, val)

            block.tensor(lambda eng: run("pe", eng))
            block.scalar(lambda eng: run("act", eng))
            block.vector(lambda eng: run("dve", eng))
            block.gpsimd(lambda eng: run("pool", eng))
            block.sync(lambda eng: run("sp", eng))


class Arena:
    def __init__(self, ap, nwords):
        self.ap = ap
        self.n = nwords
        self.off = 0

    def alloc(self, free_shape, dt, at=None):
        nel = int(np.prod(free_shape))
        nbytes = nel * (2 if dt == BF16 else 4)
        nw = (nbytes + 3) // 4
        off = self.off if at is None else at
        assert off + nw <= self.n, ("arena overflow", off, nw, self.n)
        a = self.ap[:, off:off + nw]
        if at is None:
            self.off += nw
        if dt == BF16:
            a = a.bitcast(BF16)[:, :nel]
        if len(free_shape) == 2:
            a = a.rearrange("p (a b) -> p a b", a=free_shape[0])
        elif len(free_shape) == 3:
            a = a.rearrange("p (a b c) -> p a b c", a=free_shape[0], b=free_shape[1])
        return a


class Ctx:
    pass


def build_program(debug=False):
    nc = bass.Bass("TRN2", target_bir_lowering=False)
    S = Sched()
    C = Ctx()
    C.nc, C.S = nc, S
    din = lambda n, s: nc.dram_tensor(n, list(s), F32, kind="ExternalInput").ap()
    xloc = din("xloc", [4096, D])
    win = din("win", [D, 2472])
    wout = din("wout", [D, D])
    wup = din("wup", [2, D, 4096])
    wdn = din("wdn", [2, 4096, D])
    cwin = din("cwin", [D, 2560])
    cwout = din("cwout", [D, D])
    gb = din("gb", [8, 128, D])
    biasA_d = din("biasA", [128, 2, 8, 128])
    biasB_d = din("biasB", [128, 2, 8, 128])
    cA_d = din("cA", [128, 8])
    cB_d = din("cB", [128, 8])
    lvb_d = din("lvb", [128, 4, 64])
    sgn_d = din("sgn", [128, 1])
    flags_d = din("flags", [128, 2])
    selA_d = din("selA", [128, 2])
    selI_d = din("selI", [128, 4])
    ident_d = din("ident", [128, 128])
    dwT_d = din("dwT", [128, 4, 31])
    cvec_d = din("cvec", [128, 3, 4])
    scw_d = din("scw", [128, 4, 3])
    out_d = nc.dram_tensor("out", [2048, D], F32, kind="ExternalOutput").ap()
    kind = "ExternalOutput"
    x1d = nc.dram_tensor("x1d", [NQT * 128, D], F32, kind=kind).ap()
    x2d = nc.dram_tensor("x2d", [NQT * 128, D], F32, kind=kind).ap()
    x3d = nc.dram_tensor("x3d", [2048, D], F32, kind=kind).ap()
    ydbg = nc.dram_tensor("ydbg", [NQT, 128, 8, 128], F32, kind="ExternalOutput").ap() if debug else None

    NW = 53200
    with contextlib.ExitStack() as st:
        arena_t = st.enter_context(nc.sbuf_tensor("arena", [128, NW], F32))
        A = Arena(arena_t[:], NW)
        pb = [st.enter_context(nc.psum_tensor("pb%d" % i, [128, 512], F32)) for i in range(7)]
        pbt = st.enter_context(nc.psum_tensor("pbt", [128, 1024], BF16))
        pbk = lambda i: ("pb", i)

        identf = A.alloc((128,), F32)
        identb = A.alloc((128,), BF16)
        onesf = A.alloc((128,), F32)
        onesb = A.alloc((128,), BF16)
        smallc = A.alloc((16,), F32)
        selA = A.alloc((2,), F32)
        selI = A.alloc((4,), F32)
        stat = A.alloc((64,), F32)
        stepc = A.alloc((NITER,), F32)
        P0 = A.off
        epsc, zeroc, flagc, pnegc = smallc[:, 0:1], smallc[:, 1:2], smallc[:, 2:3], smallc[:, 3:4]
        neglam, sgc = smallc[:, 4:5], smallc[:, 5:6]
        C.statctr = 0

        def newstat():
            i = C.statctr % 64
            C.statctr += 1
            return stat[:, i:i + 1], ("stat", i)

        S.dma("sp", lambda e: e.dma_start(out=identf, in_=ident_d), writes=["identf"])
        S.dma("sp", lambda e: e.dma_start(out=smallc[:, 2:4], in_=flags_d), writes=["flags"])
        S.dma("sp", lambda e: e.dma_start(out=selA, in_=selA_d), writes=["selA"])
        S.dma("sp", lambda e: e.dma_start(out=selI, in_=selI_d), writes=["selI"])
        S.op("dve", lambda e: e.tensor_copy(out=identb, in_=identf), reads=["identf"], writes=["identb"])
        S.op("pool", lambda e: e.memset(onesf, 1.0), writes=["onesf"])
        S.op("pool", lambda e: e.memset(onesb, 1.0), writes=["onesb"])
        S.op("pool", lambda e: e.memset(smallc[:, 0:1], EPS), writes=["eps"])
        S.op("pool", lambda e: e.memset(smallc[:, 1:2], 0.0), writes=["zero"])
        for it in range(NITER):
            S.op("pool", lambda e, it=it: e.memset(stepc[:, it:it + 1], 2.0 ** -(it + 1)), writes=["stepc"])

        def rmsnorm_rows(x_ap, x_key, g_ap, g_key, hb_ap, hb_key, junkb):
            ss, ssk = newstat()
            rs, rsk = newstat()
            S.op("act", lambda e: e.activation(out=junkb, in_=x_ap, func=AF.Square, accum_out=ss),
                 reads=[x_key], writes=["junkb", ssk])
            S.op("act", lambda e: e.activation(out=rs, in_=ss, func=AF.Sqrt, scale=1.0 / D, bias=epsc),
                 reads=[ssk, "eps"], writes=[rsk])
            S.op("dve", lambda e: e.reciprocal(out=rs, in_=rs), reads=[rsk], writes=[rsk])
            S.op("dve", lambda e: e.scalar_tensor_tensor(out=hb_ap, in0=x_ap, scalar=rs, in1=g_ap,
                                                         op0=ALU.mult, op1=ALU.mult),
                 reads=[x_key, rsk, g_key], writes=[hb_key])

        def transpose8(hb_ap, hb_key, dst_ap, dst_key, eng="act"):
            pv = pbt[:, 0:1024].rearrange("p (c t) -> p c t", c=8)
            for c in range(8):
                S.op("pe", lambda e, c=c: e.transpose(out=pv[:, c, :], in_=hb_ap[:, c * 128:(c + 1) * 128],
                                                      identity=identb),
                     reads=[hb_key, "identb"], writes=["pbt"])
            if eng == "act":
                S.op("act", lambda e: e.copy(out=dst_ap, in_=pv), reads=["pbt"], writes=[dst_key])
            else:
                S.op("dve", lambda e: e.tensor_copy(out=dst_ap, in_=pv), reads=["pbt"], writes=[dst_key])

        def norm_residual(bank0, bank1, g_ap, g_key, x_ap, x_key, tmp_ap, junkf):
            s0, k0 = newstat()
            s1, k1 = newstat()
            rs, rk = newstat()
            S.op("act", lambda e: e.activation(out=junkf, in_=pb[bank0][:], func=AF.Square, accum_out=s0),
                 reads=[pbk(bank0)], writes=["junkf", k0])
            S.op("act", lambda e: e.activation(out=junkf, in_=pb[bank1][:], func=AF.Square, accum_out=s1),
                 reads=[pbk(bank1)], writes=["junkf", k1])
            S.op("dve", lambda e: e.tensor_tensor(out=rs, in0=s0, in1=s1, op=ALU.add), reads=[k0, k1], writes=[rk])
            S.op("act", lambda e: e.activation(out=rs, in_=rs, func=AF.Sqrt, scale=1.0 / D, bias=epsc),
                 reads=[rk, "eps"], writes=[rk])
            S.op("dve", lambda e: e.reciprocal(out=rs, in_=rs), reads=[rk], writes=[rk])
            for hf, bk in enumerate((bank0, bank1)):
                S.op("dve", lambda e, hf=hf, bk=bk: e.scalar_tensor_tensor(
                    out=tmp_ap[:, hf * 512:(hf + 1) * 512], in0=pb[bk][:], scalar=rs,
                    in1=g_ap[:, hf * 512:(hf + 1) * 512], op0=ALU.mult, op1=ALU.mult),
                    reads=[pbk(bk), rk, g_key], writes=[("tmpn", hf)])
            S.op("pool", lambda e: e.tensor_tensor(out=x_ap, in0=x_ap, in1=tmp_ap, op=ALU.add),
                 reads=[x_key, ("tmpn", 0), ("tmpn", 1)], writes=[x_key])

        def load_w(dst, dst_keyf, src3, ncols, piece):
            for i, c0 in enumerate(range(0, ncols, piece)):
                c1 = min(ncols, c0 + piece)
                S.dma("pool", lambda e, c0=c0, c1=c1: e.dma_start(out=dst[:, :, c0:c1], in_=src3[:, :, c0:c1]),
                      writes=[dst_keyf(i)])

        A.off = P0
        KA = A.alloc((4, 4096), BF16)
        KB2 = A.alloc((4096,), BF16)
        KI4 = A.alloc((4096,), BF16)
        VA = A.alloc((32, 512), BF16)
        VB2 = A.alloc((32, 128), BF16)
        wq = A.alloc((8, 1288), BF16)
        wo = A.alloc((8, 1024), BF16)
        g0 = A.alloc((1024,), F32)
        g1 = A.alloc((1024,), F32)
        biasA = A.alloc((2, 8, 128), BF16)
        biasB = A.alloc((2, 8, 128), BF16)
        xt = A.alloc((1024,), F32)
        hb = A.alloc((1024,), BF16)
        junkb = A.alloc((1024,), BF16)
        OV = A.off
        wk = A.alloc((8, 1408), BF16)
        hT4 = A.alloc((8, 512), BF16)
        xt2 = A.alloc((1024,), F32)
        biasf = A.alloc((2, 8, 128), F32)
        cAB = A.alloc((16,), F32)
        lvb = A.alloc((4, 64), F32)
        lvj = A.alloc((64,), F32)
        kv_end = A.off
        A.off = OV
        sc = A.alloc((4096,), F32)
        junk = A.alloc((2048,), BF16)
        maskT = A.alloc((32, 128), BF16)
        hT = A.alloc((8, 128), BF16)
        qmA = A.alloc((4, 2, 128), BF16)
        qmB = A.alloc((4, 2, 128), BF16)
        qmI = A.alloc((2, 4, 128), BF16)
        wis = A.alloc((8,), F32)
        rbuf = [A.alloc((512,), F32) for _ in range(2)]
        ptb = [A.alloc((512,), BF16) for _ in range(2)]
        pmb = [A.alloc((512,), BF16) for _ in range(2)]
        rz = A.alloc((512,), F32)
        tt = A.alloc((512,), F32)
        dd = A.alloc((256,), F32)
        sq = tt[:, 0:256]
        rstd = tt[:, 256:512]
        yT = A.alloc((8, 128), BF16)
        tmpn = sc[:, 0:1024]
        junkf = tt
        junk2 = maskT.rearrange("p a b -> p (a b)")[:, 0:2048]
        xtq = [xt, A.alloc((1024,), F32)]
        qmBs = [qmB, A.alloc((4, 2, 128), BF16)]
        bis = A.alloc((8,), F32)
        Rs = A.alloc((NITER,), F32)
        q_end = A.off
        assert max(kv_end, q_end) <= NW

        win3 = win.rearrange("(c p) n -> p c n", p=128)
        wkparts = [(0, 512, 512), (512, 2048, 64), (576, 2048, 64), (640, 2432, 32), (672, 2432, 32),
                   (704, 2432, 32), (736, 2432, 32), (768, 1024, 512), (1280, 2112, 64), (1344, 2112, 64)]
        for i, (d0, s0, n) in enumerate(wkparts):
            S.dma("pool", lambda e, d0=d0, s0=s0, n=n: e.dma_start(out=wk[:, :, d0:d0 + n], in_=win3[:, :, s0:s0 + n]),
                  writes=[("wk", i)])
        WK = [("wk", i) for i in range(len(wkparts))]
        wqparts = [(0, 0, 512), (512, 1536, 512), (1024, 2176, 256), (1280, 2464, 8)]
        for i, (d0, s0, n) in enumerate(wqparts):
            S.dma("pool", lambda e, d0=d0, s0=s0, n=n: e.dma_start(out=wq[:, :, d0:d0 + n], in_=win3[:, :, s0:s0 + n]),
                  writes=[("wq", i)])
        WQ = [("wq", i) for i in range(len(wqparts))]
        load_w(wo, lambda i: ("wo", i), wout.rearrange("(c p) n -> p c n", p=128), 1024, 512)
        WO = [("wo", 0), ("wo", 1)]
        S.dma("sp", lambda e: e.dma_start(out=g0, in_=gb[0]), writes=["g0"])
        S.dma("sp", lambda e: e.dma_start(out=g1, in_=gb[1]), writes=["g1"])
        S.dma("sp", lambda e: e.dma_start(out=cAB[:, 0:8], in_=cA_d), writes=["cAB"])
        S.dma("sp", lambda e: e.dma_start(out=cAB[:, 8:16], in_=cB_d), writes=["cAB2"])
        for which, (src, dstb) in enumerate(((biasA_d, biasA), (biasB_d, biasB))):
            S.dma("sp", lambda e, src=src: e.dma_start(out=biasf, in_=src), writes=["biasf"])
            for m in range(8):
                S.op("dve", lambda e, m=m, dstb=dstb, which=which: e.tensor_scalar(
                    out=dstb[:, :, m, :], in0=biasf[:, :, m, :], scalar1=cAB[:, which * 8 + m:which * 8 + m + 1],
                    scalar2=None, op0=ALU.subtract),
                    reads=["biasf", "cAB", "cAB2"], writes=[("bias", which)])
        lam_init = 0.8 - 0.6 * math.exp(-0.3 * 0)
        S.dma("sp", lambda e: e.dma_start(out=lvb, in_=lvb_d), writes=["lvb"])
        S.dma("sp", lambda e: e.dma_start(out=smallc[:, 5:6], in_=sgn_d), writes=["sgraw"])
        e1, e1k = newstat()
        e2, e2k = newstat()
        S.op("dve", lambda e: e.tensor_tensor(out=lvj, in0=lvb[:, 0, :], in1=lvb[:, 1, :], op=ALU.mult),
             reads=["lvb"], writes=["lvj"])
        S.op("dve", lambda e: e.tensor_reduce(out=e1, in_=lvj, axis=AX.X, op=ALU.add), reads=["lvj"], writes=[e1k])
        S.op("dve", lambda e: e.tensor_tensor(out=lvj, in0=lvb[:, 2, :], in1=lvb[:, 3, :], op=ALU.mult),
             reads=["lvb", e1k], writes=["lvj"])
        S.op("dve", lambda e: e.tensor_reduce(out=e2, in_=lvj, axis=AX.X, op=ALU.add), reads=["lvj"], writes=[e2k])
        S.op("act", lambda e: e.activation(out=e1, in_=e1, func=AF.Exp), reads=[e1k], writes=[e1k])
        S.op("act", lambda e: e.activation(out=e2, in_=e2, func=AF.Exp), reads=[e2k], writes=[e2k])
        S.op("dve", lambda e: e.scalar_tensor_tensor(out=neglam, in0=e2, scalar=-lam_init, in1=e1,
                                                     op0=ALU.add, op1=ALU.subtract),
             reads=[e1k, e2k], writes=["neglam"])
        S.op("dve", lambda e: e.tensor_scalar(out=sgc, in0=sgc, scalar1=1.0 - lam_init, scalar2=None, op0=ALU.mult),
             reads=["sgraw"], writes=["sg"])
        S.op("pool", lambda e: e.memset(VB2[:, :, :], 0.0), writes=["VB2"])

        xts = [xt, xt2]
        for g in range(8):
            for i in range(4):
                t = 4 * g + i
                xa = xts[t % 2]
                xk = ("xt", t % 2)
                S.dma("sp", lambda e, t=t, xa=xa: e.dma_start(out=xa, in_=xloc[t * 128:(t + 1) * 128, :]), writes=[xk])
                rmsnorm_rows(xa, xk, g0, "g0", hb, "hb", junkb)
                transpose8(hb, "hb", hT4[:, :, i * 128:(i + 1) * 128], ("hT4", i), eng="act" if i % 2 == 0 else "dve")
            HT4 = [("hT4", i) for i in range(4)]
            for c in range(6):
                bk = c % 2
                for dc in range(8):
                    S.op("pe", lambda e, c=c, dc=dc, bk=bk: e.matmul(pb[bk][:], lhsT=wk[:, dc, c * 128:(c + 1) * 128],
                                                                    rhs=hT4[:, dc, :], start=(dc == 0), stop=(dc == 7)),
                         reads=HT4 + WK, writes=[pbk(bk)])
                if c < 4:
                    dst = KA[:, c, g * 512:(g + 1) * 512]
                elif c == 4:
                    dst = KB2[:, g * 512:(g + 1) * 512]
                else:
                    dst = KI4[:, g * 512:(g + 1) * 512]
                if c % 2 == 0:
                    S.op("act", lambda e, dst=dst, bk=bk: e.copy(out=dst, in_=pb[bk][:]), reads=[pbk(bk)], writes=[("K", c, g)])
                else:
                    S.op("dve", lambda e, dst=dst, bk=bk: e.tensor_copy(out=dst, in_=pb[bk][:]), reads=[pbk(bk)], writes=[("K", c, g)])
            for i in range(4):
                t = 4 * g + i
                for dc in range(8):
                    S.op("pe", lambda e, i=i, dc=dc: e.matmul(pb[2][:], lhsT=hT4[:, dc, i * 128:(i + 1) * 128],
                                                              rhs=wk[:, dc, 768:1280], start=(dc == 0), stop=(dc == 7)),
                         reads=HT4 + WK, writes=[pbk(2)])
                for dc in range(8):
                    S.op("pe", lambda e, i=i, dc=dc: e.matmul(pb[3][:, 0:128], lhsT=hT4[:, dc, i * 128:(i + 1) * 128],
                                                              rhs=wk[:, dc, 1280:1408], start=(dc == 0), stop=(dc == 7)),
                         reads=HT4 + WK, writes=[pbk(3)])
                S.op("act", lambda e, t=t: e.copy(out=VA[:, t, :], in_=pb[2][:]), reads=[pbk(2)], writes=[("VA", t)])
                S.op("dve", lambda e, t=t: e.tensor_copy(out=VB2[:, t, :], in_=pb[3][:, 0:128]),
                     reads=[pbk(3), "VB2"], writes=[("VB", t)])
        S.barrier()

        ALLK = [("K", c, g) for c in range(6) for g in range(8)]
        outs = []
        def q_front(qi):
            qt = QT0 + qi
            xc = xtq[qi % 2]
            xk = ("xtq", qi % 2)
            qmBc = qmBs[qi % 2]
            S.dma("sp", lambda e, qt=qt: e.dma_start(out=xc, in_=xloc[qt * 128:(qt + 1) * 128, :]), writes=[xk])
            rmsnorm_rows(xc, xk, g0, "g0", hb, "hb", junkb)
            transpose8(hb, "hb", hT, "hT")
            for c in range(10):
                bank, col = (4, c) if c < 4 else ((5, c - 4) if c < 8 else (6, c - 8))
                for dc in range(8):
                    S.op("pe", lambda e, c=c, dc=dc, bank=bank, col=col: e.matmul(
                        pb[bank][:, col * 128:(col + 1) * 128], lhsT=wq[:, dc, c * 128:(c + 1) * 128], rhs=hT[:, dc, :],
                        start=(dc == 0), stop=(dc == 7)), reads=["hT"] + WQ, writes=[pbk(bank)])
            for dc in range(8):
                S.op("pe", lambda e, dc=dc: e.matmul(pb[6][:, 256:264], lhsT=hT[:, dc, :], rhs=wq[:, dc, 1280:1288],
                                                     start=(dc == 0), stop=(dc == 7)), reads=["hT"] + WQ, writes=[pbk(6)])
            cnt = 0
            for (bank, dstq, nsub, sel) in ((4, qmA, 2, selA), (5, qmBc, 2, selA), (6, qmI, 4, selI)):
                for c in range(dstq.shape[1]):
                    for s_ in range(nsub):
                        src = pb[bank][:, c * 128:(c + 1) * 128]
                        dst = dstq[:, c, s_, :]
                        sca = sel[:, s_:s_ + 1]
                        if cnt % 2 == 0:
                            S.op("act", lambda e, src=src, dst=dst, sca=sca: e.activation(out=dst, in_=src, func=AF.Copy, scale=sca),
                                 reads=[pbk(bank), "selA", "selI"], writes=[("qm", bank) if bank != 5 else ("qmB", qi % 2)])
                        else:
                            S.op("dve", lambda e, src=src, dst=dst, sca=sca: e.tensor_scalar(out=dst, in0=src, scalar1=sca, scalar2=None, op0=ALU.mult),
                                 reads=[pbk(bank), "selA", "selI"], writes=[("qm", bank) if bank != 5 else ("qmB", qi % 2)])
                        cnt += 1
            S.op("act", lambda e: e.mul(out=wis, in_=pb[6][:, 256:264], mul=(32.0 ** -0.5) * (8.0 ** -0.5)),
                 reads=[pbk(6)], writes=["wis"])
        q_front(0)
        for qi in range(NQT):
            qt = QT0 + qi
            nk = qt + 1
            N = nk * 128
            xk = ("xtq", qi % 2)
            xc = xtq[qi % 2]
            qmBc = qmBs[qi % 2]
            nkb = (N + 511) // 512
            for kb in range(nkb):
                cols = min(512, N - kb * 512)
                for j in range(8):
                    bk = 4 + (j % 2)
                    S.op("pe", lambda e, j=j, bk=bk, kb=kb, cols=cols: e.matmul(
                        pb[bk][:, 0:cols], lhsT=qmI[:, j // 4, j % 4, :], rhs=KI4[:, kb * 512:kb * 512 + cols],
                        start=True, stop=True), reads=[("qm", 6)] + ALLK, writes=[pbk(bk)])
                    rb = rbuf[j % 2]
                    S.op("act", lambda e, bk=bk, rb=rb, cols=cols: e.activation(out=rb[:, 0:cols], in_=pb[bk][:, 0:cols], func=AF.Relu),
                         reads=[pbk(bk)], writes=[("rbuf", j % 2)])
                    scv = sc[:, kb * 512:kb * 512 + cols]
                    if j == 0:
                        S.op("dve", lambda e, rb=rb, scv=scv, cols=cols: e.tensor_scalar(
                            out=scv, in0=rb[:, 0:cols], scalar1=wis[:, 0:1], scalar2=None, op0=ALU.mult),
                            reads=[("rbuf", 0), "wis"], writes=[("sc", kb)] + ([("tmpn", 0), ("tmpn", 1)] if kb < 2 else []))
                    else:
                        S.op("dve", lambda e, rb=rb, scv=scv, cols=cols, j=j: e.scalar_tensor_tensor(
                            out=scv, in0=rb[:, 0:cols], scalar=wis[:, j:j + 1], in1=scv, op0=ALU.mult, op1=ALU.add),
                            reads=[("rbuf", j % 2), "wis", ("sc", kb)], writes=[("sc", kb)])
            SCK = [("sc", kb) for kb in range(nkb)]
            mx, mn, Rr, lo, tth, cA_, cB_, gg = [bis[:, i:i + 1] for i in range(8)]
            S.op("dve", lambda e, N=N: e.tensor_reduce(out=mx, in_=sc[:, 0:N], axis=AX.X, op=ALU.max), reads=SCK, writes=["b_mx"])
            S.op("dve", lambda e, N=N: e.tensor_reduce(out=lo, in_=sc[:, 0:N], axis=AX.X, op=ALU.min), reads=SCK, writes=["b_lo"])
            S.op("dve", lambda e: e.tensor_tensor(out=Rr, in0=mx, in1=lo, op=ALU.subtract), reads=["b_mx", "b_lo"], writes=["b_R"])
            S.op("pool", lambda e, N=N: e.memset(sc[0:64, N - 64:N], NEG), reads=SCK + ["b_mx", "b_lo"], writes=SCK)
            npv = 15 * 128 if qt == 15 else 2048
            S.op("dve", lambda e, npv=npv: e.tensor_scalar(out=sc[:, 0:npv], in0=sc[:, 0:npv], scalar1=pnegc, scalar2=None, op0=ALU.add),
                 reads=SCK + ["flags"], writes=SCK)
            halves = [(0, N)] if N <= 2048 else [(0, 2048), (2048, N)]
            S.op("dve", lambda e: e.tensor_scalar(out=Rs, in0=stepc, scalar1=Rr, scalar2=None, op0=ALU.mult),
                 reads=["b_R", "stepc"], writes=["b_Rs"])

            def bis_iter(it):
                two = len(halves) == 2
                S.op("dve", lambda e: e.tensor_tensor(out=tth, in0=Rs[:, it:it + 1], in1=lo, op=ALU.add),
                     reads=["b_Rs", "b_lo"], writes=["b_t"])
                if two:
                    S.op("dve", lambda e: e.scalar_tensor_tensor(out=mn, in0=Rs[:, it:it + 1], scalar=-1.0, in1=lo, op0=ALU.mult, op1=ALU.subtract),
                         reads=["b_Rs", "b_lo"], writes=["b_nt"])
                a0, a1 = halves[0]
                S.op("dve", lambda e: e.tensor_scalar(
                    out=junk[:, 0:a1 - a0], in0=sc[:, a0:a1], scalar1=tth, scalar2=None, op0=ALU.is_ge, op1=ALU.add, accum_out=cA_),
                    reads=SCK + ["b_t"], writes=["junk", ("b_c", 0)])
                thr = TOPK - 0.5
                if two:
                    b0, b1 = halves[1]
                    S.op("act", lambda e: e.activation(out=junk2[:, 0:b1 - b0], in_=sc[:, b0:b1], func=AF.Sign, bias=mn, scale=1.0, accum_out=cB_),
                         reads=SCK + ["b_nt"], writes=[("maskT", 0), ("maskT", 1), ("maskT", 2), ("maskT", 3), ("b_c", 1)])
                    S.op("dve", lambda e: e.scalar_tensor_tensor(out=cA_, in0=cB_, scalar=0.5, in1=cA_, op0=ALU.mult, op1=ALU.add),
                         reads=[("b_c", 0), ("b_c", 1)], writes=[("b_c", 0)])
                    thr = TOPK - 0.5 - 0.5 * (b1 - b0)
                S.op("dve", lambda e: e.tensor_scalar(out=gg, in0=cA_, scalar1=thr, scalar2=None, op0=ALU.is_ge),
                     reads=[("b_c", 0)], writes=["b_g"])
                S.op("dve", lambda e: e.scalar_tensor_tensor(out=lo, in0=gg, scalar=Rs[:, it:it + 1], in1=lo, op0=ALU.mult, op1=ALU.add),
                     reads=["b_g", "b_lo", "b_Rs"], writes=["b_lo"])

            MK = [("maskT", k4) for k4 in range((nk + 3) // 4)]

            def emit_group(typ, gi, hook=None):
                def qk_stage(kt):
                    sbk = kt % 2
                    band = kt >= qt - 1
                    if band:
                        bt = 0 if kt == qt else 1
                        bsrc = (biasA if typ == "A" else biasB)[:, bt, 4 * gi:4 * gi + 4, :]
                        S.op("pe", lambda e: e.matmul(pb[sbk][:].rearrange("p (a b) -> p a b", a=4), lhsT=identb, rhs=bsrc,
                                                      start=True, stop=False),
                             reads=["identb", ("bias", 0), ("bias", 1)], writes=[pbk(sbk)])
                    for mi in range(4):
                        if typ == "A":
                            h = 2 * gi + mi // 2
                            lhsT = KA[:, h, kt * 128:(kt + 1) * 128]
                            rhs = qmA[:, h, mi % 2, :]
                            qk = ("qm", 4)
                        else:
                            j = 4 * gi + mi
                            lhsT = KB2[:, kt * 128:(kt + 1) * 128]
                            rhs = qmBc[:, j // 2, j % 2, :]
                            qk = ("qmB", qi % 2)
                        S.op("pe", lambda e, mi=mi, lhsT=lhsT, rhs=rhs: e.matmul(
                            pb[sbk][:, mi * 128:(mi + 1) * 128], lhsT=lhsT, rhs=rhs, start=(not band), stop=((not band) or mi == 3)),
                            reads=[qk] + ALLK, writes=[pbk(sbk)])
                    masked = (kt <= 14) or (kt == 15 and qt >= 16)
                    bias_ap = pnegc if masked else zeroc
                    pt = ptb[kt % 2]
                    S.op("act", lambda e: e.activation(out=pt, in_=pb[sbk][:], func=AF.Exp, bias=bias_ap, scale=1.0),
                         reads=[pbk(sbk), "flags", "zero"], writes=[("pt", kt % 2)])
                    if typ == "B":
                        pm = pmb[kt % 2]
                        S.op("dve", lambda e: e.tensor_tensor(
                            out=pm.rearrange("p (a b) -> p a b", a=4), in0=pt.rearrange("p (a b) -> p a b", a=4),
                            in1=maskT[:, kt, :].unsqueeze(1).to_broadcast([128, 4, 128]), op=ALU.mult),
                            reads=[("pt", kt % 2)] + MK, writes=[("pm", kt % 2)])

                def pv_stage(kt):
                    first, last = (kt == 0), (kt == nk - 1)
                    if typ == "B":
                        src, srck = pmb[kt % 2], ("pm", kt % 2)
                    else:
                        src, srck = ptb[kt % 2], ("pt", kt % 2)
                    if typ == "A":
                        for hl in range(2):
                            h = 2 * gi + hl
                            ob = 2 if hl == 0 else 4
                            S.op("pe", lambda e, hl=hl, h=h, ob=ob: e.matmul(
                                pb[ob][:, 0:256], lhsT=VA[:, kt, h * 128:(h + 1) * 128], rhs=src[:, hl * 256:(hl + 1) * 256],
                                start=first, stop=last), reads=[srck, ("VA", kt)], writes=[pbk(ob)])
                    else:
                        S.op("pe", lambda e: e.matmul(pb[2][:], lhsT=VB2[:, kt, :], rhs=src, start=first, stop=last),
                             reads=[srck, ("VB", kt)], writes=[pbk(2)])
                    S.op("pe", lambda e: e.matmul(pb[3][:], lhsT=onesb, rhs=src, start=first, stop=last),
                         reads=[srck, "onesb"], writes=[pbk(3)])

                qk_stage(0)
                for kt in range(nk):
                    if kt + 1 < nk:
                        qk_stage(kt + 1)
                    pv_stage(kt)
                    if hook is not None:
                        hook(kt)
                S.op("dve", lambda e: e.reciprocal(out=rz, in_=pb[3][:]), reads=[pbk(3)], writes=["rz"])
                if typ == "A":
                    S.op("dve", lambda e: e.tensor_tensor(out=tt[:, 0:256], in0=pb[2][:, 0:256], in1=rz[:, 0:256], op=ALU.mult),
                         reads=[pbk(2), "rz"], writes=["tt"])
                    S.op("dve", lambda e: e.tensor_tensor(out=tt[:, 256:512], in0=pb[4][:, 0:256], in1=rz[:, 256:512], op=ALU.mult),
                         reads=[pbk(4), "rz", "tt"], writes=["tt"])
                    t4 = tt.rearrange("p (h m q) -> p h m q", h=2, m=2)
                    S.op("dve", lambda e: e.scalar_tensor_tensor(out=dd.rearrange("p (h q) -> p h q", h=2), in0=t4[:, :, 1, :], scalar=neglam,
                                                                 in1=t4[:, :, 0, :], op0=ALU.mult, op1=ALU.add),
                         reads=["tt", "neglam"], writes=["dd"])
                    S.op("pool", lambda e: e.tensor_tensor(out=sq, in0=dd, in1=dd, op=ALU.mult), reads=["dd"], writes=["sq"])
                    S.op("pe", lambda e: e.matmul(pb[6][:, 0:256], lhsT=onesf, rhs=sq, start=True, stop=True),
                         reads=["sq", "onesf"], writes=[pbk(6)])
                    S.op("act", lambda e: e.activation(out=rstd, in_=pb[6][:, 0:256], func=AF.Sqrt, scale=1.0 / 128, bias=epsc),
                         reads=[pbk(6), "eps"], writes=["rstd"])
                    S.op("dve", lambda e: e.reciprocal(out=rstd, in_=rstd), reads=["rstd"], writes=["rstd"])
                    S.op("dve", lambda e: e.scalar_tensor_tensor(out=yT[:, 2 * gi:2 * gi + 2, :], in0=dd.rearrange("p (h q) -> p h q", h=2),
                                                                 scalar=sgc, in1=rstd.rearrange("p (h q) -> p h q", h=2),
                                                                 op0=ALU.mult, op1=ALU.mult),
                         reads=["dd", "rstd", "sg"], writes=[("yT", gi)])
                else:
                    for cl in range(2):
                        ch = 4 + 2 * gi + cl
                        for hf in range(2):
                            jl = 2 * cl + hf
                            S.op("dve", lambda e, ch=ch, hf=hf, jl=jl: e.tensor_tensor(
                                out=yT[hf * 64:(hf + 1) * 64, ch, :], in0=pb[2][hf * 64:(hf + 1) * 64, jl * 128:(jl + 1) * 128],
                                in1=rz[hf * 64:(hf + 1) * 64, jl * 128:(jl + 1) * 128], op=ALU.mult),
                                reads=[pbk(2), "rz"], writes=[("yT", 2 + gi)])

            def make_hook(it0, it1):
                st_ = {"next": it0}

                def hook(kt):
                    want = it0 + ((kt + 1) * (it1 - it0)) // nk
                    while st_["next"] < want:
                        bis_iter(st_["next"])
                        st_["next"] += 1
                return hook
            emit_group("A", 0, make_hook(0, NITER // 2))
            emit_group("A", 1, make_hook(NITER // 2, NITER))
            S.op("dve", lambda e, N=N: e.tensor_scalar(out=sc[:, 0:N], in0=sc[:, 0:N], scalar1=lo, scalar2=None, op0=ALU.is_ge),
                 reads=SCK + ["b_lo"], writes=SCK)
            for k4 in range((nk + 3) // 4):
                n4 = min(4, nk - 4 * k4)
                for i in range(n4):
                    kt = 4 * k4 + i
                    S.op("pe", lambda e, kt=kt, i=i: e.transpose(out=pb[6][:, i * 128:(i + 1) * 128], in_=sc[:, kt * 128:(kt + 1) * 128],
                                                                 identity=identf), reads=SCK + ["identf"], writes=[pbk(6)])
                S.op("act", lambda e, k4=k4, n4=n4: e.copy(out=maskT[:, 4 * k4:4 * k4 + n4, :],
                                                           in_=pb[6][:, 0:n4 * 128].rearrange("p (a b) -> p a b", a=n4)),
                     reads=[pbk(6)], writes=[("maskT", k4)])
            if qi + 1 < NQT:
                q_front(qi + 1)
            emit_group("B", 0)
            emit_group("B", 1)
            YT = [("yT", i) for i in range(4)]
            if debug:
                S.dma("pool", lambda e, qi=qi: e.dma_start(out=ydbg[qi], in_=yT), reads=YT, writes=["ydbg"])
            for hf in range(2):
                for c in range(8):
                    S.op("pe", lambda e, hf=hf, c=c: e.matmul(pb[hf][:], lhsT=yT[:, c, :], rhs=wo[:, c, hf * 512:(hf + 1) * 512],
                                                              start=(c == 0), stop=(c == 7)), reads=YT + WO, writes=[pbk(hf)])
            norm_residual(0, 1, g1, "g1", xc, xk, tmpn, junkf)
            outs.append(S.dma("sp", lambda e, qi=qi, xc=xc: e.dma_start(out=x1d[qi * 128:(qi + 1) * 128, :], in_=xc), reads=[xk], writes=["x1d"]))
        S.barrier()

        def mlp_phase(layer, ntiles, src_d, dst_d, gi0):
            A.off = P0
            Wup = A.alloc((8, 4096), BF16)
            Wdn = A.alloc((32, 1024), BF16)
            ga = A.alloc((1024,), F32)
            gb_ = A.alloc((1024,), F32)
            xtm = [A.alloc((1024,), F32) for _ in range(4)]
            hbm = A.alloc((1024,), BF16)
            jb = A.alloc((1024,), BF16)
            hTm = A.alloc((8, 256), BF16)
            uT = A.alloc((32, 256), BF16)
            rb2 = [A.alloc((2, 256), F32) for _ in range(2)]
            tmpm = A.alloc((1024,), F32)
            jf = A.alloc((512,), F32)
            assert A.off <= NW
            load_w(Wup, lambda i: ("Wup", i), wup[layer].rearrange("(c p) n -> p c n", p=128), 4096, 512)
            for p4 in range(4):
                S.dma("pool", lambda e, p4=p4: e.dma_start(out=Wdn[:, 8 * p4:8 * p4 + 8, :],
                                                          in_=wdn[layer].rearrange("(f p) n -> p f n", p=128)[:, 8 * p4:8 * p4 + 8, :]),
                      writes=[("Wdn", p4)])
            S.dma("sp", lambda e: e.dma_start(out=ga, in_=gb[gi0]), writes=["ga"])
            S.dma("sp", lambda e: e.dma_start(out=gb_, in_=gb[gi0 + 1]), writes=["gb"])
            res = []
            groups = [list(range(i, min(i + 2, ntiles))) for i in range(0, ntiles, 2)]
            def front(gidx):
                grp = groups[gidx]
                xb = 2 * (gidx % 2)
                for i, t in enumerate(grp):
                    xk = ("xtm", xb + i)
                    S.dma("sp", lambda e, t=t, i=i: e.dma_start(out=xtm[xb + i], in_=src_d[t * 128:(t + 1) * 128, :]), reads=["srcd"], writes=[xk])
                    rmsnorm_rows(xtm[xb + i], xk, ga, "ga", hbm, "hbm", jb)
                    transpose8(hbm, "hbm", hTm[:, :, i * 128:(i + 1) * 128], ("hTm", i), eng="act" if i == 0 else "dve")

            def up(gidx):
                grp = groups[gidx]
                T = 128 * len(grp)
                HK = [("hTm", i) for i in range(len(grp))]
                for f2 in range(16):
                    bk = 4 + f2 % 2
                    for fl in range(2):
                        f = 2 * f2 + fl
                        for dc in range(8):
                            S.op("pe", lambda e, f=f, fl=fl, dc=dc, bk=bk: e.matmul(
                                pb[bk][:, fl * 256:fl * 256 + T], lhsT=Wup[:, dc, f * 128:(f + 1) * 128], rhs=hTm[:, dc, 0:T],
                                start=(dc == 0), stop=(dc == 7)), reads=HK + [("Wup", f // 4)], writes=[pbk(bk)])
                    rb = rb2[f2 % 2]
                    S.op("act", lambda e, bk=bk, rb=rb: e.activation(
                        out=rb[:, :, 0:T], in_=pb[bk][:].rearrange("p (a b) -> p a b", a=2)[:, :, 0:T], func=AF.Relu),
                        reads=[pbk(bk)], writes=[("rb2", f2 % 2)])
                    S.op("pool", lambda e, rb=rb, f2=f2: e.tensor_tensor(out=uT[:, 2 * f2:2 * f2 + 2, 0:T], in0=rb[:, :, 0:T],
                                                                    in1=rb[:, :, 0:T], op=ALU.mult),
                         reads=[("rb2", f2 % 2)], writes=[("uT", f2)])

            def down(gidx):
                grp = groups[gidx]
                xb = 2 * (gidx % 2)
                UK = [("uT", f2) for f2 in range(16)]
                for i, t in enumerate(grp):
                    b0 = 0 if i == 0 else 2
                    for hf in range(2):
                        for f in range(32):
                            S.op("pe", lambda e, i=i, hf=hf, f=f, b0=b0: e.matmul(pb[b0 + hf][:], lhsT=uT[:, f, i * 128:(i + 1) * 128],
                                                                                 rhs=Wdn[:, f, hf * 512:(hf + 1) * 512], start=(f == 0), stop=(f == 31)),
                                 reads=UK + [("Wdn", f // 8)], writes=[pbk(b0 + hf)])
                    norm_residual(b0, b0 + 1, gb_, "gb", xtm[xb + i], ("xtm", xb + i), tmpm, jf)
                    res.append(S.dma("sp", lambda e, t=t, i=i: e.dma_start(out=dst_d[t * 128:(t + 1) * 128, :], in_=xtm[xb + i]),
                                     reads=[("xtm", xb + i)], writes=["dstd"]))

            front(0)
            for gidx in range(len(groups)):
                up(gidx)
                if gidx + 1 < len(groups):
                    front(gidx + 1)
                down(gidx)
            S.barrier()
            return res

        mlp_phase(0, NQT, x1d, x2d, 2)

        A.off = P0
        wci = A.alloc((8, 2560), BF16)
        wco = A.alloc((8, 1024), BF16)
        dg = A.alloc((31, 4, 128), BF16)
        dwT = A.alloc((4, 31), F32)
        cvec = A.alloc((3, 4), F32)
        scw = A.alloc((4, 3), F32)
        g4 = A.alloc((1024,), F32)
        g5 = A.alloc((1024,), F32)
        xtc = [A.alloc((1024,), F32) for _ in range(4)]
        hbc = A.alloc((1024,), BF16)
        jbc = A.alloc((1024,), BF16)
        hTc = A.alloc((8, 256), BF16)
        uTb = A.alloc((4, 32 + 256), BF16)
        eTb = A.alloc((4, 2 + 256), F32)
        sgb = A.alloc((256,), F32)
        utmp = A.alloc((256,), F32)
        dhs = A.alloc((256,), F32)
        z1 = A.alloc((256,), F32)
        vv = A.alloc((4, 256), F32)
        sqv = A.alloc((4, 256), F32)
        mean = A.alloc((256,), F32)
        msq = A.alloc((256,), F32)
        rsd = A.alloc((256,), F32)
        tcb = A.alloc((256,), F32)
        ymT = A.alloc((8, 256), BF16)
        tmpc = A.alloc((1024,), F32)
        jfc = A.alloc((512,), F32)
        assert A.off <= NW
        load_w(wci, lambda i: ("wci", i), cwin.rearrange("(c p) n -> p c n", p=128), 2560, 512)
        WCI = [("wci", i) for i in range(5)]
        load_w(wco, lambda i: ("wco", i), cwout.rearrange("(c p) n -> p c n", p=128), 1024, 512)
        WCO = [("wco", 0), ("wco", 1)]
        S.dma("sp", lambda e: e.dma_start(out=dwT, in_=dwT_d), writes=["dwT"])
        S.dma("sp", lambda e: e.dma_start(out=cvec, in_=cvec_d), writes=["cvec"])
        S.dma("sp", lambda e: e.dma_start(out=scw, in_=scw_d), writes=["scw"])
        S.dma("sp", lambda e: e.dma_start(out=g4, in_=gb[4]), writes=["g4"])
        S.dma("sp", lambda e: e.dma_start(out=g5, in_=gb[5]), writes=["g5"])
        cnt = 0
        for j in range(31):
            for c in range(4):
                eng = "dve" if cnt % 2 == 0 else "pool"
                S.op(eng, lambda e, j=j, c=c: e.tensor_scalar(out=dg[:, j, c, :], in0=identf, scalar1=dwT[:, c, j:j + 1], scalar2=None, op0=ALU.mult),
                     reads=["identf", "dwT"], writes=[("dg", eng)])
                cnt += 1
        DG = [("dg", "dve"), ("dg", "pool")]
        S.op("pool", lambda e: e.memset(uTb[:, :, :], 0.0), writes=["uTb"])
        S.op("pool", lambda e: e.memset(eTb[:, :, :], 0.0), writes=["eTb"])
        cgroups = [[0]] + [[1 + 2 * i, 2 + 2 * i] for i in range(8)]
        for gidx, grp in enumerate(cgroups):
            T = 128 * len(grp)
            halo = (grp == [0])
            xb = 2 * (gidx % 2)
            for i, li in enumerate(grp):
                xk = ("xtc", xb + i)
                S.dma("sp", lambda e, li=li, i=i, xb=xb: e.dma_start(out=xtc[xb + i], in_=x2d[li * 128:(li + 1) * 128, :]), reads=["dstd"], writes=[xk])
                rmsnorm_rows(xtc[xb + i], xk, g4, "g4", hbc, "hbc", jbc)
                transpose8(hbc, "hbc", hTc[:, :, i * 128:(i + 1) * 128], ("hTc", i), eng="act" if i == 0 else "dve")
            HK = [("hTc", i) for i in range(len(grp))]

            def proj(bank, col0, T=T, HK=HK):
                for dc in range(8):
                    S.op("pe", lambda e, dc=dc: e.matmul(pb[bank][:, 0:T], lhsT=wci[:, dc, col0:col0 + 128], rhs=hTc[:, dc, 0:T],
                                                         start=(dc == 0), stop=(dc == 7)), reads=HK + WCI, writes=[pbk(bank)])
            for c in range(4):
                proj(0, c * 128)
                proj(1, 512 + c * 128)
                S.op("act", lambda e, T=T: e.activation(out=sgb[:, 0:T], in_=pb[1][:, 0:T], func=AF.Sigmoid), reads=[pbk(1)], writes=["sgb"])
                if halo:
                    S.op("dve", lambda e, T=T: e.tensor_tensor(out=utmp[:, 0:T], in0=pb[0][:, 0:T], in1=sgb[:, 0:T], op=ALU.mult),
                         reads=[pbk(0), "sgb"], writes=["utmp"])
                    S.op("dve", lambda e, c=c, T=T: e.tensor_scalar(out=uTb[:, c, 32:32 + T], in0=utmp[:, 0:T], scalar1=flagc, scalar2=None, op0=ALU.mult),
                         reads=["utmp", "flags", "uTb"], writes=[("uT", c)])
                else:
                    S.op("dve", lambda e, c=c, T=T: e.tensor_tensor(out=uTb[:, c, 32:32 + T], in0=pb[0][:, 0:T], in1=sgb[:, 0:T], op=ALU.mult),
                         reads=[pbk(0), "sgb", "uTb"], writes=[("uT", c)])
            for c in range(4):
                proj(2, 1536 + c * 128)
                proj(3, 2048 + c * 128)
                S.op("act", lambda e, T=T: e.copy(out=dhs[:, 0:T], in_=pb[3][:, 0:T]), reads=[pbk(3)], writes=["dhs"])
                if halo:
                    S.op("dve", lambda e, T=T: e.tensor_tensor(out=utmp[:, 0:T], in0=pb[2][:, 0:T], in1=dhs[:, 0:T], op=ALU.mult),
                         reads=[pbk(2), "dhs"], writes=["utmp"])
                    S.op("dve", lambda e, c=c, T=T: e.tensor_scalar(out=eTb[:, c, 2:2 + T], in0=utmp[:, 0:T], scalar1=flagc, scalar2=None, op0=ALU.mult),
                         reads=["utmp", "flags", "eTb"], writes=[("eT", c)])
                else:
                    S.op("dve", lambda e, c=c, T=T: e.tensor_tensor(out=eTb[:, c, 2:2 + T], in0=pb[2][:, 0:T], in1=dhs[:, 0:T], op=ALU.mult),
                         reads=[pbk(2), "dhs", "eTb"], writes=[("eT", c)])
            if not halo:
                for c in range(4):
                    proj(4, 1024 + c * 128)
                    S.op("dve", lambda e, c=c, T=T: e.tensor_scalar(out=z1[:, 0:T], in0=eTb[:, c, 0:T], scalar1=scw[:, c, 0:1], scalar2=None, op0=ALU.mult),
                         reads=[("eT", c), "scw"], writes=["z1"])
                    for jj in (1, 2):
                        S.op("dve", lambda e, c=c, T=T, jj=jj: e.scalar_tensor_tensor(out=z1[:, 0:T], in0=eTb[:, c, jj:jj + T], scalar=scw[:, c, jj:jj + 1],
                                                                                    in1=z1[:, 0:T], op0=ALU.mult, op1=ALU.add),
                             reads=[("eT", c), "scw", "z1"], writes=["z1"])
                    S.op("dve", lambda e, c=c, T=T: e.tensor_tensor(out=ymT[:, 4 + c, 0:T], in0=pb[4][:, 0:T], in1=z1[:, 0:T], op=ALU.mult),
                         reads=[pbk(4), "z1"], writes=[("ymT", 4 + c)])
                for c in range(4):
                    for j in range(31):
                        S.op("pe", lambda e, c=c, j=j, T=T: e.matmul(pb[5][:, 0:T], lhsT=dg[:, j, c, :], rhs=uTb[:, c, 2 + j:2 + j + T],
                                                                     start=(j == 0), stop=(j == 30)),
                             reads=[("uT", c)] + DG, writes=[pbk(5)])
                    S.op("act", lambda e, c=c, T=T: e.activation(out=vv[:, c, 0:T], in_=pb[5][:, 0:T], func=AF.Identity, bias=cvec[:, 0, c:c + 1], scale=1.0),
                         reads=[pbk(5), "cvec"], writes=[("vv", c)])
                    S.op("act", lambda e, c=c, T=T: e.activation(out=sqv[:, c, 0:T], in_=vv[:, c, 0:T], func=AF.Square),
                         reads=[("vv", c)], writes=[("sqv", c)])
                for c in range(4):
                    S.op("pe", lambda e, c=c, T=T: e.matmul(pb[6][:, 0:T], lhsT=onesf, rhs=vv[:, c, 0:T], start=(c == 0), stop=(c == 3)),
                         reads=[("vv", c), "onesf"], writes=[pbk(6)])
                for c in range(4):
                    S.op("pe", lambda e, c=c, T=T: e.matmul(pb[6][:, 256:256 + T], lhsT=onesf, rhs=sqv[:, c, 0:T], start=(c == 0), stop=(c == 3)),
                         reads=[("sqv", c), "onesf"], writes=[pbk(6)])
                S.op("act", lambda e, T=T: e.mul(out=mean[:, 0:T], in_=pb[6][:, 0:T], mul=1.0 / 512), reads=[pbk(6)], writes=["mean"])
                S.op("pool", lambda e, T=T: e.tensor_tensor(out=msq[:, 0:T], in0=mean[:, 0:T], in1=mean[:, 0:T], op=ALU.mult), reads=["mean"], writes=["msq"])
                S.op("dve", lambda e, T=T: e.scalar_tensor_tensor(out=rsd[:, 0:T], in0=pb[6][:, 256:256 + T], scalar=1.0 / 512, in1=msq[:, 0:T],
                                                                op0=ALU.mult, op1=ALU.subtract), reads=[pbk(6), "msq"], writes=["rsd"])
                S.op("act", lambda e, T=T: e.activation(out=rsd[:, 0:T], in_=rsd[:, 0:T], func=AF.Sqrt, scale=1.0, bias=epsc), reads=["rsd", "eps"], writes=["rsd"])
                S.op("dve", lambda e, T=T: e.reciprocal(out=rsd[:, 0:T], in_=rsd[:, 0:T]), reads=["rsd"], writes=["rsd"])
                for c in range(4):
                    S.op("pool", lambda e, c=c, T=T: e.tensor_tensor(out=tcb[:, 0:T], in0=vv[:, c, 0:T], in1=mean[:, 0:T], op=ALU.subtract),
                         reads=[("vv", c), "mean"], writes=["tcb"])
                    S.op("pool", lambda e, T=T: e.tensor_tensor(out=tcb[:, 0:T], in0=tcb[:, 0:T], in1=rsd[:, 0:T], op=ALU.mult),
                         reads=["tcb", "rsd"], writes=["tcb"])
                    S.op("act", lambda e, c=c, T=T: e.activation(out=ymT[:, c, 0:T], in_=tcb[:, 0:T], func=AF.Silu, scale=cvec[:, 1, c:c + 1],
                                                                bias=cvec[:, 2, c:c + 1]), reads=["tcb", "cvec"], writes=[("ymT", c)])
                YM = [("ymT", c) for c in range(8)]
                for i, li in enumerate(grp):
                    for hf in range(2):
                        for c in range(8):
                            S.op("pe", lambda e, i=i, hf=hf, c=c: e.matmul(pb[hf][:], lhsT=ymT[:, c, i * 128:(i + 1) * 128],
                                                                          rhs=wco[:, c, hf * 512:(hf + 1) * 512], start=(c == 0), stop=(c == 7)),
                                 reads=YM + WCO, writes=[pbk(hf)])
                    norm_residual(0, 1, g5, "g5", xtc[xb + i], ("xtc", xb + i), tmpc, jfc)
                    S.dma("sp", lambda e, li=li, i=i, xb=xb: e.dma_start(out=x3d[(li - 1) * 128:li * 128, :], in_=xtc[xb + i]),
                          reads=[("xtc", xb + i)], writes=["x3d"])
            UT = [("uT", c) for c in range(4)]
            ET = [("eT", c) for c in range(4)]
            S.op("pool", lambda e, T=T: e.tensor_copy(out=uTb[:, :, 0:32], in_=uTb[:, :, T:T + 32]), reads=UT, writes=UT + ["uTb"])
            S.op("pool", lambda e, T=T: e.tensor_copy(out=eTb[:, :, 0:2], in_=eTb[:, :, T:T + 2]), reads=ET, writes=ET + ["eTb"])
        S.barrier()

        A.off = P0

        def final_src_fix():
            pass
        res = mlp_phase_final = None
        res = mlp_phase(1, 16, x3d, out_d, 6)
        S.emit(nc, final_wait_tokens=res)
    return nc


def _t5_bucket_np(rel):
    import jax
    import jax.numpy as jnp
    cpu = jax.devices("cpu")[0]
    with jax.default_device(cpu):
        rel = jnp.asarray(rel, dtype=jnp.int32)
        nb = 16
        ret = jnp.where(rel > 0, nb, 0)
        n = jnp.abs(rel)
        max_exact = nb // 2
        nf = jnp.maximum(n, 1).astype(jnp.float32)
        large = max_exact + (jnp.log(nf / max_exact) / math.log(128 / max_exact) * (nb - max_exact)).astype(jnp.int32)
        large = jnp.minimum(large, nb - 1)
        out = ret + jnp.where(n < max_exact, n, large)
        return np.asarray(out)


_PROG = {}


def _get_prog(debug=False):
    if debug not in _PROG:
        _PROG[debug] = build_program(debug)
    return _PROG[debug]


def make_in_maps(inputs):
    f = lambda a: np.ascontiguousarray(np.asarray(a, dtype=np.float32))
    x = f(inputs["x"])
    rel_bias = f(inputs["rel_bias"])
    norm_g = f(inputs["norm_g"])
    kk = np.arange(128)[:, None]
    qq = np.arange(128)[None, :]
    bD = _t5_bucket_np(kk - qq)
    bP = _t5_bucket_np(kk - 128 - qq)
    admiss = (kk // 64) <= (qq // 64)
    tabA = rel_bias[:, :4]
    tabB = rel_bias[:, 4:]
    biasA = np.zeros((128, 2, 8, 128), np.float32)
    biasB = np.zeros((128, 2, 8, 128), np.float32)
    for m in range(8):
        biasA[:, 0, m, :] = np.where(admiss, tabA[bD, m // 2], np.float32(NEG))
        biasA[:, 1, m, :] = tabA[bP, m // 2]
        biasB[:, 0, m, :] = np.where(admiss, tabB[bD, m], np.float32(NEG))
        biasB[:, 1, m, :] = tabB[bP, m]
    cA = np.ascontiguousarray(np.broadcast_to(np.repeat(tabA[15], 2)[None, :], (128, 8)))
    cB = np.ascontiguousarray(np.broadcast_to(tabB[15][None, :], (128, 8)))
    gbb = np.ascontiguousarray(np.broadcast_to(norm_g.reshape(8, 1, D), (8, 128, D)))
    lvb = np.ascontiguousarray(np.broadcast_to(f(inputs["diff_lambda"])[0][None], (128, 4, 64)))
    sgn = np.ascontiguousarray(f(inputs["diff_subln_g"])[0].reshape(128, 1))
    selA = np.zeros((128, 2), np.float32)
    selA[:64, 0] = 0.125
    selA[64:, 1] = 0.125
    selI = np.zeros((128, 4), np.float32)
    for i in range(4):
        selI[32 * i:32 * i + 32, i] = 1.0
    dwT = np.ascontiguousarray(f(inputs["conv_dw_w"])[0].reshape(31, 4, 128).transpose(2, 1, 0))
    cvec = np.ascontiguousarray(np.stack([f(inputs["conv_dw_b"])[0].reshape(4, 128).T,
                                          f(inputs["conv_ln_g"])[0].reshape(4, 128).T,
                                          f(inputs["conv_ln_b"])[0].reshape(4, 128).T], axis=1))
    scw = np.ascontiguousarray(f(inputs["sconv_w"])[0].reshape(3, 4, 128).transpose(2, 1, 0))
    common = dict(win=f(inputs["attn_w_in"])[0], wout=f(inputs["attn_w_out"])[0], wup=f(inputs["w_mlp_up"]),
                  wdn=f(inputs["w_mlp_down"]), cwin=f(inputs["conv_w_in"])[0], cwout=f(inputs["conv_w_out"])[0],
                  gb=gbb, biasA=biasA, biasB=biasB, cA=cA, cB=cB, lvb=lvb, sgn=sgn, selA=selA, selI=selI,
                  ident=np.eye(128, dtype=np.float32), dwT=dwT, cvec=cvec, scw=scw)
    in_maps = []
    for c in range(8):
        b, h = c // 2, c % 2
        if h == 1:
            xl = x[b]
        else:
            xl = np.concatenate([np.zeros((2048, D), np.float32), x[b, :2048]], axis=0)
        flags = np.zeros((128, 2), np.float32)
        flags[:, 0] = float(h)
        flags[:, 1] = 0.0 if h == 1 else NEG
        m = dict(common)
        m["xloc"] = np.ascontiguousarray(xl)
        m["flags"] = flags
        in_maps.append(m)
    return in_maps


def kernel(**inputs):
    nc = _get_prog(False)
    in_maps = make_in_maps(inputs)
    res = run_bass_kernel_spmd(nc, in_maps, core_ids=list(range(8)))
    out = np.zeros((4, 4096, D), np.float32)
    for c in range(8):
        b, h = c // 2, c % 2
        out[b, h * 2048:(h + 1) * 2048] = res.results[c]["out"]
    return out
```
